# Optimizing a Trainium2 kernel written in Bass

```python
import math
import jax, jax.numpy as jnp
from jax import lax
import numpy as np

D_MODEL = 1024
BATCH = 8
SEQ = 2048
DEPTH = 2

GRID_W = 64
CTX_LEN = 256
N_EVEN = (DEPTH + 1) // 2
N_ODD = DEPTH // 2

DIFF_HEADS = 4
DIFF_HEAD_DIM = 64
DIFF_V_DIM = 2 * DIFF_HEAD_DIM
DIFF_QK_W = DIFF_HEADS * 2 * DIFF_HEAD_DIM
DIFF_V_W = DIFF_HEADS * DIFF_V_DIM
DIFF_IN_W = 2 * DIFF_QK_W + DIFF_V_W
HGRN_HEADS = 4
HGRN_K_DIM = 128
HGRN_V_DIM = 128
HGRN_CHUNK = 16
HGRN_KW = HGRN_HEADS * HGRN_K_DIM
HGRN_VW = HGRN_HEADS * HGRN_V_DIM
HGRN_IN_W = 3 * HGRN_KW + 2 * HGRN_VW
EVEN_IN_W = DIFF_IN_W + HGRN_IN_W
EVEN_OUT_W = DIFF_V_W + HGRN_VW
MLA_HEADS = 8
MLA_NOPE = 128
MLA_ROPE = 64
MLA_V = 128
MLA_Q_LORA = 384
MLA_KV_LORA = 256
ODD_IN_W = MLA_Q_LORA + MLA_KV_LORA + MLA_ROPE
ODD_OUT_W = MLA_HEADS * MLA_V
N_EXPERTS = 16
N_GROUPS = 4
EXPERTS_PER_GROUP = N_EXPERTS // N_GROUPS
TOP_K = 2
EXPERT_FF = 512
SHARED_FF = 512

ROPE_BASE = 10000.0
Q_BLOCK = 128
EPS = 1e-6

kernel_name = 'hybrid_diffattn_hgrn2_mla_grouped_moe_dit'


def rmsnorm(x, gain):
    xf = x.astype(jnp.float32)
    y = xf * lax.rsqrt(jnp.mean(xf * xf, axis=-1, keepdims=True) + EPS)
    return y.astype(x.dtype) * gain


def axial_rope_tables(n_tokens, rot_dim):
    rows = n_tokens // GRID_W
    row = jnp.repeat(jnp.arange(rows, dtype=jnp.int32), GRID_W)
    col = jnp.tile(jnp.arange(GRID_W, dtype=jnp.int32), rows)
    axis_dim = rot_dim // 2
    inv_freq = ROPE_BASE ** (-jnp.arange(0, axis_dim, 2, dtype=jnp.float32) / axis_dim)
    ang_r = row.astype(jnp.float32)[:, None] * inv_freq
    ang_c = col.astype(jnp.float32)[:, None] * inv_freq
    return (jnp.cos(ang_r), jnp.sin(ang_r), jnp.cos(ang_c), jnp.sin(ang_c))


def _rope_1d(x, cos, sin):
    x1, x2 = jnp.split(x, 2, axis=-1)
    cos = cos.astype(x.dtype)
    sin = sin.astype(x.dtype)
    return jnp.concatenate([x1 * cos - x2 * sin, x1 * sin + x2 * cos], axis=-1)


def apply_axial_rope(x, tables):
    cos_r, sin_r, cos_c, sin_c = tables
    xr, xc = jnp.split(x, 2, axis=-1)
    return jnp.concatenate([_rope_1d(xr, cos_r, sin_r), _rope_1d(xc, cos_c, sin_c)], axis=-1)


def sweep_query_blocks(fn, qs, axis):
    n_q = qs[0].shape[axis]
    n_blocks = n_q // Q_BLOCK

    def split(q):
        q = q.reshape(q.shape[:axis] + (n_blocks, Q_BLOCK) + q.shape[axis + 1:])
        return jnp.moveaxis(q, axis, 0)

    out = lax.map(fn, tuple(split(q) for q in qs))
    out = jnp.moveaxis(out, 0, 2)
    return out.reshape(out.shape[:2] + (n_q, out.shape[-1]))


def merge_heads(o):
    b, h, t, dv = o.shape
    return o.transpose(0, 2, 1, 3).reshape(b, t, h * dv)


def diff_heads(p, q_gain, k_gain):
    b, t, _ = p.shape
    q, k, v = jnp.split(p, [DIFF_QK_W, 2 * DIFF_QK_W], axis=-1)
    q = rmsnorm(q.reshape(b, t, DIFF_HEADS, 2, DIFF_HEAD_DIM), q_gain).transpose(0, 2, 3, 1, 4)
    k = rmsnorm(k.reshape(b, t, DIFF_HEADS, 2, DIFF_HEAD_DIM), k_gain).transpose(0, 2, 3, 1, 4)
    v = v.reshape(b, t, DIFF_HEADS, DIFF_V_DIM).transpose(0, 2, 1, 3)
    return q, k, v


def diff_attention(q, k, v, lam):
    scale = DIFF_HEAD_DIM ** -0.5

    def block(qs):
        (qb,) = qs
        s = jnp.einsum('bhcqd,bhckd->bhcqk', qb, k).astype(jnp.float32) * scale
        p = jax.nn.softmax(s, axis=-1)
        a = p[:, :, 0] - lam * p[:, :, 1]
        return jnp.einsum('bhqk,bhkv->bhqv', a.astype(v.dtype), v)

    return sweep_query_blocks(block, (q,), axis=3)


def diff_output(o, subln, lam_init):
    return merge_heads(rmsnorm(o, subln) * (1.0 - lam_init))


def gla_chunked(q, k, v, log_f, s0):
    b, h, t, dk = q.shape
    dv = v.shape[-1]
    c = HGRN_CHUNK
    n = t // c
    f32 = jnp.float32
    q = q.astype(f32).reshape(b, h, n, c, dk) * (dk ** -0.5)
    k = k.astype(f32).reshape(b, h, n, c, dk)
    v = v.astype(f32).reshape(b, h, n, c, dv)
    cum = jnp.cumsum(log_f.astype(f32).reshape(b, h, n, c, dk), axis=3)
    lower = jnp.tril(jnp.ones((c, c), dtype=bool))[:, :, None]
    decay = jnp.exp(jnp.where(lower, cum[:, :, :, :, None, :] - cum[:, :, :, None, :, :], -jnp.inf))
    scores = jnp.einsum('bhntk,bhnsk,bhntsk->bhnts', q, k, decay)
    o_intra = jnp.einsum('bhnts,bhnsv->bhntv', scores, v)
    last = cum[:, :, :, -1:, :]
    d_state = jnp.einsum('bhnsk,bhnsv->bhnkv', k * jnp.exp(last - cum), v)
    chunk_decay = jnp.exp(last[:, :, :, 0, :])

    def step(state, inp):
        a, ds = inp
        return a[..., None] * state + ds, state

    s_final, s_before = lax.scan(step, s0.astype(f32), (jnp.moveaxis(chunk_decay, 2, 0), jnp.moveaxis(d_state, 2, 0)))
    s_before = jnp.moveaxis(s_before, 0, 2)
    o_inter = jnp.einsum('bhntk,bhnkv->bhntv', q * jnp.exp(cum), s_before)
    return (o_intra + o_inter).reshape(b, h, t, dv), s_final


def hgrn_parse(p, lb_fwd, lb_bwd):
    b, t, _ = p.shape
    q, f_fw, f_bw, i, g = jnp.split(p, [HGRN_KW, 2 * HGRN_KW, 3 * HGRN_KW, 3 * HGRN_KW + HGRN_VW], axis=-1)

    def heads(z):
        return z.reshape(b, t, HGRN_HEADS, -1).transpose(0, 2, 1, 3)

    def forget(z, lb):
        lb = lb.reshape(HGRN_HEADS, 1, HGRN_K_DIM)
        f = lb + (1.0 - lb) * jax.nn.sigmoid(heads(z).astype(jnp.float32))
        return jnp.log(f), 1.0 - f

    return heads(jax.nn.silu(q)), heads(i), forget(f_fw, lb_fwd), forget(f_bw, lb_bwd), g


def hgrn_bidir(q, v, fwd, bwd, s_fwd0, s_bwd0):
    log_f_fw, k_fw = fwd
    log_f_bw, k_bw = bwd
    o_fw, s_fw = gla_chunked(q, k_fw, v, log_f_fw, s_fwd0)

    def rev(z):
        return jnp.flip(z, axis=2)

    o_bw, s_bw = gla_chunked(rev(q), rev(k_bw), rev(v), rev(log_f_bw), s_bwd0)
    return o_fw + rev(o_bw), s_fw, s_bw


def hgrn_output(o, g, gain):
    b, h, t, dv = o.shape
    o = rmsnorm(o.transpose(0, 2, 1, 3).astype(g.dtype), gain)
    return (o * jax.nn.silu(g).reshape(b, t, h, dv)).reshape(b, t, h * dv)


def even_mixer(h_ctx, h_lat, w_in, w_out, q_gain, k_gain, lam_vecs, subln, lam_init, lb_fwd, lb_bwd, o_gain, rope, with_ctx_out):
    p_ctx = h_ctx @ w_in
    p_lat = h_lat @ w_in
    qa_c, ka_c, va_c = diff_heads(p_ctx[..., :DIFF_IN_W], q_gain, k_gain)
    qa_l, ka_l, va_l = diff_heads(p_lat[..., :DIFF_IN_W], q_gain, k_gain)
    qa_l = apply_axial_rope(qa_l, rope)
    ka_l = apply_axial_rope(ka_l, rope)
    lv = lam_vecs.astype(jnp.float32)
    lam = jnp.exp(jnp.sum(lv[0] * lv[1])) - jnp.exp(jnp.sum(lv[2] * lv[3])) + lam_init
    oa_l = diff_attention(qa_l, jnp.concatenate([ka_c, ka_l], axis=3), jnp.concatenate([va_c, va_l], axis=2), lam)
    qb_c, vb_c, fw_c, bw_c, g_c = hgrn_parse(p_ctx[..., DIFF_IN_W:], lb_fwd, lb_bwd)
    qb_l, vb_l, fw_l, bw_l, g_l = hgrn_parse(p_lat[..., DIFF_IN_W:], lb_fwd, lb_bwd)
    s0 = jnp.zeros((h_ctx.shape[0], HGRN_HEADS, HGRN_K_DIM, HGRN_V_DIM), jnp.float32)
    ob_c, s_fw, s_bw = hgrn_bidir(qb_c, vb_c, fw_c, bw_c, s0, s0)
    ob_l, _, _ = hgrn_bidir(qb_l, vb_l, fw_l, bw_l, s_fw, s_bw)
    out_lat = jnp.concatenate([diff_output(oa_l, subln, lam_init), hgrn_output(ob_l, g_l, o_gain)], axis=-1) @ w_out
    out_ctx = None
    if with_ctx_out:
        oa_c = diff_attention(qa_c, ka_c, va_c, lam)
        out_ctx = jnp.concatenate([diff_output(oa_c, subln, lam_init), hgrn_output(ob_c, g_c, o_gain)], axis=-1) @ w_out
    return out_ctx, out_lat


def mla_queries(c_q, q_a_gain, w_uq, qn_gain, qr_gain):
    b, t, _ = c_q.shape
    q = (rmsnorm(c_q, q_a_gain) @ w_uq).reshape(b, t, MLA_HEADS, MLA_NOPE + MLA_ROPE).transpose(0, 2, 1, 3)
    return rmsnorm(q[..., :MLA_NOPE], qn_gain), rmsnorm(q[..., MLA_NOPE:], qr_gain)


def mla_keys_values(c_kv, k_rope, kv_a_gain, w_ukv, kn_gain, kr_gain):
    b, t, _ = c_kv.shape
    kv = (rmsnorm(c_kv, kv_a_gain) @ w_ukv).reshape(b, t, MLA_HEADS, MLA_NOPE + MLA_V).transpose(0, 2, 1, 3)
    return rmsnorm(kv[..., :MLA_NOPE], kn_gain), rmsnorm(k_rope, kr_gain), kv[..., MLA_NOPE:]


def mla_attention(q_nope, q_rope, k_nope, k_rope, v):
    scale = (MLA_NOPE + MLA_ROPE) ** -0.5

    def block(qs):
        qn, qr = qs
        s = jnp.einsum('bhqd,bhkd->bhqk', qn, k_nope) + jnp.einsum('bhqd,bkd->bhqk', qr, k_rope)
        p = jax.nn.softmax(s.astype(jnp.float32) * scale, axis=-1)
        return jnp.einsum('bhqk,bhkv->bhqv', p.astype(v.dtype), v)

    return sweep_query_blocks(block, (q_nope, q_rope), axis=2)


def odd_mixer(h_ctx, h_lat, w_in, q_a_gain, kv_a_gain, w_uq, w_ukv, qn_gain, qr_gain, kn_gain, kr_gain, w_out, rope, with_ctx_out):
    ckv_c, kr_c = jnp.split(h_ctx @ w_in[:, MLA_Q_LORA:], [MLA_KV_LORA], axis=-1)
    kn_c, kr_c, v_c = mla_keys_values(ckv_c, kr_c, kv_a_gain, w_ukv, kn_gain, kr_gain)
    cq_l, ckv_l, kr_l = jnp.split(h_lat @ w_in, [MLA_Q_LORA, MLA_Q_LORA + MLA_KV_LORA], axis=-1)
    kn_l, kr_l, v_l = mla_keys_values(ckv_l, kr_l, kv_a_gain, w_ukv, kn_gain, kr_gain)
    kr_l = apply_axial_rope(kr_l, rope)
    qn_l, qr_l = mla_queries(cq_l, q_a_gain, w_uq, qn_gain, qr_gain)
    qr_l = apply_axial_rope(qr_l, rope)
    o_l = mla_attention(qn_l, qr_l, jnp.concatenate([kn_c, kn_l], axis=2), jnp.concatenate([kr_c, kr_l], axis=1), jnp.concatenate([v_c, v_l], axis=2))
    out_lat = merge_heads(o_l) @ w_out
    out_ctx = None
    if with_ctx_out:
        qn_c, qr_c = mla_queries(h_ctx @ w_in[:, :MLA_Q_LORA], q_a_gain, w_uq, qn_gain, qr_gain)
        out_ctx = merge_heads(mla_attention(qn_c, qr_c, kn_c, kr_c, v_c)) @ w_out
    return out_ctx, out_lat


def swiglu(t, w_gate, w_up, w_down):
    return (jax.nn.silu(t @ w_gate) * (t @ w_up)) @ w_down


def moe_ffn(h, router_w, router_bias, w_gate, w_up, w_down, sw_gate, sw_up, sw_down):
    shape = h.shape
    t = h.reshape(-1, shape[-1])
    scores = jax.nn.sigmoid((t @ router_w).astype(jnp.float32))
    biased = scores + router_bias.astype(jnp.float32)
    grouped = biased.reshape(-1, N_GROUPS, EXPERTS_PER_GROUP)
    group_score = jnp.sum(lax.top_k(grouped, TOP_K)[0], axis=-1)
    best_group = jnp.argmax(group_score, axis=-1)
    in_group = (jnp.arange(N_GROUPS) == best_group[:, None])[:, :, None]
    masked = jnp.where(in_group, grouped, -jnp.inf).reshape(-1, N_EXPERTS)
    _, top_idx = lax.top_k(masked, TOP_K)
    top_w = jnp.take_along_axis(scores, top_idx, axis=-1)
    top_w = top_w / jnp.sum(top_w, axis=-1, keepdims=True)
    gates = jnp.sum(jax.nn.one_hot(top_idx, N_EXPERTS, dtype=jnp.float32) * top_w[..., None], axis=1).astype(t.dtype)
    out = swiglu(t, sw_gate, sw_up, sw_down)
    for e in range(N_EXPERTS):
        out = out + gates[:, e:e + 1] * swiglu(t, w_gate[e], w_up[e], w_down[e])
    return out.reshape(shape)


def setup_inputs(seed: int = 0) -> dict:
    key = jax.random.key(seed)
    ks = jax.random.split(key, 40)
    f32 = jnp.float32

    def nrm(k, shape, scale=1.0):
        return jax.random.normal(k, shape, f32) * scale

    def gain(k, shape):
        return 1.0 + 0.05 * jax.random.normal(k, shape, f32)

    D = D_MODEL
    return {
        'x': nrm(ks[0], (BATCH, SEQ, D)),
        'c': nrm(ks[1], (BATCH, D)),
        'ctx': nrm(ks[2], (BATCH, CTX_LEN, D)),
        'c_ctx': nrm(ks[3], (D,)),
        'mod_w': nrm(ks[4], (DEPTH, D, 6 * D), 0.5 * D ** -0.5),
        'mod_b': nrm(ks[5], (DEPTH, 6 * D), 0.02),
        'norm_mix': gain(ks[6], (DEPTH, D)),
        'norm_ffn': gain(ks[7], (DEPTH, D)),
        'even_w_in': nrm(ks[8], (N_EVEN, D, EVEN_IN_W), D ** -0.5),
        'even_w_out': nrm(ks[9], (N_EVEN, EVEN_OUT_W, D), EVEN_OUT_W ** -0.5),
        'diff_q_gain': gain(ks[10], (N_EVEN, DIFF_HEAD_DIM)),
        'diff_k_gain': gain(ks[11], (N_EVEN, DIFF_HEAD_DIM)),
        'diff_lambda': nrm(ks[12], (N_EVEN, 4, DIFF_HEAD_DIM), 0.1),
        'diff_subln': gain(ks[13], (N_EVEN, DIFF_V_DIM)),
        'hgrn_lb_logits': nrm(ks[14], (N_EVEN + 1, 2, HGRN_KW), 0.5),
        'hgrn_out_gain': gain(ks[15], (N_EVEN, HGRN_V_DIM)),
        'odd_w_in': nrm(ks[16], (N_ODD, D, ODD_IN_W), D ** -0.5),
        'mla_q_a_gain': gain(ks[17], (N_ODD, MLA_Q_LORA)),
        'mla_kv_a_gain': gain(ks[18], (N_ODD, MLA_KV_LORA)),
        'mla_w_uq': nrm(ks[19], (N_ODD, MLA_Q_LORA, MLA_HEADS * (MLA_NOPE + MLA_ROPE)), MLA_Q_LORA ** -0.5),
        'mla_w_ukv': nrm(ks[20], (N_ODD, MLA_KV_LORA, MLA_HEADS * (MLA_NOPE + MLA_V)), MLA_KV_LORA ** -0.5),
        'mla_q_nope_gain': gain(ks[21], (N_ODD, MLA_NOPE)),
        'mla_q_rope_gain': gain(ks[22], (N_ODD, MLA_ROPE)),
        'mla_k_nope_gain': gain(ks[23], (N_ODD, MLA_NOPE)),
        'mla_k_rope_gain': gain(ks[24], (N_ODD, MLA_ROPE)),
        'odd_w_out': nrm(ks[25], (N_ODD, ODD_OUT_W, D), ODD_OUT_W ** -0.5),
        'router_w': nrm(ks[26], (D, N_EXPERTS), D ** -0.5),
        'router_bias': nrm(ks[27], (N_EXPERTS,), 0.01),
        'expert_w_gate': nrm(ks[28], (DEPTH, N_EXPERTS, D, EXPERT_FF), D ** -0.5),
        'expert_w_up': nrm(ks[29], (DEPTH, N_EXPERTS, D, EXPERT_FF), D ** -0.5),
        'expert_w_down': nrm(ks[30], (DEPTH, N_EXPERTS, EXPERT_FF, D), EXPERT_FF ** -0.5),
        'shared_w_gate': nrm(ks[31], (DEPTH, D, SHARED_FF), D ** -0.5),
        'shared_w_up': nrm(ks[32], (DEPTH, D, SHARED_FF), D ** -0.5),
        'shared_w_down': nrm(ks[33], (DEPTH, SHARED_FF, D), SHARED_FF ** -0.5),
    }


def reference(x, c, ctx, c_ctx, mod_w, mod_b, norm_mix, norm_ffn,
              even_w_in, even_w_out, diff_q_gain, diff_k_gain, diff_lambda, diff_subln, hgrn_lb_logits, hgrn_out_gain,
              odd_w_in, mla_q_a_gain, mla_kv_a_gain, mla_w_uq, mla_w_ukv, mla_q_nope_gain, mla_q_rope_gain,
              mla_k_nope_gain, mla_k_rope_gain, odd_w_out,
              router_w, router_bias, expert_w_gate, expert_w_up, expert_w_down, shared_w_gate, shared_w_up, shared_w_down):
    n_lat = x.shape[1]
    rope = axial_rope_tables(n_lat, DIFF_HEAD_DIM)
    lb_all = jnp.cumsum(jax.nn.softmax(hgrn_lb_logits.astype(jnp.float32), axis=0), axis=0)
    s_c = jax.nn.silu(c)
    s_ctx = jax.nn.silu(c_ctx)
    for layer in range(DEPTH):
        with_ctx_out = layer < DEPTH - 1
        j = layer // 2
        mod_lat = (s_c @ mod_w[layer] + mod_b[layer])[:, None, :]
        mod_ctx = s_ctx @ mod_w[layer] + mod_b[layer]
        sh_m, sc_m, g_m, sh_f, sc_f, g_f = jnp.split(mod_lat, 6, axis=-1)
        csh_m, csc_m, cg_m, csh_f, csc_f, cg_f = jnp.split(mod_ctx, 6, axis=-1)
        h_lat = rmsnorm(x, norm_mix[layer]) * (1.0 + sc_m) + sh_m
        h_ctx = rmsnorm(ctx, norm_mix[layer]) * (1.0 + csc_m) + csh_m
        if layer % 2 == 0:
            lam_init = 0.8 - 0.6 * math.exp(-0.3 * layer)
            mix_ctx, mix_lat = even_mixer(h_ctx, h_lat, even_w_in[j], even_w_out[j], diff_q_gain[j], diff_k_gain[j],
                                          diff_lambda[j], diff_subln[j], lam_init, lb_all[j, 0], lb_all[j, 1],
                                          hgrn_out_gain[j], rope, with_ctx_out)
        else:
            mix_ctx, mix_lat = odd_mixer(h_ctx, h_lat, odd_w_in[j], mla_q_a_gain[j], mla_kv_a_gain[j], mla_w_uq[j],
                                         mla_w_ukv[j], mla_q_nope_gain[j], mla_q_rope_gain[j], mla_k_nope_gain[j],
                                         mla_k_rope_gain[j], odd_w_out[j], rope, with_ctx_out)
        x = x + g_m * mix_lat
        hf = rmsnorm(x, norm_ffn[layer]) * (1.0 + sc_f) + sh_f
        x = x + g_f * moe_ffn(hf, router_w, router_bias, expert_w_gate[layer], expert_w_up[layer], expert_w_down[layer],
                              shared_w_gate[layer], shared_w_up[layer], shared_w_down[layer])
        if with_ctx_out:
            ctx = ctx + cg_m * mix_ctx
            hfc = rmsnorm(ctx, norm_ffn[layer]) * (1.0 + csc_f) + csh_f
            ctx = ctx + cg_f * moe_ffn(hfc, router_w, router_bias, expert_w_gate[layer], expert_w_up[layer],
                                       expert_w_down[layer], shared_w_gate[layer], shared_w_up[layer], shared_w_down[layer])
    return x
```

```python
import math
import numpy as np
from contextlib import ExitStack
import concourse.bass as bass
import concourse.mybir as mybir
from concourse.bass_utils import run_bass_kernel_spmd

F32 = mybir.dt.float32
BF16 = mybir.dt.bfloat16
AF = mybir.ActivationFunctionType
ALU = mybir.AluOpType
AX = mybir.AxisListType

D = 1024
SEQ = 2048
CTX = 256
NTOK = SEQ + CTX
NT = NTOK // 128
BLOCKS = [(0, 256)] + [(256 + 512 * i, 512) for i in range(4)]
EPS = 1e-6
NSLOT = 4
SLOT_ELEMS = 4096

C_ID = 0
C_ONES = 128
C_BLK64 = 256
C_PERM = 384
C_TRIF = 512
C_TRIB = 640
C_AFTF = 768
C_AFTB = 896
C_N = 1024


def _consts():
    c = np.zeros((128, C_N), np.float32)
    i = np.arange(128)
    s = i[:, None]
    t = i[None, :]
    same = (s // 64) == (t // 64)
    c[:, C_ID:C_ID + 128] = (s == t)
    c[:, C_ONES:C_ONES + 128] = 1.0
    c[:, C_BLK64:C_BLK64 + 128] = same
    partner = np.where((i % 32) < 16, i + 16, i - 16)
    c[:, C_PERM:C_PERM + 128] = (s == partner[None, :])
    c[:, C_TRIF:C_TRIF + 128] = same & (s <= t)
    c[:, C_TRIB:C_TRIB + 128] = same & (s >= t)
    c[:, C_AFTF:C_AFTF + 128] = same & (s > t)
    c[:, C_AFTB:C_AFTB + 128] = same & (s < t)
    return c


def _rope_tables():
    tok = np.arange(SEQ)
    row = (tok // 64).astype(np.float32)
    col = (tok % 64).astype(np.float32)
    inv = (10000.0 ** (-np.arange(0, 32, 2, dtype=np.float32) / 32.0)).astype(np.float32)
    C = np.zeros((128, SEQ), np.float32)
    S = np.zeros((128, SEQ), np.float32)
    for p in range(128):
        d = p % 64
        pos = row if d < 32 else col
        f = inv[d % 16]
        ang = (pos * f).astype(np.float32)
        C[p] = np.cos(ang)
        S[p] = np.sin(ang) * (-1.0 if (d % 32) < 16 else 1.0)
    return np.stack([C, S], axis=1)


class PP:
    pass


def _pp_layout():
    off = {}
    n = 0

    def add(name, w):
        nonlocal n
        off[name] = (n, w)
        n += w
    add("c", 8)
    add("cctx", 8)
    add("modb", 2 * 48)
    add("nmix", 16)
    add("nffn", 16)
    add("rw", 8 * 16)
    add("rbias", 16)
    add("dqg", 1)
    add("dkg", 1)
    add("dlam", 256)
    add("subln", 1)
    add("lbfm", 2 * 2 * 4)
    add("hog", 128)
    add("qag", 3)
    add("kvag", 2)
    add("qng", 1)
    add("qrg", 1)
    add("kng", 1)
    add("krg", 1)
    return off, n


PPO, PPN = _pp_layout()


def _fm(v):
    v = np.asarray(v, np.float32)
    return np.ascontiguousarray(v.reshape(-1, 128).T)


def _pack_params(b, inp):
    pp = np.zeros((128, PPN), np.float32)

    def put(name, arr):
        o, w = PPO[name]
        arr = np.asarray(arr, np.float32).reshape(128, -1)
        assert arr.shape[1] == w, (name, arr.shape, w)
        pp[:, o:o + w] = arr
    put("c", _fm(inp["c"][b]))
    put("cctx", _fm(inp["c_ctx"]))
    put("modb", np.concatenate([_fm(inp["mod_b"][0]), _fm(inp["mod_b"][1])], axis=1))
    put("nmix", np.concatenate([_fm(inp["norm_mix"][0]), _fm(inp["norm_mix"][1])], axis=1))
    put("nffn", np.concatenate([_fm(inp["norm_ffn"][0]), _fm(inp["norm_ffn"][1])], axis=1))
    rw = np.asarray(inp["router_w"], np.float32).reshape(8, 128, 16).transpose(1, 0, 2)
    put("rw", rw.reshape(128, 128))
    put("rbias", np.broadcast_to(np.asarray(inp["router_bias"], np.float32)[None, :], (128, 16)))
    put("dqg", np.tile(np.asarray(inp["diff_q_gain"][0], np.float32), 2)[:, None])
    put("dkg", np.tile(np.asarray(inp["diff_k_gain"][0], np.float32), 2)[:, None])
    put("dlam", np.broadcast_to(np.asarray(inp["diff_lambda"][0], np.float32).reshape(1, 256), (128, 256)))
    put("subln", np.asarray(inp["diff_subln"][0], np.float32)[:, None])
    lb = np.asarray(inp["hgrn_lb_logits"], np.float32)
    put("lbfm", lb.reshape(2, 2, 4, 128).transpose(3, 0, 1, 2).reshape(128, 16))
    put("hog", np.broadcast_to(np.asarray(inp["hgrn_out_gain"][0], np.float32)[None, :], (128, 128)))
    put("qag", _fm(inp["mla_q_a_gain"][0]))
    put("kvag", _fm(inp["mla_kv_a_gain"][0]))
    put("qng", np.asarray(inp["mla_q_nope_gain"][0], np.float32)[:, None])
    put("qrg", np.tile(np.asarray(inp["mla_q_rope_gain"][0], np.float32), 2)[:, None])
    put("kng", np.asarray(inp["mla_k_nope_gain"][0], np.float32)[:, None])
    put("krg", np.tile(np.asarray(inp["mla_k_rope_gain"][0], np.float32), 2)[:, None])
    return pp


class Res:
    __slots__ = ("name", "w", "rs", "dsem", "dcnt")

    def __init__(self, name):
        self.name = name
        self.w = None
        self.rs = {}
        self.dsem = None
        self.dcnt = 0


class Eng:
    def __init__(self, name, obj, sem):
        self.name = name
        self.obj = obj
        self.sem = sem
        self.cnt = 0
        self.seen = {}


class KB:
    def __init__(self, nc, es):
        self.nc = nc
        self.es = es
        self.sems = {}
        self.E = {}
        for name, obj in (("pe", nc.tensor), ("act", nc.scalar), ("dve", nc.vector),
                          ("pool", nc.gpsimd), ("sp", nc.sync)):
            sem = es.enter_context(nc.semaphore("s_" + name))
            self.sems[name] = sem
            self.E[name] = Eng(name, obj, sem)
        self.nres = 0

    def res(self, name=None):
        self.nres += 1
        return Res(name or ("r%d" % self.nres))

    def _wait(self, E, reads, writes):
        deps = {}

        def add(d):
            if d is None:
                return
            k, v = d
            if deps.get(k, 0) < v:
                deps[k] = v
        for r in reads:
            add(r.w)
        for w in writes:
            add(w.w)
            for k, v in w.rs.items():
                add((k, v))
        for k, v in deps.items():
            if k == E.name and E.name == "pe":
                continue
            if E.seen.get(k, 0) < v:
                E.obj.wait_ge(self.sems[k], v)
                E.seen[k] = v

    def op(self, eng, fn, reads=(), writes=()):
        E = self.E[eng]
        self._wait(E, reads, writes)
        ins = None
        if callable(fn):
            ins = fn()
        else:
            for f in fn:
                ins = f()
        E.cnt += 1
        ins.then_inc(E.sem, 1)
        dep = (E.name, E.cnt)
        for r in reads:
            if r.rs.get(E.name, 0) < E.cnt:
                r.rs[E.name] = E.cnt
        for w in writes:
            w.w = dep
            w.rs = {}
        return ins

    def dma(self, queue, fn, res, is_write, reads=(), writes=()):
        E = self.E[queue]
        if res.dsem is None:
            self.nres += 1
            key = "d%d_%s" % (self.nres, res.name)
            res.dsem = key
            self.sems[key] = self.es.enter_context(self.nc.semaphore(key))
        rr = list(reads) + ([] if is_write else [res])
        ww = list(writes) + ([res] if is_write else [])
        self._wait(E, rr, ww)
        ins = fn()
        res.dcnt += 16
        ins.then_inc(self.sems[res.dsem], 16)
        dep = (res.dsem, res.dcnt)
        for r in rr:
            if r.rs.get(res.dsem, 0) < res.dcnt:
                r.rs[res.dsem] = res.dcnt
        for w in ww:
            w.w = dep
            w.rs = {}
        return dep

    def barrier(self):
        for E in self.E.values():
            for F in self.E.values():
                if F is E or F.cnt == 0:
                    continue
                if E.seen.get(F.name, 0) < F.cnt:
                    E.obj.wait_ge(self.sems[F.name], F.cnt)
                    E.seen[F.name] = F.cnt
            if E.name != "pe" and E.cnt > 0 and E.seen.get(E.name, 0) < E.cnt:
                E.obj.wait_ge(self.sems[E.name], E.cnt)
                E.seen[E.name] = E.cnt

    def final_wait(self, eng, deps):
        E = self.E[eng]
        for k, v in deps:
            E.obj.wait_ge(self.sems[k], v)


def build_program(flags=None):
    flags = flags or {}
    do_mix0 = flags.get("mix0", True)
    do_mix1 = flags.get("mix1", True)
    do_ffn = flags.get("ffn", True)
    taps = flags.get("taps", False)

    nc = bass.Bass("TRN2", target_bir_lowering=False)

    def din(name, shape):
        return nc.dram_tensor(name, list(shape), F32, kind="ExternalInput").ap()
    x_d = din("x", [SEQ, D])
    ctx_d = din("ctx", [CTX, D])
    pp_d = din("pp", [128, PPN])
    cst_d = din("cst", [128, C_N])
    rope_d = din("rope", [128, 2, SEQ])
    mod_w_d = din("mod_w", [2, D, 6 * D])
    ewin_d = din("even_w_in", [D, 4096])
    ewout_d = din("even_w_out", [D, D])
    owin_d = din("odd_w_in", [D, 704])
    wuq_d = din("mla_w_uq", [384, 1536])
    wukv_d = din("mla_w_ukv", [256, 2048])
    owout_d = din("odd_w_out", [D, D])
    xg_d = din("expert_w_gate", [2, 16, D, 512])
    xu_d = din("expert_w_up", [2, 16, D, 512])
    xd_d = din("expert_w_down", [2, 16, 512, D])
    sg_d = din("shared_w_gate", [2, D, 512])
    su_d = din("shared_w_up", [2, D, 512])
    sd_d = din("shared_w_down", [2, 512, D])
    y_d = nc.dram_tensor("y", [SEQ, D], F32, kind="ExternalOutput").ap()
    tap_d = None
    if taps:
        tap_d = nc.dram_tensor("tap", [NTOK, D], F32, kind="ExternalOutput").ap()

    es = ExitStack()
    with es:
        kb = KB(nc, es)

        def sb(name, shape, dt=F32):
            return es.enter_context(nc.sbuf_tensor(name, list(shape), dt))

        X = sb("X", [128, NT, D])
        XR = [[kb.res("x%d_%d" % (t, h)) for h in range(2)] for t in range(NT)]
        HT = sb("HT", [128, 8, NTOK], BF16)
        HTR = [kb.res("ht%d" % i) for i in range(len(BLOCKS))]
        PPT = sb("PPT", [128, PPN])
        PPR = kb.res("pp")
        CST = sb("CST", [128, C_N])
        CSTB = sb("CSTB", [128, C_N], BF16)
        CSTR = kb.res("cst")
        RING = [sb("ring%d" % i, [128, SLOT_ELEMS], BF16) for i in range(NSLOT)]
        RINGR = [kb.res("ring%d" % i) for i in range(NSLOT)]
        ring_pos = [0]
        PS = [es.enter_context(nc.psum_tensor("ps%d" % i, [128, 512], F32)) for i in range(8)]
        PSR = [kb.res("ps%d" % i) for i in range(8)]
        MODV = sb("MODV", [128, 2, 48, 2])
        MODR = kb.res("modv")
        NA = sb("NA", [128, 2, 8])
        NB = sb("NB", [128, 2, 8])
        NAR = kb.res("na")
        SS = sb("SS", [128, NT])
        SSR = kb.res("ss")
        RSTD = sb("RSTD", [128, NT])
        RSTDR = kb.res("rstd")
        GBC = sb("GBC", [128, 2, D], BF16)
        GBCR = kb.res("gbc")
        GATES = sb("GATES", [128, NT, 16])
        GATESR = kb.res("gates")
        SCB = sb("SCB", [128, 8, 2], BF16)
        SCR = kb.res("scb")

        scope_id = [0]

        class Scope:
            def __enter__(self):
                kb.barrier()
                scope_id[0] += 1
                self.sid = scope_id[0]
                self.stack = ExitStack()
                self.stack.__enter__()
                return self

            def sb(self, name, shape, dt=F32):
                return self.stack.enter_context(nc.sbuf_tensor("%s_s%d" % (name, self.sid), list(shape), dt))

            def __exit__(self, *exc):
                kb.barrier()
                return self.stack.__exit__(*exc)
        SM = [sb("SM%d" % i, [128, 64]) for i in range(8)]
        SMR = [kb.res("sm%d" % i) for i in range(8)]

        v = nc.vector
        a = nc.scalar
        g = nc.gpsimd
        pe = nc.tensor

        def ppc(name, i=0, w=1):
            o, _ = PPO[name]
            return PPT[:, o + i:o + i + w]

        kb.dma("sp", lambda: nc.sync.dma_start(out=PPT[:], in_=pp_d), PPR, True)
        kb.dma("sp", lambda: nc.sync.dma_start(out=CST[:], in_=cst_d), CSTR, True)
        kb.op("dve", lambda: v.tensor_copy(out=CSTB[:], in_=CST[:]), reads=[CSTR], writes=[CSTR])
        for t in range(NT):
            src = ctx_d[t * 128:(t + 1) * 128, :] if t < 2 else x_d[(t - 2) * 128:(t - 1) * 128, :]
            for h in range(2):
                kb.dma("sp", lambda src=src, t=t, h=h: nc.sync.dma_start(
                    out=X[:, t, h * 512:(h + 1) * 512], in_=src[:, h * 512:(h + 1) * 512]), XR[t][h], True)

        IDENT = CST[:, C_ID:C_ID + 128]

        def wload(src2d, kc, cols):
            assert kc * cols <= SLOT_ELEMS
            i = ring_pos[0] % NSLOT
            ring_pos[0] += 1
            view = RING[i][:, 0:kc * cols].rearrange("p (k c) -> p k c", k=kc)
            kb.dma("pool", lambda: g.dma_start(out=view, in_=src2d.rearrange("(k p) c -> p k c", p=128)),
                   RINGR[i], True)
            return RINGR[i], view

        def silu_small(out_ap, in_ap, sm_i, width, reads, writes):
            t1 = SM[sm_i][:, 0:width]
            kb.op("act", lambda: a.activation(out=t1, in_=in_ap, func=AF.Exp, scale=-1.0),
                  reads=reads, writes=[SMR[sm_i]])
            kb.op("dve", lambda: v.tensor_scalar(out=t1, in0=t1, scalar1=1.0, scalar2=None, op0=ALU.add),
                  reads=[SMR[sm_i]], writes=[SMR[sm_i]])
            kb.op("dve", lambda: v.reciprocal(out=t1, in_=t1), reads=[SMR[sm_i]], writes=[SMR[sm_i]])
            kb.op("dve", lambda: v.tensor_tensor(out=out_ap, in0=in_ap, in1=t1, op=ALU.mult),
                  reads=list(reads) + [SMR[sm_i]], writes=writes)

        silu_small(SCB[:, :, 0], ppc("c", 0, 8), 0, 8, [PPR], [SCR])
        silu_small(SCB[:, :, 1], ppc("cctx", 0, 8), 1, 8, [PPR, SCR], [SCR])

        for l in range(2):
            for s in range(12):
                r, wv = wload(mod_w_d[l, :, s * 512:(s + 1) * 512], 8, 512)
                pb = 0
                fns = []
                for j in range(4):
                    for k in range(8):
                        fns.append(lambda j=j, k=k, wv=wv, s=s: pe.matmul(
                            PS[pb][:, (s * 4 + j) * 2:(s * 4 + j) * 2 + 2], lhsT=wv[:, k, j * 128:(j + 1) * 128],
                            rhs=SCB[:, k, :], start=(k == 0), stop=(k == 7)))
                kb.op("pe", fns, reads=[r, SCR], writes=[PSR[pb]])
            o, _ = PPO["modb"]
            for w_ in range(2):
                kb.op("dve", lambda l=l, w_=w_: v.tensor_tensor(
                    out=MODV[:, l, :, w_], in0=PS[0][:, 0:96].rearrange("p (c w) -> p c w", w=2)[:, :, w_],
                    in1=PPT[:, o + l * 48:o + (l + 1) * 48], op=ALU.add),
                    reads=[PSR[0], PPR], writes=[MODR])

        def bc_from_fm(dst_ap_fn, vec_col_fn, dst_res, extra_reads, HF32, HF32R):
            for half in range(2):
                pb = 1 + half
                fns = []
                for kk in range(4):
                    k = half * 4 + kk
                    kb.op("dve", lambda k=k, kk=kk, half=half: v.tensor_copy(
                        out=HF32[half][:, kk, :], in_=vec_col_fn(k).to_broadcast([128, 128])),
                        reads=extra_reads, writes=[HF32R[half]])
                for kk in range(4):
                    fns.append(lambda kk=kk, half=half, pb=pb: pe.matmul(
                        PS[pb][:, kk * 128:(kk + 1) * 128], lhsT=HF32[half][:, kk, :], rhs=IDENT,
                        start=True, stop=True))
                kb.op("pe", fns, reads=[HF32R[half], CSTR], writes=[PSR[pb]])
                kb.op("act", lambda half=half, pb=pb: a.copy(out=dst_ap_fn(half), in_=PS[pb][:]),
                      reads=[PSR[pb]], writes=[dst_res])

        def norm_phase(l, which, with_ctx, router, gate_vec):
            with Scope() as sc_:
                XN = [sc_.sb("XN", [128, D])] * 2
                XNR = [kb.res("xn")] * 2
                HF32 = [sc_.sb("HF32_%d" % i, [128, 8, 128]) for i in range(2)]
                HF32R = [kb.res("hf32_%d" % i) for i in range(2)]
                JUNK = sc_.sb("JUNK", [128, D], BF16)
                JUNKR = kb.res("junk")
                _norm_phase(l, which, with_ctx, router, XN, XNR, HF32, HF32R, JUNK, JUNKR)
                for w_ in range(2):
                    bc_from_fm(lambda half, w_=w_: GBC[:, w_, half * 512:(half + 1) * 512],
                               lambda k, w_=w_: MODV[:, l, gate_vec * 8 + k, w_:w_ + 1], GBCR, [MODR], HF32, HF32R)

        def _norm_phase(l, which, with_ctx, router, XN, XNR, HF32, HF32R, JUNK, JUNKR):
            nw = "nmix" if which == 0 else "nffn"
            vsh = 0 if which == 0 else 3
            vsc = vsh + 1
            for w_ in range(2):
                kb.op("dve", lambda w_=w_: v.scalar_tensor_tensor(
                    out=NA[:, w_, :], in0=MODV[:, l, vsc * 8:(vsc + 1) * 8, w_], scalar=1.0,
                    in1=ppc(nw, l * 8, 8), op0=ALU.add, op1=ALU.mult),
                    reads=[MODR, PPR], writes=[NAR])
                kb.op("dve", lambda w_=w_: v.tensor_copy(out=NB[:, w_, :], in_=MODV[:, l, vsh * 8:(vsh + 1) * 8, w_]),
                      reads=[MODR], writes=[NAR])
            tiles = list(range(NT)) if with_ctx else list(range(2, NT))
            for t in tiles:
                kb.op("act", lambda t=t: a.activation(out=JUNK[:], in_=X[:, t, :], func=AF.Square,
                                                      accum_out=SS[:, t:t + 1]),
                      reads=[XR[t][0], XR[t][1]], writes=[JUNKR, SSR])
            t0 = tiles[0]
            kb.op("dve", lambda: v.tensor_scalar(out=RSTD[:, t0:NT], in0=SS[:, t0:NT], scalar1=1.0 / D, scalar2=EPS,
                                                 op0=ALU.mult, op1=ALU.add), reads=[SSR], writes=[RSTDR])
            kb.op("act", lambda: a.activation(out=RSTD[:, t0:NT], in_=RSTD[:, t0:NT], func=AF.Ln),
                  reads=[RSTDR], writes=[RSTDR])
            kb.op("act", lambda: a.activation(out=RSTD[:, t0:NT], in_=RSTD[:, t0:NT], func=AF.Exp, scale=-0.5),
                  reads=[RSTDR], writes=[RSTDR])
            for idx, t in enumerate(tiles):
                p = idx % 2
                w_ = 1 if t < 2 else 0
                blk = 0 if t < 2 else 1 + (t - 2) // 4
                kb.op("dve", lambda t=t, p=p: v.tensor_scalar(out=XN[p][:], in0=X[:, t, :], scalar1=RSTD[:, t:t + 1],
                                                              scalar2=None, op0=ALU.mult),
                      reads=[XR[t][0], XR[t][1], RSTDR], writes=[XNR[p]])
                for half in range(2):
                    pb = 1 + half
                    fns = [lambda kk=kk, half=half, pb=pb, p=p: pe.matmul(
                        PS[pb][:, kk * 128:(kk + 1) * 128], lhsT=XN[p][:, (half * 4 + kk) * 128:(half * 4 + kk + 1) * 128],
                        rhs=IDENT, start=True, stop=True) for kk in range(4)]
                    kb.op("pe", fns, reads=[XNR[p], CSTR], writes=[PSR[pb]])
                    for kk in range(4):
                        k = half * 4 + kk
                        kb.op("act", lambda kk=kk, k=k, pb=pb, p=p, w_=w_: a.activation(
                            out=HF32[p][:, k, :], in_=PS[pb][:, kk * 128:(kk + 1) * 128], func=AF.Identity,
                            scale=NA[:, w_, k:k + 1], bias=NB[:, w_, k:k + 1]),
                            reads=[PSR[pb], NAR], writes=[HF32R[p]])
                kb.op("pool", lambda t=t, p=p: g.tensor_copy(out=HT[:, :, t * 128:(t + 1) * 128], in_=HF32[p][:]),
                      reads=[HF32R[p]], writes=[HTR[blk]])
                if router:
                    o, _ = PPO["rw"]
                    fns = [lambda k=k, p=p: pe.matmul(PS[3][:, 0:16], lhsT=HF32[p][:, k, :],
                                                      rhs=PPT[:, o + k * 16:o + (k + 1) * 16],
                                                      start=(k == 0), stop=(k == 7)) for k in range(8)]
                    kb.op("pe", fns, reads=[HF32R[p], PPR], writes=[PSR[3]])
                    route(t)

        def route(t):
            S = SM[2]
            R = SMR[2]
            sc = S[:, 0:16]
            bi = S[:, 16:32]
            t2 = S[:, 32:48]
            m1 = SM[3][:, 0:4]
            m2 = SM[3][:, 4:8]
            gs = SM[3][:, 8:12]
            gm = SM[3][:, 12:13]
            ing = SM[3][:, 16:20]
            den = SM[3][:, 20:21]
            R3 = SMR[3]
            kb.op("act", lambda: a.activation(out=sc, in_=PS[3][:, 0:16], func=AF.Exp, scale=-1.0),
                  reads=[PSR[3]], writes=[R])
            kb.op("dve", lambda: v.tensor_scalar(out=sc, in0=sc, scalar1=1.0, scalar2=None, op0=ALU.add),
                  reads=[R], writes=[R])
            kb.op("dve", lambda: v.reciprocal(out=sc, in_=sc), reads=[R], writes=[R])
            kb.op("dve", lambda: v.tensor_tensor(out=bi, in0=sc, in1=ppc("rbias", 0, 16), op=ALU.add),
                  reads=[R, PPR], writes=[R])
            b3 = bi.rearrange("p (g e) -> p g e", e=4)
            t3 = t2.rearrange("p (g e) -> p g e", e=4)
            kb.op("dve", lambda: v.tensor_reduce(out=m1, in_=b3, axis=AX.X, op=ALU.max), reads=[R], writes=[R3])
            kb.op("dve", lambda: v.tensor_tensor(out=t3, in0=b3, in1=m1.unsqueeze(2).to_broadcast([128, 4, 4]),
                                                 op=ALU.is_equal), reads=[R, R3], writes=[R])
            kb.op("dve", lambda: v.scalar_tensor_tensor(out=t2, in0=t2, scalar=-1e9, in1=bi, op0=ALU.mult, op1=ALU.add),
                  reads=[R], writes=[R])
            kb.op("dve", lambda: v.tensor_reduce(out=m2, in_=t3, axis=AX.X, op=ALU.max), reads=[R], writes=[R3])
            kb.op("dve", lambda: v.tensor_tensor(out=gs, in0=m1, in1=m2, op=ALU.add), reads=[R3], writes=[R3])
            kb.op("dve", lambda: v.tensor_reduce(out=gm, in_=gs, axis=AX.X, op=ALU.max), reads=[R3], writes=[R3])
            kb.op("dve", lambda: v.tensor_scalar(out=ing, in0=gs, scalar1=gm, scalar2=None, op0=ALU.is_ge),
                  reads=[R3], writes=[R3])
            kb.op("dve", lambda: v.tensor_tensor(out=t3, in0=b3, in1=m2.unsqueeze(2).to_broadcast([128, 4, 4]),
                                                 op=ALU.is_ge), reads=[R, R3], writes=[R])
            kb.op("dve", lambda: v.tensor_tensor(out=t3, in0=t3, in1=ing.unsqueeze(2).to_broadcast([128, 4, 4]),
                                                 op=ALU.mult), reads=[R, R3], writes=[R])
            kb.op("dve", lambda: v.tensor_tensor(out=t2, in0=t2, in1=sc, op=ALU.mult), reads=[R], writes=[R])
            kb.op("dve", lambda: v.tensor_reduce(out=den, in_=t2, axis=AX.X, op=ALU.add), reads=[R], writes=[R3])
            kb.op("dve", lambda: v.reciprocal(out=den, in_=den), reads=[R3], writes=[R3])
            kb.op("dve", lambda: v.tensor_scalar(out=GATES[:, t, :], in0=t2, scalar1=den, scalar2=None, op0=ALU.mult),
                  reads=[R, R3], writes=[GATESR])

        def moe_phase(l, with_ctx):
            with Scope() as sc_:
                _moe_phase(l, with_ctx, sc_.sb)

        def _moe_phase(l, with_ctx, psb):
            ACTT = [psb("ACTT%d" % i, [128, 4, 512], BF16) for i in range(2)]
            ACTTR = [kb.res("actt%d" % i) for i in range(2)]
            SIL = [psb("SIL%d" % i, [128, 512], BF16) for i in range(2)]
            SILR = [kb.res("sil%d" % i) for i in range(2)]
            TMP = [psb("TMP%d" % i, [128, 512]) for i in range(4)]
            TMPR = [kb.res("tmp%d" % i) for i in range(4)]
            blocks = BLOCKS if with_ctx else BLOCKS[1:]
            cnt = 0
            for e in range(17):
                if e < 16:
                    srcs = (xg_d[l, e], xu_d[l, e], xd_d[l, e])
                else:
                    srcs = (sg_d[l], su_d[l], sd_d[l])
                rg, vg = wload(srcs[0], 8, 512)
                ru, vu = wload(srcs[1], 8, 512)
                rd, vd = wload(srcs[2], 4, 1024)
                for bi_, (t0, n) in enumerate(blocks):
                    blk = BLOCKS.index((t0, n))
                    ab = cnt % 2
                    cnt += 1
                    for j in range(4):
                        pg = (j % 2)
                        pu = 2 + (j % 2)
                        kb.op("pe", [lambda k=k, j=j, pg=pg: pe.matmul(
                            PS[pg][:, 0:n], lhsT=vg[:, k, j * 128:(j + 1) * 128], rhs=HT[:, k, t0:t0 + n],
                            start=(k == 0), stop=(k == 7)) for k in range(8)],
                            reads=[rg, HTR[blk]], writes=[PSR[pg]])
                        kb.op("pe", [lambda k=k, j=j, pu=pu: pe.matmul(
                            PS[pu][:, 0:n], lhsT=vu[:, k, j * 128:(j + 1) * 128], rhs=HT[:, k, t0:t0 + n],
                            start=(k == 0), stop=(k == 7)) for k in range(8)],
                            reads=[ru, HTR[blk]], writes=[PSR[pu]])
                        sl = j % 2
                        kb.op("act", lambda sl=sl, pg=pg: a.activation(out=SIL[sl][:, 0:n], in_=PS[pg][:, 0:n], func=AF.Silu),
                              reads=[PSR[pg]], writes=[SILR[sl]])
                        kb.op("dve", lambda sl=sl, pu=pu, j=j, ab=ab: v.tensor_tensor(
                            out=ACTT[ab][:, j, 0:n], in0=SIL[sl][:, 0:n], in1=PS[pu][:, 0:n], op=ALU.mult),
                            reads=[SILR[sl], PSR[pu]], writes=[ACTTR[ab]])
                    for tt in range(n // 128):
                        t = (t0 + tt * 128) // 128
                        w_ = 1 if t < 2 else 0
                        for nh in range(2):
                            pd = 4 + ((tt * 2 + nh) % 4)
                            kb.op("pe", [lambda j=j, tt=tt, nh=nh, pd=pd, ab=ab: pe.matmul(
                                PS[pd][:], lhsT=ACTT[ab][:, j, tt * 128:(tt + 1) * 128],
                                rhs=vd[:, j, nh * 512:(nh + 1) * 512], start=(j == 0), stop=(j == 3)) for j in range(4)],
                                reads=[rd, ACTTR[ab]], writes=[PSR[pd]])
                            ti = (tt * 2 + nh) % 4
                            if e < 16:
                                kb.op("dve", lambda pd=pd, t=t, e=e, ti=ti, nh=nh, w_=w_: v.scalar_tensor_tensor(
                                    out=TMP[ti][:], in0=PS[pd][:], scalar=GATES[:, t, e:e + 1],
                                    in1=GBC[:, w_, nh * 512:(nh + 1) * 512], op0=ALU.mult, op1=ALU.mult),
                                    reads=[PSR[pd], GATESR, GBCR], writes=[TMPR[ti]])
                            else:
                                kb.op("dve", lambda pd=pd, ti=ti, nh=nh, w_=w_: v.tensor_tensor(
                                    out=TMP[ti][:], in0=PS[pd][:], in1=GBC[:, w_, nh * 512:(nh + 1) * 512], op=ALU.mult),
                                    reads=[PSR[pd], GBCR], writes=[TMPR[ti]])
                            kb.op("pool", lambda t=t, nh=nh, ti=ti: g.tensor_tensor(
                                out=X[:, t, nh * 512:(nh + 1) * 512], in0=X[:, t, nh * 512:(nh + 1) * 512],
                                in1=TMP[ti][:], op=ALU.add),
                                reads=[TMPR[ti], XR[t][nh]], writes=[XR[t][nh]])

        ONESB = CSTB[:, C_ONES:C_ONES + 128]
        PERMB = CSTB[:, C_PERM:C_PERM + 128]
        EPSC = sb("EPSC", [128, 1])
        kb.op("dve", lambda: v.memset(EPSC[:], EPS), writes=[CSTR])

        def mm(out, lhsT, rhs, start, stop):
            return lambda: pe.matmul(out, lhsT=lhsT, rhs=rhs, start=start, stop=stop)

        class Common:
            def __init__(self, sc_):
                self.SQ = [sc_.sb("SQ%d" % i, [128, 512], BF16) for i in range(2)]
                self.SQR = [kb.res("sq%d" % i) for i in range(2)]
                self.RS = sc_.sb("RS", [128, 512])
                self.RSR = kb.res("rs")
                self.T1 = sc_.sb("T1", [128, 512])
                self.T1R = kb.res("t1")
                self.T2 = self.RS
                self.T2R = self.RSR
                self.RT = [sc_.sb("RT0", [128, 2, 512], BF16)] * 2
                self.RTR = [kb.res("rt0")] * 2
                self.rti = 0
                self.E = [sc_.sb("E%d" % i, [128, 512], BF16) for i in range(2)]
                self.ER = [kb.res("e%d" % i) for i in range(2)]
                self.sqi = 0

        def fm_rmsnorm(cm, banks, P, nfeat, gains, outs, n, out_res, ssb=7, ones_ap=None):
            srcs = [(PS[b][0:P, 0:n], PSR[b]) if isinstance(b, int) else b for b in banks]
            if ones_ap is None:
                ones_ap = ONESB[0:P, 0:P]
            nb = len(srcs)
            for j, (ap_, r_) in enumerate(srcs):
                q = cm.sqi % 2
                cm.sqi += 1
                kb.op("act", lambda ap_=ap_, q=q: a.activation(out=cm.SQ[q][0:P, 0:n], in_=ap_, func=AF.Square),
                      reads=[r_], writes=[cm.SQR[q]])
                kb.op("pe", mm(PS[ssb][0:P, 0:n], ones_ap, cm.SQ[q][0:P, 0:n], j == 0, j == nb - 1),
                      reads=[cm.SQR[q], CSTR], writes=[PSR[ssb]])
            kb.op("act", lambda: a.activation(out=cm.RS[0:P, 0:n], in_=PS[ssb][0:P, 0:n], func=AF.Ln,
                                              scale=1.0 / nfeat, bias=EPSC[0:P, :]),
                  reads=[PSR[ssb], CSTR], writes=[cm.RSR])
            kb.op("act", lambda: a.activation(out=cm.RS[0:P, 0:n], in_=cm.RS[0:P, 0:n], func=AF.Exp, scale=-0.5),
                  reads=[cm.RSR], writes=[cm.RSR])
            for j, (ap_, r_) in enumerate(srcs):
                kb.op("dve", lambda j=j, ap_=ap_: v.scalar_tensor_tensor(
                    out=outs[j], in0=ap_, scalar=gains[j], in1=cm.RS[0:P, 0:n], op0=ALU.mult, op1=ALU.mult),
                    reads=[r_, cm.RSR, PPR, CSTR], writes=[out_res])

        def rope_fm(cm, src, src_res, P, lt0, n, pb):
            i = cm.rti % 2
            cm.rti += 1
            kb.dma("pool", lambda: g.dma_start(out=cm.RT[i][:, :, 0:n], in_=rope_d[:, :, lt0:lt0 + n]), cm.RTR[i], True)
            kb.op("pe", mm(PS[pb][0:P, 0:n], PERMB[0:P, 0:P], src, True, True), reads=[src_res, CSTR], writes=[PSR[pb]])
            kb.op("dve", lambda: v.tensor_tensor(out=cm.T1[0:P, 0:n], in0=src, in1=cm.RT[i][0:P, 0, 0:n], op=ALU.mult),
                  reads=[src_res, cm.RTR[i]], writes=[cm.T1R])
            kb.op("dve", lambda: v.tensor_tensor(out=cm.T2[0:P, 0:n], in0=PS[pb][0:P, 0:n], in1=cm.RT[i][0:P, 1, 0:n], op=ALU.mult),
                  reads=[PSR[pb], cm.RTR[i]], writes=[cm.T2R])
            kb.op("dve", lambda: v.tensor_tensor(out=src, in0=cm.T1[0:P, 0:n], in1=cm.T2[0:P, 0:n], op=ALU.add),
                  reads=[cm.T1R, cm.T2R], writes=[src_res])

        def attn_core(cm, kts, n, s_terms, s_reads, v_fn, v_reads, scale, ob=2, zb=3):
            def emit_s(i):
                terms = s_terms(kts[i])
                kb.op("pe", [mm(PS[i % 2][:, 0:n], l_, r_, ti == 0, ti == len(terms) - 1) for ti, (l_, r_) in enumerate(terms)],
                      reads=s_reads, writes=[PSR[i % 2]])
            emit_s(0)
            last = len(kts) - 1
            for i, kt in enumerate(kts):
                if i < last:
                    emit_s(i + 1)
                eb = i % 2
                kb.op("act", lambda i=i, eb=eb: a.activation(out=cm.E[eb][:, 0:n], in_=PS[i % 2][:, 0:n], func=AF.Exp, scale=scale),
                      reads=[PSR[i % 2]], writes=[cm.ER[eb]])
                kb.op("pe", [mm(PS[ob][:, 0:n], v_fn(kt), cm.E[eb][:, 0:n], i == 0, i == last),
                             mm(PS[zb][:, 0:n], ONESB, cm.E[eb][:, 0:n], i == 0, i == last)],
                      reads=[cm.ER[eb], CSTR] + v_reads, writes=[PSR[ob], PSR[zb]])

        def wout_partial(cm, sc_, lhs_fn, lhs_reads, nk, wviews, wres, tiles, tag):
            TM = [cm.T1, cm.RS]
            TMR = [cm.T1R, cm.RSR]
            c_ = 0
            for t in tiles:
                w_ = 1 if t < 2 else 0
                for nh in range(2):
                    pb = 4 + (c_ % 4)
                    ti = c_ % 2
                    c_ += 1
                    kb.op("pe", [mm(PS[pb][:], lhs_fn(k, t), wviews[nh][:, k, :], k == 0, k == nk - 1) for k in range(nk)],
                          reads=lhs_reads(t) + [wres[nh]], writes=[PSR[pb]])
                    kb.op("dve", lambda pb=pb, ti=ti, nh=nh, w_=w_: v.tensor_tensor(
                        out=TM[ti][:], in0=PS[pb][:], in1=GBC[:, w_, nh * 512:(nh + 1) * 512], op=ALU.mult),
                        reads=[PSR[pb], GBCR], writes=[TMR[ti]])
                    kb.op("pool", lambda t=t, nh=nh, ti=ti: g.tensor_tensor(
                        out=X[:, t, nh * 512:(nh + 1) * 512], in0=X[:, t, nh * 512:(nh + 1) * 512], in1=TM[ti][:], op=ALU.add),
                        reads=[TMR[ti], XR[t][nh]], writes=[XR[t][nh]])

        def blk_of_tile(t):
            return 0 if t < 2 else 1 + (t - 2) // 4

        def mla_mixer():
            with Scope() as sc_:
                cm = Common(sc_)
                CQN = sc_.sb("CQN", [128, 3, SEQ], BF16)
                CQNR = kb.res("cqn")
                CKVN = sc_.sb("CKVN", [128, 2, NTOK], BF16)
                CKVNR = kb.res("ckvn")
                KRT = sc_.sb("KRT", [128, NTOK], BF16)
                KRTR = kb.res("krt")
                KNT = sc_.sb("KNT", [128, NTOK], BF16)
                KNTR = kb.res("knt")
                VH = sc_.sb("VH", [128, NT, 128], BF16)
                VHR = kb.res("vh")
                QNT = [sc_.sb("QNT%d" % i, [128, 512], BF16) for i in range(2)]
                QNTR = [kb.res("qnt%d" % i) for i in range(2)]
                QRT = [sc_.sb("QRT%d" % i, [128, 512], BF16) for i in range(2)]
                QRTR = [kb.res("qrt%d" % i) for i in range(2)]
                ra, wa = wload(owin_d[:, 0:384], 8, 384)
                rb, wb = wload(owin_d[:, 384:704], 8, 320)
                for blk, (t0, n) in enumerate(BLOCKS):
                    lat = blk > 0
                    if lat:
                        for j in range(3):
                            kb.op("pe", [mm(PS[4 + j][:, 0:n], wa[:, k, j * 128:(j + 1) * 128], HT[:, k, t0:t0 + n], k == 0, k == 7)
                                         for k in range(8)], reads=[ra, HTR[blk]], writes=[PSR[4 + j]])
                        fm_rmsnorm(cm, [4, 5, 6], 128, 384, [ppc("qag", j) for j in range(3)],
                                   [CQN[:, j, t0 - 256:t0 - 256 + n] for j in range(3)], n, CQNR)
                    for j in range(2):
                        kb.op("pe", [mm(PS[4 + j][:, 0:n], wb[:, k, j * 128:(j + 1) * 128], HT[:, k, t0:t0 + n], k == 0, k == 7)
                                     for k in range(8)], reads=[rb, HTR[blk]], writes=[PSR[4 + j]])
                    fm_rmsnorm(cm, [4, 5], 128, 256, [ppc("kvag", j) for j in range(2)],
                               [CKVN[:, j, t0:t0 + n] for j in range(2)], n, CKVNR)
                    kb.op("pe", [mm(PS[6][0:64, 0:n], wb[:, k, 256:320], HT[:, k, t0:t0 + n], k == 0, k == 7)
                                 for k in range(8)], reads=[rb, HTR[blk]], writes=[PSR[6]])
                    fm_rmsnorm(cm, [6], 64, 64, [ppc("krg")[0:64, :]], [KRT[0:64, t0:t0 + n]], n, KRTR)
                    if lat:
                        rope_fm(cm, KRT[0:64, t0:t0 + n], KRTR, 64, t0 - 256, n, 6)
                rq0, wq0 = wload(wuq_d[:, 0:768], 3, 768)
                rq1, wq1 = wload(wuq_d[:, 768:1536], 3, 768)
                rkv, wkv = wload(wukv_d, 2, 2048)
                scale = 192.0 ** -0.5
                qi = 0
                for h in range(8):
                    rq, wq = (rq0, wq0) if h < 4 else (rq1, wq1)
                    hq = h % 4
                    for blk, (t0, n) in enumerate(BLOCKS):
                        kb.op("pe", [mm(PS[4][:, 0:n], wkv[:, j, h * 256:h * 256 + 128], CKVN[:, j, t0:t0 + n], j == 0, j == 1)
                                     for j in range(2)], reads=[rkv, CKVNR], writes=[PSR[4]])
                        fm_rmsnorm(cm, [4], 128, 128, [ppc("kng")], [KNT[:, t0:t0 + n]], n, KNTR)
                    for g0 in range(0, NT, 4):
                        tl = list(range(g0, min(g0 + 4, NT)))
                        fns = []
                        for i, t in enumerate(tl):
                            for j in range(2):
                                fns.append(mm(PS[5][:, i * 128:(i + 1) * 128], CKVN[:, j, t * 128:(t + 1) * 128],
                                              wkv[:, j, h * 256 + 128:h * 256 + 256], j == 0, j == 1))
                        kb.op("pe", fns, reads=[rkv, CKVNR], writes=[PSR[5]])
                        kb.op("act", lambda g0=g0, tl=tl: a.copy(
                            out=VH[:, g0:g0 + len(tl), :],
                            in_=PS[5][:, 0:len(tl) * 128].rearrange("p (t c) -> p t c", c=128)),
                            reads=[PSR[5]], writes=[VHR])
                    for qb in range(4):
                        q0 = qb * 512
                        blk = qb + 1
                        qq = qi % 2
                        qi += 1
                        kb.op("pe", [mm(PS[4][:, 0:512], wq[:, j, hq * 192:hq * 192 + 128], CQN[:, j, q0:q0 + 512], j == 0, j == 2)
                                     for j in range(3)], reads=[rq, CQNR], writes=[PSR[4]])
                        fm_rmsnorm(cm, [4], 128, 128, [ppc("qng")], [QNT[qq][:, :]], 512, QNTR[qq])
                        kb.op("pe", [mm(PS[5][0:64, 0:512], wq[:, j, hq * 192 + 128:hq * 192 + 192], CQN[:, j, q0:q0 + 512], j == 0, j == 2)
                                     for j in range(3)], reads=[rq, CQNR], writes=[PSR[5]])
                        fm_rmsnorm(cm, [5], 64, 64, [ppc("qrg")[0:64, :]], [QRT[qq][0:64, :]], 512, QRTR[qq])
                        rope_fm(cm, QRT[qq][0:64, :], QRTR[qq], 64, q0, 512, 5)
                        attn_core(cm, list(range(NT)), 512,
                                  lambda kt, qq=qq: [(KNT[:, kt * 128:(kt + 1) * 128], QNT[qq][:, :]),
                                                     (KRT[0:64, kt * 128:(kt + 1) * 128], QRT[qq][0:64, :])],
                                  [KNTR, KRTR, QNTR[qq], QRTR[qq]],
                                  lambda kt: VH[:, kt, :], [VHR], scale)
                        kb.op("dve", lambda: v.reciprocal(out=cm.T1[:, :], in_=PS[3][:, :]), reads=[PSR[3]], writes=[cm.T1R])
                        kb.op("dve", lambda h=h, q0=q0: v.tensor_tensor(
                            out=HT[:, h, 256 + q0:256 + q0 + 512], in0=PS[2][:, :], in1=cm.T1[:, :], op=ALU.mult),
                            reads=[PSR[2], cm.T1R], writes=[HTR[blk]])
                rw0, wo0 = wload(owout_d[:, 0:512], 8, 512)
                rw1, wo1 = wload(owout_d[:, 512:1024], 8, 512)
                wout_partial(cm, sc_, lambda k, t: HT[:, k, t * 128:(t + 1) * 128], lambda t: [HTR[blk_of_tile(t)]],
                             8, [wo0, wo1], [rw0, rw1], list(range(2, NT)), "m")

        BLK64B = CSTB[:, C_BLK64:C_BLK64 + 128]
        ONEC = sb("ONEC", [128, 1])
        kb.op("dve", lambda: v.memset(ONEC[:], 1.0), writes=[CSTR])
        LAMC = sb("LAMC", [128, 4])
        LAMR = kb.res("lamc")
        OMLF = sb("OMLF", [128, 8])
        OMLFR = kb.res("omlf")
        LAM_INIT0 = 0.8 - 0.6 * math.exp(-0.3 * 0)

        def even_prep():
            o, _ = PPO["dlam"]
            S0, R0 = SM[4], SMR[4]
            S1, R1 = SM[5], SMR[5]
            for i in range(2):
                kb.op("dve", lambda i=i: v.tensor_tensor(out=S0[:, 0:64], in0=PPT[:, o + i * 128:o + i * 128 + 64],
                                                         in1=PPT[:, o + i * 128 + 64:o + i * 128 + 128], op=ALU.mult),
                      reads=[PPR], writes=[R0])
                kb.op("dve", lambda i=i: v.tensor_reduce(out=S1[:, i:i + 1], in_=S0[:, 0:64], axis=AX.X, op=ALU.add),
                      reads=[R0], writes=[R1])
            kb.op("act", lambda: a.activation(out=S1[:, 2:4], in_=S1[:, 0:2], func=AF.Exp), reads=[R1], writes=[R1])
            kb.op("dve", lambda: v.scalar_tensor_tensor(out=LAMC[:, 0:1], in0=S1[:, 3:4], scalar=-LAM_INIT0, in1=S1[:, 2:3],
                                                        op0=ALU.add, op1=ALU.subtract), reads=[R1], writes=[LAMR])
            kb.op("dve", lambda: v.tensor_scalar(out=LAMC[:, 1:2], in0=ppc("subln"), scalar1=1.0 - LAM_INIT0, scalar2=None,
                                                 op0=ALU.mult), reads=[PPR], writes=[LAMR])
            o2, _ = PPO["lbfm"]
            kb.op("dve", lambda: v.tensor_tensor(out=S0[:, 0:8], in0=PPT[:, o2:o2 + 8], in1=PPT[:, o2 + 8:o2 + 16], op=ALU.subtract),
                  reads=[PPR], writes=[R0])
            kb.op("act", lambda: a.activation(out=S0[:, 0:8], in_=S0[:, 0:8], func=AF.Exp), reads=[R0], writes=[R0])
            kb.op("dve", lambda: v.tensor_scalar(out=S0[:, 0:8], in0=S0[:, 0:8], scalar1=1.0, scalar2=None, op0=ALU.add),
                  reads=[R0], writes=[R0])
            kb.op("dve", lambda: v.reciprocal(out=OMLF[:, :], in_=S0[:, 0:8]), reads=[R0], writes=[OMLFR])

        def diff_part():
            with Scope() as sc_:
                cm = Common(sc_)
                KT = sc_.sb("KT", [128, NTOK], BF16)
                KTR = kb.res("kt")
                QT = sc_.sb("QT", [128, NTOK], BF16)
                QTR = kb.res("qt")
                VH = sc_.sb("VHd", [128, NT, 128], BF16)
                VHR = kb.res("vhd")
                CATA = sc_.sb("CATA", [128, 4, NTOK], BF16)
                CATAR = [kb.res("cata%d" % i) for i in range(len(BLOCKS))]
                rq, wq = wload(ewin_d[:, 0:512], 8, 512)
                rk, wk = wload(ewin_d[:, 512:1024], 8, 512)
                rv, wv = wload(ewin_d[:, 1024:1536], 8, 512)
                scale = 64.0 ** -0.5
                for h in range(4):
                    for (W, RW, DST, DSTR, gn) in ((wk, rk, KT, KTR, "dkg"), (wq, rq, QT, QTR, "dqg")):
                        for blk, (t0, n) in enumerate(BLOCKS):
                            kb.op("pe", [mm(PS[6][:, 0:n], W[:, k, h * 128:(h + 1) * 128], HT[:, k, t0:t0 + n], k == 0, k == 7)
                                         for k in range(8)], reads=[RW, HTR[blk]], writes=[PSR[6]])
                            fm_rmsnorm(cm, [6], 128, 64, [ppc(gn)], [DST[:, t0:t0 + n]], n, DSTR, ones_ap=BLK64B)
                            if blk > 0:
                                rope_fm(cm, DST[:, t0:t0 + n], DSTR, 128, t0 - 256, n, 6)
                    for g0 in range(0, NT, 4):
                        tl = list(range(g0, min(g0 + 4, NT)))
                        fns = []
                        for i, t in enumerate(tl):
                            for k in range(8):
                                fns.append(mm(PS[6][:, i * 128:(i + 1) * 128], HT[:, k, t * 128:(t + 1) * 128],
                                              wv[:, k, h * 128:(h + 1) * 128], k == 0, k == 7))
                        kb.op("pe", fns, reads=[rv] + [HTR[blk_of_tile(t)] for t in tl], writes=[PSR[6]])
                        kb.op("act", lambda g0=g0, tl=tl: a.copy(
                            out=VH[:, g0:g0 + len(tl), :],
                            in_=PS[6][:, 0:len(tl) * 128].rearrange("p (t c) -> p t c", c=128)),
                            reads=[PSR[6]], writes=[VHR])
                    for blk, (t0, n) in enumerate(BLOCKS):
                        kts = [0, 1] if blk == 0 else list(range(NT))
                        for c in range(2):
                            attn_core(cm, kts, n,
                                      lambda kt, c=c: [(KT[c * 64:(c + 1) * 64, kt * 128:(kt + 1) * 128],
                                                        QT[c * 64:(c + 1) * 64, t0:t0 + n])],
                                      [KTR, QTR], lambda kt: VH[:, kt, :], [VHR], scale, ob=2 + 2 * c, zb=3 + 2 * c)
                        kb.op("dve", lambda: v.reciprocal(out=cm.T1[:, 0:n], in_=PS[3][:, 0:n]), reads=[PSR[3]], writes=[cm.T1R])
                        kb.op("dve", lambda: v.tensor_tensor(out=cm.T1[:, 0:n], in0=PS[2][:, 0:n], in1=cm.T1[:, 0:n], op=ALU.mult),
                              reads=[PSR[2], cm.T1R], writes=[cm.T1R])
                        kb.op("dve", lambda: v.reciprocal(out=cm.RS[:, 0:n], in_=PS[5][:, 0:n]), reads=[PSR[5]], writes=[cm.RSR])
                        kb.op("dve", lambda: v.tensor_tensor(out=cm.RS[:, 0:n], in0=PS[4][:, 0:n], in1=cm.RS[:, 0:n], op=ALU.mult),
                              reads=[PSR[4], cm.RSR], writes=[cm.RSR])
                        kb.op("dve", lambda: v.scalar_tensor_tensor(out=cm.T1[:, 0:n], in0=cm.RS[:, 0:n], scalar=LAMC[:, 0:1],
                                                                    in1=cm.T1[:, 0:n], op0=ALU.mult, op1=ALU.add),
                              reads=[cm.RSR, cm.T1R, LAMR], writes=[cm.T1R])
                        fm_rmsnorm(cm, [(cm.T1[:, 0:n], cm.T1R)], 128, 128, [LAMC[:, 1:2]], [CATA[:, h, t0:t0 + n]], n, CATAR[blk])
                rw0, wo0 = wload(ewout_d[0:512, 0:512], 4, 512)
                rw1, wo1 = wload(ewout_d[0:512, 512:1024], 4, 512)
                wout_partial(cm, sc_, lambda k, t: CATA[:, k, t * 128:(t + 1) * 128], lambda t: [CATAR[blk_of_tile(t)]],
                             4, [wo0, wo1], [rw0, rw1], list(range(NT)), "d")

        def hgrn_part():
            with Scope() as sc_:
                _hgrn_part(sc_)

        def _hgrn_part(sc_):
            def T_(name, shape, dt=F32):
                return sc_.sb(name, shape, dt), kb.res(name)
            TA, TAR = T_("hTA", [128, 512])
            TB, TBR = T_("hTB", [128, 512])
            TC, TCR = T_("hTC", [128, 512])
            TD, TDR = T_("hTD", [128, 512])
            EC, ECR = T_("hEC", [128, 512])
            KBAR = [[T_("hKBAR%d%d" % (d, b), [128, 4, 128], BF16) for b in range(2)] for d in range(2)]
            KTIL = [[T_("hKTIL%d%d" % (d, b), [128, 512], BF16) for b in range(2)] for d in range(2)]
            QZ = [[T_("hQZ%d%d" % (d, b), [128, 4, 256], BF16) for b in range(2)] for d in range(2)]
            ALAST = [[T_("hAL%d%d" % (d, b), [128, 8]) for b in range(2)] for d in range(2)]
            VH, VHR = T_("hVH", [128, NT, 128], BF16)
            OD = [T_("hO%d" % d, [128, NT, 128], BF16) for d in range(2)]
            ST = [T_("hS%d" % d, [128, 128]) for d in range(2)]
            SBF = [T_("hSB%d" % d, [128, 128], BF16) for d in range(2)]
            SCM = [T_("hSCM%d" % d, [128, 128], BF16) for d in range(2)]
            OMLBC, OMLBCR = T_("hOMLBC", [128, 2, 128])
            CATB, CATBR = T_("hCATB", [128, 512], BF16)
            SSH, SSHR = T_("hSSH", [128, 8])
            PSS = [[kb.res("pss%d_%d" % (d, i)) for i in range(3)] for d in range(2)]
            for d in range(2):
                for b in range(2):
                    kb.op("pool", lambda d=d, b=b: g.memset(QZ[d][b][0][:], 0.0), writes=[QZ[d][b][1]])

            class Shim:
                pass
            cm = Shim()
            cm.T1, cm.T1R, cm.RS, cm.RSR = TA, TAR, TB, TBR
            G = [[0, 1], [2, 3, 4, 5], [6, 7, 8, 9], [10, 11, 12, 13], [14, 15, 16, 17]]
            GORD = [G, [G[0], G[4], G[3], G[2], G[1]]]
            TRI = [CST[:, C_TRIF:C_TRIF + 128], CST[:, C_TRIB:C_TRIB + 128]]
            AFT = [CST[:, C_AFTF:C_AFTF + 128], CST[:, C_AFTB:C_AFTB + 128]]
            hscale = 128.0 ** -0.5
            hb = 1536

            def ht_res(tiles):
                return list({id(HTR[blk_of_tile(t)]): HTR[blk_of_tile(t)] for t in tiles}.values())

            for hh in range(4):
                ia = ring_pos[0] % NSLOT
                ring_pos[0] += 1
                ib = ring_pos[0] % NSLOT
                ring_pos[0] += 1
                rA, rB = RINGR[ia], RINGR[ib]
                wA = RING[ia][:, 0:4096].rearrange("p (k c) -> p k c", k=8)
                for pi, c0 in enumerate((hb + hh * 128, hb + 512 + hh * 128, hb + 1024 + hh * 128, hb + 1536 + hh * 128)):
                    kb.dma("pool", lambda pi=pi, c0=c0: g.dma_start(
                        out=wA[:, :, pi * 128:(pi + 1) * 128],
                        in_=ewin_d[:, c0:c0 + 128].rearrange("(k p) c -> p k c", p=128)), rA, True)
                wG = RING[ib][:, 0:1024].rearrange("p (k c) -> p k c", k=8)
                wO = RING[ib][:, 1024:2048]
                c0 = hb + 2048 + hh * 128
                kb.dma("pool", lambda: g.dma_start(out=wG, in_=ewin_d[:, c0:c0 + 128].rearrange("(k p) c -> p k c", p=128)), rB, True)
                kb.dma("pool", lambda: g.dma_start(out=wO, in_=ewout_d[512 + hh * 128:512 + (hh + 1) * 128, :]), rB, True)
                for d in range(2):
                    kb.op("dve", lambda d=d: v.tensor_copy(out=TD[:, 0:128], in_=OMLF[:, d * 4 + hh:d * 4 + hh + 1].to_broadcast([128, 128])),
                          reads=[OMLFR], writes=[TDR])
                    kb.op("pe", mm(PS[0][:, 0:128], TD[:, 0:128], IDENT, True, True), reads=[TDR, CSTR], writes=[PSR[0]])
                    kb.op("act", lambda d=d: a.copy(out=OMLBC[:, d, :], in_=PS[0][:, 0:128]), reads=[PSR[0]], writes=[OMLBCR])
                for g0 in range(0, NT, 4):
                    tl = list(range(g0, min(g0 + 4, NT)))
                    fns = []
                    for i, t in enumerate(tl):
                        for k in range(8):
                            fns.append(mm(PS[3][:, i * 128:(i + 1) * 128], HT[:, k, t * 128:(t + 1) * 128], wA[:, k, 384:512], k == 0, k == 7))
                    kb.op("pe", fns, reads=[rA] + ht_res(tl), writes=[PSR[3]])
                    kb.op("act", lambda g0=g0, tl=tl: a.copy(out=VH[:, g0:g0 + len(tl), :],
                                                           in_=PS[3][:, 0:len(tl) * 128].rearrange("p (t c) -> p t c", c=128)),
                          reads=[PSR[3]], writes=[VHR])
                for d in range(2):
                    kb.op("dve", lambda d=d: v.memset(ST[d][0][:], 0.0), writes=[ST[d][1]])
                    kb.op("dve", lambda d=d: v.memset(SBF[d][0][:], 0.0), writes=[SBF[d][1]])

                def prep(d, tiles, b):
                    ng = len(tiles)
                    n = ng * 128
                    tk0 = tiles[0] * 128
                    fc = 128 + d * 128
                    hr = ht_res(tiles)
                    kbar, kbarR = KBAR[d][b]
                    ktil, ktilR = KTIL[d][b]
                    qz, qzR = QZ[d][b]
                    al, alR = ALAST[d][b]
                    fns = []
                    for i, t in enumerate(tiles):
                        for k in range(8):
                            fns.append(mm(PS[0][:, i * 128:(i + 1) * 128], HT[:, k, t * 128:(t + 1) * 128], wA[:, k, fc:fc + 128], k == 0, k == 7))
                    kb.op("pe", fns, reads=[rA] + hr, writes=[PSR[0]])
                    kb.op("pe", [mm(PS[1][:, 0:n], wA[:, k, fc:fc + 128], HT[:, k, tk0:tk0 + n], k == 0, k == 7) for k in range(8)],
                          reads=[rA] + hr, writes=[PSR[1]])
                    kb.op("pe", [mm(PS[2][:, 0:n], wA[:, k, 0:128], HT[:, k, tk0:tk0 + n], k == 0, k == 7) for k in range(8)],
                          reads=[rA] + hr, writes=[PSR[2]])
                    kb.op("act", lambda: a.activation(out=TA[:, 0:n], in_=PS[0][:, 0:n], func=AF.Exp), reads=[PSR[0]], writes=[TAR])
                    kb.op("dve", lambda: v.tensor_scalar(out=TA[:, 0:n], in0=TA[:, 0:n], scalar1=1.0, scalar2=None, op0=ALU.add),
                          reads=[TAR], writes=[TAR])
                    kb.op("dve", lambda: v.reciprocal(out=TA[:, 0:n], in_=TA[:, 0:n]), reads=[TAR], writes=[TAR])
                    kb.op("dve", lambda: v.tensor_tensor(
                        out=TB[:, 0:n].rearrange("p (t c) -> p t c", c=128), in0=TA[:, 0:n].rearrange("p (t c) -> p t c", c=128),
                        in1=OMLBC[:, d:d + 1, :].to_broadcast([128, ng, 128]), op=ALU.mult),
                        reads=[TAR, OMLBCR], writes=[TBR])
                    kb.op("act", lambda: a.activation(out=TC[:, 0:n], in_=TB[:, 0:n], func=AF.Ln, scale=-1.0, bias=ONEC[:, :]),
                          reads=[TBR, CSTR], writes=[TCR])
                    kb.op("pe", [mm(PS[3][:, i * 128:(i + 1) * 128], AFT[d], TC[:, i * 128:(i + 1) * 128], True, True) for i in range(ng)],
                          reads=[TCR, CSTR], writes=[PSR[3]])
                    kb.op("pe", [mm(PS[4][:, i * 128:(i + 1) * 128], TC[:, i * 128:(i + 1) * 128], TRI[d], True, True) for i in range(ng)],
                          reads=[TCR, CSTR], writes=[PSR[4]])
                    kb.op("act", lambda: a.activation(out=TA[:, 0:n], in_=PS[3][:, 0:n], func=AF.Exp), reads=[PSR[3]], writes=[TAR])
                    kb.op("dve", lambda: v.tensor_tensor(out=kbar[:, 0:ng, :], in0=TB[:, 0:n].rearrange("p (t c) -> p t c", c=128),
                                                         in1=TA[:, 0:n].rearrange("p (t c) -> p t c", c=128), op=ALU.mult),
                          reads=[TAR, TBR], writes=[kbarR])
                    kb.op("act", lambda: a.activation(out=TD[:, 0:n], in_=PS[1][:, 0:n], func=AF.Exp), reads=[PSR[1]], writes=[TDR])
                    kb.op("dve", lambda: v.tensor_scalar(out=TD[:, 0:n], in0=TD[:, 0:n], scalar1=1.0, scalar2=None, op0=ALU.add),
                          reads=[TDR], writes=[TDR])
                    kb.op("dve", lambda: v.reciprocal(out=TD[:, 0:n], in_=TD[:, 0:n]), reads=[TDR], writes=[TDR])
                    kb.op("act", lambda: a.activation(out=EC[:, 0:n], in_=PS[4][:, 0:n], func=AF.Exp), reads=[PSR[4]], writes=[ECR])
                    kb.op("act", lambda: a.activation(out=TA[:, 0:n], in_=PS[4][:, 0:n], func=AF.Exp, scale=-1.0), reads=[PSR[4]], writes=[TAR])
                    kb.op("dve", lambda: v.scalar_tensor_tensor(out=ktil[:, 0:n], in0=TD[:, 0:n], scalar=OMLF[:, d * 4 + hh:d * 4 + hh + 1],
                                                                in1=TA[:, 0:n], op0=ALU.mult, op1=ALU.mult),
                          reads=[TDR, TAR, OMLFR], writes=[ktilR])
                    lastcol = 63 if d == 0 else 0
                    kb.op("dve", lambda: v.tensor_copy(out=al[:, 0:2 * ng],
                                                       in_=EC[:, 0:n].rearrange("p (c j) -> p c j", j=64)[:, :, lastcol]),
                          reads=[ECR], writes=[alR])
                    kb.op("act", lambda: a.activation(out=TB[:, 0:n], in_=PS[2][:, 0:n], func=AF.Exp, scale=-1.0), reads=[PSR[2]], writes=[TBR])
                    kb.op("dve", lambda: v.tensor_scalar(out=TB[:, 0:n], in0=TB[:, 0:n], scalar1=1.0, scalar2=None, op0=ALU.add),
                          reads=[TBR], writes=[TBR])
                    kb.op("dve", lambda: v.reciprocal(out=TB[:, 0:n], in_=TB[:, 0:n]), reads=[TBR], writes=[TBR])
                    kb.op("dve", lambda: v.tensor_tensor(out=TB[:, 0:n], in0=PS[2][:, 0:n], in1=TB[:, 0:n], op=ALU.mult),
                          reads=[TBR, PSR[2]], writes=[TBR])
                    for c in range(2):
                        kb.op("dve", lambda c=c: v.scalar_tensor_tensor(
                            out=qz[:, 0:ng, c * 192:c * 192 + 64],
                            in0=TB[:, 0:n].rearrange("p (t c) -> p t c", c=128)[:, :, c * 64:(c + 1) * 64], scalar=hscale,
                            in1=EC[:, 0:n].rearrange("p (t c) -> p t c", c=128)[:, :, c * 64:(c + 1) * 64], op0=ALU.mult, op1=ALU.mult),
                            reads=[TBR, ECR], writes=[qzR])

                def recur(d, t, i, b):
                    bank = 5 + d
                    s_ap = PS[bank][:, 0:128]
                    o_ap = PS[bank][:, 128:256]
                    ds_ap = PS[7][:, 256 + d * 128:384 + d * 128]
                    sR, oR, dsR = PSS[d]
                    kbar, kbarR = KBAR[d][b]
                    ktil, ktilR = KTIL[d][b]
                    qz, qzR = QZ[d][b]
                    al, alR = ALAST[d][b]
                    st, stR = ST[d]
                    sbf, sbfR = SBF[d]
                    scm, scmR = SCM[d]
                    kt_ = ktil[:, i * 128:(i + 1) * 128]
                    kb.op("pe", [mm(s_ap[:, 0:64], kt_, qz[:, i, 0:64], True, True), mm(s_ap[:, 64:128], kt_, qz[:, i, 192:256], True, True)],
                          reads=[ktilR, qzR], writes=[sR])
                    kb.op("dve", lambda: v.tensor_tensor(out=scm[:, :], in0=s_ap, in1=TRI[d], op=ALU.mult), reads=[sR, CSTR], writes=[scmR])
                    cs = [0, 1] if d == 0 else [1, 0]

                    def upd(c):
                        kb.op("pe", mm(ds_ap, kbar[c * 64:(c + 1) * 64, i, :], VH[c * 64:(c + 1) * 64, t, :], True, True),
                              reads=[kbarR, VHR], writes=[dsR])
                        kb.op("dve", lambda: v.scalar_tensor_tensor(out=st[:, :], in0=st[:, :], scalar=al[:, 2 * i + c:2 * i + c + 1],
                                                                    in1=ds_ap, op0=ALU.mult, op1=ALU.add),
                              reads=[stR, alR, dsR], writes=[stR])
                        kb.op("act", lambda: a.copy(out=sbf[:, :], in_=st[:, :]), reads=[stR], writes=[sbfR])
                    kb.op("pe", [mm(o_ap, scm[:, :], VH[:, t, :], True, False),
                                 mm(o_ap, qz[:, i, cs[0] * 128:(cs[0] + 1) * 128], sbf[:, :], False, False)],
                          reads=[scmR, VHR, qzR, sbfR], writes=[oR])
                    upd(cs[0])
                    kb.op("pe", mm(o_ap, qz[:, i, cs[1] * 128:(cs[1] + 1) * 128], sbf[:, :], False, True),
                          reads=[qzR, sbfR], writes=[oR])
                    upd(cs[1])
                    kb.op("act", lambda: a.copy(out=OD[d][0][:, t, :], in_=o_ap), reads=[oR], writes=[OD[d][1]])

                bufi = [0, 0]
                pend = [None, None]
                for d in range(2):
                    pend[d] = (GORD[d][0], bufi[d] % 2)
                    prep(d, GORD[d][0], bufi[d] % 2)
                    bufi[d] += 1
                for gi in range(5):
                    cur = [pend[0], pend[1]]
                    if gi + 1 < 5:
                        for d in range(2):
                            pend[d] = (GORD[d][gi + 1], bufi[d] % 2)
                            prep(d, GORD[d][gi + 1], bufi[d] % 2)
                            bufi[d] += 1
                    ftiles, fb = cur[0]
                    btiles, bb = cur[1]
                    border = list(reversed(btiles))
                    for j in range(max(len(ftiles), len(border))):
                        if j < len(ftiles):
                            recur(0, ftiles[j], j, fb)
                        if j < len(border):
                            recur(1, border[j], btiles.index(border[j]), bb)
                cnt = 0
                for tiles in G:
                    ng = len(tiles)
                    n = ng * 128
                    t0_ = tiles[0]
                    fns = []
                    for i, t in enumerate(tiles):
                        for k in range(8):
                            fns.append(mm(PS[0][:, i * 128:(i + 1) * 128], HT[:, k, t * 128:(t + 1) * 128], wG[:, k, :], k == 0, k == 7))
                    kb.op("pe", fns, reads=[rB] + ht_res(tiles), writes=[PSR[0]])
                    kb.op("dve", lambda: v.tensor_tensor(out=TA[:, 0:n].rearrange("p (t c) -> p t c", c=128),
                                                         in0=OD[0][0][:, t0_:t0_ + ng, :], in1=OD[1][0][:, t0_:t0_ + ng, :], op=ALU.add),
                          reads=[OD[0][1], OD[1][1]], writes=[TAR])
                    for i in range(ng):
                        kb.op("act", lambda i=i: a.activation(out=TD[:, i * 128:(i + 1) * 128], in_=TA[:, i * 128:(i + 1) * 128],
                                                              func=AF.Square, accum_out=SSH[:, i:i + 1]),
                              reads=[TAR], writes=[TDR, SSHR])
                    kb.op("dve", lambda: v.tensor_scalar(out=SSH[:, 0:ng], in0=SSH[:, 0:ng], scalar1=1.0 / 128, scalar2=EPS,
                                                         op0=ALU.mult, op1=ALU.add), reads=[SSHR], writes=[SSHR])
                    kb.op("act", lambda: a.activation(out=SSH[:, 0:ng], in_=SSH[:, 0:ng], func=AF.Ln), reads=[SSHR], writes=[SSHR])
                    kb.op("act", lambda: a.activation(out=SSH[:, 0:ng], in_=SSH[:, 0:ng], func=AF.Exp, scale=-0.5), reads=[SSHR], writes=[SSHR])
                    for i in range(ng):
                        kb.op("dve", lambda i=i: v.scalar_tensor_tensor(
                            out=TA[:, i * 128:(i + 1) * 128], in0=TA[:, i * 128:(i + 1) * 128], scalar=SSH[:, i:i + 1],
                            in1=ppc("hog", 0, 128), op0=ALU.mult, op1=ALU.mult), reads=[TAR, SSHR, PPR], writes=[TAR])
                    kb.op("act", lambda: a.activation(out=TB[:, 0:n], in_=PS[0][:, 0:n], func=AF.Exp, scale=-1.0), reads=[PSR[0]], writes=[TBR])
                    kb.op("dve", lambda: v.tensor_scalar(out=TB[:, 0:n], in0=TB[:, 0:n], scalar1=1.0, scalar2=None, op0=ALU.add),
                          reads=[TBR], writes=[TBR])
                    kb.op("dve", lambda: v.reciprocal(out=TB[:, 0:n], in_=TB[:, 0:n]), reads=[TBR], writes=[TBR])
                    kb.op("dve", lambda: v.tensor_tensor(out=TB[:, 0:n], in0=PS[0][:, 0:n], in1=TB[:, 0:n], op=ALU.mult),
                          reads=[TBR, PSR[0]], writes=[TBR])
                    kb.op("dve", lambda: v.tensor_tensor(out=TA[:, 0:n], in0=TA[:, 0:n], in1=TB[:, 0:n], op=ALU.mult),
                          reads=[TAR, TBR], writes=[TAR])
                    kb.op("pe", [mm(PS[1][:, i * 128:(i + 1) * 128], TA[:, i * 128:(i + 1) * 128], IDENT, True, True) for i in range(ng)],
                          reads=[TAR, CSTR], writes=[PSR[1]])
                    kb.op("act", lambda: a.copy(out=CATB[:, 0:n], in_=PS[1][:, 0:n]), reads=[PSR[1]], writes=[CATBR])
                    for i, t in enumerate(tiles):
                        w_ = 1 if t < 2 else 0
                        for nh in range(2):
                            pb = 2 + (cnt % 2)
                            tm_, tmR = (TC, TCR) if cnt % 2 == 0 else (TD, TDR)
                            cnt += 1
                            kb.op("pe", mm(PS[pb][:], CATB[:, i * 128:(i + 1) * 128], wO[:, nh * 512:(nh + 1) * 512], True, True),
                                  reads=[CATBR, rB], writes=[PSR[pb]])
                            kb.op("dve", lambda pb=pb, nh=nh, w_=w_, tm_=tm_: v.tensor_tensor(
                                out=tm_[:], in0=PS[pb][:], in1=GBC[:, w_, nh * 512:(nh + 1) * 512], op=ALU.mult),
                                reads=[PSR[pb], GBCR], writes=[tmR])
                            kb.op("pool", lambda t=t, nh=nh, tm_=tm_: g.tensor_tensor(
                                out=X[:, t, nh * 512:(nh + 1) * 512], in0=X[:, t, nh * 512:(nh + 1) * 512], in1=tm_[:], op=ALU.add),
                                reads=[tmR, XR[t][nh]], writes=[XR[t][nh]])

        def even_mixer():
            even_prep()
            if flags.get("diff", True):
                diff_part()
            if flags.get("hgrn", True):
                hgrn_part()

        for l in range(2):
            with_ctx = (l == 0)
            if (l == 0 and do_mix0) or (l == 1 and do_mix1):
                norm_phase(l, 0, True, False, 2)
                if l == 0:
                    even_mixer()
                else:
                    mla_mixer()
            if do_ffn:
                norm_phase(l, 1, with_ctx, True, 5)
                moe_phase(l, with_ctx)

        outdeps = []
        for t in range(2, NT):
            for h in range(2):
                outdeps.append(kb.dma("sp", lambda t=t, h=h: nc.sync.dma_start(
                    out=y_d[(t - 2) * 128:(t - 1) * 128, h * 512:(h + 1) * 512], in_=X[:, t, h * 512:(h + 1) * 512]),
                    XR[t][h], False))
        if taps:
            for t in range(NT):
                for h in range(2):
                    outdeps.append(kb.dma("sp", lambda t=t, h=h: nc.sync.dma_start(
                        out=tap_d[t * 128:(t + 1) * 128, h * 512:(h + 1) * 512], in_=X[:, t, h * 512:(h + 1) * 512]),
                        XR[t][h], False))
        kb.final_wait("sp", outdeps)
    return nc


WEIGHT_KEYS = ["mod_w", "even_w_in", "even_w_out", "odd_w_in", "mla_w_uq", "mla_w_ukv", "odd_w_out",
               "expert_w_gate", "expert_w_up", "expert_w_down", "shared_w_gate", "shared_w_up", "shared_w_down"]


def make_in_maps(inp, cores):
    cst = _consts()
    rope = _rope_tables()
    shared = {}
    for k in WEIGHT_KEYS:
        arr = np.ascontiguousarray(np.asarray(inp[k], np.float32))
        if k in ("even_w_in", "even_w_out", "odd_w_in", "mla_w_uq", "mla_w_ukv", "odd_w_out"):
            arr = arr[0]
        shared[k] = arr
    maps = []
    for b in cores:
        m = dict(shared)
        m["x"] = np.ascontiguousarray(np.asarray(inp["x"][b], np.float32))
        m["ctx"] = np.ascontiguousarray(np.asarray(inp["ctx"][b], np.float32))
        m["pp"] = _pack_params(b, inp)
        m["cst"] = cst
        m["rope"] = rope
        maps.append(m)
    return maps


def kernel(**inputs):
    nc = build_program()
    maps = make_in_maps(inputs, list(range(8)))
    res = run_bass_kernel_spmd(nc, maps, core_ids=list(range(8)))
    return np.stack([np.asarray(r["y"], np.float32) for r in res.results], axis=0)
```

```python
import math
import numpy as np
from contextlib import ExitStack
import concourse.bass as bass
import concourse.mybir as mybir
from concourse.bass_utils import run_bass_kernel_spmd

F32 = mybir.dt.float32
BF16 = mybir.dt.bfloat16
AF = mybir.ActivationFunctionType
ALU = mybir.AluOpType
AX = mybir.AxisListType

D = 1024
SEQ = 2048
CTX = 256
NTOK = SEQ + CTX
NT = NTOK // 128
BLOCKS = [(0, 256)] + [(256 + 512 * i, 512) for i in range(4)]
EPS = 1e-6
NSLOT = 3
SLOT_ELEMS = 4096

C_ID = 0
C_ONES = 128
C_BLK64 = 256
C_PERM = 384
C_TRIF = 512
C_TRIB = 640
C_AFTF = 768
C_AFTB = 896
C_N = 1024


def _consts():
    c = np.zeros((128, C_N), np.float32)
    i = np.arange(128)
    s = i[:, None]
    t = i[None, :]
    same = (s // 64) == (t // 64)
    c[:, C_ID:C_ID + 128] = (s == t)
    c[:, C_ONES:C_ONES + 128] = 1.0
    c[:, C_BLK64:C_BLK64 + 128] = same
    partner = np.where((i % 32) < 16, i + 16, i - 16)
    c[:, C_PERM:C_PERM + 128] = (s == partner[None, :])
    c[:, C_TRIF:C_TRIF + 128] = same & (s <= t)
    c[:, C_TRIB:C_TRIB + 128] = same & (s >= t)
    c[:, C_AFTF:C_AFTF + 128] = same & (s > t)
    c[:, C_AFTB:C_AFTB + 128] = same & (s < t)
    return c


def _rope_tables():
    tok = np.arange(SEQ)
    row = (tok // 64).astype(np.float32)
    col = (tok % 64).astype(np.float32)
    inv = (10000.0 ** (-np.arange(0, 32, 2, dtype=np.float32) / 32.0)).astype(np.float32)
    C = np.zeros((128, SEQ), np.float32)
    S = np.zeros((128, SEQ), np.float32)
    for p in range(128):
        d = p % 64
        pos = row if d < 32 else col
        f = inv[d % 16]
        ang = (pos * f).astype(np.float32)
        C[p] = np.cos(ang)
        S[p] = np.sin(ang) * (-1.0 if (d % 32) < 16 else 1.0)
    return np.stack([C, S], axis=1)


class PP:
    pass


def _pp_layout():
    off = {}
    n = 0

    def add(name, w):
        nonlocal n
        off[name] = (n, w)
        n += w
    add("c", 8)
    add("cctx", 8)
    add("modb", 2 * 48)
    add("nmix", 16)
    add("nffn", 16)
    add("rw", 8 * 16)
    add("rbias", 16)
    add("dqg", 1)
    add("dkg", 1)
    add("dlam", 256)
    add("subln", 1)
    add("lbfm", 2 * 2 * 4)
    add("hog", 128)
    add("qag", 3)
    add("kvag", 2)
    add("qng", 1)
    add("qrg", 1)
    add("kng", 1)
    add("krg", 1)
    return off, n


PPO, PPN = _pp_layout()


def _fm(v):
    v = np.asarray(v, np.float32)
    return np.ascontiguousarray(v.reshape(-1, 128).T)


def _pack_params(b, inp):
    pp = np.zeros((128, PPN), np.float32)

    def put(name, arr):
        o, w = PPO[name]
        arr = np.asarray(arr, np.float32).reshape(128, -1)
        assert arr.shape[1] == w, (name, arr.shape, w)
        pp[:, o:o + w] = arr
    put("c", _fm(inp["c"][b]))
    put("cctx", _fm(inp["c_ctx"]))
    put("modb", np.concatenate([_fm(inp["mod_b"][0]), _fm(inp["mod_b"][1])], axis=1))
    put("nmix", np.concatenate([_fm(inp["norm_mix"][0]), _fm(inp["norm_mix"][1])], axis=1))
    put("nffn", np.concatenate([_fm(inp["norm_ffn"][0]), _fm(inp["norm_ffn"][1])], axis=1))
    rw = np.asarray(inp["router_w"], np.float32).reshape(8, 128, 16).transpose(1, 0, 2)
    put("rw", rw.reshape(128, 128))
    put("rbias", np.broadcast_to(np.asarray(inp["router_bias"], np.float32)[None, :], (128, 16)))
    put("dqg", np.tile(np.asarray(inp["diff_q_gain"][0], np.float32), 2)[:, None])
    put("dkg", np.tile(np.asarray(inp["diff_k_gain"][0], np.float32), 2)[:, None])
    put("dlam", np.broadcast_to(np.asarray(inp["diff_lambda"][0], np.float32).reshape(1, 256), (128, 256)))
    put("subln", np.asarray(inp["diff_subln"][0], np.float32)[:, None])
    lb = np.asarray(inp["hgrn_lb_logits"], np.float32)
    put("lbfm", lb.reshape(2, 2, 4, 128).transpose(3, 0, 1, 2).reshape(128, 16))
    put("hog", np.broadcast_to(np.asarray(inp["hgrn_out_gain"][0], np.float32)[None, :], (128, 128)))
    put("qag", _fm(inp["mla_q_a_gain"][0]))
    put("kvag", _fm(inp["mla_kv_a_gain"][0]))
    put("qng", np.asarray(inp["mla_q_nope_gain"][0], np.float32)[:, None])
    put("qrg", np.tile(np.asarray(inp["mla_q_rope_gain"][0], np.float32), 2)[:, None])
    put("kng", np.asarray(inp["mla_k_nope_gain"][0], np.float32)[:, None])
    put("krg", np.tile(np.asarray(inp["mla_k_rope_gain"][0], np.float32), 2)[:, None])
    return pp


class Res:
    __slots__ = ("name", "w", "rs", "dsem", "dcnt")

    def __init__(self, name):
        self.name = name
        self.w = None
        self.rs = {}
        self.dsem = None
        self.dcnt = 0


class Eng:
    def __init__(self, name, obj, sem):
        self.name = name
        self.obj = obj
        self.sem = sem
        self.cnt = 0
        self.seen = {}


class KB:
    def __init__(self, nc, es):
        self.nc = nc
        self.es = es
        self.sems = {}
        self.E = {}
        for name, obj in (("pe", nc.tensor), ("act", nc.scalar), ("dve", nc.vector),
                          ("pool", nc.gpsimd), ("sp", nc.sync)):
            sem = es.enter_context(nc.semaphore("s_" + name))
            self.sems[name] = sem
            self.E[name] = Eng(name, obj, sem)
        self.nres = 0

    def res(self, name=None):
        self.nres += 1
        return Res(name or ("r%d" % self.nres))

    def _wait(self, E, reads, writes):
        deps = {}

        def add(d):
            if d is None:
                return
            k, v = d
            if deps.get(k, 0) < v:
                deps[k] = v
        for r in reads:
            add(r.w)
        for w in writes:
            add(w.w)
            for k, v in w.rs.items():
                add((k, v))
        for k, v in deps.items():
            if k == E.name and E.name == "pe":
                continue
            if E.seen.get(k, 0) < v:
                E.obj.wait_ge(self.sems[k], v)
                E.seen[k] = v

    def op(self, eng, fn, reads=(), writes=()):
        E = self.E[eng]
        self._wait(E, reads, writes)
        ins = None
        if callable(fn):
            ins = fn()
        else:
            for f in fn:
                ins = f()
        E.cnt += 1
        ins.then_inc(E.sem, 1)
        dep = (E.name, E.cnt)
        for r in reads:
            if r.rs.get(E.name, 0) < E.cnt:
                r.rs[E.name] = E.cnt
        for w in writes:
            w.w = dep
            w.rs = {}
        return ins

    def dma(self, queue, fn, res, is_write, reads=(), writes=()):
        E = self.E[queue]
        if res.dsem is None:
            self.nres += 1
            key = "d%d_%s" % (self.nres, res.name)
            res.dsem = key
            self.sems[key] = self.es.enter_context(self.nc.semaphore(key))
        rr = list(reads) + ([] if is_write else [res])
        ww = list(writes) + ([res] if is_write else [])
        self._wait(E, rr, ww)
        ins = fn()
        res.dcnt += 16
        ins.then_inc(self.sems[res.dsem], 16)
        dep = (res.dsem, res.dcnt)
        for r in rr:
            if r.rs.get(res.dsem, 0) < res.dcnt:
                r.rs[res.dsem] = res.dcnt
        for w in ww:
            w.w = dep
            w.rs = {}
        return dep

    def barrier(self):
        for E in self.E.values():
            for F in self.E.values():
                if F is E or F.cnt == 0:
                    continue
                if E.seen.get(F.name, 0) < F.cnt:
                    E.obj.wait_ge(self.sems[F.name], F.cnt)
                    E.seen[F.name] = F.cnt
            if E.name != "pe" and E.cnt > 0 and E.seen.get(E.name, 0) < E.cnt:
                E.obj.wait_ge(self.sems[E.name], E.cnt)
                E.seen[E.name] = E.cnt

    def final_wait(self, eng, deps):
        E = self.E[eng]
        for k, v in deps:
            E.obj.wait_ge(self.sems[k], v)


def build_program(flags=None):
    flags = flags or {}
    do_mix0 = flags.get("mix0", True)
    do_mix1 = flags.get("mix1", True)
    do_ffn = flags.get("ffn", True)
    taps = flags.get("taps", False)

    nc = bass.Bass("TRN2", target_bir_lowering=False)

    def din(name, shape):
        return nc.dram_tensor(name, list(shape), F32, kind="ExternalInput").ap()
    x_d = din("x", [SEQ, D])
    ctx_d = din("ctx", [CTX, D])
    pp_d = din("pp", [128, PPN])
    cst_d = din("cst", [128, C_N])
    rope_d = din("rope", [128, 2, SEQ])
    mod_w_d = din("mod_w", [2, D, 6 * D])
    ewin_d = din("even_w_in", [D, 4096])
    ewout_d = din("even_w_out", [D, D])
    owin_d = din("odd_w_in", [D, 704])
    wuq_d = din("mla_w_uq", [384, 1536])
    wukv_d = din("mla_w_ukv", [256, 2048])
    owout_d = din("odd_w_out", [D, D])
    xg_d = din("expert_w_gate", [2, 16, D, 512])
    xu_d = din("expert_w_up", [2, 16, D, 512])
    xd_d = din("expert_w_down", [2, 16, 512, D])
    sg_d = din("shared_w_gate", [2, D, 512])
    su_d = din("shared_w_up", [2, D, 512])
    sd_d = din("shared_w_down", [2, 512, D])
    y_d = nc.dram_tensor("y", [SEQ, D], F32, kind="ExternalOutput").ap()
    tap_d = None
    if taps:
        tap_d = nc.dram_tensor("tap", [NTOK, D], F32, kind="ExternalOutput").ap()

    es = ExitStack()
    with es:
        kb = KB(nc, es)

        def sb(name, shape, dt=F32):
            return es.enter_context(nc.sbuf_tensor(name, list(shape), dt))

        X = sb("X", [128, NT, D])
        XR = [[kb.res("x%d_%d" % (t, h)) for h in range(2)] for t in range(NT)]
        HT = sb("HT", [128, 8, NTOK], BF16)
        HTR = [kb.res("ht%d" % i) for i in range(len(BLOCKS))]
        PPT = sb("PPT", [128, PPN])
        PPR = kb.res("pp")
        CST = sb("CST", [128, C_N])
        CSTB = sb("CSTB", [128, C_N], BF16)
        CSTR = kb.res("cst")
        RING = [sb("ring%d" % i, [128, SLOT_ELEMS], BF16) for i in range(NSLOT)]
        RINGR = [kb.res("ring%d" % i) for i in range(NSLOT)]
        ring_pos = [0]
        PS = [es.enter_context(nc.psum_tensor("ps%d" % i, [128, 512], F32)) for i in range(8)]
        PSR = [kb.res("ps%d" % i) for i in range(8)]
        MODV = sb("MODV", [128, 2, 48, 2])
        MODR = kb.res("modv")
        NA = sb("NA", [128, 2, 8])
        NB = sb("NB", [128, 2, 8])
        NAR = kb.res("na")
        SS = sb("SS", [128, NT])
        SSR = kb.res("ss")
        RSTD = sb("RSTD", [128, NT])
        RSTDR = kb.res("rstd")
        GBC = sb("GBC", [128, 2, D], BF16)
        GBCR = kb.res("gbc")
        GATES = sb("GATES", [128, NT, 16])
        GATESR = kb.res("gates")
        SCB = sb("SCB", [128, 8, 2], BF16)
        SCR = kb.res("scb")

        scope_id = [0]

        class Scope:
            def __enter__(self):
                kb.barrier()
                scope_id[0] += 1
                self.sid = scope_id[0]
                self.stack = ExitStack()
                self.stack.__enter__()
                return self

            def sb(self, name, shape, dt=F32):
                return self.stack.enter_context(nc.sbuf_tensor("%s_s%d" % (name, self.sid), list(shape), dt))

            def __exit__(self, *exc):
                kb.barrier()
                return self.stack.__exit__(*exc)
        SM = [sb("SM%d" % i, [128, 64]) for i in range(6)]
        SMR = [kb.res("sm%d" % i) for i in range(6)]

        v = nc.vector
        a = nc.scalar
        g = nc.gpsimd
        pe = nc.tensor

        def ppc(name, i=0, w=1):
            o, _ = PPO[name]
            return PPT[:, o + i:o + i + w]

        kb.dma("sp", lambda: nc.sync.dma_start(out=PPT[:], in_=pp_d), PPR, True)
        kb.dma("sp", lambda: nc.sync.dma_start(out=CST[:], in_=cst_d), CSTR, True)
        kb.op("dve", lambda: v.tensor_copy(out=CSTB[:], in_=CST[:]), reads=[CSTR], writes=[CSTR])
        for t in range(NT):
            src = ctx_d[t * 128:(t + 1) * 128, :] if t < 2 else x_d[(t - 2) * 128:(t - 1) * 128, :]
            for h in range(2):
                kb.dma("sp", lambda src=src, t=t, h=h: nc.sync.dma_start(
                    out=X[:, t, h * 512:(h + 1) * 512], in_=src[:, h * 512:(h + 1) * 512]), XR[t][h], True)

        IDENT = CST[:, C_ID:C_ID + 128]

        def wload(src2d, kc, cols):
            assert kc * cols <= SLOT_ELEMS
            i = ring_pos[0] % len(RING)
            ring_pos[0] += 1
            view = RING[i][:, 0:kc * cols].rearrange("p (k c) -> p k c", k=kc)
            kb.dma("pool", lambda: g.dma_start(out=view, in_=src2d.rearrange("(k p) c -> p k c", p=128)),
                   RINGR[i], True)
            return RINGR[i], view

        def silu_small(out_ap, in_ap, sm_i, width, reads, writes):
            t1 = SM[sm_i][:, 0:width]
            kb.op("act", lambda: a.activation(out=t1, in_=in_ap, func=AF.Exp, scale=-1.0),
                  reads=reads, writes=[SMR[sm_i]])
            kb.op("dve", lambda: v.tensor_scalar(out=t1, in0=t1, scalar1=1.0, scalar2=None, op0=ALU.add),
                  reads=[SMR[sm_i]], writes=[SMR[sm_i]])
            kb.op("dve", lambda: v.reciprocal(out=t1, in_=t1), reads=[SMR[sm_i]], writes=[SMR[sm_i]])
            kb.op("dve", lambda: v.tensor_tensor(out=out_ap, in0=in_ap, in1=t1, op=ALU.mult),
                  reads=list(reads) + [SMR[sm_i]], writes=writes)

        silu_small(SCB[:, :, 0], ppc("c", 0, 8), 0, 8, [PPR], [SCR])
        silu_small(SCB[:, :, 1], ppc("cctx", 0, 8), 1, 8, [PPR, SCR], [SCR])

        for l in range(2):
            for s in range(12):
                r, wv = wload(mod_w_d[l, :, s * 512:(s + 1) * 512], 8, 512)
                pb = 0
                fns = []
                for j in range(4):
                    for k in range(8):
                        fns.append(lambda j=j, k=k, wv=wv, s=s: pe.matmul(
                            PS[pb][:, (s * 4 + j) * 2:(s * 4 + j) * 2 + 2], lhsT=wv[:, k, j * 128:(j + 1) * 128],
                            rhs=SCB[:, k, :], start=(k == 0), stop=(k == 7)))
                kb.op("pe", fns, reads=[r, SCR], writes=[PSR[pb]])
            o, _ = PPO["modb"]
            for w_ in range(2):
                kb.op("dve", lambda l=l, w_=w_: v.tensor_tensor(
                    out=MODV[:, l, :, w_], in0=PS[0][:, 0:96].rearrange("p (c w) -> p c w", w=2)[:, :, w_],
                    in1=PPT[:, o + l * 48:o + (l + 1) * 48], op=ALU.add),
                    reads=[PSR[0], PPR], writes=[MODR])

        def bc_from_fm(dst_ap_fn, vec_col_fn, dst_res, extra_reads, HF32, HF32R):
            for half in range(2):
                pb = 1 + half
                fns = []
                for kk in range(4):
                    k = half * 4 + kk
                    kb.op("dve", lambda k=k, kk=kk, half=half: v.tensor_copy(
                        out=HF32[half][:, kk, :], in_=vec_col_fn(k).to_broadcast([128, 128])),
                        reads=extra_reads, writes=[HF32R[half]])
                for kk in range(4):
                    fns.append(lambda kk=kk, half=half, pb=pb: pe.matmul(
                        PS[pb][:, kk * 128:(kk + 1) * 128], lhsT=HF32[half][:, kk, :], rhs=IDENT,
                        start=True, stop=True))
                kb.op("pe", fns, reads=[HF32R[half], CSTR], writes=[PSR[pb]])
                kb.op("act", lambda half=half, pb=pb: a.copy(out=dst_ap_fn(half), in_=PS[pb][:]),
                      reads=[PSR[pb]], writes=[dst_res])

        def norm_phase(l, which, with_ctx, router, gate_vec):
            with Scope() as sc_:
                XN = [sc_.sb("XN", [128, D])] * 2
                XNR = [kb.res("xn")] * 2
                HF32 = [sc_.sb("HF32_%d" % i, [128, 8, 128]) for i in range(2)]
                HF32R = [kb.res("hf32_%d" % i) for i in range(2)]
                JUNK = sc_.sb("JUNK", [128, D], BF16)
                JUNKR = kb.res("junk")
                _norm_phase(l, which, with_ctx, router, XN, XNR, HF32, HF32R, JUNK, JUNKR)
                for w_ in range(2):
                    bc_from_fm(lambda half, w_=w_: GBC[:, w_, half * 512:(half + 1) * 512],
                               lambda k, w_=w_: MODV[:, l, gate_vec * 8 + k, w_:w_ + 1], GBCR, [MODR], HF32, HF32R)

        def _norm_phase(l, which, with_ctx, router, XN, XNR, HF32, HF32R, JUNK, JUNKR):
            nw = "nmix" if which == 0 else "nffn"
            vsh = 0 if which == 0 else 3
            vsc = vsh + 1
            for w_ in range(2):
                kb.op("dve", lambda w_=w_: v.scalar_tensor_tensor(
                    out=NA[:, w_, :], in0=MODV[:, l, vsc * 8:(vsc + 1) * 8, w_], scalar=1.0,
                    in1=ppc(nw, l * 8, 8), op0=ALU.add, op1=ALU.mult),
                    reads=[MODR, PPR], writes=[NAR])
                kb.op("dve", lambda w_=w_: v.tensor_copy(out=NB[:, w_, :], in_=MODV[:, l, vsh * 8:(vsh + 1) * 8, w_]),
                      reads=[MODR], writes=[NAR])
            tiles = list(range(NT)) if with_ctx else list(range(2, NT))
            for t in tiles:
                kb.op("act", lambda t=t: a.activation(out=JUNK[:], in_=X[:, t, :], func=AF.Square,
                                                      accum_out=SS[:, t:t + 1]),
                      reads=[XR[t][0], XR[t][1]], writes=[JUNKR, SSR])
            t0 = tiles[0]
            kb.op("dve", lambda: v.tensor_scalar(out=RSTD[:, t0:NT], in0=SS[:, t0:NT], scalar1=1.0 / D, scalar2=EPS,
                                                 op0=ALU.mult, op1=ALU.add), reads=[SSR], writes=[RSTDR])
            kb.op("act", lambda: a.activation(out=RSTD[:, t0:NT], in_=RSTD[:, t0:NT], func=AF.Ln),
                  reads=[RSTDR], writes=[RSTDR])
            kb.op("act", lambda: a.activation(out=RSTD[:, t0:NT], in_=RSTD[:, t0:NT], func=AF.Exp, scale=-0.5),
                  reads=[RSTDR], writes=[RSTDR])
            for idx, t in enumerate(tiles):
                p = idx % 2
                w_ = 1 if t < 2 else 0
                blk = 0 if t < 2 else 1 + (t - 2) // 4
                kb.op("dve", lambda t=t, p=p: v.tensor_scalar(out=XN[p][:], in0=X[:, t, :], scalar1=RSTD[:, t:t + 1],
                                                              scalar2=None, op0=ALU.mult),
                      reads=[XR[t][0], XR[t][1], RSTDR], writes=[XNR[p]])
                for half in range(2):
                    pb = 1 + half
                    fns = [lambda kk=kk, half=half, pb=pb, p=p: pe.matmul(
                        PS[pb][:, kk * 128:(kk + 1) * 128], lhsT=XN[p][:, (half * 4 + kk) * 128:(half * 4 + kk + 1) * 128],
                        rhs=IDENT, start=True, stop=True) for kk in range(4)]
                    kb.op("pe", fns, reads=[XNR[p], CSTR], writes=[PSR[pb]])
                    for kk in range(4):
                        k = half * 4 + kk
                        kb.op("act", lambda kk=kk, k=k, pb=pb, p=p, w_=w_: a.activation(
                            out=HF32[p][:, k, :], in_=PS[pb][:, kk * 128:(kk + 1) * 128], func=AF.Identity,
                            scale=NA[:, w_, k:k + 1], bias=NB[:, w_, k:k + 1]),
                            reads=[PSR[pb], NAR], writes=[HF32R[p]])
                kb.op("pool", lambda t=t, p=p: g.tensor_copy(out=HT[:, :, t * 128:(t + 1) * 128], in_=HF32[p][:]),
                      reads=[HF32R[p]], writes=[HTR[blk]])
                if router:
                    o, _ = PPO["rw"]
                    fns = [lambda k=k, p=p: pe.matmul(PS[3][:, 0:16], lhsT=HF32[p][:, k, :],
                                                      rhs=PPT[:, o + k * 16:o + (k + 1) * 16],
                                                      start=(k == 0), stop=(k == 7)) for k in range(8)]
                    kb.op("pe", fns, reads=[HF32R[p], PPR], writes=[PSR[3]])
                    route(t)

        def route(t):
            S = SM[2]
            R = SMR[2]
            sc = S[:, 0:16]
            bi = S[:, 16:32]
            t2 = S[:, 32:48]
            m1 = SM[3][:, 0:4]
            m2 = SM[3][:, 4:8]
            gs = SM[3][:, 8:12]
            gm = SM[3][:, 12:13]
            ing = SM[3][:, 16:20]
            den = SM[3][:, 20:21]
            R3 = SMR[3]
            kb.op("act", lambda: a.activation(out=sc, in_=PS[3][:, 0:16], func=AF.Exp, scale=-1.0),
                  reads=[PSR[3]], writes=[R])
            kb.op("dve", lambda: v.tensor_scalar(out=sc, in0=sc, scalar1=1.0, scalar2=None, op0=ALU.add),
                  reads=[R], writes=[R])
            kb.op("dve", lambda: v.reciprocal(out=sc, in_=sc), reads=[R], writes=[R])
            kb.op("dve", lambda: v.tensor_tensor(out=bi, in0=sc, in1=ppc("rbias", 0, 16), op=ALU.add),
                  reads=[R, PPR], writes=[R])
            b3 = bi.rearrange("p (g e) -> p g e", e=4)
            t3 = t2.rearrange("p (g e) -> p g e", e=4)
            kb.op("dve", lambda: v.tensor_reduce(out=m1, in_=b3, axis=AX.X, op=ALU.max), reads=[R], writes=[R3])
            kb.op("dve", lambda: v.tensor_tensor(out=t3, in0=b3, in1=m1.unsqueeze(2).to_broadcast([128, 4, 4]),
                                                 op=ALU.is_equal), reads=[R, R3], writes=[R])
            kb.op("dve", lambda: v.scalar_tensor_tensor(out=t2, in0=t2, scalar=-1e9, in1=bi, op0=ALU.mult, op1=ALU.add),
                  reads=[R], writes=[R])
            kb.op("dve", lambda: v.tensor_reduce(out=m2, in_=t3, axis=AX.X, op=ALU.max), reads=[R], writes=[R3])
            kb.op("dve", lambda: v.tensor_tensor(out=gs, in0=m1, in1=m2, op=ALU.add), reads=[R3], writes=[R3])
            kb.op("dve", lambda: v.tensor_reduce(out=gm, in_=gs, axis=AX.X, op=ALU.max), reads=[R3], writes=[R3])
            kb.op("dve", lambda: v.tensor_scalar(out=ing, in0=gs, scalar1=gm, scalar2=None, op0=ALU.is_ge),
                  reads=[R3], writes=[R3])
            kb.op("dve", lambda: v.tensor_tensor(out=t3, in0=b3, in1=m2.unsqueeze(2).to_broadcast([128, 4, 4]),
                                                 op=ALU.is_ge), reads=[R, R3], writes=[R])
            kb.op("dve", lambda: v.tensor_tensor(out=t3, in0=t3, in1=ing.unsqueeze(2).to_broadcast([128, 4, 4]),
                                                 op=ALU.mult), reads=[R, R3], writes=[R])
            kb.op("dve", lambda: v.tensor_tensor(out=t2, in0=t2, in1=sc, op=ALU.mult), reads=[R], writes=[R])
            kb.op("dve", lambda: v.tensor_reduce(out=den, in_=t2, axis=AX.X, op=ALU.add), reads=[R], writes=[R3])
            kb.op("dve", lambda: v.reciprocal(out=den, in_=den), reads=[R3], writes=[R3])
            kb.op("dve", lambda: v.tensor_scalar(out=GATES[:, t, :], in0=t2, scalar1=den, scalar2=None, op0=ALU.mult),
                  reads=[R, R3], writes=[GATESR])

        def moe_phase(l, with_ctx):
            with Scope() as sc_:
                nextra = 5
                for i in range(nextra):
                    RING.append(sc_.sb("xring%d" % i, [128, SLOT_ELEMS], BF16))
                    RINGR.append(kb.res("xring%d_%d" % (l, i)))
                ring_pos[0] = 0
                _moe_phase(l, with_ctx, sc_.sb)
                kb.barrier()
                del RING[NSLOT:]
                del RINGR[NSLOT:]
                ring_pos[0] = 0

        def _moe_phase(l, with_ctx, psb):
            ACTT = [psb("ACTT%d" % i, [128, 4, 512], BF16) for i in range(2)]
            ACTTR = [kb.res("actt%d" % i) for i in range(2)]
            SIL = [psb("SIL%d" % i, [128, 512], BF16) for i in range(2)]
            SILR = [kb.res("sil%d" % i) for i in range(2)]
            TMP = [psb("TMP%d" % i, [128, 512]) for i in range(4)]
            TMPR = [kb.res("tmp%d" % i) for i in range(4)]
            blocks = BLOCKS if with_ctx else BLOCKS[1:]
            cnt = 0
            for e in range(17):
                if e < 16:
                    srcs = (xg_d[l, e], xu_d[l, e], xd_d[l, e])
                else:
                    srcs = (sg_d[l], su_d[l], sd_d[l])
                rg, vg = wload(srcs[0], 8, 512)
                ru, vu = wload(srcs[1], 8, 512)
                rd, vd = wload(srcs[2], 4, 1024)
                for bi_, (t0, n) in enumerate(blocks):
                    blk = BLOCKS.index((t0, n))
                    ab = cnt % 2
                    cnt += 1
                    for j in range(4):
                        pg = (j % 2)
                        pu = 2 + (j % 2)
                        kb.op("pe", [lambda k=k, j=j, pg=pg: pe.matmul(
                            PS[pg][:, 0:n], lhsT=vg[:, k, j * 128:(j + 1) * 128], rhs=HT[:, k, t0:t0 + n],
                            start=(k == 0), stop=(k == 7)) for k in range(8)],
                            reads=[rg, HTR[blk]], writes=[PSR[pg]])
                        kb.op("pe", [lambda k=k, j=j, pu=pu: pe.matmul(
                            PS[pu][:, 0:n], lhsT=vu[:, k, j * 128:(j + 1) * 128], rhs=HT[:, k, t0:t0 + n],
                            start=(k == 0), stop=(k == 7)) for k in range(8)],
                            reads=[ru, HTR[blk]], writes=[PSR[pu]])
                        sl = j % 2
                        kb.op("act", lambda sl=sl, pg=pg: a.activation(out=SIL[sl][:, 0:n], in_=PS[pg][:, 0:n], func=AF.Silu),
                              reads=[PSR[pg]], writes=[SILR[sl]])
                        kb.op("dve", lambda sl=sl, pu=pu, j=j, ab=ab: v.tensor_tensor(
                            out=ACTT[ab][:, j, 0:n], in0=SIL[sl][:, 0:n], in1=PS[pu][:, 0:n], op=ALU.mult),
                            reads=[SILR[sl], PSR[pu]], writes=[ACTTR[ab]])
                    for tt in range(n // 128):
                        t = (t0 + tt * 128) // 128
                        w_ = 1 if t < 2 else 0
                        for nh in range(2):
                            pd = 4 + ((tt * 2 + nh) % 4)
                            kb.op("pe", [lambda j=j, tt=tt, nh=nh, pd=pd, ab=ab: pe.matmul(
                                PS[pd][:], lhsT=ACTT[ab][:, j, tt * 128:(tt + 1) * 128],
                                rhs=vd[:, j, nh * 512:(nh + 1) * 512], start=(j == 0), stop=(j == 3)) for j in range(4)],
                                reads=[rd, ACTTR[ab]], writes=[PSR[pd]])
                            ti = (tt * 2 + nh) % 4
                            if e < 16:
                                kb.op("dve", lambda pd=pd, t=t, e=e, ti=ti, nh=nh, w_=w_: v.scalar_tensor_tensor(
                                    out=TMP[ti][:], in0=PS[pd][:], scalar=GATES[:, t, e:e + 1],
                                    in1=GBC[:, w_, nh * 512:(nh + 1) * 512], op0=ALU.mult, op1=ALU.mult),
                                    reads=[PSR[pd], GATESR, GBCR], writes=[TMPR[ti]])
                            else:
                                kb.op("dve", lambda pd=pd, ti=ti, nh=nh, w_=w_: v.tensor_tensor(
                                    out=TMP[ti][:], in0=PS[pd][:], in1=GBC[:, w_, nh * 512:(nh + 1) * 512], op=ALU.mult),
                                    reads=[PSR[pd], GBCR], writes=[TMPR[ti]])
                            kb.op("pool", lambda t=t, nh=nh, ti=ti: g.tensor_tensor(
                                out=X[:, t, nh * 512:(nh + 1) * 512], in0=X[:, t, nh * 512:(nh + 1) * 512],
                                in1=TMP[ti][:], op=ALU.add),
                                reads=[TMPR[ti], XR[t][nh]], writes=[XR[t][nh]])

        ONESB = CSTB[:, C_ONES:C_ONES + 128]
        PERMB = CSTB[:, C_PERM:C_PERM + 128]
        EPSC = sb("EPSC", [128, 1])
        kb.op("dve", lambda: v.memset(EPSC[:], EPS), writes=[CSTR])

        def mm(out, lhsT, rhs, start, stop):
            return lambda: pe.matmul(out, lhsT=lhsT, rhs=rhs, start=start, stop=stop)

        class Common:
            def __init__(self, sc_):
                self.SQ = [sc_.sb("SQ0", [128, 512], BF16)] * 2
                self.SQR = [kb.res("sq0")] * 2
                self.RS = sc_.sb("RS", [128, 512])
                self.RSR = kb.res("rs")
                self.T1 = sc_.sb("T1", [128, 512])
                self.T1R = kb.res("t1")
                self.T2 = self.RS
                self.T2R = self.RSR
                self.T3 = sc_.sb("T3", [128, 512])
                self.T3R = kb.res("t3")
                self.RT = [sc_.sb("RT0", [128, 2, 512], BF16)] * 2
                self.RTR = [kb.res("rt0")] * 2
                self.rti = 0
                self.E = [sc_.sb("E%d" % i, [128, 512], BF16) for i in range(2)]
                self.ER = [kb.res("e%d" % i) for i in range(2)]
                self.sqi = 0

        def warmup(n=24):
            kb.op("pe", [mm(PS[7][:, 0:512], CSTB[:, 0:128], CSTB[:, 0:512], True, True) for _ in range(n)],
                  reads=[CSTR], writes=[PSR[7]])

        class BG:
            def __init__(self, gen):
                self.gen = gen
                self.safe = True
                self.done = gen is None

            def step(self):
                if self.done:
                    return False
                try:
                    self.safe = bool(next(self.gen))
                    return True
                except StopIteration:
                    self.done = True
                    self.safe = True
                    return False

            def to_safe(self):
                while not self.done and not self.safe:
                    self.step()

            def finish(self):
                while self.step():
                    pass

        def exhaust(gen):
            if gen is not None:
                for _ in gen:
                    pass

        def stepn(gen, n):
            if gen is None:
                return
            for _ in range(n):
                try:
                    next(gen)
                except StopIteration:
                    return

        def fm_rmsnorm_g(cm, banks, P, nfeat, gains, outs, n, out_res, ssb=7, ones_ap=None):
            srcs = [(PS[b][0:P, 0:n], PSR[b]) if isinstance(b, int) else b for b in banks]
            if ones_ap is None:
                ones_ap = ONESB[0:P, 0:P]
            nb = len(srcs)
            for j, (ap_, r_) in enumerate(srcs):
                q = cm.sqi % 2
                cm.sqi += 1
                kb.op("act", lambda ap_=ap_, q=q: a.activation(out=cm.SQ[q][0:P, 0:n], in_=ap_, func=AF.Square),
                      reads=[r_], writes=[cm.SQR[q]])
                yield
                kb.op("pe", mm(PS[ssb][0:P, 0:n], ones_ap, cm.SQ[q][0:P, 0:n], j == 0, j == nb - 1),
                      reads=[cm.SQR[q], CSTR], writes=[PSR[ssb]])
                yield
            kb.op("act", lambda: a.activation(out=cm.RS[0:P, 0:n], in_=PS[ssb][0:P, 0:n], func=AF.Ln,
                                              scale=1.0 / nfeat, bias=EPSC[0:P, :]),
                  reads=[PSR[ssb], CSTR], writes=[cm.RSR])
            yield
            kb.op("act", lambda: a.activation(out=cm.RS[0:P, 0:n], in_=cm.RS[0:P, 0:n], func=AF.Exp, scale=-0.5),
                  reads=[cm.RSR], writes=[cm.RSR])
            yield
            for j, (ap_, r_) in enumerate(srcs):
                kb.op("dve", lambda j=j, ap_=ap_: v.scalar_tensor_tensor(
                    out=outs[j], in0=ap_, scalar=gains[j], in1=cm.RS[0:P, 0:n], op0=ALU.mult, op1=ALU.mult),
                    reads=[r_, cm.RSR, PPR, CSTR], writes=[out_res])
                yield

        def fm_rmsnorm(*args, **kw):
            exhaust(fm_rmsnorm_g(*args, **kw))

        def rope_fm_g(cm, src, src_res, P, lt0, n, pb):
            i = cm.rti % 2
            cm.rti += 1
            kb.dma("pool", lambda: g.dma_start(out=cm.RT[i][:, :, 0:n], in_=rope_d[:, :, lt0:lt0 + n]), cm.RTR[i], True)
            kb.op("pe", mm(PS[pb][0:P, 0:n], PERMB[0:P, 0:P], src, True, True), reads=[src_res, CSTR], writes=[PSR[pb]])
            yield
            kb.op("dve", lambda: v.tensor_tensor(out=cm.T3[0:P, 0:n], in0=src, in1=cm.RT[i][0:P, 0, 0:n], op=ALU.mult),
                  reads=[src_res, cm.RTR[i]], writes=[cm.T3R])
            yield
            kb.op("dve", lambda: v.tensor_tensor(out=cm.T2[0:P, 0:n], in0=PS[pb][0:P, 0:n], in1=cm.RT[i][0:P, 1, 0:n], op=ALU.mult),
                  reads=[PSR[pb], cm.RTR[i]], writes=[cm.T2R])
            yield
            kb.op("dve", lambda: v.tensor_tensor(out=src, in0=cm.T3[0:P, 0:n], in1=cm.T2[0:P, 0:n], op=ALU.add),
                  reads=[cm.T3R, cm.T2R], writes=[src_res])
            yield

        def rope_fm(*args, **kw):
            exhaust(rope_fm_g(*args, **kw))

        def attn_core(cm, kts, n, s_terms, s_reads, v_fn, v_reads, scale, ob=2, zb=3, tick=None):
            def emit_s(i):
                terms = s_terms(kts[i])
                kb.op("pe", [mm(PS[i % 2][:, 0:n], l_, r_, ti == 0, ti == len(terms) - 1) for ti, (l_, r_) in enumerate(terms)],
                      reads=s_reads, writes=[PSR[i % 2]])
            emit_s(0)
            last = len(kts) - 1
            for i, kt in enumerate(kts):
                if i < last:
                    emit_s(i + 1)
                eb = i % 2
                kb.op("act", lambda i=i, eb=eb: a.activation(out=cm.E[eb][:, 0:n], in_=PS[i % 2][:, 0:n], func=AF.Exp, scale=scale),
                      reads=[PSR[i % 2]], writes=[cm.ER[eb]])
                kb.op("pe", [mm(PS[ob][:, 0:n], v_fn(kt), cm.E[eb][:, 0:n], i == 0, i == last),
                             mm(PS[zb][:, 0:n], ONESB, cm.E[eb][:, 0:n], i == 0, i == last)],
                      reads=[cm.ER[eb], CSTR] + v_reads, writes=[PSR[ob], PSR[zb]])
                if tick is not None:
                    tick()

        def wout_partial(cm, sc_, lhs_fn, lhs_reads, nk, wviews, wres, tiles, tag):
            TM = [cm.T1, cm.RS]
            TMR = [cm.T1R, cm.RSR]
            c_ = 0
            for t in tiles:
                w_ = 1 if t < 2 else 0
                for nh in range(2):
                    pb = 4 + (c_ % 4)
                    ti = c_ % 2
                    c_ += 1
                    kb.op("pe", [mm(PS[pb][:], lhs_fn(k, t), wviews[nh][:, k, :], k == 0, k == nk - 1) for k in range(nk)],
                          reads=lhs_reads(t) + [wres[nh]], writes=[PSR[pb]])
                    kb.op("dve", lambda pb=pb, ti=ti, nh=nh, w_=w_: v.tensor_tensor(
                        out=TM[ti][:], in0=PS[pb][:], in1=GBC[:, w_, nh * 512:(nh + 1) * 512], op=ALU.mult),
                        reads=[PSR[pb], GBCR], writes=[TMR[ti]])
                    kb.op("pool", lambda t=t, nh=nh, ti=ti: g.tensor_tensor(
                        out=X[:, t, nh * 512:(nh + 1) * 512], in0=X[:, t, nh * 512:(nh + 1) * 512], in1=TM[ti][:], op=ALU.add),
                        reads=[TMR[ti], XR[t][nh]], writes=[XR[t][nh]])

        def blk_of_tile(t):
            return 0 if t < 2 else 1 + (t - 2) // 4

        def mla_mixer():
            with Scope() as sc_:
                cm = Common(sc_)
                CQN = sc_.sb("CQN", [128, 3, SEQ], BF16)
                CQNR = kb.res("cqn")
                CKVN = sc_.sb("CKVN", [128, 2, NTOK], BF16)
                CKVNR = kb.res("ckvn")
                KRT = sc_.sb("KRT", [128, NTOK], BF16)
                KRTR = kb.res("krt")
                KNT = [sc_.sb("KNT%d" % i, [128, NTOK], BF16) for i in range(2)]
                KNTR = [kb.res("knt%d" % i) for i in range(2)]
                VH = [sc_.sb("VH%d" % i, [128, NT, 128], BF16) for i in range(2)]
                VHR = [kb.res("vh%d" % i) for i in range(2)]
                QNT = [sc_.sb("QNT%d" % i, [128, 512], BF16) for i in range(2)]
                QNTR = [kb.res("qnt%d" % i) for i in range(2)]
                QRT = [sc_.sb("QRT%d" % i, [128, 512], BF16) for i in range(2)]
                QRTR = [kb.res("qrt%d" % i) for i in range(2)]
                ra, wa = wload(owin_d[:, 0:384], 8, 384)
                rb, wb = wload(owin_d[:, 384:704], 8, 320)
                for blk, (t0, n) in enumerate(BLOCKS):
                    lat = blk > 0
                    if lat:
                        for j in range(3):
                            kb.op("pe", [mm(PS[4 + j][:, 0:n], wa[:, k, j * 128:(j + 1) * 128], HT[:, k, t0:t0 + n], k == 0, k == 7)
                                         for k in range(8)], reads=[ra, HTR[blk]], writes=[PSR[4 + j]])
                        fm_rmsnorm(cm, [4, 5, 6], 128, 384, [ppc("qag", j) for j in range(3)],
                                   [CQN[:, j, t0 - 256:t0 - 256 + n] for j in range(3)], n, CQNR)
                    for j in range(2):
                        kb.op("pe", [mm(PS[4 + j][:, 0:n], wb[:, k, j * 128:(j + 1) * 128], HT[:, k, t0:t0 + n], k == 0, k == 7)
                                     for k in range(8)], reads=[rb, HTR[blk]], writes=[PSR[4 + j]])
                    fm_rmsnorm(cm, [4, 5], 128, 256, [ppc("kvag", j) for j in range(2)],
                               [CKVN[:, j, t0:t0 + n] for j in range(2)], n, CKVNR)
                    kb.op("pe", [mm(PS[6][0:64, 0:n], wb[:, k, 256:320], HT[:, k, t0:t0 + n], k == 0, k == 7)
                                 for k in range(8)], reads=[rb, HTR[blk]], writes=[PSR[6]])
                    fm_rmsnorm(cm, [6], 64, 64, [ppc("krg")[0:64, :]], [KRT[0:64, t0:t0 + n]], n, KRTR)
                    if lat:
                        rope_fm(cm, KRT[0:64, t0:t0 + n], KRTR, 64, t0 - 256, n, 6)
                rq0, wq0 = wload(wuq_d[:, 0:768], 3, 768)
                rq1, wq1 = wload(wuq_d[:, 768:1536], 3, 768)
                rkv, wkv = wload(wukv_d, 2, 2048)
                scale = 192.0 ** -0.5
                warmup()

                def prep_head(h, bf):
                    knt, kntR = KNT[bf], KNTR[bf]
                    vh, vhR = VH[bf], VHR[bf]
                    for blk, (t0, n) in enumerate(BLOCKS):
                        kb.op("pe", [mm(PS[6][:, 0:n], wkv[:, j, h * 256:h * 256 + 128], CKVN[:, j, t0:t0 + n], j == 0, j == 1)
                                     for j in range(2)], reads=[rkv, CKVNR], writes=[PSR[6]])
                        yield
                        yield from fm_rmsnorm_g(cm, [6], 128, 128, [ppc("kng")], [knt[:, t0:t0 + n]], n, kntR)
                        yield True
                    for g0 in range(0, NT, 4):
                        tl = list(range(g0, min(g0 + 4, NT)))
                        fns = []
                        for i, t in enumerate(tl):
                            for j in range(2):
                                fns.append(mm(PS[6][:, i * 128:(i + 1) * 128], CKVN[:, j, t * 128:(t + 1) * 128],
                                              wkv[:, j, h * 256 + 128:h * 256 + 256], j == 0, j == 1))
                        kb.op("pe", fns, reads=[rkv, CKVNR], writes=[PSR[6]])
                        yield
                        kb.op("act", lambda g0=g0, tl=tl: a.copy(
                            out=vh[:, g0:g0 + len(tl), :],
                            in_=PS[6][:, 0:len(tl) * 128].rearrange("p (t c) -> p t c", c=128)),
                            reads=[PSR[6]], writes=[vhR])
                        yield True

                def prep_q(h, qb, qq):
                    rq, wq = (rq0, wq0) if h < 4 else (rq1, wq1)
                    hq = h % 4
                    q0 = qb * 512
                    kb.op("pe", [mm(PS[6][:, 0:512], wq[:, j, hq * 192:hq * 192 + 128], CQN[:, j, q0:q0 + 512], j == 0, j == 2)
                                 for j in range(3)], reads=[rq, CQNR], writes=[PSR[6]])
                    yield
                    yield from fm_rmsnorm_g(cm, [6], 128, 128, [ppc("qng")], [QNT[qq][:, :]], 512, QNTR[qq])
                    kb.op("pe", [mm(PS[6][0:64, 0:512], wq[:, j, hq * 192 + 128:hq * 192 + 192], CQN[:, j, q0:q0 + 512], j == 0, j == 2)
                                 for j in range(3)], reads=[rq, CQNR], writes=[PSR[6]])
                    yield
                    yield from fm_rmsnorm_g(cm, [6], 64, 64, [ppc("qrg")[0:64, :]], [QRT[qq][0:64, :]], 512, QRTR[qq])
                    yield from rope_fm_g(cm, QRT[qq][0:64, :], QRTR[qq], 64, q0, 512, 6)

                exhaust(prep_head(0, 0))
                exhaust(prep_q(0, 0, 0))
                qi = 0
                for h in range(8):
                    bf = h % 2
                    bg_head = BG(prep_head(h + 1, (h + 1) % 2) if h + 1 < 8 else None)
                    for qb in range(4):
                        q0 = qb * 512
                        blk = qb + 1
                        qq = qi % 2
                        qi += 1
                        if qb + 1 < 4:
                            bg_q = BG(prep_q(h, qb + 1, qi % 2))
                        elif h + 1 < 8:
                            bg_q = BG(prep_q(h + 1, 0, qi % 2))
                        else:
                            bg_q = BG(None)

                        def tick(bg_q=bg_q, bg_head=bg_head):
                            for _ in range(2):
                                if not bg_q.done:
                                    bg_q.step()
                                else:
                                    bg_head.step()
                        ob, zb = (2, 3) if qi % 2 else (4, 5)
                        attn_core(cm, list(range(NT)), 512,
                                  lambda kt, qq=qq, bf=bf: [(KNT[bf][:, kt * 128:(kt + 1) * 128], QNT[qq][:, :]),
                                                            (KRT[0:64, kt * 128:(kt + 1) * 128], QRT[qq][0:64, :])],
                                  [KNTR[bf], KRTR, QNTR[qq], QRTR[qq]],
                                  lambda kt, bf=bf: VH[bf][:, kt, :], [VHR[bf]], scale, ob=ob, zb=zb, tick=tick)
                        kb.op("dve", lambda zb=zb: v.reciprocal(out=cm.T1[:, :], in_=PS[zb][:, :]), reads=[PSR[zb]], writes=[cm.T1R])
                        kb.op("dve", lambda h=h, q0=q0, ob=ob: v.tensor_tensor(
                            out=HT[:, h, 256 + q0:256 + q0 + 512], in0=PS[ob][:, :], in1=cm.T1[:, :], op=ALU.mult),
                            reads=[PSR[ob], cm.T1R], writes=[HTR[blk]])
                        bg_q.finish()
                        bg_head.to_safe()
                    bg_head.finish()
                rw0, wo0 = wload(owout_d[:, 0:512], 8, 512)
                rw1, wo1 = wload(owout_d[:, 512:1024], 8, 512)
                wout_partial(cm, sc_, lambda k, t: HT[:, k, t * 128:(t + 1) * 128], lambda t: [HTR[blk_of_tile(t)]],
                             8, [wo0, wo1], [rw0, rw1], list(range(2, NT)), "m")

        BLK64B = CSTB[:, C_BLK64:C_BLK64 + 128]
        ONEC = sb("ONEC", [128, 1])
        kb.op("dve", lambda: v.memset(ONEC[:], 1.0), writes=[CSTR])
        LAMC = sb("LAMC", [128, 4])
        LAMR = kb.res("lamc")
        OMLF = sb("OMLF", [128, 8])
        OMLFR = kb.res("omlf")
        LAM_INIT0 = 0.8 - 0.6 * math.exp(-0.3 * 0)

        def even_prep():
            o, _ = PPO["dlam"]
            S0, R0 = SM[4], SMR[4]
            S1, R1 = SM[5], SMR[5]
            for i in range(2):
                kb.op("dve", lambda i=i: v.tensor_tensor(out=S0[:, 0:64], in0=PPT[:, o + i * 128:o + i * 128 + 64],
                                                         in1=PPT[:, o + i * 128 + 64:o + i * 128 + 128], op=ALU.mult),
                      reads=[PPR], writes=[R0])
                kb.op("dve", lambda i=i: v.tensor_reduce(out=S1[:, i:i + 1], in_=S0[:, 0:64], axis=AX.X, op=ALU.add),
                      reads=[R0], writes=[R1])
            kb.op("act", lambda: a.activation(out=S1[:, 2:4], in_=S1[:, 0:2], func=AF.Exp), reads=[R1], writes=[R1])
            kb.op("dve", lambda: v.scalar_tensor_tensor(out=LAMC[:, 0:1], in0=S1[:, 3:4], scalar=-LAM_INIT0, in1=S1[:, 2:3],
                                                        op0=ALU.add, op1=ALU.subtract), reads=[R1], writes=[LAMR])
            kb.op("dve", lambda: v.tensor_scalar(out=LAMC[:, 1:2], in0=ppc("subln"), scalar1=1.0 - LAM_INIT0, scalar2=None,
                                                 op0=ALU.mult), reads=[PPR], writes=[LAMR])
            o2, _ = PPO["lbfm"]
            kb.op("dve", lambda: v.tensor_tensor(out=S0[:, 0:8], in0=PPT[:, o2:o2 + 8], in1=PPT[:, o2 + 8:o2 + 16], op=ALU.subtract),
                  reads=[PPR], writes=[R0])
            kb.op("act", lambda: a.activation(out=S0[:, 0:8], in_=S0[:, 0:8], func=AF.Exp), reads=[R0], writes=[R0])
            kb.op("dve", lambda: v.tensor_scalar(out=S0[:, 0:8], in0=S0[:, 0:8], scalar1=1.0, scalar2=None, op0=ALU.add),
                  reads=[R0], writes=[R0])
            kb.op("dve", lambda: v.reciprocal(out=OMLF[:, :], in_=S0[:, 0:8]), reads=[R0], writes=[OMLFR])

        def diff_part():
            with Scope() as sc_:
                cm = Common(sc_)
                cm.T4 = sc_.sb("T4d", [128, 512])
                cm.T4R = kb.res("t4d")
                KT = [sc_.sb("KT%d" % i, [128, NTOK], BF16) for i in range(2)]
                KTR = [kb.res("kt%d" % i) for i in range(2)]
                QT = [sc_.sb("QTb%d" % i, [128, 512], BF16) for i in range(2)]
                QTR = [kb.res("qtb%d" % i) for i in range(2)]
                VH = [sc_.sb("VHd%d" % i, [128, NT, 128], BF16) for i in range(2)]
                VHR = [kb.res("vhd%d" % i) for i in range(2)]
                CATA = sc_.sb("CATA", [128, 4, NTOK], BF16)
                CATAR = [kb.res("cata%d" % i) for i in range(len(BLOCKS))]
                rq, wq = wload(ewin_d[:, 0:512], 8, 512)
                rk, wk = wload(ewin_d[:, 512:1024], 8, 512)
                rv, wv = wload(ewin_d[:, 1024:1536], 8, 512)
                scale = 64.0 ** -0.5
                warmup()

                def prep_head(h, bf):
                    for blk, (t0, n) in enumerate(BLOCKS):
                        kb.op("pe", [mm(PS[6][:, 0:n], wk[:, k, h * 128:(h + 1) * 128], HT[:, k, t0:t0 + n], k == 0, k == 7)
                                     for k in range(8)], reads=[rk, HTR[blk]], writes=[PSR[6]])
                        yield
                        yield from fm_rmsnorm_g(cm, [6], 128, 64, [ppc("dkg")], [KT[bf][:, t0:t0 + n]], n, KTR[bf], ones_ap=BLK64B)
                        if blk > 0:
                            yield from rope_fm_g(cm, KT[bf][:, t0:t0 + n], KTR[bf], 128, t0 - 256, n, 6)
                        yield True
                    for g0 in range(0, NT, 4):
                        tl = list(range(g0, min(g0 + 4, NT)))
                        fns = []
                        for i, t in enumerate(tl):
                            for k in range(8):
                                fns.append(mm(PS[6][:, i * 128:(i + 1) * 128], HT[:, k, t * 128:(t + 1) * 128],
                                              wv[:, k, h * 128:(h + 1) * 128], k == 0, k == 7))
                        kb.op("pe", fns, reads=[rv] + [HTR[blk_of_tile(t)] for t in tl], writes=[PSR[6]])
                        yield
                        kb.op("act", lambda g0=g0, tl=tl: a.copy(
                            out=VH[bf][:, g0:g0 + len(tl), :],
                            in_=PS[6][:, 0:len(tl) * 128].rearrange("p (t c) -> p t c", c=128)),
                            reads=[PSR[6]], writes=[VHR[bf]])
                        yield True

                def prep_q(h, blk, qq):
                    t0, n = BLOCKS[blk]
                    kb.op("pe", [mm(PS[6][:, 0:n], wq[:, k, h * 128:(h + 1) * 128], HT[:, k, t0:t0 + n], k == 0, k == 7)
                                 for k in range(8)], reads=[rq, HTR[blk]], writes=[PSR[6]])
                    yield
                    yield from fm_rmsnorm_g(cm, [6], 128, 64, [ppc("dqg")], [QT[qq][:, 0:n]], n, QTR[qq], ones_ap=BLK64B)
                    if blk > 0:
                        yield from rope_fm_g(cm, QT[qq][:, 0:n], QTR[qq], 128, t0 - 256, n, 6)

                exhaust(prep_head(0, 0))
                exhaust(prep_q(0, 0, 0))
                qi = 0
                nblk = len(BLOCKS)
                for h in range(4):
                    bf = h % 2
                    bg_head = BG(prep_head(h + 1, (h + 1) % 2) if h + 1 < 4 else None)
                    for blk, (t0, n) in enumerate(BLOCKS):
                        qq = qi % 2
                        qi += 1
                        if blk + 1 < nblk:
                            bg_q = BG(prep_q(h, blk + 1, qi % 2))
                        elif h + 1 < 4:
                            bg_q = BG(prep_q(h + 1, 0, qi % 2))
                        else:
                            bg_q = BG(None)

                        def tick(bg_q=bg_q, bg_head=bg_head):
                            if not bg_q.done:
                                bg_q.step()
                            else:
                                bg_head.step()
                        kts = [0, 1] if blk == 0 else list(range(NT))
                        for c in range(2):
                            attn_core(cm, kts, n,
                                      lambda kt, c=c, bf=bf, qq=qq: [(KT[bf][c * 64:(c + 1) * 64, kt * 128:(kt + 1) * 128],
                                                                      QT[qq][c * 64:(c + 1) * 64, 0:n])],
                                      [KTR[bf], QTR[qq]], lambda kt, bf=bf: VH[bf][:, kt, :], [VHR[bf]], scale,
                                      ob=2 + 2 * c, zb=3 + 2 * c, tick=tick)
                        kb.op("dve", lambda: v.reciprocal(out=cm.T1[:, 0:n], in_=PS[3][:, 0:n]), reads=[PSR[3]], writes=[cm.T1R])
                        kb.op("dve", lambda: v.tensor_tensor(out=cm.T1[:, 0:n], in0=PS[2][:, 0:n], in1=cm.T1[:, 0:n], op=ALU.mult),
                              reads=[PSR[2], cm.T1R], writes=[cm.T1R])
                        kb.op("dve", lambda: v.reciprocal(out=cm.T4[:, 0:n], in_=PS[5][:, 0:n]), reads=[PSR[5]], writes=[cm.T4R])
                        kb.op("dve", lambda: v.tensor_tensor(out=cm.T4[:, 0:n], in0=PS[4][:, 0:n], in1=cm.T4[:, 0:n], op=ALU.mult),
                              reads=[PSR[4], cm.T4R], writes=[cm.T4R])
                        kb.op("dve", lambda: v.scalar_tensor_tensor(out=cm.T1[:, 0:n], in0=cm.T4[:, 0:n], scalar=LAMC[:, 0:1],
                                                                    in1=cm.T1[:, 0:n], op0=ALU.mult, op1=ALU.add),
                              reads=[cm.T4R, cm.T1R, LAMR], writes=[cm.T1R])
                        bg_q.finish()
                        bg_head.to_safe()
                        fm_rmsnorm(cm, [(cm.T1[:, 0:n], cm.T1R)], 128, 128, [LAMC[:, 1:2]], [CATA[:, h, t0:t0 + n]], n, CATAR[blk])
                    bg_head.finish()
                rw0, wo0 = wload(ewout_d[0:512, 0:512], 4, 512)
                rw1, wo1 = wload(ewout_d[0:512, 512:1024], 4, 512)
                wout_partial(cm, sc_, lambda k, t: CATA[:, k, t * 128:(t + 1) * 128], lambda t: [CATAR[blk_of_tile(t)]],
                             4, [wo0, wo1], [rw0, rw1], list(range(NT)), "d")

        def hgrn_part():
            with Scope() as sc_:
                RING.append(sc_.sb("hring", [128, SLOT_ELEMS], BF16))
                RINGR.append(kb.res("hring"))
                ring_pos[0] = 0
                _hgrn_part(sc_)
                kb.barrier()
                del RING[NSLOT:]
                del RINGR[NSLOT:]
                ring_pos[0] = 0

        def _hgrn_part(sc_):
            def T_(name, shape, dt=F32):
                return sc_.sb(name, shape, dt), kb.res(name)
            TA, TAR = T_("hTA", [128, 512])
            TB, TBR = T_("hTB", [128, 512])
            TC, TCR = T_("hTC", [128, 512])
            TD, TDR = T_("hTD", [128, 512])
            EC, ECR = T_("hEC", [128, 512])
            KBAR = [[T_("hKBAR%d%d" % (d, b), [128, 4, 128], BF16) for b in range(2)] for d in range(2)]
            KTIL = [[T_("hKTIL%d%d" % (d, b), [128, 512], BF16) for b in range(2)] for d in range(2)]
            QZ = [[T_("hQZ%d%d" % (d, b), [128, 4, 256], BF16) for b in range(2)] for d in range(2)]
            ALAST = [[T_("hAL%d%d" % (d, b), [128, 8]) for b in range(2)] for d in range(2)]
            VH, VHR = T_("hVH", [128, NT, 128], BF16)
            OD = [T_("hO%d" % d, [128, NT, 128], BF16) for d in range(2)]
            ST = [T_("hS%d" % d, [128, 128]) for d in range(2)]
            SBF = [T_("hSB%d" % d, [128, 128], BF16) for d in range(2)]
            SCM = [T_("hSCM%d" % d, [128, 128], BF16) for d in range(2)]
            OMLBC, OMLBCR = T_("hOMLBC", [128, 2, 128])
            CATB, CATBR = T_("hCATB", [128, 512], BF16)
            SSH, SSHR = T_("hSSH", [128, 8])
            PSS = [[kb.res("pss%d_%d" % (d, i)) for i in range(3)] for d in range(2)]
            for d in range(2):
                for b in range(2):
                    kb.op("pool", lambda d=d, b=b: g.memset(QZ[d][b][0][:], 0.0), writes=[QZ[d][b][1]])

            class Shim:
                pass
            cm = Shim()
            cm.T1, cm.T1R, cm.RS, cm.RSR = TA, TAR, TB, TBR
            G = [[0, 1], [2, 3, 4, 5], [6, 7, 8, 9], [10, 11, 12, 13], [14, 15, 16, 17]]
            GORD = [G, [G[0], G[4], G[3], G[2], G[1]]]
            TRI = [CST[:, C_TRIF:C_TRIF + 128], CST[:, C_TRIB:C_TRIB + 128]]
            AFT = [CST[:, C_AFTF:C_AFTF + 128], CST[:, C_AFTB:C_AFTB + 128]]
            hscale = 128.0 ** -0.5
            hb = 1536

            def ht_res(tiles):
                return list({id(HTR[blk_of_tile(t)]): HTR[blk_of_tile(t)] for t in tiles}.values())

            for hh in range(4):
                ia = ring_pos[0] % len(RING)
                ring_pos[0] += 1
                ib = ring_pos[0] % len(RING)
                ring_pos[0] += 1
                rA, rB = RINGR[ia], RINGR[ib]
                wA = RING[ia][:, 0:4096].rearrange("p (k c) -> p k c", k=8)
                for pi, c0 in enumerate((hb + hh * 128, hb + 512 + hh * 128, hb + 1024 + hh * 128, hb + 1536 + hh * 128)):
                    kb.dma("pool", lambda pi=pi, c0=c0: g.dma_start(
                        out=wA[:, :, pi * 128:(pi + 1) * 128],
                        in_=ewin_d[:, c0:c0 + 128].rearrange("(k p) c -> p k c", p=128)), rA, True)
                wG = RING[ib][:, 0:1024].rearrange("p (k c) -> p k c", k=8)
                wO = RING[ib][:, 1024:2048]
                c0 = hb + 2048 + hh * 128
                kb.dma("pool", lambda: g.dma_start(out=wG, in_=ewin_d[:, c0:c0 + 128].rearrange("(k p) c -> p k c", p=128)), rB, True)
                kb.dma("pool", lambda: g.dma_start(out=wO, in_=ewout_d[512 + hh * 128:512 + (hh + 1) * 128, :]), rB, True)
                for d in range(2):
                    kb.op("dve", lambda d=d: v.tensor_copy(out=TD[:, 0:128], in_=OMLF[:, d * 4 + hh:d * 4 + hh + 1].to_broadcast([128, 128])),
                          reads=[OMLFR], writes=[TDR])
                    kb.op("pe", mm(PS[0][:, 0:128], TD[:, 0:128], IDENT, True, True), reads=[TDR, CSTR], writes=[PSR[0]])
                    kb.op("act", lambda d=d: a.copy(out=OMLBC[:, d, :], in_=PS[0][:, 0:128]), reads=[PSR[0]], writes=[OMLBCR])
                for g0 in range(0, NT, 4):
                    tl = list(range(g0, min(g0 + 4, NT)))
                    fns = []
                    for i, t in enumerate(tl):
                        for k in range(8):
                            fns.append(mm(PS[3][:, i * 128:(i + 1) * 128], HT[:, k, t * 128:(t + 1) * 128], wA[:, k, 384:512], k == 0, k == 7))
                    kb.op("pe", fns, reads=[rA] + ht_res(tl), writes=[PSR[3]])
                    kb.op("act", lambda g0=g0, tl=tl: a.copy(out=VH[:, g0:g0 + len(tl), :],
                                                           in_=PS[3][:, 0:len(tl) * 128].rearrange("p (t c) -> p t c", c=128)),
                          reads=[PSR[3]], writes=[VHR])
                for d in range(2):
                    kb.op("dve", lambda d=d: v.memset(ST[d][0][:], 0.0), writes=[ST[d][1]])
                    kb.op("dve", lambda d=d: v.memset(SBF[d][0][:], 0.0), writes=[SBF[d][1]])

                def prep(d, tiles, b):
                    ng = len(tiles)
                    n = ng * 128
                    tk0 = tiles[0] * 128
                    fc = 128 + d * 128
                    hr = ht_res(tiles)
                    kbar, kbarR = KBAR[d][b]
                    ktil, ktilR = KTIL[d][b]
                    qz, qzR = QZ[d][b]
                    al, alR = ALAST[d][b]
                    fns = []
                    for i, t in enumerate(tiles):
                        for k in range(8):
                            fns.append(mm(PS[0][:, i * 128:(i + 1) * 128], HT[:, k, t * 128:(t + 1) * 128], wA[:, k, fc:fc + 128], k == 0, k == 7))
                    kb.op("pe", fns, reads=[rA] + hr, writes=[PSR[0]])
                    kb.op("pe", [mm(PS[1][:, 0:n], wA[:, k, fc:fc + 128], HT[:, k, tk0:tk0 + n], k == 0, k == 7) for k in range(8)],
                          reads=[rA] + hr, writes=[PSR[1]])
                    kb.op("pe", [mm(PS[2][:, 0:n], wA[:, k, 0:128], HT[:, k, tk0:tk0 + n], k == 0, k == 7) for k in range(8)],
                          reads=[rA] + hr, writes=[PSR[2]])
                    kb.op("act", lambda: a.activation(out=TA[:, 0:n], in_=PS[0][:, 0:n], func=AF.Exp), reads=[PSR[0]], writes=[TAR])
                    kb.op("dve", lambda: v.tensor_scalar(out=TA[:, 0:n], in0=TA[:, 0:n], scalar1=1.0, scalar2=None, op0=ALU.add),
                          reads=[TAR], writes=[TAR])
                    kb.op("dve", lambda: v.reciprocal(out=TA[:, 0:n], in_=TA[:, 0:n]), reads=[TAR], writes=[TAR])
                    kb.op("dve", lambda: v.tensor_tensor(
                        out=TB[:, 0:n].rearrange("p (t c) -> p t c", c=128), in0=TA[:, 0:n].rearrange("p (t c) -> p t c", c=128),
                        in1=OMLBC[:, d:d + 1, :].to_broadcast([128, ng, 128]), op=ALU.mult),
                        reads=[TAR, OMLBCR], writes=[TBR])
                    kb.op("act", lambda: a.activation(out=TC[:, 0:n], in_=TB[:, 0:n], func=AF.Ln, scale=-1.0, bias=ONEC[:, :]),
                          reads=[TBR, CSTR], writes=[TCR])
                    kb.op("pe", [mm(PS[3][:, i * 128:(i + 1) * 128], AFT[d], TC[:, i * 128:(i + 1) * 128], True, True) for i in range(ng)],
                          reads=[TCR, CSTR], writes=[PSR[3]])
                    kb.op("pe", [mm(PS[4][:, i * 128:(i + 1) * 128], TC[:, i * 128:(i + 1) * 128], TRI[d], True, True) for i in range(ng)],
                          reads=[TCR, CSTR], writes=[PSR[4]])
                    kb.op("act", lambda: a.activation(out=TA[:, 0:n], in_=PS[3][:, 0:n], func=AF.Exp), reads=[PSR[3]], writes=[TAR])
                    kb.op("dve", lambda: v.tensor_tensor(out=kbar[:, 0:ng, :], in0=TB[:, 0:n].rearrange("p (t c) -> p t c", c=128),
                                                         in1=TA[:, 0:n].rearrange("p (t c) -> p t c", c=128), op=ALU.mult),
                          reads=[TAR, TBR], writes=[kbarR])
                    kb.op("act", lambda: a.activation(out=TD[:, 0:n], in_=PS[1][:, 0:n], func=AF.Exp), reads=[PSR[1]], writes=[TDR])
                    kb.op("dve", lambda: v.tensor_scalar(out=TD[:, 0:n], in0=TD[:, 0:n], scalar1=1.0, scalar2=None, op0=ALU.add),
                          reads=[TDR], writes=[TDR])
                    kb.op("dve", lambda: v.reciprocal(out=TD[:, 0:n], in_=TD[:, 0:n]), reads=[TDR], writes=[TDR])
                    kb.op("act", lambda: a.activation(out=EC[:, 0:n], in_=PS[4][:, 0:n], func=AF.Exp), reads=[PSR[4]], writes=[ECR])
                    kb.op("act", lambda: a.activation(out=TA[:, 0:n], in_=PS[4][:, 0:n], func=AF.Exp, scale=-1.0), reads=[PSR[4]], writes=[TAR])
                    kb.op("dve", lambda: v.scalar_tensor_tensor(out=ktil[:, 0:n], in0=TD[:, 0:n], scalar=OMLF[:, d * 4 + hh:d * 4 + hh + 1],
                                                                in1=TA[:, 0:n], op0=ALU.mult, op1=ALU.mult),
                          reads=[TDR, TAR, OMLFR], writes=[ktilR])
                    lastcol = 63 if d == 0 else 0
                    kb.op("dve", lambda: v.tensor_copy(out=al[:, 0:2 * ng],
                                                       in_=EC[:, 0:n].rearrange("p (c j) -> p c j", j=64)[:, :, lastcol]),
                          reads=[ECR], writes=[alR])
                    kb.op("act", lambda: a.activation(out=TB[:, 0:n], in_=PS[2][:, 0:n], func=AF.Exp, scale=-1.0), reads=[PSR[2]], writes=[TBR])
                    kb.op("dve", lambda: v.tensor_scalar(out=TB[:, 0:n], in0=TB[:, 0:n], scalar1=1.0, scalar2=None, op0=ALU.add),
                          reads=[TBR], writes=[TBR])
                    kb.op("dve", lambda: v.reciprocal(out=TB[:, 0:n], in_=TB[:, 0:n]), reads=[TBR], writes=[TBR])
                    kb.op("dve", lambda: v.tensor_tensor(out=TB[:, 0:n], in0=PS[2][:, 0:n], in1=TB[:, 0:n], op=ALU.mult),
                          reads=[TBR, PSR[2]], writes=[TBR])
                    for c in range(2):
                        kb.op("dve", lambda c=c: v.scalar_tensor_tensor(
                            out=qz[:, 0:ng, c * 192:c * 192 + 64],
                            in0=TB[:, 0:n].rearrange("p (t c) -> p t c", c=128)[:, :, c * 64:(c + 1) * 64], scalar=hscale,
                            in1=EC[:, 0:n].rearrange("p (t c) -> p t c", c=128)[:, :, c * 64:(c + 1) * 64], op0=ALU.mult, op1=ALU.mult),
                            reads=[TBR, ECR], writes=[qzR])

                def recur(d, t, i, b):
                    bank = 5 + d
                    s_ap = PS[bank][:, 0:128]
                    o_ap = PS[bank][:, 128:256]
                    ds_ap = PS[7][:, 256 + d * 128:384 + d * 128]
                    sR, oR, dsR = PSS[d]
                    kbar, kbarR = KBAR[d][b]
                    ktil, ktilR = KTIL[d][b]
                    qz, qzR = QZ[d][b]
                    al, alR = ALAST[d][b]
                    st, stR = ST[d]
                    sbf, sbfR = SBF[d]
                    scm, scmR = SCM[d]
                    kt_ = ktil[:, i * 128:(i + 1) * 128]
                    kb.op("pe", [mm(s_ap[:, 0:64], kt_, qz[:, i, 0:64], True, True), mm(s_ap[:, 64:128], kt_, qz[:, i, 192:256], True, True)],
                          reads=[ktilR, qzR], writes=[sR])
                    kb.op("dve", lambda: v.tensor_tensor(out=scm[:, :], in0=s_ap, in1=TRI[d], op=ALU.mult), reads=[sR, CSTR], writes=[scmR])
                    cs = [0, 1] if d == 0 else [1, 0]

                    def upd(c):
                        kb.op("pe", mm(ds_ap, kbar[c * 64:(c + 1) * 64, i, :], VH[c * 64:(c + 1) * 64, t, :], True, True),
                              reads=[kbarR, VHR], writes=[dsR])
                        kb.op("dve", lambda: v.scalar_tensor_tensor(out=st[:, :], in0=st[:, :], scalar=al[:, 2 * i + c:2 * i + c + 1],
                                                                    in1=ds_ap, op0=ALU.mult, op1=ALU.add),
                              reads=[stR, alR, dsR], writes=[stR])
                        kb.op("act", lambda: a.copy(out=sbf[:, :], in_=st[:, :]), reads=[stR], writes=[sbfR])
                    kb.op("pe", [mm(o_ap, scm[:, :], VH[:, t, :], True, False),
                                 mm(o_ap, qz[:, i, cs[0] * 128:(cs[0] + 1) * 128], sbf[:, :], False, False)],
                          reads=[scmR, VHR, qzR, sbfR], writes=[oR])
                    upd(cs[0])
                    kb.op("pe", mm(o_ap, qz[:, i, cs[1] * 128:(cs[1] + 1) * 128], sbf[:, :], False, True),
                          reads=[qzR, sbfR], writes=[oR])
                    upd(cs[1])
                    kb.op("act", lambda: a.copy(out=OD[d][0][:, t, :], in_=o_ap), reads=[oR], writes=[OD[d][1]])

                bufi = [0, 0]
                pend = [None, None]
                for d in range(2):
                    pend[d] = (GORD[d][0], bufi[d] % 2)
                    prep(d, GORD[d][0], bufi[d] % 2)
                    bufi[d] += 1
                for gi in range(5):
                    cur = [pend[0], pend[1]]
                    if gi + 1 < 5:
                        for d in range(2):
                            pend[d] = (GORD[d][gi + 1], bufi[d] % 2)
                            prep(d, GORD[d][gi + 1], bufi[d] % 2)
                            bufi[d] += 1
                    ftiles, fb = cur[0]
                    btiles, bb = cur[1]
                    border = list(reversed(btiles))
                    for j in range(max(len(ftiles), len(border))):
                        if j < len(ftiles):
                            recur(0, ftiles[j], j, fb)
                        if j < len(border):
                            recur(1, border[j], btiles.index(border[j]), bb)
                cnt = 0
                for tiles in G:
                    ng = len(tiles)
                    n = ng * 128
                    t0_ = tiles[0]
                    fns = []
                    for i, t in enumerate(tiles):
                        for k in range(8):
                            fns.append(mm(PS[0][:, i * 128:(i + 1) * 128], HT[:, k, t * 128:(t + 1) * 128], wG[:, k, :], k == 0, k == 7))
                    kb.op("pe", fns, reads=[rB] + ht_res(tiles), writes=[PSR[0]])
                    kb.op("dve", lambda: v.tensor_tensor(out=TA[:, 0:n].rearrange("p (t c) -> p t c", c=128),
                                                         in0=OD[0][0][:, t0_:t0_ + ng, :], in1=OD[1][0][:, t0_:t0_ + ng, :], op=ALU.add),
                          reads=[OD[0][1], OD[1][1]], writes=[TAR])
                    for i in range(ng):
                        kb.op("act", lambda i=i: a.activation(out=TD[:, i * 128:(i + 1) * 128], in_=TA[:, i * 128:(i + 1) * 128],
                                                              func=AF.Square, accum_out=SSH[:, i:i + 1]),
                              reads=[TAR], writes=[TDR, SSHR])
                    kb.op("dve", lambda: v.tensor_scalar(out=SSH[:, 0:ng], in0=SSH[:, 0:ng], scalar1=1.0 / 128, scalar2=EPS,
                                                         op0=ALU.mult, op1=ALU.add), reads=[SSHR], writes=[SSHR])
                    kb.op("act", lambda: a.activation(out=SSH[:, 0:ng], in_=SSH[:, 0:ng], func=AF.Ln), reads=[SSHR], writes=[SSHR])
                    kb.op("act", lambda: a.activation(out=SSH[:, 0:ng], in_=SSH[:, 0:ng], func=AF.Exp, scale=-0.5), reads=[SSHR], writes=[SSHR])
                    for i in range(ng):
                        kb.op("dve", lambda i=i: v.scalar_tensor_tensor(
                            out=TA[:, i * 128:(i + 1) * 128], in0=TA[:, i * 128:(i + 1) * 128], scalar=SSH[:, i:i + 1],
                            in1=ppc("hog", 0, 128), op0=ALU.mult, op1=ALU.mult), reads=[TAR, SSHR, PPR], writes=[TAR])
                    kb.op("act", lambda: a.activation(out=TB[:, 0:n], in_=PS[0][:, 0:n], func=AF.Exp, scale=-1.0), reads=[PSR[0]], writes=[TBR])
                    kb.op("dve", lambda: v.tensor_scalar(out=TB[:, 0:n], in0=TB[:, 0:n], scalar1=1.0, scalar2=None, op0=ALU.add),
                          reads=[TBR], writes=[TBR])
                    kb.op("dve", lambda: v.reciprocal(out=TB[:, 0:n], in_=TB[:, 0:n]), reads=[TBR], writes=[TBR])
                    kb.op("dve", lambda: v.tensor_tensor(out=TB[:, 0:n], in0=PS[0][:, 0:n], in1=TB[:, 0:n], op=ALU.mult),
                          reads=[TBR, PSR[0]], writes=[TBR])
                    kb.op("dve", lambda: v.tensor_tensor(out=TA[:, 0:n], in0=TA[:, 0:n], in1=TB[:, 0:n], op=ALU.mult),
                          reads=[TAR, TBR], writes=[TAR])
                    kb.op("pe", [mm(PS[1][:, i * 128:(i + 1) * 128], TA[:, i * 128:(i + 1) * 128], IDENT, True, True) for i in range(ng)],
                          reads=[TAR, CSTR], writes=[PSR[1]])
                    kb.op("act", lambda: a.copy(out=CATB[:, 0:n], in_=PS[1][:, 0:n]), reads=[PSR[1]], writes=[CATBR])
                    for i, t in enumerate(tiles):
                        w_ = 1 if t < 2 else 0
                        for nh in range(2):
                            pb = 2 + (cnt % 2)
                            tm_, tmR = (TC, TCR) if cnt % 2 == 0 else (TD, TDR)
                            cnt += 1
                            kb.op("pe", mm(PS[pb][:], CATB[:, i * 128:(i + 1) * 128], wO[:, nh * 512:(nh + 1) * 512], True, True),
                                  reads=[CATBR, rB], writes=[PSR[pb]])
                            kb.op("dve", lambda pb=pb, nh=nh, w_=w_, tm_=tm_: v.tensor_tensor(
                                out=tm_[:], in0=PS[pb][:], in1=GBC[:, w_, nh * 512:(nh + 1) * 512], op=ALU.mult),
                                reads=[PSR[pb], GBCR], writes=[tmR])
                            kb.op("pool", lambda t=t, nh=nh, tm_=tm_: g.tensor_tensor(
                                out=X[:, t, nh * 512:(nh + 1) * 512], in0=X[:, t, nh * 512:(nh + 1) * 512], in1=tm_[:], op=ALU.add),
                                reads=[tmR, XR[t][nh]], writes=[XR[t][nh]])

        def even_mixer():
            even_prep()
            if flags.get("diff", True):
                diff_part()
            if flags.get("hgrn", True):
                hgrn_part()

        for l in range(2):
            with_ctx = (l == 0)
            if (l == 0 and do_mix0) or (l == 1 and do_mix1):
                norm_phase(l, 0, True, False, 2)
                if l == 0:
                    even_mixer()
                else:
                    mla_mixer()
            if do_ffn:
                norm_phase(l, 1, with_ctx, True, 5)
                moe_phase(l, with_ctx)

        outdeps = []
        for t in range(2, NT):
            for h in range(2):
                outdeps.append(kb.dma("sp", lambda t=t, h=h: nc.sync.dma_start(
                    out=y_d[(t - 2) * 128:(t - 1) * 128, h * 512:(h + 1) * 512], in_=X[:, t, h * 512:(h + 1) * 512]),
                    XR[t][h], False))
        if taps:
            for t in range(NT):
                for h in range(2):
                    outdeps.append(kb.dma("sp", lambda t=t, h=h: nc.sync.dma_start(
                        out=tap_d[t * 128:(t + 1) * 128, h * 512:(h + 1) * 512], in_=X[:, t, h * 512:(h + 1) * 512]),
                        XR[t][h], False))
        kb.final_wait("sp", outdeps)
    return nc


WEIGHT_KEYS = ["mod_w", "even_w_in", "even_w_out", "odd_w_in", "mla_w_uq", "mla_w_ukv", "odd_w_out",
               "expert_w_gate", "expert_w_up", "expert_w_down", "shared_w_gate", "shared_w_up", "shared_w_down"]


def make_in_maps(inp, cores):
    cst = _consts()
    rope = _rope_tables()
    shared = {}
    for k in WEIGHT_KEYS:
        arr = np.ascontiguousarray(np.asarray(inp[k], np.float32))
        if k in ("even_w_in", "even_w_out", "odd_w_in", "mla_w_uq", "mla_w_ukv", "odd_w_out"):
            arr = arr[0]
        shared[k] = arr
    maps = []
    for b in cores:
        m = dict(shared)
        m["x"] = np.ascontiguousarray(np.asarray(inp["x"][b], np.float32))
        m["ctx"] = np.ascontiguousarray(np.asarray(inp["ctx"][b], np.float32))
        m["pp"] = _pack_params(b, inp)
        m["cst"] = cst
        m["rope"] = rope
        maps.append(m)
    return maps


def kernel(**inputs):
    nc = build_program()
    maps = make_in_maps(inputs, list(range(8)))
    res = run_bass_kernel_spmd(nc, maps, core_ids=list(range(8)))
    return np.stack([np.asarray(r["y"], np.float32) for r in res.results], axis=0)
```

```python
import math
import numpy as np
from contextlib import ExitStack
import concourse.bass as bass
import concourse.mybir as mybir
from concourse.bass_utils import run_bass_kernel_spmd

F32 = mybir.dt.float32
BF16 = mybir.dt.bfloat16
AF = mybir.ActivationFunctionType
ALU = mybir.AluOpType
AX = mybir.AxisListType

D = 1024
SEQ = 2048
CTX = 256
NTOK = SEQ + CTX
NT = NTOK // 128
BLOCKS = [(0, 256)] + [(256 + 512 * i, 512) for i in range(4)]
EPS = 1e-6
NSLOT = 3
SLOT_ELEMS = 4096

C_ID = 0
C_ONES = 128
C_BLK64 = 256
C_PERM = 384
C_TRIF = 512
C_TRIB = 640
C_AFTF = 768
C_AFTB = 896
C_N = 1024


def _consts():
    c = np.zeros((128, C_N), np.float32)
    i = np.arange(128)
    s = i[:, None]
    t = i[None, :]
    same = (s // 64) == (t // 64)
    c[:, C_ID:C_ID + 128] = (s == t)
    c[:, C_ONES:C_ONES + 128] = 1.0
    c[:, C_BLK64:C_BLK64 + 128] = same
    partner = np.where((i % 32) < 16, i + 16, i - 16)
    c[:, C_PERM:C_PERM + 128] = (s == partner[None, :])
    c[:, C_TRIF:C_TRIF + 128] = same & (s <= t)
    c[:, C_TRIB:C_TRIB + 128] = same & (s >= t)
    c[:, C_AFTF:C_AFTF + 128] = same & (s > t)
    c[:, C_AFTB:C_AFTB + 128] = same & (s < t)
    return c


def _rope_tables():
    tok = np.arange(SEQ)
    row = (tok // 64).astype(np.float32)
    col = (tok % 64).astype(np.float32)
    inv = (10000.0 ** (-np.arange(0, 32, 2, dtype=np.float32) / 32.0)).astype(np.float32)
    C = np.zeros((128, SEQ), np.float32)
    S = np.zeros((128, SEQ), np.float32)
    for p in range(128):
        d = p % 64
        pos = row if d < 32 else col
        f = inv[d % 16]
        ang = (pos * f).astype(np.float32)
        C[p] = np.cos(ang)
        S[p] = np.sin(ang) * (-1.0 if (d % 32) < 16 else 1.0)
    return np.stack([C, S], axis=1)


class PP:
    pass


def _pp_layout():
    off = {}
    n = 0

    def add(name, w):
        nonlocal n
        off[name] = (n, w)
        n += w
    add("c", 8)
    add("cctx", 8)
    add("modb", 2 * 48)
    add("nmix", 16)
    add("nffn", 16)
    add("rw", 8 * 16)
    add("rbias", 16)
    add("dqg", 1)
    add("dkg", 1)
    add("dlam", 256)
    add("subln", 1)
    add("lbfm", 2 * 2 * 4)
    add("hog", 128)
    add("qag", 3)
    add("kvag", 2)
    add("qng", 1)
    add("qrg", 1)
    add("kng", 1)
    add("krg", 1)
    return off, n


PPO, PPN = _pp_layout()


def _fm(v):
    v = np.asarray(v, np.float32)
    return np.ascontiguousarray(v.reshape(-1, 128).T)


def _pack_params(b, inp):
    pp = np.zeros((128, PPN), np.float32)

    def put(name, arr):
        o, w = PPO[name]
        arr = np.asarray(arr, np.float32).reshape(128, -1)
        assert arr.shape[1] == w, (name, arr.shape, w)
        pp[:, o:o + w] = arr
    put("c", _fm(inp["c"][b]))
    put("cctx", _fm(inp["c_ctx"]))
    put("modb", np.concatenate([_fm(inp["mod_b"][0]), _fm(inp["mod_b"][1])], axis=1))
    put("nmix", np.concatenate([_fm(inp["norm_mix"][0]), _fm(inp["norm_mix"][1])], axis=1))
    put("nffn", np.concatenate([_fm(inp["norm_ffn"][0]), _fm(inp["norm_ffn"][1])], axis=1))
    rw = np.asarray(inp["router_w"], np.float32).reshape(8, 128, 16).transpose(1, 0, 2)
    put("rw", rw.reshape(128, 128))
    put("rbias", np.broadcast_to(np.asarray(inp["router_bias"], np.float32)[None, :], (128, 16)))
    put("dqg", np.tile(np.asarray(inp["diff_q_gain"][0], np.float32), 2)[:, None])
    put("dkg", np.tile(np.asarray(inp["diff_k_gain"][0], np.float32), 2)[:, None])
    put("dlam", np.broadcast_to(np.asarray(inp["diff_lambda"][0], np.float32).reshape(1, 256), (128, 256)))
    put("subln", np.asarray(inp["diff_subln"][0], np.float32)[:, None])
    lb = np.asarray(inp["hgrn_lb_logits"], np.float32)
    put("lbfm", lb.reshape(2, 2, 4, 128).transpose(3, 0, 1, 2).reshape(128, 16))
    put("hog", np.broadcast_to(np.asarray(inp["hgrn_out_gain"][0], np.float32)[None, :], (128, 128)))
    put("qag", _fm(inp["mla_q_a_gain"][0]))
    put("kvag", _fm(inp["mla_kv_a_gain"][0]))
    put("qng", np.asarray(inp["mla_q_nope_gain"][0], np.float32)[:, None])
    put("qrg", np.tile(np.asarray(inp["mla_q_rope_gain"][0], np.float32), 2)[:, None])
    put("kng", np.asarray(inp["mla_k_nope_gain"][0], np.float32)[:, None])
    put("krg", np.tile(np.asarray(inp["mla_k_rope_gain"][0], np.float32), 2)[:, None])
    return pp


class Res:
    __slots__ = ("name", "w", "rs", "dsem", "dcnt")

    def __init__(self, name):
        self.name = name
        self.w = None
        self.rs = {}
        self.dsem = None
        self.dcnt = 0


class Eng:
    def __init__(self, name, obj, sem):
        self.name = name
        self.obj = obj
        self.sem = sem
        self.cnt = 0
        self.seen = {}


class KB:
    def __init__(self, nc, es):
        self.nc = nc
        self.es = es
        self.sems = {}
        self.E = {}
        for name, obj in (("pe", nc.tensor), ("act", nc.scalar), ("dve", nc.vector),
                          ("pool", nc.gpsimd), ("sp", nc.sync)):
            sem = es.enter_context(nc.semaphore("s_" + name))
            self.sems[name] = sem
            self.E[name] = Eng(name, obj, sem)
        self.nres = 0

    def res(self, name=None):
        self.nres += 1
        return Res(name or ("r%d" % self.nres))

    def _wait(self, E, reads, writes):
        deps = {}

        def add(d):
            if d is None:
                return
            k, v = d
            if deps.get(k, 0) < v:
                deps[k] = v
        for r in reads:
            add(r.w)
        for w in writes:
            add(w.w)
            for k, v in w.rs.items():
                add((k, v))
        for k, v in deps.items():
            if k == E.name and E.name == "pe":
                continue
            if E.seen.get(k, 0) < v:
                E.obj.wait_ge(self.sems[k], v)
                E.seen[k] = v

    def op(self, eng, fn, reads=(), writes=()):
        E = self.E[eng]
        self._wait(E, reads, writes)
        ins = None
        if callable(fn):
            ins = fn()
        else:
            for f in fn:
                ins = f()
        E.cnt += 1
        ins.then_inc(E.sem, 1)
        dep = (E.name, E.cnt)
        for r in reads:
            if r.rs.get(E.name, 0) < E.cnt:
                r.rs[E.name] = E.cnt
        for w in writes:
            w.w = dep
            w.rs = {}
        return ins

    def dma(self, queue, fn, res, is_write, reads=(), writes=()):
        E = self.E[queue]
        if res.dsem is None:
            self.nres += 1
            key = "d%d_%s" % (self.nres, res.name)
            res.dsem = key
            self.sems[key] = self.es.enter_context(self.nc.semaphore(key))
        rr = list(reads) + ([] if is_write else [res])
        ww = list(writes) + ([res] if is_write else [])
        self._wait(E, rr, ww)
        ins = fn()
        res.dcnt += 16
        ins.then_inc(self.sems[res.dsem], 16)
        dep = (res.dsem, res.dcnt)
        for r in rr:
            if r.rs.get(res.dsem, 0) < res.dcnt:
                r.rs[res.dsem] = res.dcnt
        for w in ww:
            w.w = dep
            w.rs = {}
        return dep

    def barrier(self):
        for E in self.E.values():
            for F in self.E.values():
                if F is E or F.cnt == 0:
                    continue
                if E.seen.get(F.name, 0) < F.cnt:
                    E.obj.wait_ge(self.sems[F.name], F.cnt)
                    E.seen[F.name] = F.cnt
            if E.name != "pe" and E.cnt > 0 and E.seen.get(E.name, 0) < E.cnt:
                E.obj.wait_ge(self.sems[E.name], E.cnt)
                E.seen[E.name] = E.cnt

    def final_wait(self, eng, deps):
        E = self.E[eng]
        for k, v in deps:
            E.obj.wait_ge(self.sems[k], v)


def build_program(flags=None):
    flags = flags or {}
    do_mix0 = flags.get("mix0", True)
    do_mix1 = flags.get("mix1", True)
    do_ffn = flags.get("ffn", True)
    taps = flags.get("taps", False)

    nc = bass.Bass("TRN2", target_bir_lowering=False)

    def din(name, shape):
        return nc.dram_tensor(name, list(shape), F32, kind="ExternalInput").ap()
    x_d = din("x", [SEQ, D])
    ctx_d = din("ctx", [CTX, D])
    pp_d = din("pp", [128, PPN])
    cst_d = din("cst", [128, C_N])
    rope_d = din("rope", [128, 2, SEQ])
    mod_w_d = din("mod_w", [2, D, 6 * D])
    ewin_d = din("even_w_in", [D, 4096])
    ewout_d = din("even_w_out", [D, D])
    owin_d = din("odd_w_in", [D, 704])
    wuq_d = din("mla_w_uq", [384, 1536])
    wukv_d = din("mla_w_ukv", [256, 2048])
    owout_d = din("odd_w_out", [D, D])
    xg_d = din("expert_w_gate", [2, 16, D, 512])
    xu_d = din("expert_w_up", [2, 16, D, 512])
    xd_d = din("expert_w_down", [2, 16, 512, D])
    sg_d = din("shared_w_gate", [2, D, 512])
    su_d = din("shared_w_up", [2, D, 512])
    sd_d = din("shared_w_down", [2, 512, D])
    y_d = nc.dram_tensor("y", [SEQ, D], F32, kind="ExternalOutput").ap()
    tap_d = None
    if taps:
        tap_d = nc.dram_tensor("tap", [NTOK, D], F32, kind="ExternalOutput").ap()

    es = ExitStack()
    with es:
        kb = KB(nc, es)

        def sb(name, shape, dt=F32):
            return es.enter_context(nc.sbuf_tensor(name, list(shape), dt))

        X = sb("X", [128, NT, D])
        XR = [[kb.res("x%d_%d" % (t, h)) for h in range(2)] for t in range(NT)]
        HT = sb("HT", [128, 8, NTOK], BF16)
        HTR = [kb.res("ht%d" % i) for i in range(len(BLOCKS))]
        PPT = sb("PPT", [128, PPN])
        PPR = kb.res("pp")
        CST = sb("CST", [128, C_N])
        CSTB = sb("CSTB", [128, C_N], BF16)
        CSTR = kb.res("cst")
        RING = [sb("ring%d" % i, [128, SLOT_ELEMS], BF16) for i in range(NSLOT)]
        RINGR = [kb.res("ring%d" % i) for i in range(NSLOT)]
        ring_pos = [0]
        PS = [es.enter_context(nc.psum_tensor("ps%d" % i, [128, 512], F32)) for i in range(8)]
        PSR = [kb.res("ps%d" % i) for i in range(8)]
        MODV = sb("MODV", [128, 2, 48, 2])
        MODR = kb.res("modv")
        NA = sb("NA", [128, 2, 8])
        NB = sb("NB", [128, 2, 8])
        NAR = kb.res("na")
        SS = sb("SS", [128, NT])
        SSR = kb.res("ss")
        RSTD = sb("RSTD", [128, NT])
        RSTDR = kb.res("rstd")
        GBC = sb("GBC", [128, 2, D], BF16)
        GBCR = kb.res("gbc")
        GATES = sb("GATES", [128, NT, 16])
        GATESR = kb.res("gates")
        SCB = sb("SCB", [128, 8, 2], BF16)
        SCR = kb.res("scb")

        scope_id = [0]

        class Scope:
            def __enter__(self):
                kb.barrier()
                scope_id[0] += 1
                self.sid = scope_id[0]
                self.stack = ExitStack()
                self.stack.__enter__()
                return self

            def sb(self, name, shape, dt=F32):
                return self.stack.enter_context(nc.sbuf_tensor("%s_s%d" % (name, self.sid), list(shape), dt))

            def __exit__(self, *exc):
                kb.barrier()
                return self.stack.__exit__(*exc)
        SM = [sb("SM%d" % i, [128, 64]) for i in range(6)]
        SMR = [kb.res("sm%d" % i) for i in range(6)]

        v = nc.vector
        a = nc.scalar
        g = nc.gpsimd
        pe = nc.tensor

        def ppc(name, i=0, w=1):
            o, _ = PPO[name]
            return PPT[:, o + i:o + i + w]

        kb.dma("sp", lambda: nc.sync.dma_start(out=PPT[:], in_=pp_d), PPR, True)
        kb.dma("sp", lambda: nc.sync.dma_start(out=CST[:], in_=cst_d), CSTR, True)
        kb.op("dve", lambda: v.tensor_copy(out=CSTB[:], in_=CST[:]), reads=[CSTR], writes=[CSTR])
        for t in range(NT):
            src = ctx_d[t * 128:(t + 1) * 128, :] if t < 2 else x_d[(t - 2) * 128:(t - 1) * 128, :]
            for h in range(2):
                kb.dma("sp", lambda src=src, t=t, h=h: nc.sync.dma_start(
                    out=X[:, t, h * 512:(h + 1) * 512], in_=src[:, h * 512:(h + 1) * 512]), XR[t][h], True)

        IDENT = CST[:, C_ID:C_ID + 128]

        def wload(src2d, kc, cols):
            assert kc * cols <= SLOT_ELEMS
            i = ring_pos[0] % len(RING)
            ring_pos[0] += 1
            view = RING[i][:, 0:kc * cols].rearrange("p (k c) -> p k c", k=kc)
            kb.dma("pool", lambda: g.dma_start(out=view, in_=src2d.rearrange("(k p) c -> p k c", p=128)),
                   RINGR[i], True)
            return RINGR[i], view

        def silu_small(out_ap, in_ap, sm_i, width, reads, writes):
            t1 = SM[sm_i][:, 0:width]
            kb.op("act", lambda: a.activation(out=t1, in_=in_ap, func=AF.Exp, scale=-1.0),
                  reads=reads, writes=[SMR[sm_i]])
            kb.op("dve", lambda: v.tensor_scalar(out=t1, in0=t1, scalar1=1.0, scalar2=None, op0=ALU.add),
                  reads=[SMR[sm_i]], writes=[SMR[sm_i]])
            kb.op("dve", lambda: v.reciprocal(out=t1, in_=t1), reads=[SMR[sm_i]], writes=[SMR[sm_i]])
            kb.op("dve", lambda: v.tensor_tensor(out=out_ap, in0=in_ap, in1=t1, op=ALU.mult),
                  reads=list(reads) + [SMR[sm_i]], writes=writes)

        silu_small(SCB[:, :, 0], ppc("c", 0, 8), 0, 8, [PPR], [SCR])
        silu_small(SCB[:, :, 1], ppc("cctx", 0, 8), 1, 8, [PPR, SCR], [SCR])

        for l in range(2):
            for s in range(12):
                r, wv = wload(mod_w_d[l, :, s * 512:(s + 1) * 512], 8, 512)
                pb = 0
                fns = []
                for j in range(4):
                    for k in range(8):
                        fns.append(lambda j=j, k=k, wv=wv, s=s: pe.matmul(
                            PS[pb][:, (s * 4 + j) * 2:(s * 4 + j) * 2 + 2], lhsT=wv[:, k, j * 128:(j + 1) * 128],
                            rhs=SCB[:, k, :], start=(k == 0), stop=(k == 7)))
                kb.op("pe", fns, reads=[r, SCR], writes=[PSR[pb]])
            o, _ = PPO["modb"]
            for w_ in range(2):
                kb.op("dve", lambda l=l, w_=w_: v.tensor_tensor(
                    out=MODV[:, l, :, w_], in0=PS[0][:, 0:96].rearrange("p (c w) -> p c w", w=2)[:, :, w_],
                    in1=PPT[:, o + l * 48:o + (l + 1) * 48], op=ALU.add),
                    reads=[PSR[0], PPR], writes=[MODR])

        def bc_from_fm(dst_ap_fn, vec_col_fn, dst_res, extra_reads, HF32, HF32R):
            for half in range(2):
                pb = 1 + half
                fns = []
                for kk in range(4):
                    k = half * 4 + kk
                    kb.op("dve", lambda k=k, kk=kk, half=half: v.tensor_copy(
                        out=HF32[half][:, kk, :], in_=vec_col_fn(k).to_broadcast([128, 128])),
                        reads=extra_reads, writes=[HF32R[half]])
                for kk in range(4):
                    fns.append(lambda kk=kk, half=half, pb=pb: pe.matmul(
                        PS[pb][:, kk * 128:(kk + 1) * 128], lhsT=HF32[half][:, kk, :], rhs=IDENT,
                        start=True, stop=True))
                kb.op("pe", fns, reads=[HF32R[half], CSTR], writes=[PSR[pb]])
                kb.op("act", lambda half=half, pb=pb: a.copy(out=dst_ap_fn(half), in_=PS[pb][:]),
                      reads=[PSR[pb]], writes=[dst_res])

        def norm_phase(l, which, with_ctx, router, gate_vec):
            with Scope() as sc_:
                XN = [sc_.sb("XN", [128, D])] * 2
                XNR = [kb.res("xn")] * 2
                HF32 = [sc_.sb("HF32_%d" % i, [128, 8, 128]) for i in range(2)]
                HF32R = [kb.res("hf32_%d" % i) for i in range(2)]
                JUNK = sc_.sb("JUNK", [128, D], BF16)
                JUNKR = kb.res("junk")
                _norm_phase(l, which, with_ctx, router, XN, XNR, HF32, HF32R, JUNK, JUNKR)
                for w_ in range(2):
                    bc_from_fm(lambda half, w_=w_: GBC[:, w_, half * 512:(half + 1) * 512],
                               lambda k, w_=w_: MODV[:, l, gate_vec * 8 + k, w_:w_ + 1], GBCR, [MODR], HF32, HF32R)

        def _norm_phase(l, which, with_ctx, router, XN, XNR, HF32, HF32R, JUNK, JUNKR):
            nw = "nmix" if which == 0 else "nffn"
            vsh = 0 if which == 0 else 3
            vsc = vsh + 1
            for w_ in range(2):
                kb.op("dve", lambda w_=w_: v.scalar_tensor_tensor(
                    out=NA[:, w_, :], in0=MODV[:, l, vsc * 8:(vsc + 1) * 8, w_], scalar=1.0,
                    in1=ppc(nw, l * 8, 8), op0=ALU.add, op1=ALU.mult),
                    reads=[MODR, PPR], writes=[NAR])
                kb.op("dve", lambda w_=w_: v.tensor_copy(out=NB[:, w_, :], in_=MODV[:, l, vsh * 8:(vsh + 1) * 8, w_]),
                      reads=[MODR], writes=[NAR])
            tiles = list(range(NT)) if with_ctx else list(range(2, NT))
            for t in tiles:
                kb.op("act", lambda t=t: a.activation(out=JUNK[:], in_=X[:, t, :], func=AF.Square,
                                                      accum_out=SS[:, t:t + 1]),
                      reads=[XR[t][0], XR[t][1]], writes=[JUNKR, SSR])
            t0 = tiles[0]
            kb.op("dve", lambda: v.tensor_scalar(out=RSTD[:, t0:NT], in0=SS[:, t0:NT], scalar1=1.0 / D, scalar2=EPS,
                                                 op0=ALU.mult, op1=ALU.add), reads=[SSR], writes=[RSTDR])
            kb.op("act", lambda: a.activation(out=RSTD[:, t0:NT], in_=RSTD[:, t0:NT], func=AF.Ln),
                  reads=[RSTDR], writes=[RSTDR])
            kb.op("act", lambda: a.activation(out=RSTD[:, t0:NT], in_=RSTD[:, t0:NT], func=AF.Exp, scale=-0.5),
                  reads=[RSTDR], writes=[RSTDR])
            for idx, t in enumerate(tiles):
                p = idx % 2
                w_ = 1 if t < 2 else 0
                blk = 0 if t < 2 else 1 + (t - 2) // 4
                kb.op("dve", lambda t=t, p=p: v.tensor_scalar(out=XN[p][:], in0=X[:, t, :], scalar1=RSTD[:, t:t + 1],
                                                              scalar2=None, op0=ALU.mult),
                      reads=[XR[t][0], XR[t][1], RSTDR], writes=[XNR[p]])
                for half in range(2):
                    pb = 1 + half
                    fns = [lambda kk=kk, half=half, pb=pb, p=p: pe.matmul(
                        PS[pb][:, kk * 128:(kk + 1) * 128], lhsT=XN[p][:, (half * 4 + kk) * 128:(half * 4 + kk + 1) * 128],
                        rhs=IDENT, start=True, stop=True) for kk in range(4)]
                    kb.op("pe", fns, reads=[XNR[p], CSTR], writes=[PSR[pb]])
                    for kk in range(4):
                        k = half * 4 + kk
                        kb.op("act", lambda kk=kk, k=k, pb=pb, p=p, w_=w_: a.activation(
                            out=HF32[p][:, k, :], in_=PS[pb][:, kk * 128:(kk + 1) * 128], func=AF.Identity,
                            scale=NA[:, w_, k:k + 1], bias=NB[:, w_, k:k + 1]),
                            reads=[PSR[pb], NAR], writes=[HF32R[p]])
                kb.op("pool", lambda t=t, p=p: g.tensor_copy(out=HT[:, :, t * 128:(t + 1) * 128], in_=HF32[p][:]),
                      reads=[HF32R[p]], writes=[HTR[blk]])
                if router:
                    o, _ = PPO["rw"]
                    fns = [lambda k=k, p=p: pe.matmul(PS[3][:, 0:16], lhsT=HF32[p][:, k, :],
                                                      rhs=PPT[:, o + k * 16:o + (k + 1) * 16],
                                                      start=(k == 0), stop=(k == 7)) for k in range(8)]
                    kb.op("pe", fns, reads=[HF32R[p], PPR], writes=[PSR[3]])
                    route(t)

        def route(t):
            S = SM[2]
            R = SMR[2]
            sc = S[:, 0:16]
            bi = S[:, 16:32]
            t2 = S[:, 32:48]
            m1 = SM[3][:, 0:4]
            m2 = SM[3][:, 4:8]
            gs = SM[3][:, 8:12]
            gm = SM[3][:, 12:13]
            ing = SM[3][:, 16:20]
            den = SM[3][:, 20:21]
            R3 = SMR[3]
            kb.op("act", lambda: a.activation(out=sc, in_=PS[3][:, 0:16], func=AF.Exp, scale=-1.0),
                  reads=[PSR[3]], writes=[R])
            kb.op("dve", lambda: v.tensor_scalar(out=sc, in0=sc, scalar1=1.0, scalar2=None, op0=ALU.add),
                  reads=[R], writes=[R])
            kb.op("dve", lambda: v.reciprocal(out=sc, in_=sc), reads=[R], writes=[R])
            kb.op("dve", lambda: v.tensor_tensor(out=bi, in0=sc, in1=ppc("rbias", 0, 16), op=ALU.add),
                  reads=[R, PPR], writes=[R])
            b3 = bi.rearrange("p (g e) -> p g e", e=4)
            t3 = t2.rearrange("p (g e) -> p g e", e=4)
            kb.op("dve", lambda: v.tensor_reduce(out=m1, in_=b3, axis=AX.X, op=ALU.max), reads=[R], writes=[R3])
            kb.op("dve", lambda: v.tensor_tensor(out=t3, in0=b3, in1=m1.unsqueeze(2).to_broadcast([128, 4, 4]),
                                                 op=ALU.is_equal), reads=[R, R3], writes=[R])
            kb.op("dve", lambda: v.scalar_tensor_tensor(out=t2, in0=t2, scalar=-1e9, in1=bi, op0=ALU.mult, op1=ALU.add),
                  reads=[R], writes=[R])
            kb.op("dve", lambda: v.tensor_reduce(out=m2, in_=t3, axis=AX.X, op=ALU.max), reads=[R], writes=[R3])
            kb.op("dve", lambda: v.tensor_tensor(out=gs, in0=m1, in1=m2, op=ALU.add), reads=[R3], writes=[R3])
            kb.op("dve", lambda: v.tensor_reduce(out=gm, in_=gs, axis=AX.X, op=ALU.max), reads=[R3], writes=[R3])
            kb.op("dve", lambda: v.tensor_scalar(out=ing, in0=gs, scalar1=gm, scalar2=None, op0=ALU.is_ge),
                  reads=[R3], writes=[R3])
            kb.op("dve", lambda: v.tensor_tensor(out=t3, in0=b3, in1=m2.unsqueeze(2).to_broadcast([128, 4, 4]),
                                                 op=ALU.is_ge), reads=[R, R3], writes=[R])
            kb.op("dve", lambda: v.tensor_tensor(out=t3, in0=t3, in1=ing.unsqueeze(2).to_broadcast([128, 4, 4]),
                                                 op=ALU.mult), reads=[R, R3], writes=[R])
            kb.op("dve", lambda: v.tensor_tensor(out=t2, in0=t2, in1=sc, op=ALU.mult), reads=[R], writes=[R])
            kb.op("dve", lambda: v.tensor_reduce(out=den, in_=t2, axis=AX.X, op=ALU.add), reads=[R], writes=[R3])
            kb.op("dve", lambda: v.reciprocal(out=den, in_=den), reads=[R3], writes=[R3])
            kb.op("dve", lambda: v.tensor_scalar(out=GATES[:, t, :], in0=t2, scalar1=den, scalar2=None, op0=ALU.mult),
                  reads=[R, R3], writes=[GATESR])

        def moe_phase(l, with_ctx):
            with Scope() as sc_:
                nextra = 5
                for i in range(nextra):
                    RING.append(sc_.sb("xring%d" % i, [128, SLOT_ELEMS], BF16))
                    RINGR.append(kb.res("xring%d_%d" % (l, i)))
                ring_pos[0] = 0
                _moe_phase(l, with_ctx, sc_.sb)
                kb.barrier()
                del RING[NSLOT:]
                del RINGR[NSLOT:]
                ring_pos[0] = 0

        def _moe_phase(l, with_ctx, psb):
            ACTT = [psb("ACTT%d" % i, [128, 4, 512], BF16) for i in range(2)]
            ACTTR = [kb.res("actt%d" % i) for i in range(2)]
            SIL = [psb("SIL%d" % i, [128, 512], BF16) for i in range(2)]
            SILR = [kb.res("sil%d" % i) for i in range(2)]
            TMP = [psb("TMP%d" % i, [128, 512]) for i in range(4)]
            TMPR = [kb.res("tmp%d" % i) for i in range(4)]
            blocks = BLOCKS if with_ctx else BLOCKS[1:]

            def load_expert(e):
                if e < 16:
                    srcs = (xg_d[l, e], xu_d[l, e], xd_d[l, e])
                else:
                    srcs = (sg_d[l], su_d[l], sd_d[l])
                return wload(srcs[0], 8, 512) + wload(srcs[1], 8, 512) + wload(srcs[2], 4, 1024)

            W = {0: load_expert(0)}
            items = [(e, bi_, t0, n) for e in range(17) for bi_, (t0, n) in enumerate(blocks)]

            def GU(idx):
                e, bi_, t0, n = items[idx]
                rg, vg, ru, vu, rd, vd = W[e]
                blk = BLOCKS.index((t0, n))
                ab = idx % 2
                for j in range(4):
                    pg = (j % 2)
                    pu = 2 + (j % 2)
                    kb.op("pe", [mm(PS[pg][:, 0:n], vg[:, k, j * 128:(j + 1) * 128], HT[:, k, t0:t0 + n], k == 0, k == 7)
                                 for k in range(8)], reads=[rg, HTR[blk]], writes=[PSR[pg]])
                    kb.op("pe", [mm(PS[pu][:, 0:n], vu[:, k, j * 128:(j + 1) * 128], HT[:, k, t0:t0 + n], k == 0, k == 7)
                                 for k in range(8)], reads=[ru, HTR[blk]], writes=[PSR[pu]])
                    sl = j % 2
                    kb.op("act", lambda sl=sl, pg=pg: a.activation(out=SIL[sl][:, 0:n], in_=PS[pg][:, 0:n], func=AF.Silu),
                          reads=[PSR[pg]], writes=[SILR[sl]])
                    kb.op("dve", lambda sl=sl, pu=pu, j=j, ab=ab: v.tensor_tensor(
                        out=ACTT[ab][:, j, 0:n], in0=SIL[sl][:, 0:n], in1=PS[pu][:, 0:n], op=ALU.mult),
                        reads=[SILR[sl], PSR[pu]], writes=[ACTTR[ab]])

            def DN(idx):
                e, bi_, t0, n = items[idx]
                rg, vg, ru, vu, rd, vd = W[e]
                ab = idx % 2
                for tt in range(n // 128):
                    t = (t0 + tt * 128) // 128
                    w_ = 1 if t < 2 else 0
                    for nh in range(2):
                        pd = 4 + ((tt * 2 + nh) % 4)
                        kb.op("pe", [mm(PS[pd][:], ACTT[ab][:, j, tt * 128:(tt + 1) * 128], vd[:, j, nh * 512:(nh + 1) * 512],
                                        j == 0, j == 3) for j in range(4)], reads=[rd, ACTTR[ab]], writes=[PSR[pd]])
                        ti = (tt * 2 + nh) % 4
                        if e < 16:
                            kb.op("dve", lambda pd=pd, t=t, e=e, ti=ti, nh=nh, w_=w_: v.scalar_tensor_tensor(
                                out=TMP[ti][:], in0=PS[pd][:], scalar=GATES[:, t, e:e + 1],
                                in1=GBC[:, w_, nh * 512:(nh + 1) * 512], op0=ALU.mult, op1=ALU.mult),
                                reads=[PSR[pd], GATESR, GBCR], writes=[TMPR[ti]])
                        else:
                            kb.op("dve", lambda pd=pd, ti=ti, nh=nh, w_=w_: v.tensor_tensor(
                                out=TMP[ti][:], in0=PS[pd][:], in1=GBC[:, w_, nh * 512:(nh + 1) * 512], op=ALU.mult),
                                reads=[PSR[pd], GBCR], writes=[TMPR[ti]])
                        kb.op("pool", lambda t=t, nh=nh, ti=ti: g.tensor_tensor(
                            out=X[:, t, nh * 512:(nh + 1) * 512], in0=X[:, t, nh * 512:(nh + 1) * 512],
                            in1=TMP[ti][:], op=ALU.add),
                            reads=[TMPR[ti], XR[t][nh]], writes=[XR[t][nh]])

            GU(0)
            for idx in range(len(items)):
                e, bi_ = items[idx][0], items[idx][1]
                if bi_ == 0 and e + 1 < 17:
                    W[e + 1] = load_expert(e + 1)
                if idx + 1 < len(items):
                    GU(idx + 1)
                DN(idx)

        ONESB = CSTB[:, C_ONES:C_ONES + 128]
        PERMB = CSTB[:, C_PERM:C_PERM + 128]
        EPSC = sb("EPSC", [128, 1])
        kb.op("dve", lambda: v.memset(EPSC[:], EPS), writes=[CSTR])

        def mm(out, lhsT, rhs, start, stop):
            return lambda: pe.matmul(out, lhsT=lhsT, rhs=rhs, start=start, stop=stop)

        class Common:
            def __init__(self, sc_):
                self.SQ = [sc_.sb("SQ0", [128, 512], BF16)] * 2
                self.SQR = [kb.res("sq0")] * 2
                self.RS = sc_.sb("RS", [128, 512])
                self.RSR = kb.res("rs")
                self.T1 = sc_.sb("T1", [128, 512])
                self.T1R = kb.res("t1")
                self.T2 = self.RS
                self.T2R = self.RSR
                self.T3 = sc_.sb("T3", [128, 512])
                self.T3R = kb.res("t3")
                self.RT = [sc_.sb("RT0", [128, 2, 512], BF16)] * 2
                self.RTR = [kb.res("rt0")] * 2
                self.rti = 0
                self.E = [sc_.sb("E%d" % i, [128, 512], BF16) for i in range(2)]
                self.ER = [kb.res("e%d" % i) for i in range(2)]
                self.sqi = 0

        def warmup(n=24):
            kb.op("pe", [mm(PS[7][:, 0:512], CSTB[:, 0:128], CSTB[:, 0:512], True, True) for _ in range(n)],
                  reads=[CSTR], writes=[PSR[7]])

        class BG:
            def __init__(self, gen):
                self.gen = gen
                self.safe = True
                self.done = gen is None

            def step(self):
                if self.done:
                    return False
                try:
                    self.safe = bool(next(self.gen))
                    return True
                except StopIteration:
                    self.done = True
                    self.safe = True
                    return False

            def to_safe(self):
                while not self.done and not self.safe:
                    self.step()

            def finish(self):
                while self.step():
                    pass

        def exhaust(gen):
            if gen is not None:
                for _ in gen:
                    pass

        def stepn(gen, n):
            if gen is None:
                return
            for _ in range(n):
                try:
                    next(gen)
                except StopIteration:
                    return

        def fm_rmsnorm_g(cm, banks, P, nfeat, gains, outs, n, out_res, ssb=7, ones_ap=None):
            srcs = [(PS[b][0:P, 0:n], PSR[b]) if isinstance(b, int) else b for b in banks]
            if ones_ap is None:
                ones_ap = ONESB[0:P, 0:P]
            nb = len(srcs)
            for j, (ap_, r_) in enumerate(srcs):
                q = cm.sqi % 2
                cm.sqi += 1
                kb.op("act", lambda ap_=ap_, q=q: a.activation(out=cm.SQ[q][0:P, 0:n], in_=ap_, func=AF.Square),
                      reads=[r_], writes=[cm.SQR[q]])
                yield
                kb.op("pe", mm(PS[ssb][0:P, 0:n], ones_ap, cm.SQ[q][0:P, 0:n], j == 0, j == nb - 1),
                      reads=[cm.SQR[q], CSTR], writes=[PSR[ssb]])
                yield
            kb.op("act", lambda: a.activation(out=cm.RS[0:P, 0:n], in_=PS[ssb][0:P, 0:n], func=AF.Ln,
                                              scale=1.0 / nfeat, bias=EPSC[0:P, :]),
                  reads=[PSR[ssb], CSTR], writes=[cm.RSR])
            yield
            kb.op("act", lambda: a.activation(out=cm.RS[0:P, 0:n], in_=cm.RS[0:P, 0:n], func=AF.Exp, scale=-0.5),
                  reads=[cm.RSR], writes=[cm.RSR])
            yield
            for j, (ap_, r_) in enumerate(srcs):
                kb.op("dve", lambda j=j, ap_=ap_: v.scalar_tensor_tensor(
                    out=outs[j], in0=ap_, scalar=gains[j], in1=cm.RS[0:P, 0:n], op0=ALU.mult, op1=ALU.mult),
                    reads=[r_, cm.RSR, PPR, CSTR], writes=[out_res])
                yield

        def fm_rmsnorm(*args, **kw):
            exhaust(fm_rmsnorm_g(*args, **kw))

        def rope_fm_g(cm, src, src_res, P, lt0, n, pb):
            i = cm.rti % 2
            cm.rti += 1
            kb.dma("pool", lambda: g.dma_start(out=cm.RT[i][:, :, 0:n], in_=rope_d[:, :, lt0:lt0 + n]), cm.RTR[i], True)
            kb.op("pe", mm(PS[pb][0:P, 0:n], PERMB[0:P, 0:P], src, True, True), reads=[src_res, CSTR], writes=[PSR[pb]])
            yield
            kb.op("dve", lambda: v.tensor_tensor(out=cm.T3[0:P, 0:n], in0=src, in1=cm.RT[i][0:P, 0, 0:n], op=ALU.mult),
                  reads=[src_res, cm.RTR[i]], writes=[cm.T3R])
            yield
            kb.op("dve", lambda: v.tensor_tensor(out=cm.T2[0:P, 0:n], in0=PS[pb][0:P, 0:n], in1=cm.RT[i][0:P, 1, 0:n], op=ALU.mult),
                  reads=[PSR[pb], cm.RTR[i]], writes=[cm.T2R])
            yield
            kb.op("dve", lambda: v.tensor_tensor(out=src, in0=cm.T3[0:P, 0:n], in1=cm.T2[0:P, 0:n], op=ALU.add),
                  reads=[cm.T3R, cm.T2R], writes=[src_res])
            yield

        def rope_fm(*args, **kw):
            exhaust(rope_fm_g(*args, **kw))

        def attn_core(cm, kts, n, s_terms, s_reads, v_fn, v_reads, scale, ob=2, zb=3, tick=None):
            def emit_s(i):
                terms = s_terms(kts[i])
                kb.op("pe", [mm(PS[i % 2][:, 0:n], l_, r_, ti == 0, ti == len(terms) - 1) for ti, (l_, r_) in enumerate(terms)],
                      reads=s_reads, writes=[PSR[i % 2]])
            emit_s(0)
            last = len(kts) - 1
            for i, kt in enumerate(kts):
                if i < last:
                    emit_s(i + 1)
                eb = i % 2
                kb.op("act", lambda i=i, eb=eb: a.activation(out=cm.E[eb][:, 0:n], in_=PS[i % 2][:, 0:n], func=AF.Exp, scale=scale),
                      reads=[PSR[i % 2]], writes=[cm.ER[eb]])
                kb.op("pe", [mm(PS[ob][:, 0:n], v_fn(kt), cm.E[eb][:, 0:n], i == 0, i == last),
                             mm(PS[zb][:, 0:n], ONESB, cm.E[eb][:, 0:n], i == 0, i == last)],
                      reads=[cm.ER[eb], CSTR] + v_reads, writes=[PSR[ob], PSR[zb]])
                if tick is not None:
                    tick()

        def wout_partial(cm, sc_, lhs_fn, lhs_reads, nk, wviews, wres, tiles, tag):
            TM = [cm.T1, cm.RS]
            TMR = [cm.T1R, cm.RSR]
            c_ = 0
            for t in tiles:
                w_ = 1 if t < 2 else 0
                for nh in range(2):
                    pb = 4 + (c_ % 4)
                    ti = c_ % 2
                    c_ += 1
                    kb.op("pe", [mm(PS[pb][:], lhs_fn(k, t), wviews[nh][:, k, :], k == 0, k == nk - 1) for k in range(nk)],
                          reads=lhs_reads(t) + [wres[nh]], writes=[PSR[pb]])
                    kb.op("dve", lambda pb=pb, ti=ti, nh=nh, w_=w_: v.tensor_tensor(
                        out=TM[ti][:], in0=PS[pb][:], in1=GBC[:, w_, nh * 512:(nh + 1) * 512], op=ALU.mult),
                        reads=[PSR[pb], GBCR], writes=[TMR[ti]])
                    kb.op("pool", lambda t=t, nh=nh, ti=ti: g.tensor_tensor(
                        out=X[:, t, nh * 512:(nh + 1) * 512], in0=X[:, t, nh * 512:(nh + 1) * 512], in1=TM[ti][:], op=ALU.add),
                        reads=[TMR[ti], XR[t][nh]], writes=[XR[t][nh]])

        def blk_of_tile(t):
            return 0 if t < 2 else 1 + (t - 2) // 4

        def mla_mixer():
            with Scope() as sc_:
                cm = Common(sc_)
                CQN = sc_.sb("CQN", [128, 3, SEQ], BF16)
                CQNR = kb.res("cqn")
                CKVN = sc_.sb("CKVN", [128, 2, NTOK], BF16)
                CKVNR = kb.res("ckvn")
                KRT = sc_.sb("KRT", [128, NTOK], BF16)
                KRTR = kb.res("krt")
                KNT = [sc_.sb("KNT%d" % i, [128, NTOK], BF16) for i in range(2)]
                KNTR = [kb.res("knt%d" % i) for i in range(2)]
                VH = [sc_.sb("VH%d" % i, [128, NT, 128], BF16) for i in range(2)]
                VHR = [kb.res("vh%d" % i) for i in range(2)]
                QNT = [sc_.sb("QNT%d" % i, [128, 512], BF16) for i in range(2)]
                QNTR = [kb.res("qnt%d" % i) for i in range(2)]
                QRT = [sc_.sb("QRT%d" % i, [128, 512], BF16) for i in range(2)]
                QRTR = [kb.res("qrt%d" % i) for i in range(2)]
                ra, wa = wload(owin_d[:, 0:384], 8, 384)
                rb, wb = wload(owin_d[:, 384:704], 8, 320)
                for blk, (t0, n) in enumerate(BLOCKS):
                    lat = blk > 0
                    if lat:
                        for j in range(3):
                            kb.op("pe", [mm(PS[4 + j][:, 0:n], wa[:, k, j * 128:(j + 1) * 128], HT[:, k, t0:t0 + n], k == 0, k == 7)
                                         for k in range(8)], reads=[ra, HTR[blk]], writes=[PSR[4 + j]])
                        fm_rmsnorm(cm, [4, 5, 6], 128, 384, [ppc("qag", j) for j in range(3)],
                                   [CQN[:, j, t0 - 256:t0 - 256 + n] for j in range(3)], n, CQNR)
                    for j in range(2):
                        kb.op("pe", [mm(PS[4 + j][:, 0:n], wb[:, k, j * 128:(j + 1) * 128], HT[:, k, t0:t0 + n], k == 0, k == 7)
                                     for k in range(8)], reads=[rb, HTR[blk]], writes=[PSR[4 + j]])
                    fm_rmsnorm(cm, [4, 5], 128, 256, [ppc("kvag", j) for j in range(2)],
                               [CKVN[:, j, t0:t0 + n] for j in range(2)], n, CKVNR)
                    kb.op("pe", [mm(PS[6][0:64, 0:n], wb[:, k, 256:320], HT[:, k, t0:t0 + n], k == 0, k == 7)
                                 for k in range(8)], reads=[rb, HTR[blk]], writes=[PSR[6]])
                    fm_rmsnorm(cm, [6], 64, 64, [ppc("krg")[0:64, :]], [KRT[0:64, t0:t0 + n]], n, KRTR)
                    if lat:
                        rope_fm(cm, KRT[0:64, t0:t0 + n], KRTR, 64, t0 - 256, n, 6)
                rq0, wq0 = wload(wuq_d[:, 0:768], 3, 768)
                rq1, wq1 = wload(wuq_d[:, 768:1536], 3, 768)
                rkv, wkv = wload(wukv_d, 2, 2048)
                scale = 192.0 ** -0.5
                warmup()

                def prep_head(h, bf):
                    knt, kntR = KNT[bf], KNTR[bf]
                    vh, vhR = VH[bf], VHR[bf]
                    for blk, (t0, n) in enumerate(BLOCKS):
                        kb.op("pe", [mm(PS[6][:, 0:n], wkv[:, j, h * 256:h * 256 + 128], CKVN[:, j, t0:t0 + n], j == 0, j == 1)
                                     for j in range(2)], reads=[rkv, CKVNR], writes=[PSR[6]])
                        yield
                        yield from fm_rmsnorm_g(cm, [6], 128, 128, [ppc("kng")], [knt[:, t0:t0 + n]], n, kntR)
                        yield True
                    for g0 in range(0, NT, 4):
                        tl = list(range(g0, min(g0 + 4, NT)))
                        fns = []
                        for i, t in enumerate(tl):
                            for j in range(2):
                                fns.append(mm(PS[6][:, i * 128:(i + 1) * 128], CKVN[:, j, t * 128:(t + 1) * 128],
                                              wkv[:, j, h * 256 + 128:h * 256 + 256], j == 0, j == 1))
                        kb.op("pe", fns, reads=[rkv, CKVNR], writes=[PSR[6]])
                        yield
                        kb.op("act", lambda g0=g0, tl=tl: a.copy(
                            out=vh[:, g0:g0 + len(tl), :],
                            in_=PS[6][:, 0:len(tl) * 128].rearrange("p (t c) -> p t c", c=128)),
                            reads=[PSR[6]], writes=[vhR])
                        yield True

                def prep_q(h, qb, qq):
                    rq, wq = (rq0, wq0) if h < 4 else (rq1, wq1)
                    hq = h % 4
                    q0 = qb * 512
                    kb.op("pe", [mm(PS[6][:, 0:512], wq[:, j, hq * 192:hq * 192 + 128], CQN[:, j, q0:q0 + 512], j == 0, j == 2)
                                 for j in range(3)], reads=[rq, CQNR], writes=[PSR[6]])
                    yield
                    yield from fm_rmsnorm_g(cm, [6], 128, 128, [ppc("qng")], [QNT[qq][:, :]], 512, QNTR[qq])
                    kb.op("pe", [mm(PS[6][0:64, 0:512], wq[:, j, hq * 192 + 128:hq * 192 + 192], CQN[:, j, q0:q0 + 512], j == 0, j == 2)
                                 for j in range(3)], reads=[rq, CQNR], writes=[PSR[6]])
                    yield
                    yield from fm_rmsnorm_g(cm, [6], 64, 64, [ppc("qrg")[0:64, :]], [QRT[qq][0:64, :]], 512, QRTR[qq])
                    yield from rope_fm_g(cm, QRT[qq][0:64, :], QRTR[qq], 64, q0, 512, 6)

                exhaust(prep_head(0, 0))
                exhaust(prep_q(0, 0, 0))
                qi = 0
                for h in range(8):
                    bf = h % 2
                    bg_head = BG(prep_head(h + 1, (h + 1) % 2) if h + 1 < 8 else None)
                    for qb in range(4):
                        q0 = qb * 512
                        blk = qb + 1
                        qq = qi % 2
                        qi += 1
                        if qb + 1 < 4:
                            bg_q = BG(prep_q(h, qb + 1, qi % 2))
                        elif h + 1 < 8:
                            bg_q = BG(prep_q(h + 1, 0, qi % 2))
                        else:
                            bg_q = BG(None)

                        def tick(bg_q=bg_q, bg_head=bg_head):
                            for _ in range(2):
                                if not bg_q.done:
                                    bg_q.step()
                                else:
                                    bg_head.step()
                        ob, zb = (2, 3) if qi % 2 else (4, 5)
                        attn_core(cm, list(range(NT)), 512,
                                  lambda kt, qq=qq, bf=bf: [(KNT[bf][:, kt * 128:(kt + 1) * 128], QNT[qq][:, :]),
                                                            (KRT[0:64, kt * 128:(kt + 1) * 128], QRT[qq][0:64, :])],
                                  [KNTR[bf], KRTR, QNTR[qq], QRTR[qq]],
                                  lambda kt, bf=bf: VH[bf][:, kt, :], [VHR[bf]], scale, ob=ob, zb=zb, tick=tick)
                        kb.op("act", lambda zb=zb: a.activation(out=cm.T1[:, :], in_=PS[zb][:, :], func=AF.Ln), reads=[PSR[zb]], writes=[cm.T1R])
                        kb.op("act", lambda: a.activation(out=cm.T1[:, :], in_=cm.T1[:, :], func=AF.Exp, scale=-1.0), reads=[cm.T1R], writes=[cm.T1R])
                        kb.op("dve", lambda h=h, q0=q0, ob=ob: v.tensor_tensor(
                            out=HT[:, h, 256 + q0:256 + q0 + 512], in0=PS[ob][:, :], in1=cm.T1[:, :], op=ALU.mult),
                            reads=[PSR[ob], cm.T1R], writes=[HTR[blk]])
                        bg_q.finish()
                        bg_head.to_safe()
                    bg_head.finish()
                rw0, wo0 = wload(owout_d[:, 0:512], 8, 512)
                rw1, wo1 = wload(owout_d[:, 512:1024], 8, 512)
                wout_partial(cm, sc_, lambda k, t: HT[:, k, t * 128:(t + 1) * 128], lambda t: [HTR[blk_of_tile(t)]],
                             8, [wo0, wo1], [rw0, rw1], list(range(2, NT)), "m")

        BLK64B = CSTB[:, C_BLK64:C_BLK64 + 128]
        ONEC = sb("ONEC", [128, 1])
        kb.op("dve", lambda: v.memset(ONEC[:], 1.0), writes=[CSTR])
        LAMC = sb("LAMC", [128, 4])
        LAMR = kb.res("lamc")
        OMLF = sb("OMLF", [128, 8])
        OMLFR = kb.res("omlf")
        LAM_INIT0 = 0.8 - 0.6 * math.exp(-0.3 * 0)

        def even_prep():
            o, _ = PPO["dlam"]
            S0, R0 = SM[4], SMR[4]
            S1, R1 = SM[5], SMR[5]
            for i in range(2):
                kb.op("dve", lambda i=i: v.tensor_tensor(out=S0[:, 0:64], in0=PPT[:, o + i * 128:o + i * 128 + 64],
                                                         in1=PPT[:, o + i * 128 + 64:o + i * 128 + 128], op=ALU.mult),
                      reads=[PPR], writes=[R0])
                kb.op("dve", lambda i=i: v.tensor_reduce(out=S1[:, i:i + 1], in_=S0[:, 0:64], axis=AX.X, op=ALU.add),
                      reads=[R0], writes=[R1])
            kb.op("act", lambda: a.activation(out=S1[:, 2:4], in_=S1[:, 0:2], func=AF.Exp), reads=[R1], writes=[R1])
            kb.op("dve", lambda: v.scalar_tensor_tensor(out=LAMC[:, 0:1], in0=S1[:, 3:4], scalar=-LAM_INIT0, in1=S1[:, 2:3],
                                                        op0=ALU.add, op1=ALU.subtract), reads=[R1], writes=[LAMR])
            kb.op("dve", lambda: v.tensor_scalar(out=LAMC[:, 1:2], in0=ppc("subln"), scalar1=1.0 - LAM_INIT0, scalar2=None,
                                                 op0=ALU.mult), reads=[PPR], writes=[LAMR])
            o2, _ = PPO["lbfm"]
            kb.op("dve", lambda: v.tensor_tensor(out=S0[:, 0:8], in0=PPT[:, o2:o2 + 8], in1=PPT[:, o2 + 8:o2 + 16], op=ALU.subtract),
                  reads=[PPR], writes=[R0])
            kb.op("act", lambda: a.activation(out=S0[:, 0:8], in_=S0[:, 0:8], func=AF.Exp), reads=[R0], writes=[R0])
            kb.op("dve", lambda: v.tensor_scalar(out=S0[:, 0:8], in0=S0[:, 0:8], scalar1=1.0, scalar2=None, op0=ALU.add),
                  reads=[R0], writes=[R0])
            kb.op("dve", lambda: v.reciprocal(out=OMLF[:, :], in_=S0[:, 0:8]), reads=[R0], writes=[OMLFR])

        def diff_part():
            with Scope() as sc_:
                cm = Common(sc_)
                cm.T4 = sc_.sb("T4d", [128, 512])
                cm.T4R = kb.res("t4d")
                KT = [sc_.sb("KT%d" % i, [128, NTOK], BF16) for i in range(2)]
                KTR = [kb.res("kt%d" % i) for i in range(2)]
                QT = [sc_.sb("QTb%d" % i, [128, 512], BF16) for i in range(2)]
                QTR = [kb.res("qtb%d" % i) for i in range(2)]
                VH = [sc_.sb("VHd%d" % i, [128, NT, 128], BF16) for i in range(2)]
                VHR = [kb.res("vhd%d" % i) for i in range(2)]
                CATA = sc_.sb("CATA", [128, 4, NTOK], BF16)
                CATAR = [kb.res("cata%d" % i) for i in range(len(BLOCKS))]
                rq, wq = wload(ewin_d[:, 0:512], 8, 512)
                rk, wk = wload(ewin_d[:, 512:1024], 8, 512)
                rv, wv = wload(ewin_d[:, 1024:1536], 8, 512)
                scale = 64.0 ** -0.5
                warmup()

                def prep_head(h, bf):
                    for blk, (t0, n) in enumerate(BLOCKS):
                        kb.op("pe", [mm(PS[6][:, 0:n], wk[:, k, h * 128:(h + 1) * 128], HT[:, k, t0:t0 + n], k == 0, k == 7)
                                     for k in range(8)], reads=[rk, HTR[blk]], writes=[PSR[6]])
                        yield
                        yield from fm_rmsnorm_g(cm, [6], 128, 64, [ppc("dkg")], [KT[bf][:, t0:t0 + n]], n, KTR[bf], ones_ap=BLK64B)
                        if blk > 0:
                            yield from rope_fm_g(cm, KT[bf][:, t0:t0 + n], KTR[bf], 128, t0 - 256, n, 6)
                        yield True
                    for g0 in range(0, NT, 4):
                        tl = list(range(g0, min(g0 + 4, NT)))
                        fns = []
                        for i, t in enumerate(tl):
                            for k in range(8):
                                fns.append(mm(PS[6][:, i * 128:(i + 1) * 128], HT[:, k, t * 128:(t + 1) * 128],
                                              wv[:, k, h * 128:(h + 1) * 128], k == 0, k == 7))
                        kb.op("pe", fns, reads=[rv] + [HTR[blk_of_tile(t)] for t in tl], writes=[PSR[6]])
                        yield
                        kb.op("act", lambda g0=g0, tl=tl: a.copy(
                            out=VH[bf][:, g0:g0 + len(tl), :],
                            in_=PS[6][:, 0:len(tl) * 128].rearrange("p (t c) -> p t c", c=128)),
                            reads=[PSR[6]], writes=[VHR[bf]])
                        yield True

                def prep_q(h, blk, qq):
                    t0, n = BLOCKS[blk]
                    kb.op("pe", [mm(PS[6][:, 0:n], wq[:, k, h * 128:(h + 1) * 128], HT[:, k, t0:t0 + n], k == 0, k == 7)
                                 for k in range(8)], reads=[rq, HTR[blk]], writes=[PSR[6]])
                    yield
                    yield from fm_rmsnorm_g(cm, [6], 128, 64, [ppc("dqg")], [QT[qq][:, 0:n]], n, QTR[qq], ones_ap=BLK64B)
                    if blk > 0:
                        yield from rope_fm_g(cm, QT[qq][:, 0:n], QTR[qq], 128, t0 - 256, n, 6)

                exhaust(prep_head(0, 0))
                exhaust(prep_q(0, 0, 0))
                qi = 0
                nblk = len(BLOCKS)
                for h in range(4):
                    bf = h % 2
                    bg_head = BG(prep_head(h + 1, (h + 1) % 2) if h + 1 < 4 else None)
                    for blk, (t0, n) in enumerate(BLOCKS):
                        qq = qi % 2
                        qi += 1
                        if blk + 1 < nblk:
                            bg_q = BG(prep_q(h, blk + 1, qi % 2))
                        elif h + 1 < 4:
                            bg_q = BG(prep_q(h + 1, 0, qi % 2))
                        else:
                            bg_q = BG(None)

                        def tick(bg_q=bg_q, bg_head=bg_head):
                            if not bg_q.done:
                                bg_q.step()
                            else:
                                bg_head.step()
                        kts = [0, 1] if blk == 0 else list(range(NT))
                        for c in range(2):
                            attn_core(cm, kts, n,
                                      lambda kt, c=c, bf=bf, qq=qq: [(KT[bf][c * 64:(c + 1) * 64, kt * 128:(kt + 1) * 128],
                                                                      QT[qq][c * 64:(c + 1) * 64, 0:n])],
                                      [KTR[bf], QTR[qq]], lambda kt, bf=bf: VH[bf][:, kt, :], [VHR[bf]], scale,
                                      ob=2 + 2 * c, zb=3 + 2 * c, tick=tick)
                        kb.op("act", lambda: a.activation(out=cm.T1[:, 0:n], in_=PS[3][:, 0:n], func=AF.Ln), reads=[PSR[3]], writes=[cm.T1R])
                        kb.op("act", lambda: a.activation(out=cm.T1[:, 0:n], in_=cm.T1[:, 0:n], func=AF.Exp, scale=-1.0), reads=[cm.T1R], writes=[cm.T1R])
                        kb.op("dve", lambda: v.tensor_tensor(out=cm.T1[:, 0:n], in0=PS[2][:, 0:n], in1=cm.T1[:, 0:n], op=ALU.mult),
                              reads=[PSR[2], cm.T1R], writes=[cm.T1R])
                        kb.op("act", lambda: a.activation(out=cm.T4[:, 0:n], in_=PS[5][:, 0:n], func=AF.Ln), reads=[PSR[5]], writes=[cm.T4R])
                        kb.op("act", lambda: a.activation(out=cm.T4[:, 0:n], in_=cm.T4[:, 0:n], func=AF.Exp, scale=-1.0), reads=[cm.T4R], writes=[cm.T4R])
                        kb.op("dve", lambda: v.tensor_tensor(out=cm.T4[:, 0:n], in0=PS[4][:, 0:n], in1=cm.T4[:, 0:n], op=ALU.mult),
                              reads=[PSR[4], cm.T4R], writes=[cm.T4R])
                        kb.op("dve", lambda: v.scalar_tensor_tensor(out=cm.T1[:, 0:n], in0=cm.T4[:, 0:n], scalar=LAMC[:, 0:1],
                                                                    in1=cm.T1[:, 0:n], op0=ALU.mult, op1=ALU.add),
                              reads=[cm.T4R, cm.T1R, LAMR], writes=[cm.T1R])
                        bg_q.finish()
                        bg_head.to_safe()
                        fm_rmsnorm(cm, [(cm.T1[:, 0:n], cm.T1R)], 128, 128, [LAMC[:, 1:2]], [CATA[:, h, t0:t0 + n]], n, CATAR[blk])
                    bg_head.finish()
                rw0, wo0 = wload(ewout_d[0:512, 0:512], 4, 512)
                rw1, wo1 = wload(ewout_d[0:512, 512:1024], 4, 512)
                wout_partial(cm, sc_, lambda k, t: CATA[:, k, t * 128:(t + 1) * 128], lambda t: [CATAR[blk_of_tile(t)]],
                             4, [wo0, wo1], [rw0, rw1], list(range(NT)), "d")

        def hgrn_part():
            with Scope() as sc_:
                RING.append(sc_.sb("hring", [128, SLOT_ELEMS], BF16))
                RINGR.append(kb.res("hring"))
                ring_pos[0] = 0
                _hgrn_part(sc_)
                kb.barrier()
                del RING[NSLOT:]
                del RINGR[NSLOT:]
                ring_pos[0] = 0

        def _hgrn_part(sc_):
            def T_(name, shape, dt=F32):
                return sc_.sb(name, shape, dt), kb.res(name)
            TA, TAR = T_("hTA", [128, 512])
            TB, TBR = T_("hTB", [128, 512])
            TC, TCR = T_("hTC", [128, 512])
            TD, TDR = T_("hTD", [128, 512])
            EC, ECR = T_("hEC", [128, 512])
            KBAR = [[T_("hKBAR%d%d" % (d, b), [128, 4, 128], BF16) for b in range(2)] for d in range(2)]
            KTIL = [[T_("hKTIL%d%d" % (d, b), [128, 512], BF16) for b in range(2)] for d in range(2)]
            QZ = [[T_("hQZ%d%d" % (d, b), [128, 4, 256], BF16) for b in range(2)] for d in range(2)]
            ALAST = [[T_("hAL%d%d" % (d, b), [128, 8]) for b in range(2)] for d in range(2)]
            VH, VHR = T_("hVH", [128, NT, 128], BF16)
            OD = [T_("hO%d" % d, [128, NT, 128], BF16) for d in range(2)]
            ST = [T_("hS%d" % d, [128, 128]) for d in range(2)]
            SBF = [T_("hSB%d" % d, [128, 128], BF16) for d in range(2)]
            SCM = [T_("hSCM%d" % d, [128, 128], BF16) for d in range(2)]
            OMLBC, OMLBCR = T_("hOMLBC", [128, 2, 128])
            CATB, CATBR = T_("hCATB", [128, 512], BF16)
            SSH, SSHR = T_("hSSH", [128, 8])
            PSS = [[kb.res("pss%d_%d" % (d, i)) for i in range(3)] for d in range(2)]
            for d in range(2):
                for b in range(2):
                    kb.op("pool", lambda d=d, b=b: g.memset(QZ[d][b][0][:], 0.0), writes=[QZ[d][b][1]])

            class Shim:
                pass
            cm = Shim()
            cm.T1, cm.T1R, cm.RS, cm.RSR = TA, TAR, TB, TBR
            G = [[0, 1], [2, 3, 4, 5], [6, 7, 8, 9], [10, 11, 12, 13], [14, 15, 16, 17]]
            GORD = [G, [G[0], G[4], G[3], G[2], G[1]]]
            TRI = [CST[:, C_TRIF:C_TRIF + 128], CST[:, C_TRIB:C_TRIB + 128]]
            AFT = [CST[:, C_AFTF:C_AFTF + 128], CST[:, C_AFTB:C_AFTB + 128]]
            hscale = 128.0 ** -0.5
            hb = 1536

            def ht_res(tiles):
                return list({id(HTR[blk_of_tile(t)]): HTR[blk_of_tile(t)] for t in tiles}.values())

            for hh in range(4):
                ia = ring_pos[0] % len(RING)
                ring_pos[0] += 1
                ib = ring_pos[0] % len(RING)
                ring_pos[0] += 1
                rA, rB = RINGR[ia], RINGR[ib]
                wA = RING[ia][:, 0:4096].rearrange("p (k c) -> p k c", k=8)
                for pi, c0 in enumerate((hb + hh * 128, hb + 512 + hh * 128, hb + 1024 + hh * 128, hb + 1536 + hh * 128)):
                    kb.dma("pool", lambda pi=pi, c0=c0: g.dma_start(
                        out=wA[:, :, pi * 128:(pi + 1) * 128],
                        in_=ewin_d[:, c0:c0 + 128].rearrange("(k p) c -> p k c", p=128)), rA, True)
                wG = RING[ib][:, 0:1024].rearrange("p (k c) -> p k c", k=8)
                wO = RING[ib][:, 1024:2048]
                c0 = hb + 2048 + hh * 128
                kb.dma("pool", lambda: g.dma_start(out=wG, in_=ewin_d[:, c0:c0 + 128].rearrange("(k p) c -> p k c", p=128)), rB, True)
                kb.dma("pool", lambda: g.dma_start(out=wO, in_=ewout_d[512 + hh * 128:512 + (hh + 1) * 128, :]), rB, True)
                for d in range(2):
                    kb.op("dve", lambda d=d: v.tensor_copy(out=TD[:, 0:128], in_=OMLF[:, d * 4 + hh:d * 4 + hh + 1].to_broadcast([128, 128])),
                          reads=[OMLFR], writes=[TDR])
                    kb.op("pe", mm(PS[0][:, 0:128], TD[:, 0:128], IDENT, True, True), reads=[TDR, CSTR], writes=[PSR[0]])
                    kb.op("act", lambda d=d: a.copy(out=OMLBC[:, d, :], in_=PS[0][:, 0:128]), reads=[PSR[0]], writes=[OMLBCR])
                for g0 in range(0, NT, 4):
                    tl = list(range(g0, min(g0 + 4, NT)))
                    fns = []
                    for i, t in enumerate(tl):
                        for k in range(8):
                            fns.append(mm(PS[3][:, i * 128:(i + 1) * 128], HT[:, k, t * 128:(t + 1) * 128], wA[:, k, 384:512], k == 0, k == 7))
                    kb.op("pe", fns, reads=[rA] + ht_res(tl), writes=[PSR[3]])
                    kb.op("act", lambda g0=g0, tl=tl: a.copy(out=VH[:, g0:g0 + len(tl), :],
                                                           in_=PS[3][:, 0:len(tl) * 128].rearrange("p (t c) -> p t c", c=128)),
                          reads=[PSR[3]], writes=[VHR])
                for d in range(2):
                    kb.op("dve", lambda d=d: v.memset(ST[d][0][:], 0.0), writes=[ST[d][1]])
                    kb.op("dve", lambda d=d: v.memset(SBF[d][0][:], 0.0), writes=[SBF[d][1]])

                def prep(d, tiles, b):
                    ng = len(tiles)
                    n = ng * 128
                    tk0 = tiles[0] * 128
                    fc = 128 + d * 128
                    hr = ht_res(tiles)
                    kbar, kbarR = KBAR[d][b]
                    ktil, ktilR = KTIL[d][b]
                    qz, qzR = QZ[d][b]
                    al, alR = ALAST[d][b]
                    fns = []
                    for i, t in enumerate(tiles):
                        for k in range(8):
                            fns.append(mm(PS[0][:, i * 128:(i + 1) * 128], HT[:, k, t * 128:(t + 1) * 128], wA[:, k, fc:fc + 128], k == 0, k == 7))
                    kb.op("pe", fns, reads=[rA] + hr, writes=[PSR[0]])
                    kb.op("pe", [mm(PS[1][:, 0:n], wA[:, k, fc:fc + 128], HT[:, k, tk0:tk0 + n], k == 0, k == 7) for k in range(8)],
                          reads=[rA] + hr, writes=[PSR[1]])
                    kb.op("pe", [mm(PS[2][:, 0:n], wA[:, k, 0:128], HT[:, k, tk0:tk0 + n], k == 0, k == 7) for k in range(8)],
                          reads=[rA] + hr, writes=[PSR[2]])
                    kb.op("act", lambda: a.activation(out=TA[:, 0:n], in_=PS[0][:, 0:n], func=AF.Exp), reads=[PSR[0]], writes=[TAR])
                    kb.op("act", lambda: a.activation(out=TA[:, 0:n], in_=TA[:, 0:n], func=AF.Ln, bias=ONEC[:, :]),
                          reads=[TAR, CSTR], writes=[TAR])
                    kb.op("act", lambda: a.activation(out=TA[:, 0:n], in_=TA[:, 0:n], func=AF.Exp, scale=-1.0), reads=[TAR], writes=[TAR])
                    kb.op("dve", lambda: v.tensor_tensor(
                        out=TB[:, 0:n].rearrange("p (t c) -> p t c", c=128), in0=TA[:, 0:n].rearrange("p (t c) -> p t c", c=128),
                        in1=OMLBC[:, d:d + 1, :].to_broadcast([128, ng, 128]), op=ALU.mult),
                        reads=[TAR, OMLBCR], writes=[TBR])
                    kb.op("act", lambda: a.activation(out=TC[:, 0:n], in_=TB[:, 0:n], func=AF.Ln, scale=-1.0, bias=ONEC[:, :]),
                          reads=[TBR, CSTR], writes=[TCR])
                    kb.op("pe", [mm(PS[3][:, i * 128:(i + 1) * 128], AFT[d], TC[:, i * 128:(i + 1) * 128], True, True) for i in range(ng)],
                          reads=[TCR, CSTR], writes=[PSR[3]])
                    kb.op("pe", [mm(PS[4][:, i * 128:(i + 1) * 128], TC[:, i * 128:(i + 1) * 128], TRI[d], True, True) for i in range(ng)],
                          reads=[TCR, CSTR], writes=[PSR[4]])
                    kb.op("act", lambda: a.activation(out=TA[:, 0:n], in_=PS[3][:, 0:n], func=AF.Exp), reads=[PSR[3]], writes=[TAR])
                    kb.op("dve", lambda: v.tensor_tensor(out=kbar[:, 0:ng, :], in0=TB[:, 0:n].rearrange("p (t c) -> p t c", c=128),
                                                         in1=TA[:, 0:n].rearrange("p (t c) -> p t c", c=128), op=ALU.mult),
                          reads=[TAR, TBR], writes=[kbarR])
                    kb.op("act", lambda: a.activation(out=TD[:, 0:n], in_=PS[1][:, 0:n], func=AF.Exp), reads=[PSR[1]], writes=[TDR])
                    kb.op("act", lambda: a.activation(out=TD[:, 0:n], in_=TD[:, 0:n], func=AF.Ln, bias=ONEC[:, :]),
                          reads=[TDR, CSTR], writes=[TDR])
                    kb.op("act", lambda: a.activation(out=TD[:, 0:n], in_=TD[:, 0:n], func=AF.Exp, scale=-1.0), reads=[TDR], writes=[TDR])
                    kb.op("act", lambda: a.activation(out=EC[:, 0:n], in_=PS[4][:, 0:n], func=AF.Exp), reads=[PSR[4]], writes=[ECR])
                    kb.op("act", lambda: a.activation(out=TA[:, 0:n], in_=PS[4][:, 0:n], func=AF.Exp, scale=-1.0), reads=[PSR[4]], writes=[TAR])
                    kb.op("dve", lambda: v.scalar_tensor_tensor(out=ktil[:, 0:n], in0=TD[:, 0:n], scalar=OMLF[:, d * 4 + hh:d * 4 + hh + 1],
                                                                in1=TA[:, 0:n], op0=ALU.mult, op1=ALU.mult),
                          reads=[TDR, TAR, OMLFR], writes=[ktilR])
                    lastcol = 63 if d == 0 else 0
                    kb.op("dve", lambda: v.tensor_copy(out=al[:, 0:2 * ng],
                                                       in_=EC[:, 0:n].rearrange("p (c j) -> p c j", j=64)[:, :, lastcol]),
                          reads=[ECR], writes=[alR])
                    kb.op("act", lambda: a.activation(out=TB[:, 0:n], in_=PS[2][:, 0:n], func=AF.Exp, scale=-1.0), reads=[PSR[2]], writes=[TBR])
                    kb.op("act", lambda: a.activation(out=TB[:, 0:n], in_=TB[:, 0:n], func=AF.Ln, bias=ONEC[:, :]),
                          reads=[TBR, CSTR], writes=[TBR])
                    kb.op("act", lambda: a.activation(out=TB[:, 0:n], in_=TB[:, 0:n], func=AF.Exp, scale=-1.0), reads=[TBR], writes=[TBR])
                    kb.op("dve", lambda: v.tensor_tensor(out=TB[:, 0:n], in0=PS[2][:, 0:n], in1=TB[:, 0:n], op=ALU.mult),
                          reads=[TBR, PSR[2]], writes=[TBR])
                    for c in range(2):
                        kb.op("dve", lambda c=c: v.scalar_tensor_tensor(
                            out=qz[:, 0:ng, c * 192:c * 192 + 64],
                            in0=TB[:, 0:n].rearrange("p (t c) -> p t c", c=128)[:, :, c * 64:(c + 1) * 64], scalar=hscale,
                            in1=EC[:, 0:n].rearrange("p (t c) -> p t c", c=128)[:, :, c * 64:(c + 1) * 64], op0=ALU.mult, op1=ALU.mult),
                            reads=[TBR, ECR], writes=[qzR])

                def recur(d, t, i, b):
                    bank = 5 + d
                    s_ap = PS[bank][:, 0:128]
                    o_ap = PS[bank][:, 128:256]
                    ds_ap = PS[7][:, 256 + d * 128:384 + d * 128]
                    sR, oR, dsR = PSS[d]
                    kbar, kbarR = KBAR[d][b]
                    ktil, ktilR = KTIL[d][b]
                    qz, qzR = QZ[d][b]
                    al, alR = ALAST[d][b]
                    st, stR = ST[d]
                    sbf, sbfR = SBF[d]
                    scm, scmR = SCM[d]
                    kt_ = ktil[:, i * 128:(i + 1) * 128]
                    kb.op("pe", [mm(s_ap[:, 0:64], kt_, qz[:, i, 0:64], True, True), mm(s_ap[:, 64:128], kt_, qz[:, i, 192:256], True, True)],
                          reads=[ktilR, qzR], writes=[sR])
                    kb.op("dve", lambda: v.tensor_tensor(out=scm[:, :], in0=s_ap, in1=TRI[d], op=ALU.mult), reads=[sR, CSTR], writes=[scmR])
                    cs = [0, 1] if d == 0 else [1, 0]

                    def upd(c):
                        kb.op("pe", mm(ds_ap, kbar[c * 64:(c + 1) * 64, i, :], VH[c * 64:(c + 1) * 64, t, :], True, True),
                              reads=[kbarR, VHR], writes=[dsR])
                        kb.op("dve", lambda: v.scalar_tensor_tensor(out=st[:, :], in0=st[:, :], scalar=al[:, 2 * i + c:2 * i + c + 1],
                                                                    in1=ds_ap, op0=ALU.mult, op1=ALU.add),
                              reads=[stR, alR, dsR], writes=[stR])
                        kb.op("act", lambda: a.copy(out=sbf[:, :], in_=st[:, :]), reads=[stR], writes=[sbfR])
                    kb.op("pe", [mm(o_ap, scm[:, :], VH[:, t, :], True, False),
                                 mm(o_ap, qz[:, i, cs[0] * 128:(cs[0] + 1) * 128], sbf[:, :], False, False)],
                          reads=[scmR, VHR, qzR, sbfR], writes=[oR])
                    upd(cs[0])
                    kb.op("pe", mm(o_ap, qz[:, i, cs[1] * 128:(cs[1] + 1) * 128], sbf[:, :], False, True),
                          reads=[qzR, sbfR], writes=[oR])
                    upd(cs[1])
                    kb.op("act", lambda: a.copy(out=OD[d][0][:, t, :], in_=o_ap), reads=[oR], writes=[OD[d][1]])

                bufi = [0, 0]
                pend = [None, None]
                for d in range(2):
                    pend[d] = (GORD[d][0], bufi[d] % 2)
                    prep(d, GORD[d][0], bufi[d] % 2)
                    bufi[d] += 1
                for gi in range(5):
                    cur = [pend[0], pend[1]]
                    if gi + 1 < 5:
                        for d in range(2):
                            pend[d] = (GORD[d][gi + 1], bufi[d] % 2)
                            prep(d, GORD[d][gi + 1], bufi[d] % 2)
                            bufi[d] += 1
                    ftiles, fb = cur[0]
                    btiles, bb = cur[1]
                    border = list(reversed(btiles))
                    for j in range(max(len(ftiles), len(border))):
                        if j < len(ftiles):
                            recur(0, ftiles[j], j, fb)
                        if j < len(border):
                            recur(1, border[j], btiles.index(border[j]), bb)
                cnt = 0
                for tiles in G:
                    ng = len(tiles)
                    n = ng * 128
                    t0_ = tiles[0]
                    fns = []
                    for i, t in enumerate(tiles):
                        for k in range(8):
                            fns.append(mm(PS[0][:, i * 128:(i + 1) * 128], HT[:, k, t * 128:(t + 1) * 128], wG[:, k, :], k == 0, k == 7))
                    kb.op("pe", fns, reads=[rB] + ht_res(tiles), writes=[PSR[0]])
                    kb.op("dve", lambda: v.tensor_tensor(out=TA[:, 0:n].rearrange("p (t c) -> p t c", c=128),
                                                         in0=OD[0][0][:, t0_:t0_ + ng, :], in1=OD[1][0][:, t0_:t0_ + ng, :], op=ALU.add),
                          reads=[OD[0][1], OD[1][1]], writes=[TAR])
                    for i in range(ng):
                        kb.op("act", lambda i=i: a.activation(out=TD[:, i * 128:(i + 1) * 128], in_=TA[:, i * 128:(i + 1) * 128],
                                                              func=AF.Square, accum_out=SSH[:, i:i + 1]),
                              reads=[TAR], writes=[TDR, SSHR])
                    kb.op("dve", lambda: v.tensor_scalar(out=SSH[:, 0:ng], in0=SSH[:, 0:ng], scalar1=1.0 / 128, scalar2=EPS,
                                                         op0=ALU.mult, op1=ALU.add), reads=[SSHR], writes=[SSHR])
                    kb.op("act", lambda: a.activation(out=SSH[:, 0:ng], in_=SSH[:, 0:ng], func=AF.Ln), reads=[SSHR], writes=[SSHR])
                    kb.op("act", lambda: a.activation(out=SSH[:, 0:ng], in_=SSH[:, 0:ng], func=AF.Exp, scale=-0.5), reads=[SSHR], writes=[SSHR])
                    for i in range(ng):
                        kb.op("dve", lambda i=i: v.scalar_tensor_tensor(
                            out=TA[:, i * 128:(i + 1) * 128], in0=TA[:, i * 128:(i + 1) * 128], scalar=SSH[:, i:i + 1],
                            in1=ppc("hog", 0, 128), op0=ALU.mult, op1=ALU.mult), reads=[TAR, SSHR, PPR], writes=[TAR])
                    kb.op("act", lambda: a.activation(out=TB[:, 0:n], in_=PS[0][:, 0:n], func=AF.Exp, scale=-1.0), reads=[PSR[0]], writes=[TBR])
                    kb.op("act", lambda: a.activation(out=TB[:, 0:n], in_=TB[:, 0:n], func=AF.Ln, bias=ONEC[:, :]),
                          reads=[TBR, CSTR], writes=[TBR])
                    kb.op("act", lambda: a.activation(out=TB[:, 0:n], in_=TB[:, 0:n], func=AF.Exp, scale=-1.0), reads=[TBR], writes=[TBR])
                    kb.op("dve", lambda: v.tensor_tensor(out=TB[:, 0:n], in0=PS[0][:, 0:n], in1=TB[:, 0:n], op=ALU.mult),
                          reads=[TBR, PSR[0]], writes=[TBR])
                    kb.op("dve", lambda: v.tensor_tensor(out=TA[:, 0:n], in0=TA[:, 0:n], in1=TB[:, 0:n], op=ALU.mult),
                          reads=[TAR, TBR], writes=[TAR])
                    kb.op("pe", [mm(PS[1][:, i * 128:(i + 1) * 128], TA[:, i * 128:(i + 1) * 128], IDENT, True, True) for i in range(ng)],
                          reads=[TAR, CSTR], writes=[PSR[1]])
                    kb.op("act", lambda: a.copy(out=CATB[:, 0:n], in_=PS[1][:, 0:n]), reads=[PSR[1]], writes=[CATBR])
                    for i, t in enumerate(tiles):
                        w_ = 1 if t < 2 else 0
                        for nh in range(2):
                            pb = 2 + (cnt % 2)
                            tm_, tmR = (TC, TCR) if cnt % 2 == 0 else (TD, TDR)
                            cnt += 1
                            kb.op("pe", mm(PS[pb][:], CATB[:, i * 128:(i + 1) * 128], wO[:, nh * 512:(nh + 1) * 512], True, True),
                                  reads=[CATBR, rB], writes=[PSR[pb]])
                            kb.op("dve", lambda pb=pb, nh=nh, w_=w_, tm_=tm_: v.tensor_tensor(
                                out=tm_[:], in0=PS[pb][:], in1=GBC[:, w_, nh * 512:(nh + 1) * 512], op=ALU.mult),
                                reads=[PSR[pb], GBCR], writes=[tmR])
                            kb.op("pool", lambda t=t, nh=nh, tm_=tm_: g.tensor_tensor(
                                out=X[:, t, nh * 512:(nh + 1) * 512], in0=X[:, t, nh * 512:(nh + 1) * 512], in1=tm_[:], op=ALU.add),
                                reads=[tmR, XR[t][nh]], writes=[XR[t][nh]])

        def even_mixer():
            even_prep()
            if flags.get("diff", True):
                diff_part()
            if flags.get("hgrn", True):
                hgrn_part()

        for l in range(2):
            with_ctx = (l == 0)
            if (l == 0 and do_mix0) or (l == 1 and do_mix1):
                norm_phase(l, 0, True, False, 2)
                if l == 0:
                    even_mixer()
                else:
                    mla_mixer()
            if do_ffn:
                norm_phase(l, 1, with_ctx, True, 5)
                moe_phase(l, with_ctx)

        outdeps = []
        for t in range(2, NT):
            for h in range(2):
                outdeps.append(kb.dma("sp", lambda t=t, h=h: nc.sync.dma_start(
                    out=y_d[(t - 2) * 128:(t - 1) * 128, h * 512:(h + 1) * 512], in_=X[:, t, h * 512:(h + 1) * 512]),
                    XR[t][h], False))
        if taps:
            for t in range(NT):
                for h in range(2):
                    outdeps.append(kb.dma("sp", lambda t=t, h=h: nc.sync.dma_start(
                        out=tap_d[t * 128:(t + 1) * 128, h * 512:(h + 1) * 512], in_=X[:, t, h * 512:(h + 1) * 512]),
                        XR[t][h], False))
        kb.final_wait("sp", outdeps)
    return nc


WEIGHT_KEYS = ["mod_w", "even_w_in", "even_w_out", "odd_w_in", "mla_w_uq", "mla_w_ukv", "odd_w_out",
               "expert_w_gate", "expert_w_up", "expert_w_down", "shared_w_gate", "shared_w_up", "shared_w_down"]


def make_in_maps(inp, cores):
    cst = _consts()
    rope = _rope_tables()
    shared = {}
    for k in WEIGHT_KEYS:
        arr = np.ascontiguousarray(np.asarray(inp[k], np.float32))
        if k in ("even_w_in", "even_w_out", "odd_w_in", "mla_w_uq", "mla_w_ukv", "odd_w_out"):
            arr = arr[0]
        shared[k] = arr
    maps = []
    for b in cores:
        m = dict(shared)
        m["x"] = np.ascontiguousarray(np.asarray(inp["x"][b], np.float32))
        m["ctx"] = np.ascontiguousarray(np.asarray(inp["ctx"][b], np.float32))
        m["pp"] = _pack_params(b, inp)
        m["cst"] = cst
        m["rope"] = rope
        maps.append(m)
    return maps


def kernel(**inputs):
    nc = build_program()
    maps = make_in_maps(inputs, list(range(8)))
    res = run_bass_kernel_spmd(nc, maps, core_ids=list(range(8)))
    return np.stack([np.asarray(r["y"], np.float32) for r in res.results], axis=0)
```

```python
import math
import numpy as np
from contextlib import ExitStack
import concourse.bass as bass
import concourse.mybir as mybir
from concourse.bass_utils import run_bass_kernel_spmd

F32 = mybir.dt.float32
BF16 = mybir.dt.bfloat16
AF = mybir.ActivationFunctionType
ALU = mybir.AluOpType
AX = mybir.AxisListType

D = 1024
SEQ = 2048
CTX = 256
NTOK = SEQ + CTX
NT = NTOK // 128
BLOCKS = [(0, 256)] + [(256 + 512 * i, 512) for i in range(4)]
EPS = 1e-6
NSLOT = 3
SLOT_ELEMS = 4096

C_ID = 0
C_ONES = 128
C_BLK64 = 256
C_PERM = 384
C_TRIF = 512
C_TRIB = 640
C_AFTF = 768
C_AFTB = 896
C_N = 1024


def _consts():
    c = np.zeros((128, C_N), np.float32)
    i = np.arange(128)
    s = i[:, None]
    t = i[None, :]
    same = (s // 64) == (t // 64)
    c[:, C_ID:C_ID + 128] = (s == t)
    c[:, C_ONES:C_ONES + 128] = 1.0
    c[:, C_BLK64:C_BLK64 + 128] = same
    partner = np.where((i % 32) < 16, i + 16, i - 16)
    c[:, C_PERM:C_PERM + 128] = (s == partner[None, :])
    c[:, C_TRIF:C_TRIF + 128] = same & (s <= t)
    c[:, C_TRIB:C_TRIB + 128] = same & (s >= t)
    c[:, C_AFTF:C_AFTF + 128] = same & (s > t)
    c[:, C_AFTB:C_AFTB + 128] = same & (s < t)
    return c


def _rope_tables():
    tok = np.arange(SEQ)
    row = (tok // 64).astype(np.float32)
    col = (tok % 64).astype(np.float32)
    inv = (10000.0 ** (-np.arange(0, 32, 2, dtype=np.float32) / 32.0)).astype(np.float32)
    C = np.zeros((128, SEQ), np.float32)
    S = np.zeros((128, SEQ), np.float32)
    for p in range(128):
        d = p % 64
        pos = row if d < 32 else col
        f = inv[d % 16]
        ang = (pos * f).astype(np.float32)
        C[p] = np.cos(ang)
        S[p] = np.sin(ang) * (-1.0 if (d % 32) < 16 else 1.0)
    return np.stack([C, S], axis=1)


class PP:
    pass


def _pp_layout():
    off = {}
    n = 0

    def add(name, w):
        nonlocal n
        off[name] = (n, w)
        n += w
    add("c", 8)
    add("cctx", 8)
    add("modb", 2 * 48)
    add("nmix", 16)
    add("nffn", 16)
    add("rw", 8 * 16)
    add("rbias", 16)
    add("dqg", 1)
    add("dkg", 1)
    add("dlam", 256)
    add("subln", 1)
    add("lbfm", 2 * 2 * 4)
    add("hog", 128)
    add("qag", 3)
    add("kvag", 2)
    add("qng", 1)
    add("qrg", 1)
    add("kng", 1)
    add("krg", 1)
    return off, n


PPO, PPN = _pp_layout()


def _fm(v):
    v = np.asarray(v, np.float32)
    return np.ascontiguousarray(v.reshape(-1, 128).T)


def _pack_params(b, inp):
    pp = np.zeros((128, PPN), np.float32)

    def put(name, arr):
        o, w = PPO[name]
        arr = np.asarray(arr, np.float32).reshape(128, -1)
        assert arr.shape[1] == w, (name, arr.shape, w)
        pp[:, o:o + w] = arr
    put("c", _fm(inp["c"][b]))
    put("cctx", _fm(inp["c_ctx"]))
    put("modb", np.concatenate([_fm(inp["mod_b"][0]), _fm(inp["mod_b"][1])], axis=1))
    put("nmix", np.concatenate([_fm(inp["norm_mix"][0]), _fm(inp["norm_mix"][1])], axis=1))
    put("nffn", np.concatenate([_fm(inp["norm_ffn"][0]), _fm(inp["norm_ffn"][1])], axis=1))
    rw = np.asarray(inp["router_w"], np.float32).reshape(8, 128, 16).transpose(1, 0, 2)
    put("rw", rw.reshape(128, 128))
    put("rbias", np.broadcast_to(np.asarray(inp["router_bias"], np.float32)[None, :], (128, 16)))
    put("dqg", np.tile(np.asarray(inp["diff_q_gain"][0], np.float32), 2)[:, None])
    put("dkg", np.tile(np.asarray(inp["diff_k_gain"][0], np.float32), 2)[:, None])
    put("dlam", np.broadcast_to(np.asarray(inp["diff_lambda"][0], np.float32).reshape(1, 256), (128, 256)))
    put("subln", np.asarray(inp["diff_subln"][0], np.float32)[:, None])
    lb = np.asarray(inp["hgrn_lb_logits"], np.float32)
    put("lbfm", lb.reshape(2, 2, 4, 128).transpose(3, 0, 1, 2).reshape(128, 16))
    put("hog", np.broadcast_to(np.asarray(inp["hgrn_out_gain"][0], np.float32)[None, :], (128, 128)))
    put("qag", _fm(inp["mla_q_a_gain"][0]))
    put("kvag", _fm(inp["mla_kv_a_gain"][0]))
    put("qng", np.asarray(inp["mla_q_nope_gain"][0], np.float32)[:, None])
    put("qrg", np.tile(np.asarray(inp["mla_q_rope_gain"][0], np.float32), 2)[:, None])
    put("kng", np.asarray(inp["mla_k_nope_gain"][0], np.float32)[:, None])
    put("krg", np.tile(np.asarray(inp["mla_k_rope_gain"][0], np.float32), 2)[:, None])
    return pp


class Res:
    __slots__ = ("name", "w", "rs", "dsem", "dcnt")

    def __init__(self, name):
        self.name = name
        self.w = None
        self.rs = {}
        self.dsem = None
        self.dcnt = 0


class Eng:
    def __init__(self, name, obj, sem):
        self.name = name
        self.obj = obj
        self.sem = sem
        self.cnt = 0
        self.seen = {}


class KB:
    def __init__(self, nc, es):
        self.nc = nc
        self.es = es
        self.sems = {}
        self.E = {}
        for name, obj in (("pe", nc.tensor), ("act", nc.scalar), ("dve", nc.vector),
                          ("pool", nc.gpsimd), ("sp", nc.sync)):
            sem = es.enter_context(nc.semaphore("s_" + name))
            self.sems[name] = sem
            self.E[name] = Eng(name, obj, sem)
        self.nres = 0

    def res(self, name=None):
        self.nres += 1
        return Res(name or ("r%d" % self.nres))

    def _wait(self, E, reads, writes):
        deps = {}

        def add(d):
            if d is None:
                return
            k, v = d
            if deps.get(k, 0) < v:
                deps[k] = v
        for r in reads:
            add(r.w)
        for w in writes:
            add(w.w)
            for k, v in w.rs.items():
                add((k, v))
        for k, v in deps.items():
            if k == E.name and E.name == "pe":
                continue
            if E.seen.get(k, 0) < v:
                E.obj.wait_ge(self.sems[k], v)
                E.seen[k] = v

    def op(self, eng, fn, reads=(), writes=()):
        E = self.E[eng]
        self._wait(E, reads, writes)
        ins = None
        if callable(fn):
            ins = fn()
        else:
            for f in fn:
                ins = f()
        E.cnt += 1
        ins.then_inc(E.sem, 1)
        dep = (E.name, E.cnt)
        for r in reads:
            if r.rs.get(E.name, 0) < E.cnt:
                r.rs[E.name] = E.cnt
        for w in writes:
            w.w = dep
            w.rs = {}
        return ins

    def dma(self, queue, fn, res, is_write, reads=(), writes=()):
        E = self.E[queue]
        if res.dsem is None:
            self.nres += 1
            key = "d%d_%s" % (self.nres, res.name)
            res.dsem = key
            self.sems[key] = self.es.enter_context(self.nc.semaphore(key))
        rr = list(reads) + ([] if is_write else [res])
        ww = list(writes) + ([res] if is_write else [])
        self._wait(E, rr, ww)
        ins = fn()
        res.dcnt += 16
        ins.then_inc(self.sems[res.dsem], 16)
        dep = (res.dsem, res.dcnt)
        for r in rr:
            if r.rs.get(res.dsem, 0) < res.dcnt:
                r.rs[res.dsem] = res.dcnt
        for w in ww:
            w.w = dep
            w.rs = {}
        return dep

    def barrier(self):
        for E in self.E.values():
            for F in self.E.values():
                if F is E or F.cnt == 0:
                    continue
                if E.seen.get(F.name, 0) < F.cnt:
                    E.obj.wait_ge(self.sems[F.name], F.cnt)
                    E.seen[F.name] = F.cnt
            if E.name != "pe" and E.cnt > 0 and E.seen.get(E.name, 0) < E.cnt:
                E.obj.wait_ge(self.sems[E.name], E.cnt)
                E.seen[E.name] = E.cnt

    def final_wait(self, eng, deps):
        E = self.E[eng]
        for k, v in deps:
            E.obj.wait_ge(self.sems[k], v)


def build_program(flags=None):
    flags = flags or {}
    do_mix0 = flags.get("mix0", True)
    do_mix1 = flags.get("mix1", True)
    do_ffn = flags.get("ffn", True)
    taps = flags.get("taps", False)

    nc = bass.Bass("TRN2", target_bir_lowering=False)

    def din(name, shape):
        return nc.dram_tensor(name, list(shape), F32, kind="ExternalInput").ap()
    x_d = din("x", [SEQ, D])
    ctx_d = din("ctx", [CTX, D])
    pp_d = din("pp", [128, PPN])
    cst_d = din("cst", [128, C_N])
    rope_d = din("rope", [128, 2, SEQ])
    mod_w_d = din("mod_w", [2, D, 6 * D])
    ewin_d = din("even_w_in", [D, 4096])
    ewout_d = din("even_w_out", [D, D])
    owin_d = din("odd_w_in", [D, 704])
    wuq_d = din("mla_w_uq", [384, 1536])
    wukv_d = din("mla_w_ukv", [256, 2048])
    owout_d = din("odd_w_out", [D, D])
    xg_d = din("expert_w_gate", [2, 16, D, 512])
    xu_d = din("expert_w_up", [2, 16, D, 512])
    xd_d = din("expert_w_down", [2, 16, 512, D])
    sg_d = din("shared_w_gate", [2, D, 512])
    su_d = din("shared_w_up", [2, D, 512])
    sd_d = din("shared_w_down", [2, 512, D])
    y_d = nc.dram_tensor("y", [SEQ, D], F32, kind="ExternalOutput").ap()
    tap_d = None
    if taps:
        tap_d = nc.dram_tensor("tap", [NTOK, D], F32, kind="ExternalOutput").ap()

    es = ExitStack()
    with es:
        kb = KB(nc, es)

        def sb(name, shape, dt=F32):
            return es.enter_context(nc.sbuf_tensor(name, list(shape), dt))

        X = sb("X", [128, NT, D])
        XR = [[kb.res("x%d_%d" % (t, h)) for h in range(2)] for t in range(NT)]
        HT = sb("HT", [128, 8, NTOK], BF16)
        HTR = [kb.res("ht%d" % i) for i in range(len(BLOCKS))]
        PPT = sb("PPT", [128, PPN])
        PPR = kb.res("pp")
        CST = sb("CST", [128, C_N])
        CSTB = sb("CSTB", [128, C_N], BF16)
        CSTR = kb.res("cst")
        RING = [sb("ring%d" % i, [128, SLOT_ELEMS], BF16) for i in range(NSLOT)]
        RINGR = [kb.res("ring%d" % i) for i in range(NSLOT)]
        ring_pos = [0]
        PS = [es.enter_context(nc.psum_tensor("ps%d" % i, [128, 512], F32)) for i in range(8)]
        PSR = [kb.res("ps%d" % i) for i in range(8)]
        MODV = sb("MODV", [128, 2, 48, 2])
        MODR = kb.res("modv")
        NA = sb("NA", [128, 2, 8])
        NB = sb("NB", [128, 2, 8])
        NAR = kb.res("na")
        SS = sb("SS", [128, NT])
        SSR = kb.res("ss")
        RSTD = sb("RSTD", [128, NT])
        RSTDR = kb.res("rstd")
        GBC = sb("GBC", [128, 2, D], BF16)
        GBCR = kb.res("gbc")
        GATES = sb("GATES", [128, NT, 16])
        GATESR = kb.res("gates")
        SCB = sb("SCB", [128, 8, 2], BF16)
        SCR = kb.res("scb")

        scope_id = [0]

        class Scope:
            def __enter__(self):
                kb.barrier()
                scope_id[0] += 1
                self.sid = scope_id[0]
                self.stack = ExitStack()
                self.stack.__enter__()
                return self

            def sb(self, name, shape, dt=F32):
                return self.stack.enter_context(nc.sbuf_tensor("%s_s%d" % (name, self.sid), list(shape), dt))

            def __exit__(self, *exc):
                kb.barrier()
                return self.stack.__exit__(*exc)
        SM = [sb("SM%d" % i, [128, 64]) for i in range(6)]
        SMR = [kb.res("sm%d" % i) for i in range(6)]

        v = nc.vector
        a = nc.scalar
        g = nc.gpsimd
        pe = nc.tensor

        def ppc(name, i=0, w=1):
            o, _ = PPO[name]
            return PPT[:, o + i:o + i + w]

        kb.dma("sp", lambda: nc.sync.dma_start(out=PPT[:], in_=pp_d), PPR, True)
        kb.dma("sp", lambda: nc.sync.dma_start(out=CST[:], in_=cst_d), CSTR, True)
        kb.op("dve", lambda: v.tensor_copy(out=CSTB[:], in_=CST[:]), reads=[CSTR], writes=[CSTR])
        for t in range(NT):
            src = ctx_d[t * 128:(t + 1) * 128, :] if t < 2 else x_d[(t - 2) * 128:(t - 1) * 128, :]
            for h in range(2):
                kb.dma("sp", lambda src=src, t=t, h=h: nc.sync.dma_start(
                    out=X[:, t, h * 512:(h + 1) * 512], in_=src[:, h * 512:(h + 1) * 512]), XR[t][h], True)

        IDENT = CST[:, C_ID:C_ID + 128]

        def wload(src2d, kc, cols):
            assert kc * cols <= SLOT_ELEMS
            i = ring_pos[0] % len(RING)
            ring_pos[0] += 1
            view = RING[i][:, 0:kc * cols].rearrange("p (k c) -> p k c", k=kc)
            kb.dma("pool", lambda: g.dma_start(out=view, in_=src2d.rearrange("(k p) c -> p k c", p=128)),
                   RINGR[i], True)
            return RINGR[i], view

        def silu_small(out_ap, in_ap, sm_i, width, reads, writes):
            t1 = SM[sm_i][:, 0:width]
            kb.op("act", lambda: a.activation(out=t1, in_=in_ap, func=AF.Exp, scale=-1.0),
                  reads=reads, writes=[SMR[sm_i]])
            kb.op("dve", lambda: v.tensor_scalar(out=t1, in0=t1, scalar1=1.0, scalar2=None, op0=ALU.add),
                  reads=[SMR[sm_i]], writes=[SMR[sm_i]])
            kb.op("dve", lambda: v.reciprocal(out=t1, in_=t1), reads=[SMR[sm_i]], writes=[SMR[sm_i]])
            kb.op("dve", lambda: v.tensor_tensor(out=out_ap, in0=in_ap, in1=t1, op=ALU.mult),
                  reads=list(reads) + [SMR[sm_i]], writes=writes)

        silu_small(SCB[:, :, 0], ppc("c", 0, 8), 0, 8, [PPR], [SCR])
        silu_small(SCB[:, :, 1], ppc("cctx", 0, 8), 1, 8, [PPR, SCR], [SCR])

        for l in range(2):
            for s in range(12):
                r, wv = wload(mod_w_d[l, :, s * 512:(s + 1) * 512], 8, 512)
                pb = 0
                fns = []
                for j in range(4):
                    for k in range(8):
                        fns.append(lambda j=j, k=k, wv=wv, s=s: pe.matmul(
                            PS[pb][:, (s * 4 + j) * 2:(s * 4 + j) * 2 + 2], lhsT=wv[:, k, j * 128:(j + 1) * 128],
                            rhs=SCB[:, k, :], start=(k == 0), stop=(k == 7)))
                kb.op("pe", fns, reads=[r, SCR], writes=[PSR[pb]])
            o, _ = PPO["modb"]
            for w_ in range(2):
                kb.op("dve", lambda l=l, w_=w_: v.tensor_tensor(
                    out=MODV[:, l, :, w_], in0=PS[0][:, 0:96].rearrange("p (c w) -> p c w", w=2)[:, :, w_],
                    in1=PPT[:, o + l * 48:o + (l + 1) * 48], op=ALU.add),
                    reads=[PSR[0], PPR], writes=[MODR])

        def bc_from_fm(dst_ap_fn, vec_col_fn, dst_res, extra_reads, HF32, HF32R):
            for half in range(2):
                pb = 1 + half
                fns = []
                for kk in range(4):
                    k = half * 4 + kk
                    kb.op("dve", lambda k=k, kk=kk, half=half: v.tensor_copy(
                        out=HF32[half][:, kk, :], in_=vec_col_fn(k).to_broadcast([128, 128])),
                        reads=extra_reads, writes=[HF32R[half]])
                for kk in range(4):
                    fns.append(lambda kk=kk, half=half, pb=pb: pe.matmul(
                        PS[pb][:, kk * 128:(kk + 1) * 128], lhsT=HF32[half][:, kk, :], rhs=IDENT,
                        start=True, stop=True))
                kb.op("pe", fns, reads=[HF32R[half], CSTR], writes=[PSR[pb]])
                kb.op("act", lambda half=half, pb=pb: a.copy(out=dst_ap_fn(half), in_=PS[pb][:]),
                      reads=[PSR[pb]], writes=[dst_res])

        def norm_phase(l, which, with_ctx, router, gate_vec):
            with Scope() as sc_:
                XN = [sc_.sb("XN", [128, D])] * 2
                XNR = [kb.res("xn")] * 2
                HF32 = [sc_.sb("HF32_%d" % i, [128, 8, 128]) for i in range(2)]
                HF32R = [kb.res("hf32_%d" % i) for i in range(2)]
                JUNK = sc_.sb("JUNK", [128, D], BF16)
                JUNKR = kb.res("junk")
                _norm_phase(l, which, with_ctx, router, XN, XNR, HF32, HF32R, JUNK, JUNKR)
                for w_ in range(2):
                    bc_from_fm(lambda half, w_=w_: GBC[:, w_, half * 512:(half + 1) * 512],
                               lambda k, w_=w_: MODV[:, l, gate_vec * 8 + k, w_:w_ + 1], GBCR, [MODR], HF32, HF32R)

        def _norm_phase(l, which, with_ctx, router, XN, XNR, HF32, HF32R, JUNK, JUNKR):
            nw = "nmix" if which == 0 else "nffn"
            vsh = 0 if which == 0 else 3
            vsc = vsh + 1
            for w_ in range(2):
                kb.op("dve", lambda w_=w_: v.scalar_tensor_tensor(
                    out=NA[:, w_, :], in0=MODV[:, l, vsc * 8:(vsc + 1) * 8, w_], scalar=1.0,
                    in1=ppc(nw, l * 8, 8), op0=ALU.add, op1=ALU.mult),
                    reads=[MODR, PPR], writes=[NAR])
                kb.op("dve", lambda w_=w_: v.tensor_copy(out=NB[:, w_, :], in_=MODV[:, l, vsh * 8:(vsh + 1) * 8, w_]),
                      reads=[MODR], writes=[NAR])
            tiles = list(range(NT)) if with_ctx else list(range(2, NT))
            for t in tiles:
                kb.op("act", lambda t=t: a.activation(out=JUNK[:], in_=X[:, t, :], func=AF.Square,
                                                      accum_out=SS[:, t:t + 1]),
                      reads=[XR[t][0], XR[t][1]], writes=[JUNKR, SSR])
            t0 = tiles[0]
            kb.op("dve", lambda: v.tensor_scalar(out=RSTD[:, t0:NT], in0=SS[:, t0:NT], scalar1=1.0 / D, scalar2=EPS,
                                                 op0=ALU.mult, op1=ALU.add), reads=[SSR], writes=[RSTDR])
            kb.op("act", lambda: a.activation(out=RSTD[:, t0:NT], in_=RSTD[:, t0:NT], func=AF.Ln),
                  reads=[RSTDR], writes=[RSTDR])
            kb.op("act", lambda: a.activation(out=RSTD[:, t0:NT], in_=RSTD[:, t0:NT], func=AF.Exp, scale=-0.5),
                  reads=[RSTDR], writes=[RSTDR])
            for idx, t in enumerate(tiles):
                p = idx % 2
                w_ = 1 if t < 2 else 0
                blk = 0 if t < 2 else 1 + (t - 2) // 4
                kb.op("dve", lambda t=t, p=p: v.tensor_scalar(out=XN[p][:], in0=X[:, t, :], scalar1=RSTD[:, t:t + 1],
                                                              scalar2=None, op0=ALU.mult),
                      reads=[XR[t][0], XR[t][1], RSTDR], writes=[XNR[p]])
                for half in range(2):
                    pb = 1 + half
                    fns = [lambda kk=kk, half=half, pb=pb, p=p: pe.matmul(
                        PS[pb][:, kk * 128:(kk + 1) * 128], lhsT=XN[p][:, (half * 4 + kk) * 128:(half * 4 + kk + 1) * 128],
                        rhs=IDENT, start=True, stop=True) for kk in range(4)]
                    kb.op("pe", fns, reads=[XNR[p], CSTR], writes=[PSR[pb]])
                    for kk in range(4):
                        k = half * 4 + kk
                        kb.op("act", lambda kk=kk, k=k, pb=pb, p=p, w_=w_: a.activation(
                            out=HF32[p][:, k, :], in_=PS[pb][:, kk * 128:(kk + 1) * 128], func=AF.Identity,
                            scale=NA[:, w_, k:k + 1], bias=NB[:, w_, k:k + 1]),
                            reads=[PSR[pb], NAR], writes=[HF32R[p]])
                kb.op("pool", lambda t=t, p=p: g.tensor_copy(out=HT[:, :, t * 128:(t + 1) * 128], in_=HF32[p][:]),
                      reads=[HF32R[p]], writes=[HTR[blk]])
                if router:
                    o, _ = PPO["rw"]
                    fns = [lambda k=k, p=p: pe.matmul(PS[3][:, 0:16], lhsT=HF32[p][:, k, :],
                                                      rhs=PPT[:, o + k * 16:o + (k + 1) * 16],
                                                      start=(k == 0), stop=(k == 7)) for k in range(8)]
                    kb.op("pe", fns, reads=[HF32R[p], PPR], writes=[PSR[3]])
                    route(t)

        def route(t):
            S = SM[2]
            R = SMR[2]
            sc = S[:, 0:16]
            bi = S[:, 16:32]
            t2 = S[:, 32:48]
            m1 = SM[3][:, 0:4]
            m2 = SM[3][:, 4:8]
            gs = SM[3][:, 8:12]
            gm = SM[3][:, 12:13]
            ing = SM[3][:, 16:20]
            den = SM[3][:, 20:21]
            R3 = SMR[3]
            kb.op("act", lambda: a.activation(out=sc, in_=PS[3][:, 0:16], func=AF.Exp, scale=-1.0),
                  reads=[PSR[3]], writes=[R])
            kb.op("dve", lambda: v.tensor_scalar(out=sc, in0=sc, scalar1=1.0, scalar2=None, op0=ALU.add),
                  reads=[R], writes=[R])
            kb.op("dve", lambda: v.reciprocal(out=sc, in_=sc), reads=[R], writes=[R])
            kb.op("dve", lambda: v.tensor_tensor(out=bi, in0=sc, in1=ppc("rbias", 0, 16), op=ALU.add),
                  reads=[R, PPR], writes=[R])
            b3 = bi.rearrange("p (g e) -> p g e", e=4)
            t3 = t2.rearrange("p (g e) -> p g e", e=4)
            kb.op("dve", lambda: v.tensor_reduce(out=m1, in_=b3, axis=AX.X, op=ALU.max), reads=[R], writes=[R3])
            kb.op("dve", lambda: v.tensor_tensor(out=t3, in0=b3, in1=m1.unsqueeze(2).to_broadcast([128, 4, 4]),
                                                 op=ALU.is_equal), reads=[R, R3], writes=[R])
            kb.op("dve", lambda: v.scalar_tensor_tensor(out=t2, in0=t2, scalar=-1e9, in1=bi, op0=ALU.mult, op1=ALU.add),
                  reads=[R], writes=[R])
            kb.op("dve", lambda: v.tensor_reduce(out=m2, in_=t3, axis=AX.X, op=ALU.max), reads=[R], writes=[R3])
            kb.op("dve", lambda: v.tensor_tensor(out=gs, in0=m1, in1=m2, op=ALU.add), reads=[R3], writes=[R3])
            kb.op("dve", lambda: v.tensor_reduce(out=gm, in_=gs, axis=AX.X, op=ALU.max), reads=[R3], writes=[R3])
            kb.op("dve", lambda: v.tensor_scalar(out=ing, in0=gs, scalar1=gm, scalar2=None, op0=ALU.is_ge),
                  reads=[R3], writes=[R3])
            kb.op("dve", lambda: v.tensor_tensor(out=t3, in0=b3, in1=m2.unsqueeze(2).to_broadcast([128, 4, 4]),
                                                 op=ALU.is_ge), reads=[R, R3], writes=[R])
            kb.op("dve", lambda: v.tensor_tensor(out=t3, in0=t3, in1=ing.unsqueeze(2).to_broadcast([128, 4, 4]),
                                                 op=ALU.mult), reads=[R, R3], writes=[R])
            kb.op("dve", lambda: v.tensor_tensor(out=t2, in0=t2, in1=sc, op=ALU.mult), reads=[R], writes=[R])
            kb.op("dve", lambda: v.tensor_reduce(out=den, in_=t2, axis=AX.X, op=ALU.add), reads=[R], writes=[R3])
            kb.op("dve", lambda: v.reciprocal(out=den, in_=den), reads=[R3], writes=[R3])
            kb.op("dve", lambda: v.tensor_scalar(out=GATES[:, t, :], in0=t2, scalar1=den, scalar2=None, op0=ALU.mult),
                  reads=[R, R3], writes=[GATESR])

        def moe_phase(l, with_ctx):
            with Scope() as sc_:
                nextra = 5
                for i in range(nextra):
                    RING.append(sc_.sb("xring%d" % i, [128, SLOT_ELEMS], BF16))
                    RINGR.append(kb.res("xring%d_%d" % (l, i)))
                ring_pos[0] = 0
                _moe_phase(l, with_ctx, sc_.sb)
                kb.barrier()
                del RING[NSLOT:]
                del RINGR[NSLOT:]
                ring_pos[0] = 0

        def _moe_phase(l, with_ctx, psb):
            ACTT = [psb("ACTT%d" % i, [128, 4, 512], BF16) for i in range(2)]
            ACTTR = [kb.res("actt%d" % i) for i in range(2)]
            SIL = [psb("SIL%d" % i, [128, 512], BF16) for i in range(2)]
            SILR = [kb.res("sil%d" % i) for i in range(2)]
            TMP = [psb("TMP%d" % i, [128, 512]) for i in range(4)]
            TMPR = [kb.res("tmp%d" % i) for i in range(4)]
            blocks = BLOCKS if with_ctx else BLOCKS[1:]

            def load_expert(e):
                if e < 16:
                    srcs = (xg_d[l, e], xu_d[l, e], xd_d[l, e])
                else:
                    srcs = (sg_d[l], su_d[l], sd_d[l])
                return wload(srcs[0], 8, 512) + wload(srcs[1], 8, 512) + wload(srcs[2], 4, 1024)

            W = {0: load_expert(0)}
            items = [(e, bi_, t0, n) for e in range(17) for bi_, (t0, n) in enumerate(blocks)]

            def GU(idx):
                e, bi_, t0, n = items[idx]
                rg, vg, ru, vu, rd, vd = W[e]
                blk = BLOCKS.index((t0, n))
                ab = idx % 2
                for j in range(4):
                    pg = (j % 2)
                    pu = 2 + (j % 2)
                    kb.op("pe", [mm(PS[pg][:, 0:n], vg[:, k, j * 128:(j + 1) * 128], HT[:, k, t0:t0 + n], k == 0, k == 7)
                                 for k in range(8)], reads=[rg, HTR[blk]], writes=[PSR[pg]])
                    kb.op("pe", [mm(PS[pu][:, 0:n], vu[:, k, j * 128:(j + 1) * 128], HT[:, k, t0:t0 + n], k == 0, k == 7)
                                 for k in range(8)], reads=[ru, HTR[blk]], writes=[PSR[pu]])
                    sl = j % 2
                    kb.op("act", lambda sl=sl, pg=pg: a.activation(out=SIL[sl][:, 0:n], in_=PS[pg][:, 0:n], func=AF.Silu),
                          reads=[PSR[pg]], writes=[SILR[sl]])
                    kb.op("dve", lambda sl=sl, pu=pu, j=j, ab=ab: v.tensor_tensor(
                        out=ACTT[ab][:, j, 0:n], in0=SIL[sl][:, 0:n], in1=PS[pu][:, 0:n], op=ALU.mult),
                        reads=[SILR[sl], PSR[pu]], writes=[ACTTR[ab]])

            def DN(idx):
                e, bi_, t0, n = items[idx]
                rg, vg, ru, vu, rd, vd = W[e]
                ab = idx % 2
                for tt in range(n // 128):
                    t = (t0 + tt * 128) // 128
                    w_ = 1 if t < 2 else 0
                    for nh in range(2):
                        pd = 4 + ((tt * 2 + nh) % 4)
                        kb.op("pe", [mm(PS[pd][:], ACTT[ab][:, j, tt * 128:(tt + 1) * 128], vd[:, j, nh * 512:(nh + 1) * 512],
                                        j == 0, j == 3) for j in range(4)], reads=[rd, ACTTR[ab]], writes=[PSR[pd]])
                        ti = (tt * 2 + nh) % 4
                        if e < 16:
                            kb.op("dve", lambda pd=pd, t=t, e=e, ti=ti, nh=nh, w_=w_: v.scalar_tensor_tensor(
                                out=TMP[ti][:], in0=PS[pd][:], scalar=GATES[:, t, e:e + 1],
                                in1=GBC[:, w_, nh * 512:(nh + 1) * 512], op0=ALU.mult, op1=ALU.mult),
                                reads=[PSR[pd], GATESR, GBCR], writes=[TMPR[ti]])
                        else:
                            kb.op("dve", lambda pd=pd, ti=ti, nh=nh, w_=w_: v.tensor_tensor(
                                out=TMP[ti][:], in0=PS[pd][:], in1=GBC[:, w_, nh * 512:(nh + 1) * 512], op=ALU.mult),
                                reads=[PSR[pd], GBCR], writes=[TMPR[ti]])
                        kb.op("pool", lambda t=t, nh=nh, ti=ti: g.tensor_tensor(
                            out=X[:, t, nh * 512:(nh + 1) * 512], in0=X[:, t, nh * 512:(nh + 1) * 512],
                            in1=TMP[ti][:], op=ALU.add),
                            reads=[TMPR[ti], XR[t][nh]], writes=[XR[t][nh]])

            GU(0)
            for idx in range(len(items)):
                e, bi_ = items[idx][0], items[idx][1]
                if bi_ == 0 and e + 1 < 17:
                    W[e + 1] = load_expert(e + 1)
                if idx + 1 < len(items):
                    GU(idx + 1)
                DN(idx)

        ONESB = CSTB[:, C_ONES:C_ONES + 128]
        PERMB = CSTB[:, C_PERM:C_PERM + 128]
        EPSC = sb("EPSC", [128, 1])
        kb.op("dve", lambda: v.memset(EPSC[:], EPS), writes=[CSTR])

        def mm(out, lhsT, rhs, start, stop):
            return lambda: pe.matmul(out, lhsT=lhsT, rhs=rhs, start=start, stop=stop)

        class Common:
            def __init__(self, sc_):
                self.SQ = [sc_.sb("SQ0", [128, 512], BF16)] * 2
                self.SQR = [kb.res("sq0")] * 2
                self.RS = sc_.sb("RS", [128, 512])
                self.RSR = kb.res("rs")
                self.T1 = sc_.sb("T1", [128, 512])
                self.T1R = kb.res("t1")
                self.T2 = self.RS
                self.T2R = self.RSR
                self.T3 = sc_.sb("T3", [128, 512])
                self.T3R = kb.res("t3")
                self.RT = [sc_.sb("RT0", [128, 2, 512], BF16)] * 2
                self.RTR = [kb.res("rt0")] * 2
                self.rti = 0
                self.E = [sc_.sb("E%d" % i, [128, 512], BF16) for i in range(2)]
                self.ER = [kb.res("e%d" % i) for i in range(2)]
                self.sqi = 0

        def warmup(n=24):
            kb.op("pe", [mm(PS[7][:, 0:512], CSTB[:, 0:128], CSTB[:, 0:512], True, True) for _ in range(n)],
                  reads=[CSTR], writes=[PSR[7]])

        class BG:
            def __init__(self, gen):
                self.gen = gen
                self.safe = True
                self.done = gen is None

            def step(self):
                if self.done:
                    return False
                try:
                    self.safe = bool(next(self.gen))
                    return True
                except StopIteration:
                    self.done = True
                    self.safe = True
                    return False

            def to_safe(self):
                while not self.done and not self.safe:
                    self.step()

            def finish(self):
                while self.step():
                    pass

        def exhaust(gen):
            if gen is not None:
                for _ in gen:
                    pass

        def stepn(gen, n):
            if gen is None:
                return
            for _ in range(n):
                try:
                    next(gen)
                except StopIteration:
                    return

        def fm_rmsnorm_g(cm, banks, P, nfeat, gains, outs, n, out_res, ssb=7, ones_ap=None):
            srcs = [(PS[b][0:P, 0:n], PSR[b]) if isinstance(b, int) else b for b in banks]
            if ones_ap is None:
                ones_ap = ONESB[0:P, 0:P]
            nb = len(srcs)
            for j, (ap_, r_) in enumerate(srcs):
                q = cm.sqi % 2
                cm.sqi += 1
                kb.op("act", lambda ap_=ap_, q=q: a.activation(out=cm.SQ[q][0:P, 0:n], in_=ap_, func=AF.Square),
                      reads=[r_], writes=[cm.SQR[q]])
                yield
                kb.op("pe", mm(PS[ssb][0:P, 0:n], ones_ap, cm.SQ[q][0:P, 0:n], j == 0, j == nb - 1),
                      reads=[cm.SQR[q], CSTR], writes=[PSR[ssb]])
                yield
            kb.op("act", lambda: a.activation(out=cm.RS[0:P, 0:n], in_=PS[ssb][0:P, 0:n], func=AF.Ln,
                                              scale=1.0 / nfeat, bias=EPSC[0:P, :]),
                  reads=[PSR[ssb], CSTR], writes=[cm.RSR])
            yield
            kb.op("act", lambda: a.activation(out=cm.RS[0:P, 0:n], in_=cm.RS[0:P, 0:n], func=AF.Exp, scale=-0.5),
                  reads=[cm.RSR], writes=[cm.RSR])
            yield
            for j, (ap_, r_) in enumerate(srcs):
                kb.op("dve", lambda j=j, ap_=ap_: v.scalar_tensor_tensor(
                    out=outs[j], in0=ap_, scalar=gains[j], in1=cm.RS[0:P, 0:n], op0=ALU.mult, op1=ALU.mult),
                    reads=[r_, cm.RSR, PPR, CSTR], writes=[out_res])
                yield

        def fm_rmsnorm(*args, **kw):
            exhaust(fm_rmsnorm_g(*args, **kw))

        def rope_fm_g(cm, src, src_res, P, lt0, n, pb):
            i = cm.rti % 2
            cm.rti += 1
            kb.dma("pool", lambda: g.dma_start(out=cm.RT[i][:, :, 0:n], in_=rope_d[:, :, lt0:lt0 + n]), cm.RTR[i], True)
            kb.op("pe", mm(PS[pb][0:P, 0:n], PERMB[0:P, 0:P], src, True, True), reads=[src_res, CSTR], writes=[PSR[pb]])
            yield
            kb.op("dve", lambda: v.tensor_tensor(out=cm.T3[0:P, 0:n], in0=src, in1=cm.RT[i][0:P, 0, 0:n], op=ALU.mult),
                  reads=[src_res, cm.RTR[i]], writes=[cm.T3R])
            yield
            kb.op("dve", lambda: v.tensor_tensor(out=cm.T2[0:P, 0:n], in0=PS[pb][0:P, 0:n], in1=cm.RT[i][0:P, 1, 0:n], op=ALU.mult),
                  reads=[PSR[pb], cm.RTR[i]], writes=[cm.T2R])
            yield
            kb.op("dve", lambda: v.tensor_tensor(out=src, in0=cm.T3[0:P, 0:n], in1=cm.T2[0:P, 0:n], op=ALU.add),
                  reads=[cm.T3R, cm.T2R], writes=[src_res])
            yield

        def rope_fm(*args, **kw):
            exhaust(rope_fm_g(*args, **kw))

        def attn_core(cm, kts, n, s_terms, s_reads, v_fn, v_reads, scale, ob=2, zb=3, tick=None):
            def emit_s(i):
                terms = s_terms(kts[i])
                kb.op("pe", [mm(PS[i % 2][:, 0:n], l_, r_, ti == 0, ti == len(terms) - 1) for ti, (l_, r_) in enumerate(terms)],
                      reads=s_reads, writes=[PSR[i % 2]])
            emit_s(0)
            last = len(kts) - 1
            for i, kt in enumerate(kts):
                if i < last:
                    emit_s(i + 1)
                eb = i % 2
                kb.op("act", lambda i=i, eb=eb: a.activation(out=cm.E[eb][:, 0:n], in_=PS[i % 2][:, 0:n], func=AF.Exp, scale=scale),
                      reads=[PSR[i % 2]], writes=[cm.ER[eb]])
                kb.op("pe", [mm(PS[ob][:, 0:n], v_fn(kt), cm.E[eb][:, 0:n], i == 0, i == last),
                             mm(PS[zb][:, 0:n], ONESB, cm.E[eb][:, 0:n], i == 0, i == last)],
                      reads=[cm.ER[eb], CSTR] + v_reads, writes=[PSR[ob], PSR[zb]])
                if tick is not None:
                    tick()

        def wout_partial(cm, sc_, lhs_fn, lhs_reads, nk, wviews, wres, tiles, tag):
            TM = [cm.T1, cm.RS]
            TMR = [cm.T1R, cm.RSR]
            c_ = 0
            for t in tiles:
                w_ = 1 if t < 2 else 0
                for nh in range(2):
                    pb = 4 + (c_ % 4)
                    ti = c_ % 2
                    c_ += 1
                    kb.op("pe", [mm(PS[pb][:], lhs_fn(k, t), wviews[nh][:, k, :], k == 0, k == nk - 1) for k in range(nk)],
                          reads=lhs_reads(t) + [wres[nh]], writes=[PSR[pb]])
                    kb.op("dve", lambda pb=pb, ti=ti, nh=nh, w_=w_: v.tensor_tensor(
                        out=TM[ti][:], in0=PS[pb][:], in1=GBC[:, w_, nh * 512:(nh + 1) * 512], op=ALU.mult),
                        reads=[PSR[pb], GBCR], writes=[TMR[ti]])
                    kb.op("pool", lambda t=t, nh=nh, ti=ti: g.tensor_tensor(
                        out=X[:, t, nh * 512:(nh + 1) * 512], in0=X[:, t, nh * 512:(nh + 1) * 512], in1=TM[ti][:], op=ALU.add),
                        reads=[TMR[ti], XR[t][nh]], writes=[XR[t][nh]])

        def blk_of_tile(t):
            return 0 if t < 2 else 1 + (t - 2) // 4

        def mla_mixer():
            with Scope() as sc_:
                cm = Common(sc_)
                CQN = sc_.sb("CQN", [128, 3, SEQ], BF16)
                CQNR = kb.res("cqn")
                CKVN = sc_.sb("CKVN", [128, 2, NTOK], BF16)
                CKVNR = kb.res("ckvn")
                KRT = sc_.sb("KRT", [128, NTOK], BF16)
                KRTR = kb.res("krt")
                KNT = [sc_.sb("KNT%d" % i, [128, NTOK], BF16) for i in range(2)]
                KNTR = [kb.res("knt%d" % i) for i in range(2)]
                VH = [sc_.sb("VH%d" % i, [128, NT, 128], BF16) for i in range(2)]
                VHR = [kb.res("vh%d" % i) for i in range(2)]
                QNT = [sc_.sb("QNT%d" % i, [128, 512], BF16) for i in range(2)]
                QNTR = [kb.res("qnt%d" % i) for i in range(2)]
                QRT = [sc_.sb("QRT%d" % i, [128, 512], BF16) for i in range(2)]
                QRTR = [kb.res("qrt%d" % i) for i in range(2)]
                ra, wa = wload(owin_d[:, 0:384], 8, 384)
                rb, wb = wload(owin_d[:, 384:704], 8, 320)
                for blk, (t0, n) in enumerate(BLOCKS):
                    lat = blk > 0
                    if lat:
                        for j in range(3):
                            kb.op("pe", [mm(PS[4 + j][:, 0:n], wa[:, k, j * 128:(j + 1) * 128], HT[:, k, t0:t0 + n], k == 0, k == 7)
                                         for k in range(8)], reads=[ra, HTR[blk]], writes=[PSR[4 + j]])
                        fm_rmsnorm(cm, [4, 5, 6], 128, 384, [ppc("qag", j) for j in range(3)],
                                   [CQN[:, j, t0 - 256:t0 - 256 + n] for j in range(3)], n, CQNR)
                    for j in range(2):
                        kb.op("pe", [mm(PS[4 + j][:, 0:n], wb[:, k, j * 128:(j + 1) * 128], HT[:, k, t0:t0 + n], k == 0, k == 7)
                                     for k in range(8)], reads=[rb, HTR[blk]], writes=[PSR[4 + j]])
                    fm_rmsnorm(cm, [4, 5], 128, 256, [ppc("kvag", j) for j in range(2)],
                               [CKVN[:, j, t0:t0 + n] for j in range(2)], n, CKVNR)
                    kb.op("pe", [mm(PS[6][0:64, 0:n], wb[:, k, 256:320], HT[:, k, t0:t0 + n], k == 0, k == 7)
                                 for k in range(8)], reads=[rb, HTR[blk]], writes=[PSR[6]])
                    fm_rmsnorm(cm, [6], 64, 64, [ppc("krg")[0:64, :]], [KRT[0:64, t0:t0 + n]], n, KRTR)
                    if lat:
                        rope_fm(cm, KRT[0:64, t0:t0 + n], KRTR, 64, t0 - 256, n, 6)
                rq0, wq0 = wload(wuq_d[:, 0:768], 3, 768)
                rq1, wq1 = wload(wuq_d[:, 768:1536], 3, 768)
                rkv, wkv = wload(wukv_d, 2, 2048)
                scale = 192.0 ** -0.5
                warmup()

                def prep_head(h, bf):
                    knt, kntR = KNT[bf], KNTR[bf]
                    vh, vhR = VH[bf], VHR[bf]
                    for blk, (t0, n) in enumerate(BLOCKS):
                        kb.op("pe", [mm(PS[6][:, 0:n], wkv[:, j, h * 256:h * 256 + 128], CKVN[:, j, t0:t0 + n], j == 0, j == 1)
                                     for j in range(2)], reads=[rkv, CKVNR], writes=[PSR[6]])
                        yield
                        yield from fm_rmsnorm_g(cm, [6], 128, 128, [ppc("kng")], [knt[:, t0:t0 + n]], n, kntR)
                        yield True
                    for g0 in range(0, NT, 4):
                        tl = list(range(g0, min(g0 + 4, NT)))
                        fns = []
                        for i, t in enumerate(tl):
                            for j in range(2):
                                fns.append(mm(PS[6][:, i * 128:(i + 1) * 128], CKVN[:, j, t * 128:(t + 1) * 128],
                                              wkv[:, j, h * 256 + 128:h * 256 + 256], j == 0, j == 1))
                        kb.op("pe", fns, reads=[rkv, CKVNR], writes=[PSR[6]])
                        yield
                        kb.op("act", lambda g0=g0, tl=tl: a.copy(
                            out=vh[:, g0:g0 + len(tl), :],
                            in_=PS[6][:, 0:len(tl) * 128].rearrange("p (t c) -> p t c", c=128)),
                            reads=[PSR[6]], writes=[vhR])
                        yield True

                def prep_q(h, qb, qq):
                    rq, wq = (rq0, wq0) if h < 4 else (rq1, wq1)
                    hq = h % 4
                    q0 = qb * 512
                    kb.op("pe", [mm(PS[6][:, 0:512], wq[:, j, hq * 192:hq * 192 + 128], CQN[:, j, q0:q0 + 512], j == 0, j == 2)
                                 for j in range(3)], reads=[rq, CQNR], writes=[PSR[6]])
                    yield
                    yield from fm_rmsnorm_g(cm, [6], 128, 128, [ppc("qng")], [QNT[qq][:, :]], 512, QNTR[qq])
                    kb.op("pe", [mm(PS[6][0:64, 0:512], wq[:, j, hq * 192 + 128:hq * 192 + 192], CQN[:, j, q0:q0 + 512], j == 0, j == 2)
                                 for j in range(3)], reads=[rq, CQNR], writes=[PSR[6]])
                    yield
                    yield from fm_rmsnorm_g(cm, [6], 64, 64, [ppc("qrg")[0:64, :]], [QRT[qq][0:64, :]], 512, QRTR[qq])
                    yield from rope_fm_g(cm, QRT[qq][0:64, :], QRTR[qq], 64, q0, 512, 6)

                exhaust(prep_head(0, 0))
                exhaust(prep_q(0, 0, 0))
                qi = 0
                for h in range(8):
                    bf = h % 2
                    bg_head = BG(prep_head(h + 1, (h + 1) % 2) if h + 1 < 8 else None)
                    for qb in range(4):
                        q0 = qb * 512
                        blk = qb + 1
                        qq = qi % 2
                        qi += 1
                        if qb + 1 < 4:
                            bg_q = BG(prep_q(h, qb + 1, qi % 2))
                        elif h + 1 < 8:
                            bg_q = BG(prep_q(h + 1, 0, qi % 2))
                        else:
                            bg_q = BG(None)

                        def tick(bg_q=bg_q, bg_head=bg_head):
                            for _ in range(2):
                                if not bg_q.done:
                                    bg_q.step()
                                else:
                                    bg_head.step()
                        ob, zb = (2, 3) if qi % 2 else (4, 5)
                        attn_core(cm, list(range(NT)), 512,
                                  lambda kt, qq=qq, bf=bf: [(KNT[bf][:, kt * 128:(kt + 1) * 128], QNT[qq][:, :]),
                                                            (KRT[0:64, kt * 128:(kt + 1) * 128], QRT[qq][0:64, :])],
                                  [KNTR[bf], KRTR, QNTR[qq], QRTR[qq]],
                                  lambda kt, bf=bf: VH[bf][:, kt, :], [VHR[bf]], scale, ob=ob, zb=zb, tick=tick)
                        kb.op("act", lambda zb=zb: a.activation(out=cm.T1[:, :], in_=PS[zb][:, :], func=AF.Ln), reads=[PSR[zb]], writes=[cm.T1R])
                        kb.op("act", lambda: a.activation(out=cm.T1[:, :], in_=cm.T1[:, :], func=AF.Exp, scale=-1.0), reads=[cm.T1R], writes=[cm.T1R])
                        kb.op("dve", lambda h=h, q0=q0, ob=ob: v.tensor_tensor(
                            out=HT[:, h, 256 + q0:256 + q0 + 512], in0=PS[ob][:, :], in1=cm.T1[:, :], op=ALU.mult),
                            reads=[PSR[ob], cm.T1R], writes=[HTR[blk]])
                        bg_q.finish()
                        bg_head.to_safe()
                    bg_head.finish()
                rw0, wo0 = wload(owout_d[:, 0:512], 8, 512)
                rw1, wo1 = wload(owout_d[:, 512:1024], 8, 512)
                wout_partial(cm, sc_, lambda k, t: HT[:, k, t * 128:(t + 1) * 128], lambda t: [HTR[blk_of_tile(t)]],
                             8, [wo0, wo1], [rw0, rw1], list(range(2, NT)), "m")

        BLK64B = CSTB[:, C_BLK64:C_BLK64 + 128]
        ONEC = sb("ONEC", [128, 1])
        kb.op("dve", lambda: v.memset(ONEC[:], 1.0), writes=[CSTR])
        LAMC = sb("LAMC", [128, 4])
        LAMR = kb.res("lamc")
        OMLF = sb("OMLF", [128, 8])
        OMLFR = kb.res("omlf")
        LAM_INIT0 = 0.8 - 0.6 * math.exp(-0.3 * 0)

        def even_prep():
            o, _ = PPO["dlam"]
            S0, R0 = SM[4], SMR[4]
            S1, R1 = SM[5], SMR[5]
            for i in range(2):
                kb.op("dve", lambda i=i: v.tensor_tensor(out=S0[:, 0:64], in0=PPT[:, o + i * 128:o + i * 128 + 64],
                                                         in1=PPT[:, o + i * 128 + 64:o + i * 128 + 128], op=ALU.mult),
                      reads=[PPR], writes=[R0])
                kb.op("dve", lambda i=i: v.tensor_reduce(out=S1[:, i:i + 1], in_=S0[:, 0:64], axis=AX.X, op=ALU.add),
                      reads=[R0], writes=[R1])
            kb.op("act", lambda: a.activation(out=S1[:, 2:4], in_=S1[:, 0:2], func=AF.Exp), reads=[R1], writes=[R1])
            kb.op("dve", lambda: v.scalar_tensor_tensor(out=LAMC[:, 0:1], in0=S1[:, 3:4], scalar=-LAM_INIT0, in1=S1[:, 2:3],
                                                        op0=ALU.add, op1=ALU.subtract), reads=[R1], writes=[LAMR])
            kb.op("dve", lambda: v.tensor_scalar(out=LAMC[:, 1:2], in0=ppc("subln"), scalar1=1.0 - LAM_INIT0, scalar2=None,
                                                 op0=ALU.mult), reads=[PPR], writes=[LAMR])
            o2, _ = PPO["lbfm"]
            kb.op("dve", lambda: v.tensor_tensor(out=S0[:, 0:8], in0=PPT[:, o2:o2 + 8], in1=PPT[:, o2 + 8:o2 + 16], op=ALU.subtract),
                  reads=[PPR], writes=[R0])
            kb.op("act", lambda: a.activation(out=S0[:, 0:8], in_=S0[:, 0:8], func=AF.Exp), reads=[R0], writes=[R0])
            kb.op("dve", lambda: v.tensor_scalar(out=S0[:, 0:8], in0=S0[:, 0:8], scalar1=1.0, scalar2=None, op0=ALU.add),
                  reads=[R0], writes=[R0])
            kb.op("dve", lambda: v.reciprocal(out=OMLF[:, :], in_=S0[:, 0:8]), reads=[R0], writes=[OMLFR])

        def diff_part():
            with Scope() as sc_:
                cm = Common(sc_)
                cm.T4 = sc_.sb("T4d", [128, 512])
                cm.T4R = kb.res("t4d")
                KT = [sc_.sb("KT%d" % i, [128, NTOK], BF16) for i in range(2)]
                KTR = [kb.res("kt%d" % i) for i in range(2)]
                QT = [sc_.sb("QTb%d" % i, [128, 512], BF16) for i in range(2)]
                QTR = [kb.res("qtb%d" % i) for i in range(2)]
                VH = [sc_.sb("VHd%d" % i, [128, NT, 128], BF16) for i in range(2)]
                VHR = [kb.res("vhd%d" % i) for i in range(2)]
                CATA = sc_.sb("CATA", [128, 4, NTOK], BF16)
                CATAR = [kb.res("cata%d" % i) for i in range(len(BLOCKS))]
                rq, wq = wload(ewin_d[:, 0:512], 8, 512)
                rk, wk = wload(ewin_d[:, 512:1024], 8, 512)
                rv, wv = wload(ewin_d[:, 1024:1536], 8, 512)
                scale = 64.0 ** -0.5
                warmup()

                def prep_head(h, bf):
                    for blk, (t0, n) in enumerate(BLOCKS):
                        kb.op("pe", [mm(PS[6][:, 0:n], wk[:, k, h * 128:(h + 1) * 128], HT[:, k, t0:t0 + n], k == 0, k == 7)
                                     for k in range(8)], reads=[rk, HTR[blk]], writes=[PSR[6]])
                        yield
                        yield from fm_rmsnorm_g(cm, [6], 128, 64, [ppc("dkg")], [KT[bf][:, t0:t0 + n]], n, KTR[bf], ones_ap=BLK64B)
                        if blk > 0:
                            yield from rope_fm_g(cm, KT[bf][:, t0:t0 + n], KTR[bf], 128, t0 - 256, n, 6)
                        yield True
                    for g0 in range(0, NT, 4):
                        tl = list(range(g0, min(g0 + 4, NT)))
                        fns = []
                        for i, t in enumerate(tl):
                            for k in range(8):
                                fns.append(mm(PS[6][:, i * 128:(i + 1) * 128], HT[:, k, t * 128:(t + 1) * 128],
                                              wv[:, k, h * 128:(h + 1) * 128], k == 0, k == 7))
                        kb.op("pe", fns, reads=[rv] + [HTR[blk_of_tile(t)] for t in tl], writes=[PSR[6]])
                        yield
                        kb.op("act", lambda g0=g0, tl=tl: a.copy(
                            out=VH[bf][:, g0:g0 + len(tl), :],
                            in_=PS[6][:, 0:len(tl) * 128].rearrange("p (t c) -> p t c", c=128)),
                            reads=[PSR[6]], writes=[VHR[bf]])
                        yield True

                def prep_q(h, blk, qq):
                    t0, n = BLOCKS[blk]
                    kb.op("pe", [mm(PS[6][:, 0:n], wq[:, k, h * 128:(h + 1) * 128], HT[:, k, t0:t0 + n], k == 0, k == 7)
                                 for k in range(8)], reads=[rq, HTR[blk]], writes=[PSR[6]])
                    yield
                    yield from fm_rmsnorm_g(cm, [6], 128, 64, [ppc("dqg")], [QT[qq][:, 0:n]], n, QTR[qq], ones_ap=BLK64B)
                    if blk > 0:
                        yield from rope_fm_g(cm, QT[qq][:, 0:n], QTR[qq], 128, t0 - 256, n, 6)

                exhaust(prep_head(0, 0))
                exhaust(prep_q(0, 0, 0))
                qi = 0
                nblk = len(BLOCKS)
                for h in range(4):
                    bf = h % 2
                    bg_head = BG(prep_head(h + 1, (h + 1) % 2) if h + 1 < 4 else None)
                    for blk, (t0, n) in enumerate(BLOCKS):
                        qq = qi % 2
                        qi += 1
                        if blk + 1 < nblk:
                            bg_q = BG(prep_q(h, blk + 1, qi % 2))
                        elif h + 1 < 4:
                            bg_q = BG(prep_q(h + 1, 0, qi % 2))
                        else:
                            bg_q = BG(None)

                        def tick(bg_q=bg_q, bg_head=bg_head):
                            if not bg_q.done:
                                bg_q.step()
                            else:
                                bg_head.step()
                        kts = [0, 1] if blk == 0 else list(range(NT))
                        for c in range(2):
                            attn_core(cm, kts, n,
                                      lambda kt, c=c, bf=bf, qq=qq: [(KT[bf][c * 64:(c + 1) * 64, kt * 128:(kt + 1) * 128],
                                                                      QT[qq][c * 64:(c + 1) * 64, 0:n])],
                                      [KTR[bf], QTR[qq]], lambda kt, bf=bf: VH[bf][:, kt, :], [VHR[bf]], scale,
                                      ob=2 + 2 * c, zb=3 + 2 * c, tick=tick)
                        kb.op("act", lambda: a.activation(out=cm.T1[:, 0:n], in_=PS[3][:, 0:n], func=AF.Ln), reads=[PSR[3]], writes=[cm.T1R])
                        kb.op("act", lambda: a.activation(out=cm.T1[:, 0:n], in_=cm.T1[:, 0:n], func=AF.Exp, scale=-1.0), reads=[cm.T1R], writes=[cm.T1R])
                        kb.op("dve", lambda: v.tensor_tensor(out=cm.T1[:, 0:n], in0=PS[2][:, 0:n], in1=cm.T1[:, 0:n], op=ALU.mult),
                              reads=[PSR[2], cm.T1R], writes=[cm.T1R])
                        kb.op("act", lambda: a.activation(out=cm.T4[:, 0:n], in_=PS[5][:, 0:n], func=AF.Ln), reads=[PSR[5]], writes=[cm.T4R])
                        kb.op("act", lambda: a.activation(out=cm.T4[:, 0:n], in_=cm.T4[:, 0:n], func=AF.Exp, scale=-1.0), reads=[cm.T4R], writes=[cm.T4R])
                        kb.op("dve", lambda: v.tensor_tensor(out=cm.T4[:, 0:n], in0=PS[4][:, 0:n], in1=cm.T4[:, 0:n], op=ALU.mult),
                              reads=[PSR[4], cm.T4R], writes=[cm.T4R])
                        kb.op("dve", lambda: v.scalar_tensor_tensor(out=cm.T1[:, 0:n], in0=cm.T4[:, 0:n], scalar=LAMC[:, 0:1],
                                                                    in1=cm.T1[:, 0:n], op0=ALU.mult, op1=ALU.add),
                              reads=[cm.T4R, cm.T1R, LAMR], writes=[cm.T1R])
                        bg_q.finish()
                        bg_head.to_safe()
                        fm_rmsnorm(cm, [(cm.T1[:, 0:n], cm.T1R)], 128, 128, [LAMC[:, 1:2]], [CATA[:, h, t0:t0 + n]], n, CATAR[blk])
                    bg_head.finish()
                rw0, wo0 = wload(ewout_d[0:512, 0:512], 4, 512)
                rw1, wo1 = wload(ewout_d[0:512, 512:1024], 4, 512)
                wout_partial(cm, sc_, lambda k, t: CATA[:, k, t * 128:(t + 1) * 128], lambda t: [CATAR[blk_of_tile(t)]],
                             4, [wo0, wo1], [rw0, rw1], list(range(NT)), "d")

        def hgrn_part():
            with Scope() as sc_:
                RING.append(sc_.sb("hring", [128, SLOT_ELEMS], BF16))
                RINGR.append(kb.res("hring"))
                ring_pos[0] = 0
                _hgrn_part(sc_)
                kb.barrier()
                del RING[NSLOT:]
                del RINGR[NSLOT:]
                ring_pos[0] = 0

        def _hgrn_part(sc_):
            def T_(name, shape, dt=F32):
                return sc_.sb(name, shape, dt), kb.res(name)
            TA, TAR = T_("hTA", [128, 512])
            TB, TBR = T_("hTB", [128, 512])
            TC, TCR = T_("hTC", [128, 512])
            TD, TDR = T_("hTD", [128, 512])
            EC, ECR = T_("hEC", [128, 512])
            KBAR = [[T_("hKBAR%d%d" % (d, b), [128, 4, 128], BF16) for b in range(2)] for d in range(2)]
            KTIL = [[T_("hKTIL%d%d" % (d, b), [128, 512], BF16) for b in range(2)] for d in range(2)]
            QZ = [[T_("hQZ%d%d" % (d, b), [128, 4, 256], BF16) for b in range(2)] for d in range(2)]
            ALAST = [[T_("hAL%d%d" % (d, b), [128, 8]) for b in range(2)] for d in range(2)]
            VH, VHR = T_("hVH", [128, NT, 128], BF16)
            ODS = [T_("hOS%d" % p_, [128, NT, 128], BF16) for p_ in range(2)]
            prev_fin = [None]
            ST = [T_("hS%d" % d, [128, 128]) for d in range(2)]
            SBF = [T_("hSB%d" % d, [128, 128], BF16) for d in range(2)]
            SCM = [T_("hSCM%d" % d, [128, 128], BF16) for d in range(2)]
            OMLBC, OMLBCR = T_("hOMLBC", [128, 2, 128])
            CATB, CATBR = T_("hCATB", [128, 512], BF16)
            SSH, SSHR = T_("hSSH", [128, 8])
            PSS = [[kb.res("pss%d_%d" % (d, i)) for i in range(3)] for d in range(2)]
            for d in range(2):
                for b in range(2):
                    kb.op("pool", lambda d=d, b=b: g.memset(QZ[d][b][0][:], 0.0), writes=[QZ[d][b][1]])

            class Shim:
                pass
            cm = Shim()
            cm.T1, cm.T1R, cm.RS, cm.RSR = TA, TAR, TB, TBR
            G = [[0, 1], [2, 3, 4, 5], [6, 7, 8, 9], [10, 11, 12, 13], [14, 15, 16, 17]]
            GORD = [G, [G[0], G[4], G[3], G[2], G[1]]]
            TRI = [CST[:, C_TRIF:C_TRIF + 128], CST[:, C_TRIB:C_TRIB + 128]]
            AFT = [CST[:, C_AFTF:C_AFTF + 128], CST[:, C_AFTB:C_AFTB + 128]]
            hscale = 128.0 ** -0.5
            hb = 1536

            def ht_res(tiles):
                return list({id(HTR[blk_of_tile(t)]): HTR[blk_of_tile(t)] for t in tiles}.values())

            for hh in range(4):
                ia = ring_pos[0] % len(RING)
                ring_pos[0] += 1
                ib = ring_pos[0] % len(RING)
                ring_pos[0] += 1
                rA, rB = RINGR[ia], RINGR[ib]
                wA = RING[ia][:, 0:4096].rearrange("p (k c) -> p k c", k=8)
                for pi, c0 in enumerate((hb + hh * 128, hb + 512 + hh * 128, hb + 1024 + hh * 128, hb + 1536 + hh * 128)):
                    kb.dma("pool", lambda pi=pi, c0=c0: g.dma_start(
                        out=wA[:, :, pi * 128:(pi + 1) * 128],
                        in_=ewin_d[:, c0:c0 + 128].rearrange("(k p) c -> p k c", p=128)), rA, True)
                wG = RING[ib][:, 0:1024].rearrange("p (k c) -> p k c", k=8)
                wO = RING[ib][:, 1024:2048]
                c0 = hb + 2048 + hh * 128
                kb.dma("pool", lambda: g.dma_start(out=wG, in_=ewin_d[:, c0:c0 + 128].rearrange("(k p) c -> p k c", p=128)), rB, True)
                kb.dma("pool", lambda: g.dma_start(out=wO, in_=ewout_d[512 + hh * 128:512 + (hh + 1) * 128, :]), rB, True)
                for d in range(2):
                    kb.op("dve", lambda d=d: v.tensor_copy(out=TD[:, 0:128], in_=OMLF[:, d * 4 + hh:d * 4 + hh + 1].to_broadcast([128, 128])),
                          reads=[OMLFR], writes=[TDR])
                    kb.op("pe", mm(PS[0][:, 0:128], TD[:, 0:128], IDENT, True, True), reads=[TDR, CSTR], writes=[PSR[0]])
                    kb.op("act", lambda d=d: a.copy(out=OMLBC[:, d, :], in_=PS[0][:, 0:128]), reads=[PSR[0]], writes=[OMLBCR])
                for g0 in range(0, NT, 4):
                    tl = list(range(g0, min(g0 + 4, NT)))
                    fns = []
                    for i, t in enumerate(tl):
                        for k in range(8):
                            fns.append(mm(PS[3][:, i * 128:(i + 1) * 128], HT[:, k, t * 128:(t + 1) * 128], wA[:, k, 384:512], k == 0, k == 7))
                    kb.op("pe", fns, reads=[rA] + ht_res(tl), writes=[PSR[3]])
                    kb.op("act", lambda g0=g0, tl=tl: a.copy(out=VH[:, g0:g0 + len(tl), :],
                                                           in_=PS[3][:, 0:len(tl) * 128].rearrange("p (t c) -> p t c", c=128)),
                          reads=[PSR[3]], writes=[VHR])
                for d in range(2):
                    kb.op("dve", lambda d=d: v.memset(ST[d][0][:], 0.0), writes=[ST[d][1]])
                    kb.op("dve", lambda d=d: v.memset(SBF[d][0][:], 0.0), writes=[SBF[d][1]])

                def prep(d, tiles, b):
                    ops = []

                    def Q(*a_, **k_):
                        ops.append(lambda: kb.op(*a_, **k_))
                    ng = len(tiles)
                    n = ng * 128
                    tk0 = tiles[0] * 128
                    fc = 128 + d * 128
                    hr = ht_res(tiles)
                    kbar, kbarR = KBAR[d][b]
                    ktil, ktilR = KTIL[d][b]
                    qz, qzR = QZ[d][b]
                    al, alR = ALAST[d][b]
                    fns = []
                    for i, t in enumerate(tiles):
                        for k in range(8):
                            fns.append(mm(PS[0][:, i * 128:(i + 1) * 128], HT[:, k, t * 128:(t + 1) * 128], wA[:, k, fc:fc + 128], k == 0, k == 7))
                    Q("pe", fns, reads=[rA] + hr, writes=[PSR[0]])
                    Q("pe", [mm(PS[1][:, 0:n], wA[:, k, fc:fc + 128], HT[:, k, tk0:tk0 + n], k == 0, k == 7) for k in range(8)],
                          reads=[rA] + hr, writes=[PSR[1]])
                    Q("pe", [mm(PS[2][:, 0:n], wA[:, k, 0:128], HT[:, k, tk0:tk0 + n], k == 0, k == 7) for k in range(8)],
                          reads=[rA] + hr, writes=[PSR[2]])
                    Q("act", lambda: a.activation(out=TA[:, 0:n], in_=PS[0][:, 0:n], func=AF.Exp), reads=[PSR[0]], writes=[TAR])
                    Q("act", lambda: a.activation(out=TA[:, 0:n], in_=TA[:, 0:n], func=AF.Ln, bias=ONEC[:, :]),
                          reads=[TAR, CSTR], writes=[TAR])
                    Q("act", lambda: a.activation(out=TA[:, 0:n], in_=TA[:, 0:n], func=AF.Exp, scale=-1.0), reads=[TAR], writes=[TAR])
                    Q("dve", lambda: v.tensor_tensor(
                        out=TB[:, 0:n].rearrange("p (t c) -> p t c", c=128), in0=TA[:, 0:n].rearrange("p (t c) -> p t c", c=128),
                        in1=OMLBC[:, d:d + 1, :].to_broadcast([128, ng, 128]), op=ALU.mult),
                        reads=[TAR, OMLBCR], writes=[TBR])
                    Q("act", lambda: a.activation(out=TC[:, 0:n], in_=TB[:, 0:n], func=AF.Ln, scale=-1.0, bias=ONEC[:, :]),
                          reads=[TBR, CSTR], writes=[TCR])
                    Q("pe", [mm(PS[3][:, i * 128:(i + 1) * 128], AFT[d], TC[:, i * 128:(i + 1) * 128], True, True) for i in range(ng)],
                          reads=[TCR, CSTR], writes=[PSR[3]])
                    Q("pe", [mm(PS[4][:, i * 128:(i + 1) * 128], TC[:, i * 128:(i + 1) * 128], TRI[d], True, True) for i in range(ng)],
                          reads=[TCR, CSTR], writes=[PSR[4]])
                    Q("act", lambda: a.activation(out=TA[:, 0:n], in_=PS[3][:, 0:n], func=AF.Exp), reads=[PSR[3]], writes=[TAR])
                    Q("dve", lambda: v.tensor_tensor(out=kbar[:, 0:ng, :], in0=TB[:, 0:n].rearrange("p (t c) -> p t c", c=128),
                                                         in1=TA[:, 0:n].rearrange("p (t c) -> p t c", c=128), op=ALU.mult),
                          reads=[TAR, TBR], writes=[kbarR])
                    Q("act", lambda: a.activation(out=TD[:, 0:n], in_=PS[1][:, 0:n], func=AF.Exp), reads=[PSR[1]], writes=[TDR])
                    Q("act", lambda: a.activation(out=TD[:, 0:n], in_=TD[:, 0:n], func=AF.Ln, bias=ONEC[:, :]),
                          reads=[TDR, CSTR], writes=[TDR])
                    Q("act", lambda: a.activation(out=TD[:, 0:n], in_=TD[:, 0:n], func=AF.Exp, scale=-1.0), reads=[TDR], writes=[TDR])
                    Q("act", lambda: a.activation(out=EC[:, 0:n], in_=PS[4][:, 0:n], func=AF.Exp), reads=[PSR[4]], writes=[ECR])
                    Q("act", lambda: a.activation(out=TA[:, 0:n], in_=PS[4][:, 0:n], func=AF.Exp, scale=-1.0), reads=[PSR[4]], writes=[TAR])
                    Q("dve", lambda: v.scalar_tensor_tensor(out=ktil[:, 0:n], in0=TD[:, 0:n], scalar=OMLF[:, d * 4 + hh:d * 4 + hh + 1],
                                                                in1=TA[:, 0:n], op0=ALU.mult, op1=ALU.mult),
                          reads=[TDR, TAR, OMLFR], writes=[ktilR])
                    lastcol = 63 if d == 0 else 0
                    Q("dve", lambda: v.tensor_copy(out=al[:, 0:2 * ng],
                                                       in_=EC[:, 0:n].rearrange("p (c j) -> p c j", j=64)[:, :, lastcol]),
                          reads=[ECR], writes=[alR])
                    Q("act", lambda: a.activation(out=TB[:, 0:n], in_=PS[2][:, 0:n], func=AF.Exp, scale=-1.0), reads=[PSR[2]], writes=[TBR])
                    Q("act", lambda: a.activation(out=TB[:, 0:n], in_=TB[:, 0:n], func=AF.Ln, bias=ONEC[:, :]),
                          reads=[TBR, CSTR], writes=[TBR])
                    Q("act", lambda: a.activation(out=TB[:, 0:n], in_=TB[:, 0:n], func=AF.Exp, scale=-1.0), reads=[TBR], writes=[TBR])
                    Q("dve", lambda: v.tensor_tensor(out=TB[:, 0:n], in0=PS[2][:, 0:n], in1=TB[:, 0:n], op=ALU.mult),
                          reads=[TBR, PSR[2]], writes=[TBR])
                    for c in range(2):
                        Q("dve", lambda c=c: v.scalar_tensor_tensor(
                            out=qz[:, 0:ng, c * 192:c * 192 + 64],
                            in0=TB[:, 0:n].rearrange("p (t c) -> p t c", c=128)[:, :, c * 64:(c + 1) * 64], scalar=hscale,
                            in1=EC[:, 0:n].rearrange("p (t c) -> p t c", c=128)[:, :, c * 64:(c + 1) * 64], op0=ALU.mult, op1=ALU.mult),
                            reads=[TBR, ECR], writes=[qzR])
                    return ops

                def recur(d, t, i, b, tick, par, written):
                    bank = 5 + d
                    s_ap = PS[bank][:, 0:128]
                    o_ap = PS[bank][:, 128:256]
                    ds_ap = PS[7][:, 256 + d * 128:384 + d * 128]
                    sR, oR, dsR = PSS[d]
                    kbar, kbarR = KBAR[d][b]
                    ktil, ktilR = KTIL[d][b]
                    qz, qzR = QZ[d][b]
                    al, alR = ALAST[d][b]
                    st, stR = ST[d]
                    sbf, sbfR = SBF[d]
                    scm, scmR = SCM[d]
                    od, odR = ODS[par]
                    kt_ = ktil[:, i * 128:(i + 1) * 128]
                    kb.op("pe", [mm(s_ap[:, 0:64], kt_, qz[:, i, 0:64], True, True), mm(s_ap[:, 64:128], kt_, qz[:, i, 192:256], True, True)],
                          reads=[ktilR, qzR], writes=[sR])
                    tick()
                    kb.op("dve", lambda: v.tensor_tensor(out=scm[:, :], in0=s_ap, in1=TRI[d], op=ALU.mult), reads=[sR, CSTR], writes=[scmR])
                    tick()
                    cs = [0, 1] if d == 0 else [1, 0]

                    def upd(c):
                        kb.op("pe", mm(ds_ap, kbar[c * 64:(c + 1) * 64, i, :], VH[c * 64:(c + 1) * 64, t, :], True, True),
                              reads=[kbarR, VHR], writes=[dsR])
                        tick()
                        kb.op("dve", lambda: v.scalar_tensor_tensor(out=sbf[:, :], in0=st[:, :], scalar=al[:, 2 * i + c:2 * i + c + 1],
                                                                    in1=ds_ap, op0=ALU.mult, op1=ALU.add),
                              reads=[stR, alR, dsR], writes=[sbfR])
                        kb.op("dve", lambda: v.scalar_tensor_tensor(out=st[:, :], in0=st[:, :], scalar=al[:, 2 * i + c:2 * i + c + 1],
                                                                    in1=ds_ap, op0=ALU.mult, op1=ALU.add),
                              reads=[stR, alR, dsR], writes=[stR])
                        tick()
                    kb.op("pe", [mm(o_ap, scm[:, :], VH[:, t, :], True, False),
                                 mm(o_ap, qz[:, i, cs[0] * 128:(cs[0] + 1) * 128], sbf[:, :], False, False)],
                          reads=[scmR, VHR, qzR, sbfR], writes=[oR])
                    tick()
                    upd(cs[0])
                    kb.op("pe", mm(o_ap, qz[:, i, cs[1] * 128:(cs[1] + 1) * 128], sbf[:, :], False, True),
                          reads=[qzR, sbfR], writes=[oR])
                    tick()
                    upd(cs[1])
                    if t not in written:
                        written.add(t)
                        kb.op("act", lambda: a.copy(out=od[:, t, :], in_=o_ap), reads=[oR], writes=[odR])
                    else:
                        kb.op("dve", lambda: v.tensor_tensor(out=od[:, t, :], in0=od[:, t, :], in1=o_ap, op=ALU.add),
                              reads=[oR, odR], writes=[odR])
                    tick()
                    tick()

                def fin_group(par, tiles, wG, wO, rB, cntl):
                    ops = []

                    def Q(*a_, **k_):
                        ops.append(lambda: kb.op(*a_, **k_))
                    od = ODS[par][0]
                    odR = ODS[par][1]
                    ng = len(tiles)
                    n = ng * 128
                    t0_ = tiles[0]
                    fns = []
                    for i, t in enumerate(tiles):
                        for k in range(8):
                            fns.append(mm(PS[0][:, i * 128:(i + 1) * 128], HT[:, k, t * 128:(t + 1) * 128], wG[:, k, :], k == 0, k == 7))
                    Q("pe", fns, reads=[rB] + ht_res(tiles), writes=[PSR[0]])
                    Q("dve", lambda: v.tensor_copy(out=TA[:, 0:n].rearrange("p (t c) -> p t c", c=128), in_=od[:, t0_:t0_ + ng, :]),
                          reads=[odR], writes=[TAR])
                    for i in range(ng):
                        Q("act", lambda i=i: a.activation(out=TD[:, i * 128:(i + 1) * 128], in_=TA[:, i * 128:(i + 1) * 128],
                                                              func=AF.Square, accum_out=SSH[:, i:i + 1]),
                              reads=[TAR], writes=[TDR, SSHR])
                    Q("dve", lambda: v.tensor_scalar(out=SSH[:, 0:ng], in0=SSH[:, 0:ng], scalar1=1.0 / 128, scalar2=EPS,
                                                         op0=ALU.mult, op1=ALU.add), reads=[SSHR], writes=[SSHR])
                    Q("act", lambda: a.activation(out=SSH[:, 0:ng], in_=SSH[:, 0:ng], func=AF.Ln), reads=[SSHR], writes=[SSHR])
                    Q("act", lambda: a.activation(out=SSH[:, 0:ng], in_=SSH[:, 0:ng], func=AF.Exp, scale=-0.5), reads=[SSHR], writes=[SSHR])
                    for i in range(ng):
                        Q("dve", lambda i=i: v.scalar_tensor_tensor(
                            out=TA[:, i * 128:(i + 1) * 128], in0=TA[:, i * 128:(i + 1) * 128], scalar=SSH[:, i:i + 1],
                            in1=ppc("hog", 0, 128), op0=ALU.mult, op1=ALU.mult), reads=[TAR, SSHR, PPR], writes=[TAR])
                    Q("act", lambda: a.activation(out=TB[:, 0:n], in_=PS[0][:, 0:n], func=AF.Exp, scale=-1.0), reads=[PSR[0]], writes=[TBR])
                    Q("act", lambda: a.activation(out=TB[:, 0:n], in_=TB[:, 0:n], func=AF.Ln, bias=ONEC[:, :]),
                          reads=[TBR, CSTR], writes=[TBR])
                    Q("act", lambda: a.activation(out=TB[:, 0:n], in_=TB[:, 0:n], func=AF.Exp, scale=-1.0), reads=[TBR], writes=[TBR])
                    Q("dve", lambda: v.tensor_tensor(out=TB[:, 0:n], in0=PS[0][:, 0:n], in1=TB[:, 0:n], op=ALU.mult),
                          reads=[TBR, PSR[0]], writes=[TBR])
                    Q("dve", lambda: v.tensor_tensor(out=TA[:, 0:n], in0=TA[:, 0:n], in1=TB[:, 0:n], op=ALU.mult),
                          reads=[TAR, TBR], writes=[TAR])
                    Q("pe", [mm(PS[1][:, i * 128:(i + 1) * 128], TA[:, i * 128:(i + 1) * 128], IDENT, True, True) for i in range(ng)],
                          reads=[TAR, CSTR], writes=[PSR[1]])
                    Q("act", lambda: a.copy(out=CATB[:, 0:n], in_=PS[1][:, 0:n]), reads=[PSR[1]], writes=[CATBR])
                    for i, t in enumerate(tiles):
                        w_ = 1 if t < 2 else 0
                        for nh in range(2):
                            pb = 2 + (cntl[0] % 2)
                            tm_, tmR = (TC, TCR) if cntl[0] % 2 == 0 else (TD, TDR)
                            cntl[0] += 1
                            Q("pe", mm(PS[pb][:], CATB[:, i * 128:(i + 1) * 128], wO[:, nh * 512:(nh + 1) * 512], True, True),
                                  reads=[CATBR, rB], writes=[PSR[pb]])
                            Q("dve", lambda pb=pb, nh=nh, w_=w_, tm_=tm_: v.tensor_tensor(
                                out=tm_[:], in0=PS[pb][:], in1=GBC[:, w_, nh * 512:(nh + 1) * 512], op=ALU.mult),
                                reads=[PSR[pb], GBCR], writes=[tmR])
                            Q("pool", lambda t=t, nh=nh, tm_=tm_: g.tensor_tensor(
                                out=X[:, t, nh * 512:(nh + 1) * 512], in0=X[:, t, nh * 512:(nh + 1) * 512], in1=tm_[:], op=ALU.add),
                                reads=[tmR, XR[t][nh]], writes=[XR[t][nh]])
                    return ops

                def run_ops(ops):
                    for f in ops:
                        f()

                def ops_gen(ops, safe_end=True):
                    for f in ops:
                        f()
                        yield False
                    if safe_end:
                        yield True

                par = hh % 2
                written = set()
                if prev_fin[0] is not None:
                    ppar, pwG, pwO, prB = prev_fin[0]
                    cntl = [0]

                    def fin_stream(ppar=ppar, pwG=pwG, pwO=pwO, prB=prB, cntl=cntl):
                        for tiles in G:
                            yield from ops_gen(fin_group(ppar, tiles, pwG, pwO, prB, cntl))
                    bg_fin = BG(fin_stream())
                else:
                    bg_fin = BG(None)
                bufi = [0, 0]
                cur = [None, None]
                for d in range(2):
                    cur[d] = (GORD[d][0], bufi[d] % 2)
                    run_ops(prep(d, GORD[d][0], bufi[d] % 2))
                    bufi[d] += 1
                for gi in range(5):
                    nxt = [None, None]
                    if gi + 1 < 5:
                        pops = []
                        for d in range(2):
                            nxt[d] = (GORD[d][gi + 1], bufi[d] % 2)
                            pops += prep(d, GORD[d][gi + 1], bufi[d] % 2)
                            bufi[d] += 1
                        bg_prep = BG(ops_gen(pops))
                    else:
                        bg_prep = BG(None)

                    def tick(bg_prep=bg_prep):
                        for _ in range(2):
                            if not bg_prep.done:
                                bg_prep.step()
                            else:
                                bg_fin.step()
                    ftiles, fb = cur[0]
                    btiles, bb = cur[1]
                    border = list(reversed(btiles))
                    for j in range(max(len(ftiles), len(border))):
                        if j < len(ftiles):
                            recur(0, ftiles[j], j, fb, tick, par, written)
                        if j < len(border):
                            recur(1, border[j], btiles.index(border[j]), bb, tick, par, written)
                    bg_prep.finish()
                    bg_fin.to_safe()
                    cur = nxt
                bg_fin.finish()
                prev_fin[0] = (par, wG, wO, rB)
            ppar, pwG, pwO, prB = prev_fin[0]
            cntl = [0]
            for tiles in G:
                for f in fin_group(ppar, tiles, pwG, pwO, prB, cntl):
                    f()

        def even_mixer():
            even_prep()
            if flags.get("diff", True):
                diff_part()
            if flags.get("hgrn", True):
                hgrn_part()

        for l in range(2):
            with_ctx = (l == 0)
            if (l == 0 and do_mix0) or (l == 1 and do_mix1):
                norm_phase(l, 0, True, False, 2)
                if l == 0:
                    even_mixer()
                else:
                    mla_mixer()
            if do_ffn:
                norm_phase(l, 1, with_ctx, True, 5)
                moe_phase(l, with_ctx)

        outdeps = []
        for t in range(2, NT):
            for h in range(2):
                outdeps.append(kb.dma("sp", lambda t=t, h=h: nc.sync.dma_start(
                    out=y_d[(t - 2) * 128:(t - 1) * 128, h * 512:(h + 1) * 512], in_=X[:, t, h * 512:(h + 1) * 512]),
                    XR[t][h], False))
        if taps:
            for t in range(NT):
                for h in range(2):
                    outdeps.append(kb.dma("sp", lambda t=t, h=h: nc.sync.dma_start(
                        out=tap_d[t * 128:(t + 1) * 128, h * 512:(h + 1) * 512], in_=X[:, t, h * 512:(h + 1) * 512]),
                        XR[t][h], False))
        kb.final_wait("sp", outdeps)
    return nc


WEIGHT_KEYS = ["mod_w", "even_w_in", "even_w_out", "odd_w_in", "mla_w_uq", "mla_w_ukv", "odd_w_out",
               "expert_w_gate", "expert_w_up", "expert_w_down", "shared_w_gate", "shared_w_up", "shared_w_down"]


def make_in_maps(inp, cores):
    cst = _consts()
    rope = _rope_tables()
    shared = {}
    for k in WEIGHT_KEYS:
        arr = np.ascontiguousarray(np.asarray(inp[k], np.float32))
        if k in ("even_w_in", "even_w_out", "odd_w_in", "mla_w_uq", "mla_w_ukv", "odd_w_out"):
            arr = arr[0]
        shared[k] = arr
    maps = []
    for b in cores:
        m = dict(shared)
        m["x"] = np.ascontiguousarray(np.asarray(inp["x"][b], np.float32))
        m["ctx"] = np.ascontiguousarray(np.asarray(inp["ctx"][b], np.float32))
        m["pp"] = _pack_params(b, inp)
        m["cst"] = cst
        m["rope"] = rope
        maps.append(m)
    return maps


def kernel(**inputs):
    nc = build_program()
    maps = make_in_maps(inputs, list(range(8)))
    res = run_bass_kernel_spmd(nc, maps, core_ids=list(range(8)))
    return np.stack([np.asarray(r["y"], np.float32) for r in res.results], axis=0)
```

```python
import math
import numpy as np
from contextlib import ExitStack
import concourse.bass as bass
import concourse.mybir as mybir
from concourse.bass_utils import run_bass_kernel_spmd

F32 = mybir.dt.float32
BF16 = mybir.dt.bfloat16
AF = mybir.ActivationFunctionType
ALU = mybir.AluOpType
AX = mybir.AxisListType

D = 1024
SEQ = 2048
CTX = 256
NTOK = SEQ + CTX
NT = NTOK // 128
BLOCKS = [(0, 256)] + [(256 + 512 * i, 512) for i in range(4)]
EPS = 1e-6
NSLOT = 3
SLOT_ELEMS = 4096

C_ID = 0
C_ONES = 128
C_BLK64 = 256
C_PERM = 384
C_TRIF = 512
C_TRIB = 640
C_AFTF = 768
C_AFTB = 896
C_N = 1024


def _consts():
    c = np.zeros((128, C_N), np.float32)
    i = np.arange(128)
    s = i[:, None]
    t = i[None, :]
    same = (s // 64) == (t // 64)
    c[:, C_ID:C_ID + 128] = (s == t)
    c[:, C_ONES:C_ONES + 128] = 1.0
    c[:, C_BLK64:C_BLK64 + 128] = same
    partner = np.where((i % 32) < 16, i + 16, i - 16)
    c[:, C_PERM:C_PERM + 128] = (s == partner[None, :])
    c[:, C_TRIF:C_TRIF + 128] = same & (s <= t)
    c[:, C_TRIB:C_TRIB + 128] = same & (s >= t)
    c[:, C_AFTF:C_AFTF + 128] = same & (s > t)
    c[:, C_AFTB:C_AFTB + 128] = same & (s < t)
    return c


def _rope_tables():
    tok = np.arange(SEQ)
    row = (tok // 64).astype(np.float32)
    col = (tok % 64).astype(np.float32)
    inv = (10000.0 ** (-np.arange(0, 32, 2, dtype=np.float32) / 32.0)).astype(np.float32)
    C = np.zeros((128, SEQ), np.float32)
    S = np.zeros((128, SEQ), np.float32)
    for p in range(128):
        d = p % 64
        pos = row if d < 32 else col
        f = inv[d % 16]
        ang = (pos * f).astype(np.float32)
        C[p] = np.cos(ang)
        S[p] = np.sin(ang) * (-1.0 if (d % 32) < 16 else 1.0)
    return np.stack([C, S], axis=1)


class PP:
    pass


def _pp_layout():
    off = {}
    n = 0

    def add(name, w):
        nonlocal n
        off[name] = (n, w)
        n += w
    add("c", 8)
    add("cctx", 8)
    add("modb", 2 * 48)
    add("nmix", 16)
    add("nffn", 16)
    add("rw", 8 * 16)
    add("rbias", 16)
    add("dqg", 1)
    add("dkg", 1)
    add("dlam", 256)
    add("subln", 1)
    add("lbfm", 2 * 2 * 4)
    add("hog", 128)
    add("qag", 3)
    add("kvag", 2)
    add("qng", 1)
    add("qrg", 1)
    add("kng", 1)
    add("krg", 1)
    return off, n


PPO, PPN = _pp_layout()


def _fm(v):
    v = np.asarray(v, np.float32)
    return np.ascontiguousarray(v.reshape(-1, 128).T)


def _pack_params(b, inp):
    pp = np.zeros((128, PPN), np.float32)

    def put(name, arr):
        o, w = PPO[name]
        arr = np.asarray(arr, np.float32).reshape(128, -1)
        assert arr.shape[1] == w, (name, arr.shape, w)
        pp[:, o:o + w] = arr
    put("c", _fm(inp["c"][b]))
    put("cctx", _fm(inp["c_ctx"]))
    put("modb", np.concatenate([_fm(inp["mod_b"][0]), _fm(inp["mod_b"][1])], axis=1))
    put("nmix", np.concatenate([_fm(inp["norm_mix"][0]), _fm(inp["norm_mix"][1])], axis=1))
    put("nffn", np.concatenate([_fm(inp["norm_ffn"][0]), _fm(inp["norm_ffn"][1])], axis=1))
    rw = np.asarray(inp["router_w"], np.float32).reshape(8, 128, 16).transpose(1, 0, 2)
    put("rw", rw.reshape(128, 128))
    put("rbias", np.broadcast_to(np.asarray(inp["router_bias"], np.float32)[None, :], (128, 16)))
    put("dqg", np.tile(np.asarray(inp["diff_q_gain"][0], np.float32), 2)[:, None])
    put("dkg", np.tile(np.asarray(inp["diff_k_gain"][0], np.float32), 2)[:, None])
    put("dlam", np.broadcast_to(np.asarray(inp["diff_lambda"][0], np.float32).reshape(1, 256), (128, 256)))
    put("subln", np.asarray(inp["diff_subln"][0], np.float32)[:, None])
    lb = np.asarray(inp["hgrn_lb_logits"], np.float32)
    put("lbfm", lb.reshape(2, 2, 4, 128).transpose(3, 0, 1, 2).reshape(128, 16))
    put("hog", np.broadcast_to(np.asarray(inp["hgrn_out_gain"][0], np.float32)[None, :], (128, 128)))
    put("qag", _fm(inp["mla_q_a_gain"][0]))
    put("kvag", _fm(inp["mla_kv_a_gain"][0]))
    put("qng", np.asarray(inp["mla_q_nope_gain"][0], np.float32)[:, None])
    put("qrg", np.tile(np.asarray(inp["mla_q_rope_gain"][0], np.float32), 2)[:, None])
    put("kng", np.asarray(inp["mla_k_nope_gain"][0], np.float32)[:, None])
    put("krg", np.tile(np.asarray(inp["mla_k_rope_gain"][0], np.float32), 2)[:, None])
    return pp


class Res:
    __slots__ = ("name", "w", "rs", "dsem", "dcnt")

    def __init__(self, name):
        self.name = name
        self.w = None
        self.rs = {}
        self.dsem = None
        self.dcnt = 0


class Eng:
    def __init__(self, name, obj, sem):
        self.name = name
        self.obj = obj
        self.sem = sem
        self.cnt = 0
        self.seen = {}


class KB:
    def __init__(self, nc, es):
        self.nc = nc
        self.es = es
        self.sems = {}
        self.E = {}
        for name, obj in (("pe", nc.tensor), ("act", nc.scalar), ("dve", nc.vector),
                          ("pool", nc.gpsimd), ("sp", nc.sync)):
            sem = es.enter_context(nc.semaphore("s_" + name))
            self.sems[name] = sem
            self.E[name] = Eng(name, obj, sem)
        self.nres = 0

    def res(self, name=None):
        self.nres += 1
        return Res(name or ("r%d" % self.nres))

    def _wait(self, E, reads, writes):
        deps = {}

        def add(d):
            if d is None:
                return
            k, v = d
            if deps.get(k, 0) < v:
                deps[k] = v
        for r in reads:
            add(r.w)
        for w in writes:
            add(w.w)
            for k, v in w.rs.items():
                add((k, v))
        for k, v in deps.items():
            if k == E.name and E.name == "pe":
                continue
            if E.seen.get(k, 0) < v:
                E.obj.wait_ge(self.sems[k], v)
                E.seen[k] = v

    def op(self, eng, fn, reads=(), writes=()):
        E = self.E[eng]
        self._wait(E, reads, writes)
        ins = None
        if callable(fn):
            ins = fn()
        else:
            for f in fn:
                ins = f()
        E.cnt += 1
        ins.then_inc(E.sem, 1)
        dep = (E.name, E.cnt)
        for r in reads:
            if r.rs.get(E.name, 0) < E.cnt:
                r.rs[E.name] = E.cnt
        for w in writes:
            w.w = dep
            w.rs = {}
        return ins

    def dma(self, queue, fn, res, is_write, reads=(), writes=()):
        E = self.E[queue]
        if res.dsem is None:
            self.nres += 1
            key = "d%d_%s" % (self.nres, res.name)
            res.dsem = key
            self.sems[key] = self.es.enter_context(self.nc.semaphore(key))
        rr = list(reads) + ([] if is_write else [res])
        ww = list(writes) + ([res] if is_write else [])
        self._wait(E, rr, ww)
        ins = fn()
        res.dcnt += 16
        ins.then_inc(self.sems[res.dsem], 16)
        dep = (res.dsem, res.dcnt)
        for r in rr:
            if r.rs.get(res.dsem, 0) < res.dcnt:
                r.rs[res.dsem] = res.dcnt
        for w in ww:
            w.w = dep
            w.rs = {}
        return dep

    def barrier(self):
        for E in self.E.values():
            for F in self.E.values():
                if F is E or F.cnt == 0:
                    continue
                if E.seen.get(F.name, 0) < F.cnt:
                    E.obj.wait_ge(self.sems[F.name], F.cnt)
                    E.seen[F.name] = F.cnt
            if E.name != "pe" and E.cnt > 0 and E.seen.get(E.name, 0) < E.cnt:
                E.obj.wait_ge(self.sems[E.name], E.cnt)
                E.seen[E.name] = E.cnt

    def final_wait(self, eng, deps):
        E = self.E[eng]
        for k, v in deps:
            E.obj.wait_ge(self.sems[k], v)


def build_program(flags=None):
    flags = flags or {}
    do_mix0 = flags.get("mix0", True)
    do_mix1 = flags.get("mix1", True)
    do_ffn = flags.get("ffn", True)
    taps = flags.get("taps", False)

    nc = bass.Bass("TRN2", target_bir_lowering=False)

    def din(name, shape):
        return nc.dram_tensor(name, list(shape), F32, kind="ExternalInput").ap()
    x_d = din("x", [SEQ, D])
    ctx_d = din("ctx", [CTX, D])
    pp_d = din("pp", [128, PPN])
    cst_d = din("cst", [128, C_N])
    rope_d = din("rope", [128, 2, SEQ])
    mod_w_d = din("mod_w", [2, D, 6 * D])
    ewin_d = din("even_w_in", [D, 4096])
    ewout_d = din("even_w_out", [D, D])
    owin_d = din("odd_w_in", [D, 704])
    wuq_d = din("mla_w_uq", [384, 1536])
    wukv_d = din("mla_w_ukv", [256, 2048])
    owout_d = din("odd_w_out", [D, D])
    xg_d = din("expert_w_gate", [2, 16, D, 512])
    xu_d = din("expert_w_up", [2, 16, D, 512])
    xd_d = din("expert_w_down", [2, 16, 512, D])
    sg_d = din("shared_w_gate", [2, D, 512])
    su_d = din("shared_w_up", [2, D, 512])
    sd_d = din("shared_w_down", [2, 512, D])
    y_d = nc.dram_tensor("y", [SEQ, D], F32, kind="ExternalOutput").ap()
    tap_d = None
    if taps:
        tap_d = nc.dram_tensor("tap", [NTOK, D], F32, kind="ExternalOutput").ap()

    es = ExitStack()
    with es:
        kb = KB(nc, es)

        def sb(name, shape, dt=F32):
            return es.enter_context(nc.sbuf_tensor(name, list(shape), dt))

        X = sb("X", [128, NT, D])
        XR = [[kb.res("x%d_%d" % (t, h)) for h in range(2)] for t in range(NT)]
        HT = sb("HT", [128, 8, NTOK], BF16)
        HTR = [kb.res("ht%d" % i) for i in range(len(BLOCKS))]
        PPT = sb("PPT", [128, PPN])
        PPR = kb.res("pp")
        CST = sb("CST", [128, C_N])
        CSTB = sb("CSTB", [128, C_N], BF16)
        CSTR = kb.res("cst")
        RING = [sb("ring%d" % i, [128, SLOT_ELEMS], BF16) for i in range(NSLOT)]
        RINGR = [kb.res("ring%d" % i) for i in range(NSLOT)]
        ring_pos = [0]
        PS = [es.enter_context(nc.psum_tensor("ps%d" % i, [128, 512], F32)) for i in range(8)]
        PSR = [kb.res("ps%d" % i) for i in range(8)]
        MODV = sb("MODV", [128, 2, 48, 2])
        MODR = kb.res("modv")
        NA = sb("NA", [128, 2, 8])
        NB = sb("NB", [128, 2, 8])
        NAR = kb.res("na")
        SS = sb("SS", [128, NT])
        SSR = kb.res("ss")
        RSTD = sb("RSTD", [128, NT])
        RSTDR = kb.res("rstd")
        GBC = sb("GBC", [128, 2, D], BF16)
        GBCR = kb.res("gbc")
        GATES = sb("GATES", [128, NT, 16])
        GATESR = kb.res("gates")
        SCB = sb("SCB", [128, 8, 2], BF16)
        SCR = kb.res("scb")

        scope_id = [0]

        class Scope:
            def __enter__(self):
                kb.barrier()
                scope_id[0] += 1
                self.sid = scope_id[0]
                self.stack = ExitStack()
                self.stack.__enter__()
                return self

            def sb(self, name, shape, dt=F32):
                return self.stack.enter_context(nc.sbuf_tensor("%s_s%d" % (name, self.sid), list(shape), dt))

            def __exit__(self, *exc):
                kb.barrier()
                return self.stack.__exit__(*exc)
        SM = [sb("SM%d" % i, [128, 64]) for i in range(6)]
        SMR = [kb.res("sm%d" % i) for i in range(6)]

        v = nc.vector
        a = nc.scalar
        g = nc.gpsimd
        pe = nc.tensor

        def ppc(name, i=0, w=1):
            o, _ = PPO[name]
            return PPT[:, o + i:o + i + w]

        kb.dma("sp", lambda: nc.sync.dma_start(out=PPT[:], in_=pp_d), PPR, True)
        kb.dma("sp", lambda: nc.sync.dma_start(out=CST[:], in_=cst_d), CSTR, True)
        kb.op("dve", lambda: v.tensor_copy(out=CSTB[:], in_=CST[:]), reads=[CSTR], writes=[CSTR])
        for t in range(NT):
            src = ctx_d[t * 128:(t + 1) * 128, :] if t < 2 else x_d[(t - 2) * 128:(t - 1) * 128, :]
            for h in range(2):
                kb.dma("sp", lambda src=src, t=t, h=h: nc.sync.dma_start(
                    out=X[:, t, h * 512:(h + 1) * 512], in_=src[:, h * 512:(h + 1) * 512]), XR[t][h], True)

        IDENT = CST[:, C_ID:C_ID + 128]

        def wload(src2d, kc, cols):
            assert kc * cols <= SLOT_ELEMS
            i = ring_pos[0] % len(RING)
            ring_pos[0] += 1
            view = RING[i][:, 0:kc * cols].rearrange("p (k c) -> p k c", k=kc)
            kb.dma("pool", lambda: g.dma_start(out=view, in_=src2d.rearrange("(k p) c -> p k c", p=128)),
                   RINGR[i], True)
            return RINGR[i], view

        def silu_small(out_ap, in_ap, sm_i, width, reads, writes):
            t1 = SM[sm_i][:, 0:width]
            kb.op("act", lambda: a.activation(out=t1, in_=in_ap, func=AF.Exp, scale=-1.0),
                  reads=reads, writes=[SMR[sm_i]])
            kb.op("dve", lambda: v.tensor_scalar(out=t1, in0=t1, scalar1=1.0, scalar2=None, op0=ALU.add),
                  reads=[SMR[sm_i]], writes=[SMR[sm_i]])
            kb.op("dve", lambda: v.reciprocal(out=t1, in_=t1), reads=[SMR[sm_i]], writes=[SMR[sm_i]])
            kb.op("dve", lambda: v.tensor_tensor(out=out_ap, in0=in_ap, in1=t1, op=ALU.mult),
                  reads=list(reads) + [SMR[sm_i]], writes=writes)

        silu_small(SCB[:, :, 0], ppc("c", 0, 8), 0, 8, [PPR], [SCR])
        silu_small(SCB[:, :, 1], ppc("cctx", 0, 8), 1, 8, [PPR, SCR], [SCR])

        for l in range(2):
            for s in range(12):
                r, wv = wload(mod_w_d[l, :, s * 512:(s + 1) * 512], 8, 512)
                pb = 0
                fns = []
                for j in range(4):
                    for k in range(8):
                        fns.append(lambda j=j, k=k, wv=wv, s=s: pe.matmul(
                            PS[pb][:, (s * 4 + j) * 2:(s * 4 + j) * 2 + 2], lhsT=wv[:, k, j * 128:(j + 1) * 128],
                            rhs=SCB[:, k, :], start=(k == 0), stop=(k == 7)))
                kb.op("pe", fns, reads=[r, SCR], writes=[PSR[pb]])
            o, _ = PPO["modb"]
            for w_ in range(2):
                kb.op("dve", lambda l=l, w_=w_: v.tensor_tensor(
                    out=MODV[:, l, :, w_], in0=PS[0][:, 0:96].rearrange("p (c w) -> p c w", w=2)[:, :, w_],
                    in1=PPT[:, o + l * 48:o + (l + 1) * 48], op=ALU.add),
                    reads=[PSR[0], PPR], writes=[MODR])

        def bc_from_fm(dst_ap_fn, vec_col_fn, dst_res, extra_reads, HF32, HF32R):
            for half in range(2):
                pb = 1 + half
                fns = []
                for kk in range(4):
                    k = half * 4 + kk
                    kb.op("dve", lambda k=k, kk=kk, half=half: v.tensor_copy(
                        out=HF32[half][:, kk, :], in_=vec_col_fn(k).to_broadcast([128, 128])),
                        reads=extra_reads, writes=[HF32R[half]])
                for kk in range(4):
                    fns.append(lambda kk=kk, half=half, pb=pb: pe.matmul(
                        PS[pb][:, kk * 128:(kk + 1) * 128], lhsT=HF32[half][:, kk, :], rhs=IDENT,
                        start=True, stop=True))
                kb.op("pe", fns, reads=[HF32R[half], CSTR], writes=[PSR[pb]])
                kb.op("act", lambda half=half, pb=pb: a.copy(out=dst_ap_fn(half), in_=PS[pb][:]),
                      reads=[PSR[pb]], writes=[dst_res])

        def norm_phase(l, which, with_ctx, router, gate_vec):
            with Scope() as sc_:
                XN = [sc_.sb("XN%d" % i, [128, D]) for i in range(2)]
                XNR = [kb.res("xn%d" % i) for i in range(2)]
                HF32 = [sc_.sb("HF32_%d" % i, [128, 8, 128]) for i in range(2)]
                HF32R = [kb.res("hf32_%d" % i) for i in range(2)]
                JUNK = sc_.sb("JUNK", [128, D], BF16)
                JUNKR = kb.res("junk")
                _norm_phase(l, which, with_ctx, router, XN, XNR, HF32, HF32R, JUNK, JUNKR)
                for w_ in range(2):
                    bc_from_fm(lambda half, w_=w_: GBC[:, w_, half * 512:(half + 1) * 512],
                               lambda k, w_=w_: MODV[:, l, gate_vec * 8 + k, w_:w_ + 1], GBCR, [MODR], HF32, HF32R)

        def _norm_phase(l, which, with_ctx, router, XN, XNR, HF32, HF32R, JUNK, JUNKR):
            nw = "nmix" if which == 0 else "nffn"
            vsh = 0 if which == 0 else 3
            vsc = vsh + 1
            for w_ in range(2):
                kb.op("dve", lambda w_=w_: v.scalar_tensor_tensor(
                    out=NA[:, w_, :], in0=MODV[:, l, vsc * 8:(vsc + 1) * 8, w_], scalar=1.0,
                    in1=ppc(nw, l * 8, 8), op0=ALU.add, op1=ALU.mult),
                    reads=[MODR, PPR], writes=[NAR])
                kb.op("dve", lambda w_=w_: v.tensor_copy(out=NB[:, w_, :], in_=MODV[:, l, vsh * 8:(vsh + 1) * 8, w_]),
                      reads=[MODR], writes=[NAR])
            tiles = list(range(NT)) if with_ctx else list(range(2, NT))
            for t in tiles:
                kb.op("act", lambda t=t: a.activation(out=JUNK[:], in_=X[:, t, :], func=AF.Square,
                                                      accum_out=SS[:, t:t + 1]),
                      reads=[XR[t][0], XR[t][1]], writes=[JUNKR, SSR])
            t0 = tiles[0]
            kb.op("dve", lambda: v.tensor_scalar(out=RSTD[:, t0:NT], in0=SS[:, t0:NT], scalar1=1.0 / D, scalar2=EPS,
                                                 op0=ALU.mult, op1=ALU.add), reads=[SSR], writes=[RSTDR])
            kb.op("act", lambda: a.activation(out=RSTD[:, t0:NT], in_=RSTD[:, t0:NT], func=AF.Ln),
                  reads=[RSTDR], writes=[RSTDR])
            kb.op("act", lambda: a.activation(out=RSTD[:, t0:NT], in_=RSTD[:, t0:NT], func=AF.Exp, scale=-0.5),
                  reads=[RSTDR], writes=[RSTDR])
            def stage1(idx, t):
                p = idx % 2
                kb.op("dve", lambda: v.tensor_scalar(out=XN[p][:], in0=X[:, t, :], scalar1=RSTD[:, t:t + 1],
                                                     scalar2=None, op0=ALU.mult),
                      reads=[XR[t][0], XR[t][1], RSTDR], writes=[XNR[p]])
                for half in range(2):
                    pb = (1 if p == 0 else 4) + half
                    fns = [lambda kk=kk: pe.transpose(
                        PS[pb][:, kk * 128:(kk + 1) * 128], XN[p][:, (half * 4 + kk) * 128:(half * 4 + kk + 1) * 128],
                        IDENT) for kk in range(4)]
                    kb.op("pe", fns, reads=[XNR[p], CSTR], writes=[PSR[pb]])

            def stage2(idx, t):
                p = idx % 2
                w_ = 1 if t < 2 else 0
                blk = 0 if t < 2 else 1 + (t - 2) // 4
                for half in range(2):
                    pb = (1 if p == 0 else 4) + half
                    for kk in range(4):
                        k = half * 4 + kk
                        if router:
                            kb.op("act", lambda kk=kk, k=k, pb=pb: a.activation(
                                out=HF32[p][:, k, :], in_=PS[pb][:, kk * 128:(kk + 1) * 128], func=AF.Identity,
                                scale=NA[:, w_, k:k + 1], bias=NB[:, w_, k:k + 1]),
                                reads=[PSR[pb], NAR], writes=[HF32R[p]])
                        else:
                            kb.op("act", lambda kk=kk, k=k, pb=pb: a.activation(
                                out=HT[:, k, t * 128:(t + 1) * 128], in_=PS[pb][:, kk * 128:(kk + 1) * 128], func=AF.Identity,
                                scale=NA[:, w_, k:k + 1], bias=NB[:, w_, k:k + 1]),
                                reads=[PSR[pb], NAR], writes=[HTR[blk]])
                if router:
                    kb.op("dve", lambda: v.tensor_copy(out=HT[:, :, t * 128:(t + 1) * 128], in_=HF32[p][:]),
                          reads=[HF32R[p]], writes=[HTR[blk]])
                    o, _ = PPO["rw"]
                    rb = 3 if p == 0 else 6
                    fns = [lambda k=k: pe.matmul(PS[rb][:, 0:16], lhsT=HF32[p][:, k, :],
                                                 rhs=PPT[:, o + k * 16:o + (k + 1) * 16],
                                                 start=(k == 0), stop=(k == 7)) for k in range(8)]
                    kb.op("pe", fns, reads=[HF32R[p], PPR], writes=[PSR[rb]])
                    route(t, rb)

            stage1(0, tiles[0])
            for idx, t in enumerate(tiles):
                if idx + 1 < len(tiles):
                    stage1(idx + 1, tiles[idx + 1])
                stage2(idx, t)

        def route(t, rb):
            S = SM[2]
            R = SMR[2]
            sc = S[:, 0:16]
            bi = S[:, 16:32]
            t2 = S[:, 32:48]
            m1 = SM[3][:, 0:4]
            m2 = SM[3][:, 4:8]
            gs = SM[3][:, 8:12]
            gm = SM[3][:, 12:13]
            ing = SM[3][:, 16:20]
            den = SM[3][:, 20:21]
            R3 = SMR[3]
            kb.op("act", lambda: a.activation(out=sc, in_=PS[rb][:, 0:16], func=AF.Exp, scale=-1.0),
                  reads=[PSR[rb]], writes=[R])
            kb.op("dve", lambda: v.tensor_scalar(out=sc, in0=sc, scalar1=1.0, scalar2=None, op0=ALU.add),
                  reads=[R], writes=[R])
            kb.op("dve", lambda: v.reciprocal(out=sc, in_=sc), reads=[R], writes=[R])
            kb.op("dve", lambda: v.tensor_tensor(out=bi, in0=sc, in1=ppc("rbias", 0, 16), op=ALU.add),
                  reads=[R, PPR], writes=[R])
            b3 = bi.rearrange("p (g e) -> p g e", e=4)
            t3 = t2.rearrange("p (g e) -> p g e", e=4)
            kb.op("dve", lambda: v.tensor_reduce(out=m1, in_=b3, axis=AX.X, op=ALU.max), reads=[R], writes=[R3])
            kb.op("dve", lambda: v.tensor_tensor(out=t3, in0=b3, in1=m1.unsqueeze(2).to_broadcast([128, 4, 4]),
                                                 op=ALU.is_equal), reads=[R, R3], writes=[R])
            kb.op("dve", lambda: v.scalar_tensor_tensor(out=t2, in0=t2, scalar=-1e9, in1=bi, op0=ALU.mult, op1=ALU.add),
                  reads=[R], writes=[R])
            kb.op("dve", lambda: v.tensor_reduce(out=m2, in_=t3, axis=AX.X, op=ALU.max), reads=[R], writes=[R3])
            kb.op("dve", lambda: v.tensor_tensor(out=gs, in0=m1, in1=m2, op=ALU.add), reads=[R3], writes=[R3])
            kb.op("dve", lambda: v.tensor_reduce(out=gm, in_=gs, axis=AX.X, op=ALU.max), reads=[R3], writes=[R3])
            kb.op("dve", lambda: v.tensor_scalar(out=ing, in0=gs, scalar1=gm, scalar2=None, op0=ALU.is_ge),
                  reads=[R3], writes=[R3])
            kb.op("dve", lambda: v.tensor_tensor(out=t3, in0=b3, in1=m2.unsqueeze(2).to_broadcast([128, 4, 4]),
                                                 op=ALU.is_ge), reads=[R, R3], writes=[R])
            kb.op("dve", lambda: v.tensor_tensor(out=t3, in0=t3, in1=ing.unsqueeze(2).to_broadcast([128, 4, 4]),
                                                 op=ALU.mult), reads=[R, R3], writes=[R])
            kb.op("dve", lambda: v.tensor_tensor(out=t2, in0=t2, in1=sc, op=ALU.mult), reads=[R], writes=[R])
            kb.op("dve", lambda: v.tensor_reduce(out=den, in_=t2, axis=AX.X, op=ALU.add), reads=[R], writes=[R3])
            kb.op("dve", lambda: v.reciprocal(out=den, in_=den), reads=[R3], writes=[R3])
            kb.op("dve", lambda: v.tensor_scalar(out=GATES[:, t, :], in0=t2, scalar1=den, scalar2=None, op0=ALU.mult),
                  reads=[R, R3], writes=[GATESR])

        def moe_phase(l, with_ctx):
            with Scope() as sc_:
                nextra = 5
                for i in range(nextra):
                    RING.append(sc_.sb("xring%d" % i, [128, SLOT_ELEMS], BF16))
                    RINGR.append(kb.res("xring%d_%d" % (l, i)))
                ring_pos[0] = 0
                _moe_phase(l, with_ctx, sc_.sb)
                kb.barrier()
                del RING[NSLOT:]
                del RINGR[NSLOT:]
                ring_pos[0] = 0

        def _moe_phase(l, with_ctx, psb):
            ACTT = [psb("ACTT%d" % i, [128, 4, 512], BF16) for i in range(2)]
            ACTTR = [kb.res("actt%d" % i) for i in range(2)]
            SIL = [psb("SIL%d" % i, [128, 512], BF16) for i in range(2)]
            SILR = [kb.res("sil%d" % i) for i in range(2)]
            TMP = [psb("TMP%d" % i, [128, 512]) for i in range(4)]
            TMPR = [kb.res("tmp%d" % i) for i in range(4)]
            blocks = BLOCKS if with_ctx else BLOCKS[1:]

            def load_expert(e):
                if e < 16:
                    srcs = (xg_d[l, e], xu_d[l, e], xd_d[l, e])
                else:
                    srcs = (sg_d[l], su_d[l], sd_d[l])
                return wload(srcs[0], 8, 512) + wload(srcs[1], 8, 512) + wload(srcs[2], 4, 1024)

            W = {0: load_expert(0)}
            items = [(e, bi_, t0, n) for e in range(17) for bi_, (t0, n) in enumerate(blocks)]

            def GU(idx):
                e, bi_, t0, n = items[idx]
                rg, vg, ru, vu, rd, vd = W[e]
                blk = BLOCKS.index((t0, n))
                ab = idx % 2
                for j in range(4):
                    pg = (j % 2)
                    pu = 2 + (j % 2)
                    kb.op("pe", [mm(PS[pg][:, 0:n], vg[:, k, j * 128:(j + 1) * 128], HT[:, k, t0:t0 + n], k == 0, k == 7)
                                 for k in range(8)], reads=[rg, HTR[blk]], writes=[PSR[pg]])
                    kb.op("pe", [mm(PS[pu][:, 0:n], vu[:, k, j * 128:(j + 1) * 128], HT[:, k, t0:t0 + n], k == 0, k == 7)
                                 for k in range(8)], reads=[ru, HTR[blk]], writes=[PSR[pu]])
                    sl = j % 2
                    kb.op("act", lambda sl=sl, pg=pg: a.activation(out=SIL[sl][:, 0:n], in_=PS[pg][:, 0:n], func=AF.Silu),
                          reads=[PSR[pg]], writes=[SILR[sl]])
                    kb.op("dve", lambda sl=sl, pu=pu, j=j, ab=ab: v.tensor_tensor(
                        out=ACTT[ab][:, j, 0:n], in0=SIL[sl][:, 0:n], in1=PS[pu][:, 0:n], op=ALU.mult),
                        reads=[SILR[sl], PSR[pu]], writes=[ACTTR[ab]])

            def DN(idx):
                e, bi_, t0, n = items[idx]
                rg, vg, ru, vu, rd, vd = W[e]
                ab = idx % 2
                for tt in range(n // 128):
                    t = (t0 + tt * 128) // 128
                    w_ = 1 if t < 2 else 0
                    for nh in range(2):
                        pd = 4 + ((tt * 2 + nh) % 4)
                        kb.op("pe", [mm(PS[pd][:], ACTT[ab][:, j, tt * 128:(tt + 1) * 128], vd[:, j, nh * 512:(nh + 1) * 512],
                                        j == 0, j == 3) for j in range(4)], reads=[rd, ACTTR[ab]], writes=[PSR[pd]])
                        ti = (tt * 2 + nh) % 4
                        if e < 16:
                            kb.op("dve", lambda pd=pd, t=t, e=e, ti=ti, nh=nh, w_=w_: v.scalar_tensor_tensor(
                                out=TMP[ti][:], in0=PS[pd][:], scalar=GATES[:, t, e:e + 1],
                                in1=GBC[:, w_, nh * 512:(nh + 1) * 512], op0=ALU.mult, op1=ALU.mult),
                                reads=[PSR[pd], GATESR, GBCR], writes=[TMPR[ti]])
                        else:
                            kb.op("dve", lambda pd=pd, ti=ti, nh=nh, w_=w_: v.tensor_tensor(
                                out=TMP[ti][:], in0=PS[pd][:], in1=GBC[:, w_, nh * 512:(nh + 1) * 512], op=ALU.mult),
                                reads=[PSR[pd], GBCR], writes=[TMPR[ti]])
                        kb.op("pool", lambda t=t, nh=nh, ti=ti: g.tensor_tensor(
                            out=X[:, t, nh * 512:(nh + 1) * 512], in0=X[:, t, nh * 512:(nh + 1) * 512],
                            in1=TMP[ti][:], op=ALU.add),
                            reads=[TMPR[ti], XR[t][nh]], writes=[XR[t][nh]])

            GU(0)
            for idx in range(len(items)):
                e, bi_ = items[idx][0], items[idx][1]
                if bi_ == 0 and e + 1 < 17:
                    W[e + 1] = load_expert(e + 1)
                if idx + 1 < len(items):
                    GU(idx + 1)
                DN(idx)

        ONESB = CSTB[:, C_ONES:C_ONES + 128]
        PERMB = CSTB[:, C_PERM:C_PERM + 128]
        EPSC = sb("EPSC", [128, 1])
        kb.op("dve", lambda: v.memset(EPSC[:], EPS), writes=[CSTR])

        def mm(out, lhsT, rhs, start, stop):
            return lambda: pe.matmul(out, lhsT=lhsT, rhs=rhs, start=start, stop=stop)

        class Common:
            def __init__(self, sc_):
                self.SQ = [sc_.sb("SQ0", [128, 512], BF16)] * 2
                self.SQR = [kb.res("sq0")] * 2
                self.RS = sc_.sb("RS", [128, 512])
                self.RSR = kb.res("rs")
                self.T1 = sc_.sb("T1", [128, 512])
                self.T1R = kb.res("t1")
                self.T2 = self.RS
                self.T2R = self.RSR
                self.T3 = sc_.sb("T3", [128, 512])
                self.T3R = kb.res("t3")
                self.RT = [sc_.sb("RT0", [128, 2, 512], BF16)] * 2
                self.RTR = [kb.res("rt0")] * 2
                self.rti = 0
                self.E = [sc_.sb("E%d" % i, [128, 512], BF16) for i in range(2)]
                self.ER = [kb.res("e%d" % i) for i in range(2)]
                self.sqi = 0

        def warmup(n=24):
            kb.op("pe", [mm(PS[7][:, 0:512], CSTB[:, 0:128], CSTB[:, 0:512], True, True) for _ in range(n)],
                  reads=[CSTR], writes=[PSR[7]])

        class BG:
            def __init__(self, gen):
                self.gen = gen
                self.safe = True
                self.done = gen is None

            def step(self):
                if self.done:
                    return False
                try:
                    self.safe = bool(next(self.gen))
                    return True
                except StopIteration:
                    self.done = True
                    self.safe = True
                    return False

            def to_safe(self):
                while not self.done and not self.safe:
                    self.step()

            def finish(self):
                while self.step():
                    pass

        def exhaust(gen):
            if gen is not None:
                for _ in gen:
                    pass

        def stepn(gen, n):
            if gen is None:
                return
            for _ in range(n):
                try:
                    next(gen)
                except StopIteration:
                    return

        def fm_rmsnorm_g(cm, banks, P, nfeat, gains, outs, n, out_res, ssb=7, ones_ap=None):
            srcs = [(PS[b][0:P, 0:n], PSR[b]) if isinstance(b, int) else b for b in banks]
            if ones_ap is None:
                ones_ap = ONESB[0:P, 0:P]
            nb = len(srcs)
            for j, (ap_, r_) in enumerate(srcs):
                q = cm.sqi % 2
                cm.sqi += 1
                kb.op("act", lambda ap_=ap_, q=q: a.activation(out=cm.SQ[q][0:P, 0:n], in_=ap_, func=AF.Square),
                      reads=[r_], writes=[cm.SQR[q]])
                yield
                kb.op("pe", mm(PS[ssb][0:P, 0:n], ones_ap, cm.SQ[q][0:P, 0:n], j == 0, j == nb - 1),
                      reads=[cm.SQR[q], CSTR], writes=[PSR[ssb]])
                yield
            kb.op("act", lambda: a.activation(out=cm.RS[0:P, 0:n], in_=PS[ssb][0:P, 0:n], func=AF.Ln,
                                              scale=1.0 / nfeat, bias=EPSC[0:P, :]),
                  reads=[PSR[ssb], CSTR], writes=[cm.RSR])
            yield
            kb.op("act", lambda: a.activation(out=cm.RS[0:P, 0:n], in_=cm.RS[0:P, 0:n], func=AF.Exp, scale=-0.5),
                  reads=[cm.RSR], writes=[cm.RSR])
            yield
            for j, (ap_, r_) in enumerate(srcs):
                kb.op("dve", lambda j=j, ap_=ap_: v.scalar_tensor_tensor(
                    out=outs[j], in0=ap_, scalar=gains[j], in1=cm.RS[0:P, 0:n], op0=ALU.mult, op1=ALU.mult),
                    reads=[r_, cm.RSR, PPR, CSTR], writes=[out_res])
                yield

        def fm_rmsnorm(*args, **kw):
            exhaust(fm_rmsnorm_g(*args, **kw))

        def rope_fm_g(cm, src, src_res, P, lt0, n, pb):
            i = cm.rti % 2
            cm.rti += 1
            kb.dma("pool", lambda: g.dma_start(out=cm.RT[i][:, :, 0:n], in_=rope_d[:, :, lt0:lt0 + n]), cm.RTR[i], True)
            kb.op("pe", mm(PS[pb][0:P, 0:n], PERMB[0:P, 0:P], src, True, True), reads=[src_res, CSTR], writes=[PSR[pb]])
            yield
            kb.op("dve", lambda: v.tensor_tensor(out=cm.T3[0:P, 0:n], in0=src, in1=cm.RT[i][0:P, 0, 0:n], op=ALU.mult),
                  reads=[src_res, cm.RTR[i]], writes=[cm.T3R])
            yield
            kb.op("dve", lambda: v.tensor_tensor(out=cm.T2[0:P, 0:n], in0=PS[pb][0:P, 0:n], in1=cm.RT[i][0:P, 1, 0:n], op=ALU.mult),
                  reads=[PSR[pb], cm.RTR[i]], writes=[cm.T2R])
            yield
            kb.op("dve", lambda: v.tensor_tensor(out=src, in0=cm.T3[0:P, 0:n], in1=cm.T2[0:P, 0:n], op=ALU.add),
                  reads=[cm.T3R, cm.T2R], writes=[src_res])
            yield

        def rope_fm(*args, **kw):
            exhaust(rope_fm_g(*args, **kw))

        def attn_core(cm, kts, n, s_terms, s_reads, v_fn, v_reads, scale, ob=2, zb=3, tick=None):
            def emit_s(i):
                terms = s_terms(kts[i])
                kb.op("pe", [mm(PS[i % 2][:, 0:n], l_, r_, ti == 0, ti == len(terms) - 1) for ti, (l_, r_) in enumerate(terms)],
                      reads=s_reads, writes=[PSR[i % 2]])
            emit_s(0)
            last = len(kts) - 1
            for i, kt in enumerate(kts):
                if i < last:
                    emit_s(i + 1)
                eb = i % 2
                kb.op("act", lambda i=i, eb=eb: a.activation(out=cm.E[eb][:, 0:n], in_=PS[i % 2][:, 0:n], func=AF.Exp, scale=scale),
                      reads=[PSR[i % 2]], writes=[cm.ER[eb]])
                kb.op("pe", [mm(PS[ob][:, 0:n], v_fn(kt), cm.E[eb][:, 0:n], i == 0, i == last),
                             mm(PS[zb][:, 0:n], ONESB, cm.E[eb][:, 0:n], i == 0, i == last)],
                      reads=[cm.ER[eb], CSTR] + v_reads, writes=[PSR[ob], PSR[zb]])
                if tick is not None:
                    tick()

        def wout_partial(cm, sc_, lhs_fn, lhs_reads, nk, wviews, wres, tiles, tag):
            TM = [cm.T1, cm.RS]
            TMR = [cm.T1R, cm.RSR]
            c_ = 0
            for t in tiles:
                w_ = 1 if t < 2 else 0
                for nh in range(2):
                    pb = 4 + (c_ % 4)
                    ti = c_ % 2
                    c_ += 1
                    kb.op("pe", [mm(PS[pb][:], lhs_fn(k, t), wviews[nh][:, k, :], k == 0, k == nk - 1) for k in range(nk)],
                          reads=lhs_reads(t) + [wres[nh]], writes=[PSR[pb]])
                    kb.op("dve", lambda pb=pb, ti=ti, nh=nh, w_=w_: v.tensor_tensor(
                        out=TM[ti][:], in0=PS[pb][:], in1=GBC[:, w_, nh * 512:(nh + 1) * 512], op=ALU.mult),
                        reads=[PSR[pb], GBCR], writes=[TMR[ti]])
                    kb.op("pool", lambda t=t, nh=nh, ti=ti: g.tensor_tensor(
                        out=X[:, t, nh * 512:(nh + 1) * 512], in0=X[:, t, nh * 512:(nh + 1) * 512], in1=TM[ti][:], op=ALU.add),
                        reads=[TMR[ti], XR[t][nh]], writes=[XR[t][nh]])

        def blk_of_tile(t):
            return 0 if t < 2 else 1 + (t - 2) // 4

        def mla_mixer():
            with Scope() as sc_:
                cm = Common(sc_)
                CQN = sc_.sb("CQN", [128, 3, SEQ], BF16)
                CQNR = kb.res("cqn")
                CKVN = sc_.sb("CKVN", [128, 2, NTOK], BF16)
                CKVNR = kb.res("ckvn")
                KRT = sc_.sb("KRT", [128, NTOK], BF16)
                KRTR = kb.res("krt")
                KNT = [sc_.sb("KNT%d" % i, [128, NTOK], BF16) for i in range(2)]
                KNTR = [kb.res("knt%d" % i) for i in range(2)]
                VH = [sc_.sb("VH%d" % i, [128, NT, 128], BF16) for i in range(2)]
                VHR = [kb.res("vh%d" % i) for i in range(2)]
                QNT = [sc_.sb("QNT%d" % i, [128, 512], BF16) for i in range(2)]
                QNTR = [kb.res("qnt%d" % i) for i in range(2)]
                QRT = [sc_.sb("QRT%d" % i, [128, 512], BF16) for i in range(2)]
                QRTR = [kb.res("qrt%d" % i) for i in range(2)]
                ra, wa = wload(owin_d[:, 0:384], 8, 384)
                rb, wb = wload(owin_d[:, 384:704], 8, 320)
                for blk, (t0, n) in enumerate(BLOCKS):
                    lat = blk > 0
                    if lat:
                        for j in range(3):
                            kb.op("pe", [mm(PS[4 + j][:, 0:n], wa[:, k, j * 128:(j + 1) * 128], HT[:, k, t0:t0 + n], k == 0, k == 7)
                                         for k in range(8)], reads=[ra, HTR[blk]], writes=[PSR[4 + j]])
                        fm_rmsnorm(cm, [4, 5, 6], 128, 384, [ppc("qag", j) for j in range(3)],
                                   [CQN[:, j, t0 - 256:t0 - 256 + n] for j in range(3)], n, CQNR)
                    for j in range(2):
                        kb.op("pe", [mm(PS[4 + j][:, 0:n], wb[:, k, j * 128:(j + 1) * 128], HT[:, k, t0:t0 + n], k == 0, k == 7)
                                     for k in range(8)], reads=[rb, HTR[blk]], writes=[PSR[4 + j]])
                    fm_rmsnorm(cm, [4, 5], 128, 256, [ppc("kvag", j) for j in range(2)],
                               [CKVN[:, j, t0:t0 + n] for j in range(2)], n, CKVNR)
                    kb.op("pe", [mm(PS[6][0:64, 0:n], wb[:, k, 256:320], HT[:, k, t0:t0 + n], k == 0, k == 7)
                                 for k in range(8)], reads=[rb, HTR[blk]], writes=[PSR[6]])
                    fm_rmsnorm(cm, [6], 64, 64, [ppc("krg")[0:64, :]], [KRT[0:64, t0:t0 + n]], n, KRTR)
                    if lat:
                        rope_fm(cm, KRT[0:64, t0:t0 + n], KRTR, 64, t0 - 256, n, 6)
                rq0, wq0 = wload(wuq_d[:, 0:768], 3, 768)
                rq1, wq1 = wload(wuq_d[:, 768:1536], 3, 768)
                rkv, wkv = wload(wukv_d, 2, 2048)
                scale = 192.0 ** -0.5
                warmup()

                def prep_head(h, bf):
                    knt, kntR = KNT[bf], KNTR[bf]
                    vh, vhR = VH[bf], VHR[bf]
                    for blk, (t0, n) in enumerate(BLOCKS):
                        kb.op("pe", [mm(PS[6][:, 0:n], wkv[:, j, h * 256:h * 256 + 128], CKVN[:, j, t0:t0 + n], j == 0, j == 1)
                                     for j in range(2)], reads=[rkv, CKVNR], writes=[PSR[6]])
                        yield
                        yield from fm_rmsnorm_g(cm, [6], 128, 128, [ppc("kng")], [knt[:, t0:t0 + n]], n, kntR)
                        yield True
                    for g0 in range(0, NT, 4):
                        tl = list(range(g0, min(g0 + 4, NT)))
                        fns = []
                        for i, t in enumerate(tl):
                            for j in range(2):
                                fns.append(mm(PS[6][:, i * 128:(i + 1) * 128], CKVN[:, j, t * 128:(t + 1) * 128],
                                              wkv[:, j, h * 256 + 128:h * 256 + 256], j == 0, j == 1))
                        kb.op("pe", fns, reads=[rkv, CKVNR], writes=[PSR[6]])
                        yield
                        kb.op("act", lambda g0=g0, tl=tl: a.copy(
                            out=vh[:, g0:g0 + len(tl), :],
                            in_=PS[6][:, 0:len(tl) * 128].rearrange("p (t c) -> p t c", c=128)),
                            reads=[PSR[6]], writes=[vhR])
                        yield True

                def prep_q(h, qb, qq):
                    rq, wq = (rq0, wq0) if h < 4 else (rq1, wq1)
                    hq = h % 4
                    q0 = qb * 512
                    kb.op("pe", [mm(PS[6][:, 0:512], wq[:, j, hq * 192:hq * 192 + 128], CQN[:, j, q0:q0 + 512], j == 0, j == 2)
                                 for j in range(3)], reads=[rq, CQNR], writes=[PSR[6]])
                    yield
                    yield from fm_rmsnorm_g(cm, [6], 128, 128, [ppc("qng")], [QNT[qq][:, :]], 512, QNTR[qq])
                    kb.op("pe", [mm(PS[6][0:64, 0:512], wq[:, j, hq * 192 + 128:hq * 192 + 192], CQN[:, j, q0:q0 + 512], j == 0, j == 2)
                                 for j in range(3)], reads=[rq, CQNR], writes=[PSR[6]])
                    yield
                    yield from fm_rmsnorm_g(cm, [6], 64, 64, [ppc("qrg")[0:64, :]], [QRT[qq][0:64, :]], 512, QRTR[qq])
                    yield from rope_fm_g(cm, QRT[qq][0:64, :], QRTR[qq], 64, q0, 512, 6)

                exhaust(prep_head(0, 0))
                exhaust(prep_q(0, 0, 0))
                qi = 0
                for h in range(8):
                    bf = h % 2
                    bg_head = BG(prep_head(h + 1, (h + 1) % 2) if h + 1 < 8 else None)
                    for qb in range(4):
                        q0 = qb * 512
                        blk = qb + 1
                        qq = qi % 2
                        qi += 1
                        if qb + 1 < 4:
                            bg_q = BG(prep_q(h, qb + 1, qi % 2))
                        elif h + 1 < 8:
                            bg_q = BG(prep_q(h + 1, 0, qi % 2))
                        else:
                            bg_q = BG(None)

                        def tick(bg_q=bg_q, bg_head=bg_head):
                            for _ in range(2):
                                if not bg_q.done:
                                    bg_q.step()
                                else:
                                    bg_head.step()
                        ob, zb = (2, 3) if qi % 2 else (4, 5)
                        attn_core(cm, list(range(NT)), 512,
                                  lambda kt, qq=qq, bf=bf: [(KNT[bf][:, kt * 128:(kt + 1) * 128], QNT[qq][:, :]),
                                                            (KRT[0:64, kt * 128:(kt + 1) * 128], QRT[qq][0:64, :])],
                                  [KNTR[bf], KRTR, QNTR[qq], QRTR[qq]],
                                  lambda kt, bf=bf: VH[bf][:, kt, :], [VHR[bf]], scale, ob=ob, zb=zb, tick=tick)
                        kb.op("act", lambda zb=zb: a.activation(out=cm.T1[:, :], in_=PS[zb][:, :], func=AF.Ln), reads=[PSR[zb]], writes=[cm.T1R])
                        kb.op("act", lambda: a.activation(out=cm.T1[:, :], in_=cm.T1[:, :], func=AF.Exp, scale=-1.0), reads=[cm.T1R], writes=[cm.T1R])
                        kb.op("dve", lambda h=h, q0=q0, ob=ob: v.tensor_tensor(
                            out=HT[:, h, 256 + q0:256 + q0 + 512], in0=PS[ob][:, :], in1=cm.T1[:, :], op=ALU.mult),
                            reads=[PSR[ob], cm.T1R], writes=[HTR[blk]])
                        bg_q.finish()
                        bg_head.to_safe()
                    bg_head.finish()
                rw0, wo0 = wload(owout_d[:, 0:512], 8, 512)
                rw1, wo1 = wload(owout_d[:, 512:1024], 8, 512)
                wout_partial(cm, sc_, lambda k, t: HT[:, k, t * 128:(t + 1) * 128], lambda t: [HTR[blk_of_tile(t)]],
                             8, [wo0, wo1], [rw0, rw1], list(range(2, NT)), "m")

        BLK64B = CSTB[:, C_BLK64:C_BLK64 + 128]
        ONEC = sb("ONEC", [128, 1])
        kb.op("dve", lambda: v.memset(ONEC[:], 1.0), writes=[CSTR])
        LAMC = sb("LAMC", [128, 4])
        LAMR = kb.res("lamc")
        OMLF = sb("OMLF", [128, 8])
        OMLFR = kb.res("omlf")
        LAM_INIT0 = 0.8 - 0.6 * math.exp(-0.3 * 0)

        def even_prep():
            o, _ = PPO["dlam"]
            S0, R0 = SM[4], SMR[4]
            S1, R1 = SM[5], SMR[5]
            for i in range(2):
                kb.op("dve", lambda i=i: v.tensor_tensor(out=S0[:, 0:64], in0=PPT[:, o + i * 128:o + i * 128 + 64],
                                                         in1=PPT[:, o + i * 128 + 64:o + i * 128 + 128], op=ALU.mult),
                      reads=[PPR], writes=[R0])
                kb.op("dve", lambda i=i: v.tensor_reduce(out=S1[:, i:i + 1], in_=S0[:, 0:64], axis=AX.X, op=ALU.add),
                      reads=[R0], writes=[R1])
            kb.op("act", lambda: a.activation(out=S1[:, 2:4], in_=S1[:, 0:2], func=AF.Exp), reads=[R1], writes=[R1])
            kb.op("dve", lambda: v.scalar_tensor_tensor(out=LAMC[:, 0:1], in0=S1[:, 3:4], scalar=-LAM_INIT0, in1=S1[:, 2:3],
                                                        op0=ALU.add, op1=ALU.subtract), reads=[R1], writes=[LAMR])
            kb.op("dve", lambda: v.tensor_scalar(out=LAMC[:, 1:2], in0=ppc("subln"), scalar1=1.0 - LAM_INIT0, scalar2=None,
                                                 op0=ALU.mult), reads=[PPR], writes=[LAMR])
            o2, _ = PPO["lbfm"]
            kb.op("dve", lambda: v.tensor_tensor(out=S0[:, 0:8], in0=PPT[:, o2:o2 + 8], in1=PPT[:, o2 + 8:o2 + 16], op=ALU.subtract),
                  reads=[PPR], writes=[R0])
            kb.op("act", lambda: a.activation(out=S0[:, 0:8], in_=S0[:, 0:8], func=AF.Exp), reads=[R0], writes=[R0])
            kb.op("dve", lambda: v.tensor_scalar(out=S0[:, 0:8], in0=S0[:, 0:8], scalar1=1.0, scalar2=None, op0=ALU.add),
                  reads=[R0], writes=[R0])
            kb.op("dve", lambda: v.reciprocal(out=OMLF[:, :], in_=S0[:, 0:8]), reads=[R0], writes=[OMLFR])

        def diff_part():
            with Scope() as sc_:
                cm = Common(sc_)
                cm.T4 = sc_.sb("T4d", [128, 512])
                cm.T4R = kb.res("t4d")
                KT = [sc_.sb("KT%d" % i, [128, NTOK], BF16) for i in range(2)]
                KTR = [kb.res("kt%d" % i) for i in range(2)]
                QT = [sc_.sb("QTb%d" % i, [128, 512], BF16) for i in range(2)]
                QTR = [kb.res("qtb%d" % i) for i in range(2)]
                VH = [sc_.sb("VHd%d" % i, [128, NT, 128], BF16) for i in range(2)]
                VHR = [kb.res("vhd%d" % i) for i in range(2)]
                CATA = sc_.sb("CATA", [128, 4, NTOK], BF16)
                CATAR = [kb.res("cata%d" % i) for i in range(len(BLOCKS))]
                rq, wq = wload(ewin_d[:, 0:512], 8, 512)
                rk, wk = wload(ewin_d[:, 512:1024], 8, 512)
                rv, wv = wload(ewin_d[:, 1024:1536], 8, 512)
                scale = 64.0 ** -0.5
                warmup()

                def prep_head(h, bf):
                    for blk, (t0, n) in enumerate(BLOCKS):
                        kb.op("pe", [mm(PS[6][:, 0:n], wk[:, k, h * 128:(h + 1) * 128], HT[:, k, t0:t0 + n], k == 0, k == 7)
                                     for k in range(8)], reads=[rk, HTR[blk]], writes=[PSR[6]])
                        yield
                        yield from fm_rmsnorm_g(cm, [6], 128, 64, [ppc("dkg")], [KT[bf][:, t0:t0 + n]], n, KTR[bf], ones_ap=BLK64B)
                        if blk > 0:
                            yield from rope_fm_g(cm, KT[bf][:, t0:t0 + n], KTR[bf], 128, t0 - 256, n, 6)
                        yield True
                    for g0 in range(0, NT, 4):
                        tl = list(range(g0, min(g0 + 4, NT)))
                        fns = []
                        for i, t in enumerate(tl):
                            for k in range(8):
                                fns.append(mm(PS[6][:, i * 128:(i + 1) * 128], HT[:, k, t * 128:(t + 1) * 128],
                                              wv[:, k, h * 128:(h + 1) * 128], k == 0, k == 7))
                        kb.op("pe", fns, reads=[rv] + [HTR[blk_of_tile(t)] for t in tl], writes=[PSR[6]])
                        yield
                        kb.op("act", lambda g0=g0, tl=tl: a.copy(
                            out=VH[bf][:, g0:g0 + len(tl), :],
                            in_=PS[6][:, 0:len(tl) * 128].rearrange("p (t c) -> p t c", c=128)),
                            reads=[PSR[6]], writes=[VHR[bf]])
                        yield True

                def prep_q(h, blk, qq):
                    t0, n = BLOCKS[blk]
                    kb.op("pe", [mm(PS[6][:, 0:n], wq[:, k, h * 128:(h + 1) * 128], HT[:, k, t0:t0 + n], k == 0, k == 7)
                                 for k in range(8)], reads=[rq, HTR[blk]], writes=[PSR[6]])
                    yield
                    yield from fm_rmsnorm_g(cm, [6], 128, 64, [ppc("dqg")], [QT[qq][:, 0:n]], n, QTR[qq], ones_ap=BLK64B)
                    if blk > 0:
                        yield from rope_fm_g(cm, QT[qq][:, 0:n], QTR[qq], 128, t0 - 256, n, 6)

                exhaust(prep_head(0, 0))
                exhaust(prep_q(0, 0, 0))
                qi = 0
                nblk = len(BLOCKS)
                for h in range(4):
                    bf = h % 2
                    bg_head = BG(prep_head(h + 1, (h + 1) % 2) if h + 1 < 4 else None)
                    for blk, (t0, n) in enumerate(BLOCKS):
                        qq = qi % 2
                        qi += 1
                        if blk + 1 < nblk:
                            bg_q = BG(prep_q(h, blk + 1, qi % 2))
                        elif h + 1 < 4:
                            bg_q = BG(prep_q(h + 1, 0, qi % 2))
                        else:
                            bg_q = BG(None)

                        def tick(bg_q=bg_q, bg_head=bg_head):
                            if not bg_q.done:
                                bg_q.step()
                            else:
                                bg_head.step()
                        kts = [0, 1] if blk == 0 else list(range(NT))
                        for c in range(2):
                            attn_core(cm, kts, n,
                                      lambda kt, c=c, bf=bf, qq=qq: [(KT[bf][c * 64:(c + 1) * 64, kt * 128:(kt + 1) * 128],
                                                                      QT[qq][c * 64:(c + 1) * 64, 0:n])],
                                      [KTR[bf], QTR[qq]], lambda kt, bf=bf: VH[bf][:, kt, :], [VHR[bf]], scale,
                                      ob=2 + 2 * c, zb=3 + 2 * c, tick=tick)
                        kb.op("act", lambda: a.activation(out=cm.T1[:, 0:n], in_=PS[3][:, 0:n], func=AF.Ln), reads=[PSR[3]], writes=[cm.T1R])
                        kb.op("act", lambda: a.activation(out=cm.T1[:, 0:n], in_=cm.T1[:, 0:n], func=AF.Exp, scale=-1.0), reads=[cm.T1R], writes=[cm.T1R])
                        kb.op("dve", lambda: v.tensor_tensor(out=cm.T1[:, 0:n], in0=PS[2][:, 0:n], in1=cm.T1[:, 0:n], op=ALU.mult),
                              reads=[PSR[2], cm.T1R], writes=[cm.T1R])
                        kb.op("act", lambda: a.activation(out=cm.T4[:, 0:n], in_=PS[5][:, 0:n], func=AF.Ln), reads=[PSR[5]], writes=[cm.T4R])
                        kb.op("act", lambda: a.activation(out=cm.T4[:, 0:n], in_=cm.T4[:, 0:n], func=AF.Exp, scale=-1.0), reads=[cm.T4R], writes=[cm.T4R])
                        kb.op("dve", lambda: v.tensor_tensor(out=cm.T4[:, 0:n], in0=PS[4][:, 0:n], in1=cm.T4[:, 0:n], op=ALU.mult),
                              reads=[PSR[4], cm.T4R], writes=[cm.T4R])
                        kb.op("dve", lambda: v.scalar_tensor_tensor(out=cm.T1[:, 0:n], in0=cm.T4[:, 0:n], scalar=LAMC[:, 0:1],
                                                                    in1=cm.T1[:, 0:n], op0=ALU.mult, op1=ALU.add),
                              reads=[cm.T4R, cm.T1R, LAMR], writes=[cm.T1R])
                        bg_q.finish()
                        bg_head.to_safe()
                        fm_rmsnorm(cm, [(cm.T1[:, 0:n], cm.T1R)], 128, 128, [LAMC[:, 1:2]], [CATA[:, h, t0:t0 + n]], n, CATAR[blk])
                    bg_head.finish()
                rw0, wo0 = wload(ewout_d[0:512, 0:512], 4, 512)
                rw1, wo1 = wload(ewout_d[0:512, 512:1024], 4, 512)
                wout_partial(cm, sc_, lambda k, t: CATA[:, k, t * 128:(t + 1) * 128], lambda t: [CATAR[blk_of_tile(t)]],
                             4, [wo0, wo1], [rw0, rw1], list(range(NT)), "d")

        def hgrn_part():
            with Scope() as sc_:
                RING.append(sc_.sb("hring", [128, SLOT_ELEMS], BF16))
                RINGR.append(kb.res("hring"))
                ring_pos[0] = 0
                _hgrn_part(sc_)
                kb.barrier()
                del RING[NSLOT:]
                del RINGR[NSLOT:]
                ring_pos[0] = 0

        def _hgrn_part(sc_):
            def T_(name, shape, dt=F32):
                return sc_.sb(name, shape, dt), kb.res(name)
            TA, TAR = T_("hTA", [128, 512])
            TB, TBR = T_("hTB", [128, 512])
            TC, TCR = T_("hTC", [128, 512])
            TD, TDR = T_("hTD", [128, 512])
            EC, ECR = T_("hEC", [128, 512])
            KBAR = [[T_("hKBAR%d%d" % (d, b), [128, 4, 128], BF16) for b in range(2)] for d in range(2)]
            KTIL = [[T_("hKTIL%d%d" % (d, b), [128, 512], BF16) for b in range(2)] for d in range(2)]
            QZ = [[T_("hQZ%d%d" % (d, b), [128, 4, 256], BF16) for b in range(2)] for d in range(2)]
            ALAST = [[T_("hAL%d%d" % (d, b), [128, 8]) for b in range(2)] for d in range(2)]
            VH, VHR = T_("hVH", [128, NT, 128], BF16)
            ODS = [T_("hOS%d" % p_, [128, NT, 128], BF16) for p_ in range(2)]
            prev_fin = [None]
            ST = [T_("hS%d" % d, [128, 128]) for d in range(2)]
            SBF = [T_("hSB%d" % d, [128, 128], BF16) for d in range(2)]
            SCM = [T_("hSCM%d" % d, [128, 128], BF16) for d in range(2)]
            OMLBC, OMLBCR = T_("hOMLBC", [128, 2, 128])
            CATB, CATBR = T_("hCATB", [128, 512], BF16)
            SSH, SSHR = T_("hSSH", [128, 8])
            PSS = [[kb.res("pss%d_%d" % (d, i)) for i in range(3)] for d in range(2)]
            for d in range(2):
                for b in range(2):
                    kb.op("pool", lambda d=d, b=b: g.memset(QZ[d][b][0][:], 0.0), writes=[QZ[d][b][1]])

            class Shim:
                pass
            cm = Shim()
            cm.T1, cm.T1R, cm.RS, cm.RSR = TA, TAR, TB, TBR
            G = [[0, 1], [2, 3, 4, 5], [6, 7, 8, 9], [10, 11, 12, 13], [14, 15, 16, 17]]
            GORD = [G, [G[0], G[4], G[3], G[2], G[1]]]
            TRI = [CST[:, C_TRIF:C_TRIF + 128], CST[:, C_TRIB:C_TRIB + 128]]
            AFT = [CST[:, C_AFTF:C_AFTF + 128], CST[:, C_AFTB:C_AFTB + 128]]
            hscale = 128.0 ** -0.5
            hb = 1536

            def ht_res(tiles):
                return list({id(HTR[blk_of_tile(t)]): HTR[blk_of_tile(t)] for t in tiles}.values())

            for hh in range(4):
                ia = ring_pos[0] % len(RING)
                ring_pos[0] += 1
                ib = ring_pos[0] % len(RING)
                ring_pos[0] += 1
                rA, rB = RINGR[ia], RINGR[ib]
                wA = RING[ia][:, 0:4096].rearrange("p (k c) -> p k c", k=8)
                for pi, c0 in enumerate((hb + hh * 128, hb + 512 + hh * 128, hb + 1024 + hh * 128, hb + 1536 + hh * 128)):
                    kb.dma("pool", lambda pi=pi, c0=c0: g.dma_start(
                        out=wA[:, :, pi * 128:(pi + 1) * 128],
                        in_=ewin_d[:, c0:c0 + 128].rearrange("(k p) c -> p k c", p=128)), rA, True)
                wG = RING[ib][:, 0:1024].rearrange("p (k c) -> p k c", k=8)
                wO = RING[ib][:, 1024:2048]
                c0 = hb + 2048 + hh * 128
                kb.dma("pool", lambda: g.dma_start(out=wG, in_=ewin_d[:, c0:c0 + 128].rearrange("(k p) c -> p k c", p=128)), rB, True)
                kb.dma("pool", lambda: g.dma_start(out=wO, in_=ewout_d[512 + hh * 128:512 + (hh + 1) * 128, :]), rB, True)
                for d in range(2):
                    kb.op("dve", lambda d=d: v.tensor_copy(out=TD[:, 0:128], in_=OMLF[:, d * 4 + hh:d * 4 + hh + 1].to_broadcast([128, 128])),
                          reads=[OMLFR], writes=[TDR])
                    kb.op("pe", mm(PS[0][:, 0:128], TD[:, 0:128], IDENT, True, True), reads=[TDR, CSTR], writes=[PSR[0]])
                    kb.op("act", lambda d=d: a.copy(out=OMLBC[:, d, :], in_=PS[0][:, 0:128]), reads=[PSR[0]], writes=[OMLBCR])
                for g0 in range(0, NT, 4):
                    tl = list(range(g0, min(g0 + 4, NT)))
                    fns = []
                    for i, t in enumerate(tl):
                        for k in range(8):
                            fns.append(mm(PS[3][:, i * 128:(i + 1) * 128], HT[:, k, t * 128:(t + 1) * 128], wA[:, k, 384:512], k == 0, k == 7))
                    kb.op("pe", fns, reads=[rA] + ht_res(tl), writes=[PSR[3]])
                    kb.op("act", lambda g0=g0, tl=tl: a.copy(out=VH[:, g0:g0 + len(tl), :],
                                                           in_=PS[3][:, 0:len(tl) * 128].rearrange("p (t c) -> p t c", c=128)),
                          reads=[PSR[3]], writes=[VHR])
                for d in range(2):
                    kb.op("dve", lambda d=d: v.memset(ST[d][0][:], 0.0), writes=[ST[d][1]])
                    kb.op("dve", lambda d=d: v.memset(SBF[d][0][:], 0.0), writes=[SBF[d][1]])

                def prep(d, tiles, b):
                    ops = []

                    def Q(*a_, **k_):
                        ops.append(lambda: kb.op(*a_, **k_))
                    ng = len(tiles)
                    n = ng * 128
                    tk0 = tiles[0] * 128
                    fc = 128 + d * 128
                    hr = ht_res(tiles)
                    kbar, kbarR = KBAR[d][b]
                    ktil, ktilR = KTIL[d][b]
                    qz, qzR = QZ[d][b]
                    al, alR = ALAST[d][b]
                    fns = []
                    for i, t in enumerate(tiles):
                        for k in range(8):
                            fns.append(mm(PS[0][:, i * 128:(i + 1) * 128], HT[:, k, t * 128:(t + 1) * 128], wA[:, k, fc:fc + 128], k == 0, k == 7))
                    Q("pe", fns, reads=[rA] + hr, writes=[PSR[0]])
                    Q("pe", [mm(PS[1][:, 0:n], wA[:, k, fc:fc + 128], HT[:, k, tk0:tk0 + n], k == 0, k == 7) for k in range(8)],
                          reads=[rA] + hr, writes=[PSR[1]])
                    Q("pe", [mm(PS[2][:, 0:n], wA[:, k, 0:128], HT[:, k, tk0:tk0 + n], k == 0, k == 7) for k in range(8)],
                          reads=[rA] + hr, writes=[PSR[2]])
                    Q("act", lambda: a.activation(out=TA[:, 0:n], in_=PS[0][:, 0:n], func=AF.Exp), reads=[PSR[0]], writes=[TAR])
                    Q("act", lambda: a.activation(out=TA[:, 0:n], in_=TA[:, 0:n], func=AF.Ln, bias=ONEC[:, :]),
                          reads=[TAR, CSTR], writes=[TAR])
                    Q("act", lambda: a.activation(out=TA[:, 0:n], in_=TA[:, 0:n], func=AF.Exp, scale=-1.0), reads=[TAR], writes=[TAR])
                    Q("dve", lambda: v.tensor_tensor(
                        out=TB[:, 0:n].rearrange("p (t c) -> p t c", c=128), in0=TA[:, 0:n].rearrange("p (t c) -> p t c", c=128),
                        in1=OMLBC[:, d:d + 1, :].to_broadcast([128, ng, 128]), op=ALU.mult),
                        reads=[TAR, OMLBCR], writes=[TBR])
                    Q("act", lambda: a.activation(out=TC[:, 0:n], in_=TB[:, 0:n], func=AF.Ln, scale=-1.0, bias=ONEC[:, :]),
                          reads=[TBR, CSTR], writes=[TCR])
                    Q("pe", [mm(PS[3][:, i * 128:(i + 1) * 128], AFT[d], TC[:, i * 128:(i + 1) * 128], True, True) for i in range(ng)],
                          reads=[TCR, CSTR], writes=[PSR[3]])
                    Q("pe", [mm(PS[4][:, i * 128:(i + 1) * 128], TC[:, i * 128:(i + 1) * 128], TRI[d], True, True) for i in range(ng)],
                          reads=[TCR, CSTR], writes=[PSR[4]])
                    Q("act", lambda: a.activation(out=TA[:, 0:n], in_=PS[3][:, 0:n], func=AF.Exp), reads=[PSR[3]], writes=[TAR])
                    Q("dve", lambda: v.tensor_tensor(out=kbar[:, 0:ng, :], in0=TB[:, 0:n].rearrange("p (t c) -> p t c", c=128),
                                                         in1=TA[:, 0:n].rearrange("p (t c) -> p t c", c=128), op=ALU.mult),
                          reads=[TAR, TBR], writes=[kbarR])
                    Q("act", lambda: a.activation(out=TD[:, 0:n], in_=PS[1][:, 0:n], func=AF.Exp), reads=[PSR[1]], writes=[TDR])
                    Q("act", lambda: a.activation(out=TD[:, 0:n], in_=TD[:, 0:n], func=AF.Ln, bias=ONEC[:, :]),
                          reads=[TDR, CSTR], writes=[TDR])
                    Q("act", lambda: a.activation(out=TD[:, 0:n], in_=TD[:, 0:n], func=AF.Exp, scale=-1.0), reads=[TDR], writes=[TDR])
                    Q("act", lambda: a.activation(out=EC[:, 0:n], in_=PS[4][:, 0:n], func=AF.Exp), reads=[PSR[4]], writes=[ECR])
                    Q("act", lambda: a.activation(out=TA[:, 0:n], in_=PS[4][:, 0:n], func=AF.Exp, scale=-1.0), reads=[PSR[4]], writes=[TAR])
                    Q("dve", lambda: v.scalar_tensor_tensor(out=ktil[:, 0:n], in0=TD[:, 0:n], scalar=OMLF[:, d * 4 + hh:d * 4 + hh + 1],
                                                                in1=TA[:, 0:n], op0=ALU.mult, op1=ALU.mult),
                          reads=[TDR, TAR, OMLFR], writes=[ktilR])
                    lastcol = 63 if d == 0 else 0
                    Q("dve", lambda: v.tensor_copy(out=al[:, 0:2 * ng],
                                                       in_=EC[:, 0:n].rearrange("p (c j) -> p c j", j=64)[:, :, lastcol]),
                          reads=[ECR], writes=[alR])
                    Q("act", lambda: a.activation(out=TB[:, 0:n], in_=PS[2][:, 0:n], func=AF.Exp, scale=-1.0), reads=[PSR[2]], writes=[TBR])
                    Q("act", lambda: a.activation(out=TB[:, 0:n], in_=TB[:, 0:n], func=AF.Ln, bias=ONEC[:, :]),
                          reads=[TBR, CSTR], writes=[TBR])
                    Q("act", lambda: a.activation(out=TB[:, 0:n], in_=TB[:, 0:n], func=AF.Exp, scale=-1.0), reads=[TBR], writes=[TBR])
                    Q("dve", lambda: v.tensor_tensor(out=TB[:, 0:n], in0=PS[2][:, 0:n], in1=TB[:, 0:n], op=ALU.mult),
                          reads=[TBR, PSR[2]], writes=[TBR])
                    for c in range(2):
                        Q("dve", lambda c=c: v.scalar_tensor_tensor(
                            out=qz[:, 0:ng, c * 192:c * 192 + 64],
                            in0=TB[:, 0:n].rearrange("p (t c) -> p t c", c=128)[:, :, c * 64:(c + 1) * 64], scalar=hscale,
                            in1=EC[:, 0:n].rearrange("p (t c) -> p t c", c=128)[:, :, c * 64:(c + 1) * 64], op0=ALU.mult, op1=ALU.mult),
                            reads=[TBR, ECR], writes=[qzR])
                    return ops

                def recur(d, t, i, b, tick, par, written):
                    bank = 5 + d
                    s_ap = PS[bank][:, 0:128]
                    o_ap = PS[bank][:, 128:256]
                    ds_ap = PS[7][:, 256 + d * 128:384 + d * 128]
                    sR, oR, dsR = PSS[d]
                    kbar, kbarR = KBAR[d][b]
                    ktil, ktilR = KTIL[d][b]
                    qz, qzR = QZ[d][b]
                    al, alR = ALAST[d][b]
                    st, stR = ST[d]
                    sbf, sbfR = SBF[d]
                    scm, scmR = SCM[d]
                    od, odR = ODS[par]
                    kt_ = ktil[:, i * 128:(i + 1) * 128]
                    kb.op("pe", [mm(s_ap[:, 0:64], kt_, qz[:, i, 0:64], True, True), mm(s_ap[:, 64:128], kt_, qz[:, i, 192:256], True, True)],
                          reads=[ktilR, qzR], writes=[sR])
                    tick()
                    kb.op("dve", lambda: v.tensor_tensor(out=scm[:, :], in0=s_ap, in1=TRI[d], op=ALU.mult), reads=[sR, CSTR], writes=[scmR])
                    tick()
                    cs = [0, 1] if d == 0 else [1, 0]

                    def upd(c):
                        kb.op("pe", mm(ds_ap, kbar[c * 64:(c + 1) * 64, i, :], VH[c * 64:(c + 1) * 64, t, :], True, True),
                              reads=[kbarR, VHR], writes=[dsR])
                        tick()
                        kb.op("dve", lambda: v.scalar_tensor_tensor(out=sbf[:, :], in0=st[:, :], scalar=al[:, 2 * i + c:2 * i + c + 1],
                                                                    in1=ds_ap, op0=ALU.mult, op1=ALU.add),
                              reads=[stR, alR, dsR], writes=[sbfR])
                        kb.op("dve", lambda: v.scalar_tensor_tensor(out=st[:, :], in0=st[:, :], scalar=al[:, 2 * i + c:2 * i + c + 1],
                                                                    in1=ds_ap, op0=ALU.mult, op1=ALU.add),
                              reads=[stR, alR, dsR], writes=[stR])
                        tick()
                    kb.op("pe", [mm(o_ap, scm[:, :], VH[:, t, :], True, False),
                                 mm(o_ap, qz[:, i, cs[0] * 128:(cs[0] + 1) * 128], sbf[:, :], False, False)],
                          reads=[scmR, VHR, qzR, sbfR], writes=[oR])
                    tick()
                    upd(cs[0])
                    kb.op("pe", mm(o_ap, qz[:, i, cs[1] * 128:(cs[1] + 1) * 128], sbf[:, :], False, True),
                          reads=[qzR, sbfR], writes=[oR])
                    tick()
                    upd(cs[1])
                    if t not in written:
                        written.add(t)
                        kb.op("act", lambda: a.copy(out=od[:, t, :], in_=o_ap), reads=[oR], writes=[odR])
                    else:
                        kb.op("dve", lambda: v.tensor_tensor(out=od[:, t, :], in0=od[:, t, :], in1=o_ap, op=ALU.add),
                              reads=[oR, odR], writes=[odR])
                    tick()
                    tick()

                def fin_group(par, tiles, wG, wO, rB, cntl):
                    ops = []

                    def Q(*a_, **k_):
                        ops.append(lambda: kb.op(*a_, **k_))
                    od = ODS[par][0]
                    odR = ODS[par][1]
                    ng = len(tiles)
                    n = ng * 128
                    t0_ = tiles[0]
                    fns = []
                    for i, t in enumerate(tiles):
                        for k in range(8):
                            fns.append(mm(PS[0][:, i * 128:(i + 1) * 128], HT[:, k, t * 128:(t + 1) * 128], wG[:, k, :], k == 0, k == 7))
                    Q("pe", fns, reads=[rB] + ht_res(tiles), writes=[PSR[0]])
                    Q("dve", lambda: v.tensor_copy(out=TA[:, 0:n].rearrange("p (t c) -> p t c", c=128), in_=od[:, t0_:t0_ + ng, :]),
                          reads=[odR], writes=[TAR])
                    for i in range(ng):
                        Q("act", lambda i=i: a.activation(out=TD[:, i * 128:(i + 1) * 128], in_=TA[:, i * 128:(i + 1) * 128],
                                                              func=AF.Square, accum_out=SSH[:, i:i + 1]),
                              reads=[TAR], writes=[TDR, SSHR])
                    Q("dve", lambda: v.tensor_scalar(out=SSH[:, 0:ng], in0=SSH[:, 0:ng], scalar1=1.0 / 128, scalar2=EPS,
                                                         op0=ALU.mult, op1=ALU.add), reads=[SSHR], writes=[SSHR])
                    Q("act", lambda: a.activation(out=SSH[:, 0:ng], in_=SSH[:, 0:ng], func=AF.Ln), reads=[SSHR], writes=[SSHR])
                    Q("act", lambda: a.activation(out=SSH[:, 0:ng], in_=SSH[:, 0:ng], func=AF.Exp, scale=-0.5), reads=[SSHR], writes=[SSHR])
                    for i in range(ng):
                        Q("dve", lambda i=i: v.scalar_tensor_tensor(
                            out=TA[:, i * 128:(i + 1) * 128], in0=TA[:, i * 128:(i + 1) * 128], scalar=SSH[:, i:i + 1],
                            in1=ppc("hog", 0, 128), op0=ALU.mult, op1=ALU.mult), reads=[TAR, SSHR, PPR], writes=[TAR])
                    Q("act", lambda: a.activation(out=TB[:, 0:n], in_=PS[0][:, 0:n], func=AF.Exp, scale=-1.0), reads=[PSR[0]], writes=[TBR])
                    Q("act", lambda: a.activation(out=TB[:, 0:n], in_=TB[:, 0:n], func=AF.Ln, bias=ONEC[:, :]),
                          reads=[TBR, CSTR], writes=[TBR])
                    Q("act", lambda: a.activation(out=TB[:, 0:n], in_=TB[:, 0:n], func=AF.Exp, scale=-1.0), reads=[TBR], writes=[TBR])
                    Q("dve", lambda: v.tensor_tensor(out=TB[:, 0:n], in0=PS[0][:, 0:n], in1=TB[:, 0:n], op=ALU.mult),
                          reads=[TBR, PSR[0]], writes=[TBR])
                    Q("dve", lambda: v.tensor_tensor(out=TA[:, 0:n], in0=TA[:, 0:n], in1=TB[:, 0:n], op=ALU.mult),
                          reads=[TAR, TBR], writes=[TAR])
                    Q("pe", [mm(PS[1][:, i * 128:(i + 1) * 128], TA[:, i * 128:(i + 1) * 128], IDENT, True, True) for i in range(ng)],
                          reads=[TAR, CSTR], writes=[PSR[1]])
                    Q("act", lambda: a.copy(out=CATB[:, 0:n], in_=PS[1][:, 0:n]), reads=[PSR[1]], writes=[CATBR])
                    for i, t in enumerate(tiles):
                        w_ = 1 if t < 2 else 0
                        for nh in range(2):
                            pb = 2 + (cntl[0] % 2)
                            tm_, tmR = (TC, TCR) if cntl[0] % 2 == 0 else (TD, TDR)
                            cntl[0] += 1
                            Q("pe", mm(PS[pb][:], CATB[:, i * 128:(i + 1) * 128], wO[:, nh * 512:(nh + 1) * 512], True, True),
                                  reads=[CATBR, rB], writes=[PSR[pb]])
                            Q("dve", lambda pb=pb, nh=nh, w_=w_, tm_=tm_: v.tensor_tensor(
                                out=tm_[:], in0=PS[pb][:], in1=GBC[:, w_, nh * 512:(nh + 1) * 512], op=ALU.mult),
                                reads=[PSR[pb], GBCR], writes=[tmR])
                            Q("pool", lambda t=t, nh=nh, tm_=tm_: g.tensor_tensor(
                                out=X[:, t, nh * 512:(nh + 1) * 512], in0=X[:, t, nh * 512:(nh + 1) * 512], in1=tm_[:], op=ALU.add),
                                reads=[tmR, XR[t][nh]], writes=[XR[t][nh]])
                    return ops

                def run_ops(ops):
                    for f in ops:
                        f()

                def ops_gen(ops, safe_end=True):
                    for f in ops:
                        f()
                        yield False
                    if safe_end:
                        yield True

                par = hh % 2
                written = set()
                if prev_fin[0] is not None:
                    ppar, pwG, pwO, prB = prev_fin[0]
                    cntl = [0]

                    def fin_stream(ppar=ppar, pwG=pwG, pwO=pwO, prB=prB, cntl=cntl):
                        for tiles in G:
                            yield from ops_gen(fin_group(ppar, tiles, pwG, pwO, prB, cntl))
                    bg_fin = BG(fin_stream())
                else:
                    bg_fin = BG(None)
                bufi = [0, 0]
                cur = [None, None]
                for d in range(2):
                    cur[d] = (GORD[d][0], bufi[d] % 2)
                    run_ops(prep(d, GORD[d][0], bufi[d] % 2))
                    bufi[d] += 1
                for gi in range(5):
                    nxt = [None, None]
                    if gi + 1 < 5:
                        pops = []
                        for d in range(2):
                            nxt[d] = (GORD[d][gi + 1], bufi[d] % 2)
                            pops += prep(d, GORD[d][gi + 1], bufi[d] % 2)
                            bufi[d] += 1
                        bg_prep = BG(ops_gen(pops))
                    else:
                        bg_prep = BG(None)

                    def tick(bg_prep=bg_prep):
                        for _ in range(2):
                            if not bg_prep.done:
                                bg_prep.step()
                            else:
                                bg_fin.step()
                    ftiles, fb = cur[0]
                    btiles, bb = cur[1]
                    border = list(reversed(btiles))
                    for j in range(max(len(ftiles), len(border))):
                        if j < len(ftiles):
                            recur(0, ftiles[j], j, fb, tick, par, written)
                        if j < len(border):
                            recur(1, border[j], btiles.index(border[j]), bb, tick, par, written)
                    bg_prep.finish()
                    bg_fin.to_safe()
                    cur = nxt
                bg_fin.finish()
                prev_fin[0] = (par, wG, wO, rB)
            ppar, pwG, pwO, prB = prev_fin[0]
            cntl = [0]
            for tiles in G:
                for f in fin_group(ppar, tiles, pwG, pwO, prB, cntl):
                    f()

        def even_mixer():
            even_prep()
            if flags.get("diff", True):
                diff_part()
            if flags.get("hgrn", True):
                hgrn_part()

        for l in range(2):
            with_ctx = (l == 0)
            if (l == 0 and do_mix0) or (l == 1 and do_mix1):
                norm_phase(l, 0, True, False, 2)
                if l == 0:
                    even_mixer()
                else:
                    mla_mixer()
            if do_ffn:
                norm_phase(l, 1, with_ctx, True, 5)
                moe_phase(l, with_ctx)

        outdeps = []
        for t in range(2, NT):
            for h in range(2):
                outdeps.append(kb.dma("sp", lambda t=t, h=h: nc.sync.dma_start(
                    out=y_d[(t - 2) * 128:(t - 1) * 128, h * 512:(h + 1) * 512], in_=X[:, t, h * 512:(h + 1) * 512]),
                    XR[t][h], False))
        if taps:
            for t in range(NT):
                for h in range(2):
                    outdeps.append(kb.dma("sp", lambda t=t, h=h: nc.sync.dma_start(
                        out=tap_d[t * 128:(t + 1) * 128, h * 512:(h + 1) * 512], in_=X[:, t, h * 512:(h + 1) * 512]),
                        XR[t][h], False))
        kb.final_wait("sp", outdeps)
    return nc


WEIGHT_KEYS = ["mod_w", "even_w_in", "even_w_out", "odd_w_in", "mla_w_uq", "mla_w_ukv", "odd_w_out",
               "expert_w_gate", "expert_w_up", "expert_w_down", "shared_w_gate", "shared_w_up", "shared_w_down"]


def make_in_maps(inp, cores):
    cst = _consts()
    rope = _rope_tables()
    shared = {}
    for k in WEIGHT_KEYS:
        arr = np.ascontiguousarray(np.asarray(inp[k], np.float32))
        if k in ("even_w_in", "even_w_out", "odd_w_in", "mla_w_uq", "mla_w_ukv", "odd_w_out"):
            arr = arr[0]
        shared[k] = arr
    maps = []
    for b in cores:
        m = dict(shared)
        m["x"] = np.ascontiguousarray(np.asarray(inp["x"][b], np.float32))
        m["ctx"] = np.ascontiguousarray(np.asarray(inp["ctx"][b], np.float32))
        m["pp"] = _pack_params(b, inp)
        m["cst"] = cst
        m["rope"] = rope
        maps.append(m)
    return maps


def kernel(**inputs):
    nc = build_program()
    maps = make_in_maps(inputs, list(range(8)))
    res = run_bass_kernel_spmd(nc, maps, core_ids=list(range(8)))
    return np.stack([np.asarray(r["y"], np.float32) for r in res.results], axis=0)
```
